# Optimizing a Trainium2 kernel written in Bass

```python
import jax, jax.numpy as jnp
from jax import lax
import numpy as np

D_MODEL = 1024
BATCH = 8
SEQ = 8192
DEPTH = 2

GDN_HEADS = 4
GDN_DK = 128
GDN_DV = 128
GDN_CHUNK = 64
CONV_WIDTH = 5
ATT_GROUPS = ((128, 1), (512, 4), (2048, 16))
ATT_HEADS_PER_GROUP = 4
ATT_HEADS = ATT_HEADS_PER_GROUP * len(ATT_GROUPS)
ATT_HEAD_DIM = 64
ROPE_THETA = 500000.0
ROPE_DIMS = ATT_HEAD_DIM // 4
N_EXPERTS = 16
EXPERT_FF = 1024
EC_CAPACITY_FACTOR = 2
RMS_EPS = 1e-6
NEG_BIG = -1e30

GDN_QK_W = GDN_HEADS * GDN_DK
GDN_V_W = GDN_HEADS * GDN_DV
GDN_CONV_CH = 2 * GDN_QK_W + GDN_V_W
ATT_W = ATT_HEADS * ATT_HEAD_DIM
ATT_OUT_W = ATT_HEADS_PER_GROUP * ATT_HEAD_DIM
N_BRANCHES = 2
OFF_Z = GDN_CONV_CH
OFF_BETA = OFF_Z + GDN_V_W
OFF_DECAY = OFF_BETA + 2 * GDN_HEADS
OFF_ATT = OFF_DECAY + 2 * GDN_HEADS
OFF_GATE = OFF_ATT + 3 * ATT_W
P_IN = OFF_GATE + N_BRANCHES * D_MODEL

kernel_name = "hybrid_gdn_dilated_attn_ec_moe"


def rms_norm(x, w):
    xf = x.astype(jnp.float32)
    y = xf * lax.rsqrt(jnp.mean(xf * xf, axis=-1, keepdims=True) + RMS_EPS)
    return (y * w.astype(jnp.float32)).astype(x.dtype)


def l2_normalize(x):
    return x * lax.rsqrt(jnp.sum(x * x, axis=-1, keepdims=True) + 1e-6)


def short_conv(u, w):
    c = u.shape[-1]
    pad = (CONV_WIDTH - 1) // 2
    return lax.conv_general_dilated(u, w[:, None, :].astype(u.dtype), window_strides=(1,),
                                    padding=((pad, pad),), dimension_numbers=('NWC', 'WIO', 'NWC'),
                                    feature_group_count=c)


def chunked_gated_delta(q, k, v, g, beta):
    B, T, H, DK = q.shape
    DV = v.shape[-1]
    N = T // GDN_CHUNK

    def to_chunks(a):
        a = a.reshape((B, N, GDN_CHUNK, H) + a.shape[3:])
        return jnp.moveaxis(a, (1, 3), (0, 2))

    q, k, v, g, beta = (to_chunks(a) for a in (q, k, v, g, beta))
    g = jnp.cumsum(g, axis=-1)
    idx = jnp.arange(GDN_CHUNK)
    lower = idx[:, None] >= idx[None, :]
    strict = idx[:, None] > idx[None, :]
    decay = jnp.exp(jnp.where(lower, g[..., :, None] - g[..., None, :], -jnp.inf))
    kb = k * beta[..., None]
    L = jnp.where(strict, jnp.einsum('nbhid,nbhjd->nbhij', kb, k) * decay, 0.0)
    rhs = jnp.concatenate([v * beta[..., None], kb * jnp.exp(g)[..., None]], axis=-1)
    uw = lax.linalg.triangular_solve(L, rhs, left_side=True, lower=True, unit_diagonal=True)
    u, w = uw[..., :DV], uw[..., DV:]
    a_qk = jnp.einsum('nbhid,nbhjd->nbhij', q, k) * decay
    q_dec = q * jnp.exp(g)[..., None]
    k_dec = k * jnp.exp(g[..., -1:] - g)[..., None]
    g_end = jnp.exp(g[..., -1])

    def step(S, xs):
        qd, kd, uu, ww, aa, ge = xs
        v_new = uu - jnp.einsum('bhck,bhkv->bhcv', ww, S)
        o = jnp.einsum('bhck,bhkv->bhcv', qd, S) + jnp.einsum('bhij,bhjv->bhiv', aa, v_new)
        S = S * ge[..., None, None] + jnp.einsum('bhck,bhcv->bhkv', kd, v_new)
        return S, o

    S0 = jnp.zeros((B, H, DK, DV), jnp.float32)
    _, o = lax.scan(step, S0, (q_dec, k_dec, u, w, a_qk, g_end))
    return jnp.moveaxis(o, (0, 2), (1, 3)).reshape(B, T, H, DV)


def gdn_branch(qkv, z, b_pre, a_pre, conv_w, a_log, dt_bias, norm_w):
    B, T, _ = qkv.shape
    H = GDN_HEADS
    u = jax.nn.silu(short_conv(qkv, conv_w)).astype(jnp.float32)
    q = l2_normalize(u[..., :GDN_QK_W].reshape(B, T, H, GDN_DK)) * (GDN_DK ** -0.5)
    k = l2_normalize(u[..., GDN_QK_W:2 * GDN_QK_W].reshape(B, T, H, GDN_DK))
    v = u[..., 2 * GDN_QK_W:].reshape(B, T, H, GDN_DV)
    beta = jax.nn.sigmoid(b_pre.astype(jnp.float32)).reshape(B, T, 2, H)
    g = -jnp.exp(a_log.astype(jnp.float32)) * jax.nn.softplus(
        a_pre.astype(jnp.float32).reshape(B, T, 2, H) + dt_bias.astype(jnp.float32))
    flip = lambda a: jnp.flip(a, axis=1)
    q2 = jnp.concatenate([q, flip(q)], axis=0)
    k2 = jnp.concatenate([k, flip(k)], axis=0)
    v2 = jnp.concatenate([v, flip(v)], axis=0)
    g2 = jnp.concatenate([g[:, :, 0], flip(g[:, :, 1])], axis=0)
    b2 = jnp.concatenate([beta[:, :, 0], flip(beta[:, :, 1])], axis=0)
    o2 = chunked_gated_delta(q2, k2, v2, g2, b2)
    o = o2[:B] + flip(o2[B:])
    o = o * lax.rsqrt(jnp.mean(o * o, axis=-1, keepdims=True) + RMS_EPS) * norm_w.astype(jnp.float32)
    o = o * jax.nn.silu(z.astype(jnp.float32).reshape(B, T, H, GDN_DV))
    return o.reshape(B, T, GDN_V_W).astype(qkv.dtype)


def partial_rope(x, pos):
    half = ROPE_DIMS // 2
    inv_freq = jnp.power(ROPE_THETA, -jnp.arange(half, dtype=jnp.float32) * 2.0 / ROPE_DIMS)
    ang = pos[:, None] * inv_freq[None, :]
    cos = jnp.cos(ang)[:, None, :]
    sin = jnp.sin(ang)[:, None, :]
    xf = x.astype(jnp.float32)
    x1, x2, rest = xf[..., :half], xf[..., half:ROPE_DIMS], xf[..., ROPE_DIMS:]
    return jnp.concatenate([x1 * cos - x2 * sin, x2 * cos + x1 * sin, rest], axis=-1)


def dilated_window_attention(q, k, v, dilation, span):
    B, T, H, D = q.shape
    M = T // dilation
    nb = -(-M // span)
    Mp = nb * span

    def subseq(a):
        a = a.reshape(B, M, dilation, H, D).transpose(0, 2, 1, 3, 4)
        return jnp.pad(a, ((0, 0), (0, 0), (0, Mp - M), (0, 0), (0, 0)))

    def windows(a):
        a = jnp.pad(subseq(a), ((0, 0), (0, 0), (span, span), (0, 0), (0, 0)))
        a = a.reshape(B, dilation, nb + 2, span, H, D)
        return jnp.concatenate([a[:, :, :-2], a[:, :, 1:-1], a[:, :, 2:]], axis=3)

    qs = subseq(q).reshape(B, dilation, nb, span, H, D)
    kw, vw = windows(k), windows(v)
    mq = jnp.arange(nb)[:, None] * span + jnp.arange(span)[None, :]
    mk = jnp.arange(nb)[:, None] * span - span + jnp.arange(3 * span)[None, :]
    valid = ((mk[:, None, :] >= 0) & (mk[:, None, :] < M)
             & (jnp.abs(mq[:, :, None] - mk[:, None, :]) <= span))
    s = jnp.einsum('brnqhd,brnkhd->brnhqk', qs, kw) * (D ** -0.5)
    s = jnp.where(valid[:, None], s, NEG_BIG)
    m = jnp.max(s, axis=-1, keepdims=True)
    p = jnp.exp(s - m)
    l = jnp.sum(p, axis=-1, keepdims=True)
    lse = jnp.swapaxes((m + jnp.log(l))[..., 0], -1, -2)
    o = jnp.einsum('brnhqk,brnkhd->brnqhd', p, vw) / jnp.swapaxes(l[..., 0], -1, -2)[..., None]
    o = o.reshape(B, dilation, Mp, H, D)[:, :, :M].transpose(0, 2, 1, 3, 4).reshape(B, T, H, D)
    lse = lse.reshape(B, dilation, Mp, H)[:, :, :M].transpose(0, 2, 1, 3).reshape(B, T, H)
    return o, lse


def dilated_attention_branch(qkv):
    B, T, _ = qkv.shape
    q = qkv[..., :ATT_W].reshape(B, T, ATT_HEADS, ATT_HEAD_DIM)
    k = qkv[..., ATT_W:2 * ATT_W].reshape(B, T, ATT_HEADS, ATT_HEAD_DIM)
    v = qkv[..., 2 * ATT_W:].reshape(B, T, ATT_HEADS, ATT_HEAD_DIM).astype(jnp.float32)
    pos = jnp.arange(T, dtype=jnp.float32)
    q = partial_rope(q, pos)
    k = partial_rope(k, pos)
    outs, lses = [], []
    for gi, (window, dilation) in enumerate(ATT_GROUPS):
        hs = slice(gi * ATT_HEADS_PER_GROUP, (gi + 1) * ATT_HEADS_PER_GROUP)
        o, lse = dilated_window_attention(q[:, :, hs], k[:, :, hs], v[:, :, hs], dilation, window // 2 // dilation)
        outs.append(o)
        lses.append(lse)
    wts = jax.nn.softmax(jnp.stack(lses, axis=0), axis=0)
    o = jnp.sum(wts[..., None] * jnp.stack(outs, axis=0), axis=0)
    return o.reshape(B, T, ATT_OUT_W).astype(qkv.dtype)


def expert_choice_ffn(h, w_router, w_gate, w_up, w_down):
    B, T, D = h.shape
    cap = EC_CAPACITY_FACTOR * T // N_EXPERTS
    aff = jax.nn.softmax(h.astype(jnp.float32) @ w_router.astype(jnp.float32), axis=-1)
    gate, idx = lax.top_k(jnp.swapaxes(aff, 1, 2), cap)
    xe = jax.vmap(lambda hb, ib: hb[ib])(h, idx)
    a = jnp.einsum('becd,edf->becf', xe, w_gate)
    u = jnp.einsum('becd,edf->becf', xe, w_up)
    y = jnp.einsum('becf,efd->becd', jax.nn.silu(a) * u, w_down)
    y = y * gate[..., None].astype(y.dtype)
    return jax.vmap(lambda yb, ib: jnp.zeros((T, D), y.dtype).at[ib.reshape(-1)].add(yb.reshape(-1, D)))(y, idx)


def setup_inputs(seed: int = 0) -> dict:
    key = jax.random.key(seed)
    ks = jax.random.split(key, 20)
    f32 = jnp.float32

    def dense(k, shape, fan_in):
        return jax.random.normal(k, shape, f32) * (fan_in ** -0.5)

    def gain(k, shape):
        return 1.0 + 0.05 * jax.random.normal(k, shape, f32)

    x = jax.random.normal(ks[0], (BATCH, SEQ, D_MODEL), f32)
    dt = jnp.exp(jax.random.uniform(ks[5], (DEPTH, 2, GDN_HEADS), f32, np.log(1e-3), np.log(1e-1)))
    return {
        "x": x,
        "norm_mix": gain(ks[1], (DEPTH, D_MODEL)),
        "w_in": dense(ks[2], (DEPTH, D_MODEL, P_IN), D_MODEL),
        "conv_w": dense(ks[3], (DEPTH, CONV_WIDTH, GDN_CONV_CH), CONV_WIDTH),
        "a_log": jnp.log(jax.random.uniform(ks[4], (DEPTH, 2, GDN_HEADS), f32, 1.0, 16.0)),
        "dt_bias": dt + jnp.log(-jnp.expm1(-dt)),
        "gdn_norm": gain(ks[6], (DEPTH, GDN_DV)),
        "w_branch_a": dense(ks[7], (DEPTH, GDN_V_W, D_MODEL), GDN_V_W),
        "w_branch_b": dense(ks[8], (DEPTH, ATT_OUT_W, D_MODEL), ATT_OUT_W),
        "w_out": dense(ks[9], (DEPTH, D_MODEL, D_MODEL), D_MODEL),
        "norm_ffn": gain(ks[10], (DEPTH, D_MODEL)),
        "w_router": dense(ks[11], (DEPTH, D_MODEL, N_EXPERTS), D_MODEL),
        "w_expert_gate": dense(ks[12], (DEPTH, N_EXPERTS, D_MODEL, EXPERT_FF), D_MODEL),
        "w_expert_up": dense(ks[13], (DEPTH, N_EXPERTS, D_MODEL, EXPERT_FF), D_MODEL),
        "w_expert_down": dense(ks[14], (DEPTH, N_EXPERTS, EXPERT_FF, D_MODEL), EXPERT_FF),
        "norm_final": gain(ks[15], (D_MODEL,)),
    }


def reference(x, norm_mix, w_in, conv_w, a_log, dt_bias, gdn_norm, w_branch_a, w_branch_b, w_out,
              norm_ffn, w_router, w_expert_gate, w_expert_up, w_expert_down, norm_final):
    for layer in range(DEPTH):
        h = rms_norm(x, norm_mix[layer])
        proj = h @ w_in[layer]
        qkv_a, z_a, b_a, a_a, qkv_b, gates = jnp.split(
            proj, [OFF_Z, OFF_BETA, OFF_DECAY, OFF_ATT, OFF_GATE], axis=-1)
        y_a = gdn_branch(qkv_a, z_a, b_a, a_a, conv_w[layer], a_log[layer], dt_bias[layer], gdn_norm[layer])
        y_b = dilated_attention_branch(qkv_b)
        g = jax.nn.sigmoid(gates.astype(jnp.float32)).astype(x.dtype)
        mixed = g[..., :D_MODEL] * (y_a @ w_branch_a[layer]) + g[..., D_MODEL:] * (y_b @ w_branch_b[layer])
        x = x + mixed @ w_out[layer]
        h = rms_norm(x, norm_ffn[layer])
        x = x + expert_choice_ffn(h, w_router[layer], w_expert_gate[layer], w_expert_up[layer], w_expert_down[layer])
    return rms_norm(x, norm_final)
```

```python
import numpy as np
import concourse.bass as bass
import concourse.mybir as mybir
from concourse.bass_utils import run_bass_kernel_spmd
from contextlib import ExitStack

F32 = mybir.dt.float32
BF16 = mybir.dt.bfloat16
I32 = mybir.dt.int32
U32 = mybir.dt.uint32
AF = mybir.ActivationFunctionType
ALU = mybir.AluOpType
AX = mybir.AxisListType

import os as _os0
SAME_ENGINE_SYNC = not _os0.environ.get("NO_SES")
INTERLEAVE_B1_C = bool(_os0.environ.get("ILV"))
GDN_F32R = False

D_MODEL = 1024
P_IN = 6416
OFF_Z = 1536
OFF_BETA = 2048
OFF_ATT = 2064
OFF_V = OFF_ATT + 1536
OFF_GATE = 4368
RMS_EPS = 1e-6


class _Rec:
    def __init__(self):
        self.call = None

    def __getattr__(self, name):
        def f(*a, **k):
            self.call = (name, a, k)
            return self
        return f


def _ap_free(ap):
    try:
        sh = tuple(ap.shape)
        n = 1
        for v in sh[1:]:
            n *= int(v)
        return int(sh[0]), n
    except Exception:
        return 128, 128


_DT_SIZE = {}


def _dtsize(dt):
    if dt not in _DT_SIZE:
        _DT_SIZE[dt] = 2 if dt == BF16 else 4
    return _DT_SIZE[dt]


class Prog:
    def __init__(self, nc, es, defer=True):
        self.nc = nc
        self.es = es
        self.engs = {}
        for name in ["tensor", "vector", "scalar", "gpsimd", "sync"]:
            sem = es.enter_context(nc.semaphore(f"sem_{name}"))
            self.engs[name] = dict(h=getattr(nc, name), sem=sem, cnt=0, waited={}, name=name)
        self.rings = {}
        for q in ["sync", "gpsimd", "scalar"]:
            K = 8
            sems = [es.enter_context(nc.semaphore(f"dq_{q}_{i}")) for i in range(K)]
            self.rings[q] = dict(sems=sems, n=0, K=K, vals=[0] * K)
        self.lastw = {}
        self.readers = {}
        self.nwaits = 0
        self.nops = 0
        self.defer = defer
        self.pending = []

    def _wait(self, E, tok):
        sid, sem, val = tok
        if E["waited"].get(sid, 0) >= val:
            return
        if sid == id(E["sem"]):
            if E["name"] == "tensor" or (not SAME_ENGINE_SYNC and E["name"] != "gpsimd"):
                return
        E["h"].wait_ge(sem, val)
        E["waited"][sid] = val
        self.nwaits += 1

    def _deps(self, E, reads, writes):
        for k in reads:
            t = self.lastw.get(k)
            if t is not None:
                self._wait(E, t)
        for k in writes:
            t = self.lastw.get(k)
            if t is not None:
                self._wait(E, t)
            for t in self.readers.get(k, {}).values():
                self._wait(E, t)

    def _record(self, tok, reads, writes):
        for k in reads:
            d = self.readers.setdefault(k, {})
            d[tok[0]] = tok
        for k in writes:
            self.lastw[k] = tok
            self.readers[k] = {}

    def _emit_op(self, eng, call, reads, writes):
        E = self.engs[eng]
        self._deps(E, reads, writes)
        name, a, k = call
        inst = getattr(E["h"], name)(*a, **k)
        E["cnt"] += 1
        inst.then_inc(E["sem"], 1)
        tok = (id(E["sem"]), E["sem"], E["cnt"])
        self._record(tok, reads, writes)
        self.nops += 1

    def _emit_dma(self, q, call, reads, writes):
        E = self.engs[q]
        R = self.rings[q]
        i = R["n"] % R["K"]
        R["n"] += 1
        sem = R["sems"][i]
        if R["vals"][i] > 0:
            self._wait(E, (id(sem), sem, R["vals"][i]))
        self._deps(E, reads, writes)
        name, a, k = call
        inst = getattr(E["h"], name)(*a, **k)
        R["vals"][i] += 16
        inst.then_inc(sem, 16)
        tok = (id(sem), sem, R["vals"][i])
        self._record(tok, reads, writes)
        self.nops += 1

    @staticmethod
    def _cost(eng, call):
        name, a, k = call
        try:
            if eng == "tensor":
                if name == "transpose":
                    ap = k.get("in_", a[1] if len(a) > 1 else None)
                    p_, f_ = _ap_free(ap)
                    c = 0.03 + max(p_, 64) / 2400.0 * (2 if ap.dtype == F32 else 1)
                    return c, c + 0.15
                rhs = k.get("rhs", a[2] if len(a) > 2 else None)
                lhs = k.get("lhsT", a[1] if len(a) > 1 else None)
                _, n = _ap_free(rhs)
                passes = 4 if rhs.dtype == F32 else 1
                c = 0.02 + (max(n, 64) + 0) * passes / 2400.0 + (0.05 if passes == 4 else 0.0)
                return c, c + 0.15
            out = k.get("out", a[0] if a else None)
            _, f_ = _ap_free(out)
            if eng == "vector":
                c = 0.07 + f_ / 960.0
                if name in ("tensor_tensor", "scalar_tensor_tensor", "tensor_tensor_scan"):
                    c = 0.07 + f_ / 700.0
            elif eng == "scalar":
                c = 0.12 + f_ / 1100.0
            else:
                c = 0.16 + f_ / 900.0
            return c, c
        except Exception:
            return 0.3, 0.3

    def op(self, eng, fn, reads=(), writes=()):
        psr = [k for k in reads if isinstance(k, str) and k.startswith("ps")]
        if psr:
            reads = [k for k in reads if k not in psr]
            writes = list(writes) + psr
        rec = _Rec()
        fn(rec)
        occ, lat = self._cost(eng, rec.call)
        self.pending.append(("op", eng, rec.call, list(reads), list(writes), occ, lat))
        if not self.defer:
            self.flush()

    def dma(self, q, out, in_, reads=(), writes=(), fn=None, **kw):
        if fn is None:
            call = ("dma_start", (), dict(out=out, in_=in_, **kw))
            p_, f_ = _ap_free(out)
            nbytes = p_ * f_ * _dtsize(out.dtype)
        else:
            rec = _Rec()
            fn(rec)
            call = rec.call
            nbytes = 128 * 2048
        lat = 2.0 + nbytes / 120000.0
        if q == "gpsimd":
            lat += 3.0
        occ = 0.06 if q != "gpsimd" else 1.0
        self.pending.append(("dma", q, call, list(reads), list(writes), occ, lat))
        if not self.defer:
            self.flush()

    def flush(self, window=20):
        ops = self.pending
        self.pending = []
        n = len(ops)
        if n == 0:
            return
        if n < 4:
            order = range(n)
        else:
            order = self._schedule(ops, window)
        for j in order:
            kind, eng, call, reads, writes, occ, lat = ops[j]
            if kind == "op":
                self._emit_op(eng, call, reads, writes)
            else:
                self._emit_dma(eng, call, reads, writes)

    @staticmethod
    def _schedule(ops, W):
        n = len(ops)
        engof = [o[1] if o[0] == "op" else "q_" + o[1] for o in ops]
        lastw = {}
        readers = {}
        preds = [None] * n
        succ_eng = [None] * n
        for j, o in enumerate(ops):
            ps = set()
            for k in o[3]:
                t = lastw.get(k)
                if t is not None:
                    ps.add(t)
            for k in o[4]:
                t = lastw.get(k)
                if t is not None:
                    ps.add(t)
                for t in readers.get(k, ()):
                    ps.add(t)
            ps.discard(j)
            preds[j] = tuple(ps)
            for k in o[3]:
                readers.setdefault(k, []).append(j)
            for k in o[4]:
                lastw[k] = j
                readers[k] = []
        qhist = {}
        for j, o in enumerate(ops):
            if o[0] == "dma":
                h = qhist.setdefault(o[1], [])
                if len(h) >= 8:
                    preds[j] = preds[j] + (h[-8],)
                h.append(j)
        succs = [[] for _ in range(n)]
        for j in range(n):
            for pj in preds[j]:
                succs[pj].append(j)
        lists = {}
        for j in range(n):
            lists.setdefault(engof[j], []).append(j)
        head = {e: 0 for e in lists}
        done = [False] * n
        fin = [0.0] * n
        start = [0.0] * n
        tE = {e: 0.0 for e in lists}
        LATX, LATS = 0.35, 0.12
        cache = {}

        def candidate(e):
            lst = lists[e]
            h = head[e]
            L = len(lst)
            while h < L and done[lst[h]]:
                h += 1
            head[e] = h
            best = None
            cnt = 0
            i = h
            te = tE[e]
            while i < L and cnt < W:
                idx = lst[i]
                i += 1
                if done[idx]:
                    continue
                cnt += 1
                r = 0.0
                ok = True
                for pj in preds[idx]:
                    if not done[pj]:
                        ok = False
                        break
                    v = fin[pj] + (LATS if engof[pj] == e else LATX)
                    if v > r:
                        r = v
                if not ok:
                    continue
                s_ = r if r > te else te
                if best is None or s_ < best[0] - 1e-9:
                    best = (s_, idx)
                if r <= te:
                    break
            return best

        for e in lists:
            cache[e] = candidate(e)
        remaining = n
        out = []
        while remaining:
            bs = None
            be = None
            for e, cnd in cache.items():
                if cnd is not None and (bs is None or cnd[0] < bs[0] - 1e-9 or (abs(cnd[0] - bs[0]) <= 1e-9 and cnd[1] < bs[1])):
                    bs = cnd
                    be = e
            s_, idx = bs
            o = ops[idx]
            done[idx] = True
            start[idx] = s_
            fin[idx] = s_ + o[6]
            tE[be] = s_ + o[5]
            out.append(idx)
            remaining -= 1
            dirty = {be}
            for sj in succs[idx]:
                dirty.add(engof[sj])
            for e in dirty:
                cache[e] = candidate(e)
        out.sort(key=lambda j: (start[j], j))
        return out

    def barrier(self):
        self.flush()
        toks = []
        for E in self.engs.values():
            if E["cnt"] > 0:
                toks.append((id(E["sem"]), E["sem"], E["cnt"]))
        for R in self.rings.values():
            for i, sem in enumerate(R["sems"]):
                if R["vals"][i] > 0:
                    toks.append((id(sem), sem, R["vals"][i]))
        for E in self.engs.values():
            for t in toks:
                if t[0] != id(E["sem"]):
                    self._wait(E, t)
        self.lastw = {}
        self.readers = {}

    def finish(self):
        self.flush()
        E = self.engs["sync"]
        for X in self.engs.values():
            if X["cnt"] > 0 and X is not E:
                self._wait(E, (id(X["sem"]), X["sem"], X["cnt"]))
        for q, R in self.rings.items():
            for i, sem in enumerate(R["sems"]):
                if R["vals"][i] > 0:
                    self._wait(E, (id(sem), sem, R["vals"][i]))


class Ctx:
    pass


_UIDC = [0]


def _uid():
    _UIDC[0] += 1
    return _UIDC[0]


def alloc_common(p, es):
    nc = p.nc
    c = Ctx()
    c.ps = [es.enter_context(nc.psum_tensor(f"ps{i}", [128, 512], F32)) for i in range(8)]
    c.psb = c.ps[7][:, :].bitcast(BF16)
    c.identf = es.enter_context(nc.sbuf_tensor("identf", [128, 128], F32))
    c.identb = es.enter_context(nc.sbuf_tensor("identb", [128, 128], BF16))
    c.onesf = es.enter_context(nc.sbuf_tensor("onesf", [128, 128], F32))
    p.op("gpsimd", lambda e: e.memset(c.onesf[:], 1.0), writes=["onesf"])
    p.op("gpsimd", lambda e: e.memset(c.identf[:], 1.0), writes=["identf"])
    p.op("gpsimd", lambda e: e.affine_select(out=c.identf[:], in_=c.identf[:], pattern=[[-1, 128]],
                                              compare_op=ALU.is_equal, fill=0.0, base=0, channel_multiplier=1),
         reads=["identf"], writes=["identf"])
    p.op("vector", lambda e: e.tensor_copy(out=c.identb[:], in_=c.identf[:]), reads=["identf"], writes=["identb"])
    c.bias_q = es.enter_context(nc.sbuf_tensor("bias_q", [128, 1], F32))
    c.bias_k = es.enter_context(nc.sbuf_tensor("bias_k", [128, 1], F32))
    p.op("gpsimd", lambda e: e.memset(c.bias_q[:], 128.0e-6), writes=["bias_q"])
    p.op("gpsimd", lambda e: e.memset(c.bias_k[:], 1.0e-6), writes=["bias_k"])
    c.bias_one = es.enter_context(nc.sbuf_tensor("bias_one", [128, 1], F32))
    p.op("gpsimd", lambda e: e.memset(c.bias_one[:], 1.0), writes=["bias_one"])
    return c


def phase_a(p, c, T, x_src, w_in, norm_w, cos_t, sin_t, o):
    nc = p.nc
    NS = T // 512
    with ExitStack() as es:
        def sb(name, shape, dt):
            return es.enter_context(nc.sbuf_tensor(f"A_{_uid()}_" + name, shape, dt))
        NMAIN = 1536 + 528 + 768 + 2048
        Wm = sb("Wm", [128, 8, NMAIN], BF16)
        M_QKV, M_Z, M_BD, M_V, M_G = 0, 1536, 2048, 2064, 2832
        Wrope = sb("Wrope", [128, 8, 384], BF16)
        Wrest = sb("Wrest", [128, 8, 1152], BF16)
        Wp = sb("Wp", [128, 8, 384], BF16)
        stg = [sb(f"stg{i}", [128, 2048], F32) for i in range(2)]
        nwb = sb("nwb", [128, 1024], F32)
        xt = [sb(f"xt{i}", [128, 1024], F32) for i in range(2)]
        junk = sb("junk", [128, 1024], BF16)
        hb = sb("hb", [128, 1024], BF16)
        st = sb("st", [128, 8], F32)
        hT = [sb(f"hT{i}", [128, 8, 512], BF16) for i in range(2)]
        cs = [sb(f"cs{i}", [128, 2, 512], F32) for i in range(2)]
        of32 = [sb(f"of32_{i}", [128, 512], F32) for i in range(3)]
        ob16 = [sb(f"ob16_{i}", [128, 512], BF16) for i in range(3)]
        t1 = sb("t1", [128, 512], F32)
        t2 = sb("t2", [128, 512], F32)
        obd = sb("obd", [128, 4, 16], F32)

        p.dma("sync", nwb[:], norm_w.to_broadcast([128, 1024]), writes=["nwb"])
        pieces = [(0, 1536), (1536, 2064), (2064, 3600), (3600, 4368), (4368, 6416)]
        si = 0
        for k in range(8):
            for pi, (a, b) in enumerate(pieces):
                s = stg[si % 2]
                sk = f"stg{si % 2}"
                si += 1
                p.dma("sync", s[:, 0:b - a], w_in[k * 128:(k + 1) * 128, a:b], writes=[sk])
                if pi == 0:
                    p.op("gpsimd", lambda e: e.tensor_copy(out=Wm[:, k, M_QKV:M_QKV + 1536], in_=s[:, 0:1536]),
                         reads=[sk], writes=["Wm"])
                elif pi == 1:
                    p.op("gpsimd", lambda e: e.tensor_copy(out=Wm[:, k, M_Z:M_Z + 528], in_=s[:, 0:528]),
                         reads=[sk], writes=["Wm"])
                elif pi == 3:
                    p.op("gpsimd", lambda e: e.tensor_copy(out=Wm[:, k, M_V:M_V + 768], in_=s[:, 0:768]),
                         reads=[sk], writes=["Wm"])
                elif pi == 4:
                    p.op("gpsimd", lambda e: e.tensor_copy(out=Wm[:, k, M_G:M_G + 2048], in_=s[:, 0:2048]),
                         reads=[sk], writes=["Wm"])
                else:
                    sv = s[:, 0:1536].rearrange("p (h d) -> p h d", d=64)
                    p.op("gpsimd", lambda e: e.tensor_copy(
                        out=Wrope[:, k, :].rearrange("p (h d) -> p h d", d=16), in_=sv[:, :, 0:16]),
                        reads=[sk], writes=["Wrope"])
                    p.op("gpsimd", lambda e: e.tensor_copy(
                        out=Wrest[:, k, :].rearrange("p (h d) -> p h d", d=48), in_=sv[:, :, 16:64]),
                        reads=[sk], writes=["Wrest"])
                    wpv = Wp[:, k, :].rearrange("p (h d) -> p h d", d=16)
                    p.op("gpsimd", lambda e: e.tensor_scalar(out=wpv[:, :, 0:8], in0=sv[:, :, 8:16], scalar1=-1.0,
                                                             scalar2=None, op0=ALU.mult),
                         reads=[sk], writes=["Wp"])
                    p.op("gpsimd", lambda e: e.tensor_copy(out=wpv[:, :, 8:16], in_=sv[:, :, 0:8]),
                         reads=[sk], writes=["Wp"])

        rot = {"f": 0, "b": 0, "ps": 0, "ev": 0}

        def prep_front(s):
            slot = s % 2
            for ti in range(4):
                r0 = s * 512 + ti * 128
                xs = xt[ti % 2]
                xk = f"xt{ti % 2}"
                p.dma("sync", xs[:], x_src[r0:r0 + 128, :], writes=[xk])
                p.op("scalar", lambda e: e.activation(out=junk[:], in_=xs[:], func=AF.Square, accum_out=st[:, 0:1]),
                     reads=[xk], writes=["junk", "st0"])
                p.op("vector", lambda e: e.tensor_scalar(out=st[:, 1:2], in0=st[:, 0:1], scalar1=1.0 / 1024, scalar2=RMS_EPS,
                                                         op0=ALU.mult, op1=ALU.add), reads=["st0"], writes=["st1"])
                p.op("scalar", lambda e: e.activation(out=st[:, 2:3], in_=st[:, 1:2], func=AF.Sqrt), reads=["st1"], writes=["st2"])
                p.op("vector", lambda e: e.reciprocal(out=st[:, 3:4], in_=st[:, 2:3]), reads=["st2"], writes=["st3"])
                p.op("vector", lambda e: e.scalar_tensor_tensor(out=hb[:], in0=xs[:], scalar=st[:, 3:4], in1=nwb[:],
                                                                op0=ALU.mult, op1=ALU.mult),
                     reads=[xk, "st3", "nwb"], writes=["hb"])
                for k in range(8):
                    p.op("tensor", lambda e: e.transpose(out=c.psb[:, k * 128:(k + 1) * 128], in_=hb[:, k * 128:(k + 1) * 128],
                                                         identity=c.identb[:]),
                         reads=["hb", "identb"], writes=["psb"])
                p.op("scalar", lambda e: e.activation(out=hT[slot][:, :, ti * 128:(ti + 1) * 128],
                                                      in_=c.psb[:, :].rearrange("p (k t) -> p k t", t=128), func=AF.Copy),
                     reads=["psb"], writes=[f"hT{slot}"])

        def fm_group(s, Wsrc, wkey, col0, nchunks, dst, dst_row0, mode):
            slot = s % 2
            hk = f"hT{slot}"
            for ci in range(nchunks):
                pi = rot["ps"] % 6
                rot["ps"] += 1
                ps = c.ps[pi]
                pk = f"ps{pi}"
                for k in range(8):
                    p.op("tensor", lambda e: e.matmul(ps[:, :], lhsT=Wsrc[:, k, col0 + ci * 128: col0 + (ci + 1) * 128],
                                                      rhs=hT[slot][:, k, :], start=(k == 0), stop=(k == 7)),
                         reads=[wkey, hk], writes=[pk])
                if mode == "f32":
                    oi = rot["f"] % 3
                    rot["f"] += 1
                    ot, ok = of32[oi], f"of32_{oi}"
                else:
                    oi = rot["b"] % 3
                    rot["b"] += 1
                    ot, ok = ob16[oi], f"ob16_{oi}"
                ev = "vector" if rot["ev"] % 2 == 0 else "scalar"
                rot["ev"] += 1
                if mode == "sig":
                    p.op("scalar", lambda e: e.activation(out=ot[:], in_=ps[:, :], func=AF.Sigmoid), reads=[pk], writes=[ok])
                elif ev == "vector":
                    p.op("vector", lambda e: e.tensor_copy(out=ot[:], in_=ps[:, :]), reads=[pk], writes=[ok])
                else:
                    p.op("scalar", lambda e: e.activation(out=ot[:], in_=ps[:, :], func=AF.Copy), reads=[pk], writes=[ok])
                r = dst_row0 + ci * 128
                p.dma("sync", dst(r, s), ot[:], reads=[ok])

        def body(s):
            slot = s % 2
            hk = f"hT{slot}"
            p.dma("sync", cs[slot][:, 0, :], cos_t[:, s * 512:(s + 1) * 512], writes=[f"cs{slot}"])
            p.dma("sync", cs[slot][:, 1, :], sin_t[:, s * 512:(s + 1) * 512], writes=[f"cs{slot}"])
            fm_group(s, Wm, "Wm", M_QKV, 12, lambda r, s_: o["qkvTa"][r:r + 128, 2 + s_ * 512: 2 + (s_ + 1) * 512], 0, "bf16")
            for ti in range(4):
                pi = rot["ps"] % 6
                rot["ps"] += 1
                ps = c.ps[pi]
                pk = f"ps{pi}"
                for k in range(8):
                    p.op("tensor", lambda e: e.matmul(ps[:, :], lhsT=hT[slot][:, k, ti * 128:(ti + 1) * 128],
                                                      rhs=Wm[:, k, M_Z:M_Z + 512], start=(k == 0), stop=(k == 7)),
                         reads=["Wm", hk], writes=[pk])
                oi = rot["f"] % 3
                rot["f"] += 1
                p.op("vector", lambda e: e.tensor_copy(out=of32[oi][:], in_=ps[:, :]), reads=[pk], writes=[f"of32_{oi}"])
                r0 = s * 512 + ti * 128
                p.dma("sync", o["ztok"][r0:r0 + 128, :], of32[oi][:], reads=[f"of32_{oi}"])
            ps = c.ps[6]
            for ti in range(4):
                for k in range(8):
                    p.op("tensor", lambda e: e.matmul(ps[:, ti * 16:(ti + 1) * 16], lhsT=hT[slot][:, k, ti * 128:(ti + 1) * 128],
                                                      rhs=Wm[:, k, M_BD:M_BD + 16], start=(k == 0), stop=(k == 7)),
                         reads=["Wm", hk], writes=["ps6"])
            p.op("vector", lambda e: e.tensor_copy(out=obd[:, :, :], in_=ps[:, 0:64].rearrange("p (t c) -> p t c", c=16)),
                 reads=["ps6"], writes=["obd"])
            p.dma("sync", o["bdtok"][s * 512:(s + 1) * 512, :].rearrange("(t p) c -> p t c", p=128), obd[:, :, :],
                  reads=["obd"])
            pi = rot["ps"] % 6
            rot["ps"] += 1
            for k in range(8):
                p.op("tensor", lambda e: e.matmul(c.ps[pi][0:16, :], lhsT=Wm[:, k, M_BD:M_BD + 16],
                                                  rhs=hT[slot][:, k, :], start=(k == 0), stop=(k == 7)),
                     reads=["Wm", hk], writes=[f"ps{pi}"])
            oi = rot["f"] % 3
            rot["f"] += 1
            p.op("vector", lambda e: e.tensor_copy(out=of32[oi][0:16, :], in_=c.ps[pi][0:16, :]), reads=[f"ps{pi}"], writes=[f"of32_{oi}"])
            p.dma("sync", o["bdT"][:, s * 512:(s + 1) * 512], of32[oi][0:16, :], reads=[f"of32_{oi}"])
            for ci in range(3):
                pa = rot["ps"] % 6
                rot["ps"] += 1
                pb = rot["ps"] % 6
                rot["ps"] += 1
                for k in range(8):
                    p.op("tensor", lambda e: e.matmul(c.ps[pa][:, :], lhsT=Wrope[:, k, ci * 128:(ci + 1) * 128],
                                                      rhs=hT[slot][:, k, :], start=(k == 0), stop=(k == 7)),
                         reads=["Wrope", hk], writes=[f"ps{pa}"])
                for k in range(8):
                    p.op("tensor", lambda e: e.matmul(c.ps[pb][:, :], lhsT=Wp[:, k, ci * 128:(ci + 1) * 128],
                                                      rhs=hT[slot][:, k, :], start=(k == 0), stop=(k == 7)),
                         reads=["Wp", hk], writes=[f"ps{pb}"])
                p.op("vector", lambda e: e.tensor_tensor(out=t1[:], in0=c.ps[pa][:, :], in1=cs[slot][:, 0, :], op=ALU.mult),
                     reads=[f"ps{pa}", f"cs{slot}"], writes=["t1"])
                p.op("vector", lambda e: e.tensor_tensor(out=t2[:], in0=c.ps[pb][:, :], in1=cs[slot][:, 1, :], op=ALU.mult),
                     reads=[f"ps{pb}", f"cs{slot}"], writes=["t2"])
                oi = rot["b"] % 3
                rot["b"] += 1
                p.op("gpsimd", lambda e: e.tensor_tensor(out=ob16[oi][:], in0=t1[:], in1=t2[:], op=ALU.add),
                     reads=["t1", "t2"], writes=[f"ob16_{oi}"])
                p.dma("sync", o["qkrope"][ci * 128:(ci + 1) * 128, s * 512:(s + 1) * 512], ob16[oi][:],
                      reads=[f"ob16_{oi}"])
            fm_group(s, Wrest, "Wrest", 0, 9, lambda r, s_: o["qkrest"][r:r + 128, s_ * 512:(s_ + 1) * 512], 0, "bf16")
            fm_group(s, Wm, "Wm", M_V, 6, lambda r, s_: o["vTb"][r:r + 128, s_ * 512:(s_ + 1) * 512], 0, "bf16")
            fm_group(s, Wm, "Wm", M_G, 16, lambda r, s_: o["gatesT"][r:r + 128, s_ * 512:(s_ + 1) * 512], 0, "sig")

        prep_front(0)
        for s in range(NS):
            body(s)
            if s + 1 < NS:
                prep_front(s + 1)
        p.barrier()


def phase_b1(p, c, T, qkvTa, conv_w, o):
    for _ in phase_b1_gen(p, c, T, qkvTa, conv_w, o):
        pass


def phase_b1_gen(p, c, T, qkvTa, conv_w, o, es_ext=None, shared=False):
    nc = p.nc
    NS = T // 512
    with ExitStack() as es_own:
        es = es_ext if es_ext is not None else es_own
        def sb(name, shape, dt):
            return es.enter_context(nc.sbuf_tensor(f"B1_{_uid()}_" + name, shape, dt))
        cw = sb("cw", [128, 12, 5], F32)
        uin = [sb(f"uin{i}", [128, 516], BF16) for i in range(3)]
        Dg = sb("Dg", [128, 60, 128], BF16)
        su = [sb(f"su{i}", [128, 512], F32) for i in range(2)]
        sq = sb("sq", [128, 512], BF16)
        onesb = sb("onesb", [128, 128], BF16)
        svb = [sb(f"svb{i}", [128, 512], BF16) for i in range(2)]
        rn = sb("rn", [128, 512], F32)
        rn2 = sb("rn2", [128, 512], F32)
        un = [sb(f"un{i}", [128, 512], F32) for i in range(2)]
        tk = [sb(f"tk{i}", [128, 4, 128], BF16) for i in range(2)]
        p.op("vector", lambda e: e.tensor_copy(out=onesb[:], in_=c.onesf[:]), reads=["onesf"], writes=["onesb"])
        for j in range(5):
            p.dma("sync", cw[:, :, j:j + 1], conv_w[j:j + 1, :].rearrange("j (k c) -> c k j", c=128), writes=["cw"], allow_slow_non_contiguous=True)
        for ch in range(12):
            for j in range(5):
                p.op("vector", lambda e: e.tensor_scalar(out=Dg[:, ch * 5 + j, :], in0=c.identb[:], scalar1=cw[:, ch, j:j + 1], scalar2=None, op0=ALU.mult),
                     reads=["cw", "identb"], writes=["Dg"])
        n = 0
        for s in range(NS):
            for ch in range(12):
                ui, uk = uin[n % 3], f"uin{n % 3}"
                sv, sk = su[n % 2], f"su{n % 2}"
                u, unk = un[n % 2], f"un{n % 2}"
                if shared:
                    cb, pi, tb = 5, 6, 7
                else:
                    cb = n % 3
                    pi = 3 + n % 3
                    tb = 6 + n % 2
                n += 1
                p.dma("sync", ui[:], qkvTa[ch * 128:(ch + 1) * 128, s * 512: s * 512 + 516], writes=[uk])
                for j in range(5):
                    p.op("tensor", lambda e: e.matmul(c.ps[cb][:, :], lhsT=Dg[:, ch * 5 + j, :], rhs=ui[:, j:j + 512], start=(j == 0), stop=(j == 4)),
                         reads=[uk, "Dg"], writes=[f"ps{cb}"])
                p.op("scalar", lambda e: e.activation(out=sv[:], in_=c.ps[cb][:, :], func=AF.Silu), reads=[f"ps{cb}"], writes=[sk])
                if ch < 8:
                    p.op("gpsimd", lambda e: e.tensor_tensor(out=sq[:], in0=sv[:], in1=sv[:], op=ALU.mult), reads=[sk], writes=["sq"])
                    ps = c.ps[pi]
                    p.op("tensor", lambda e: e.matmul(ps[:, :], lhsT=onesb[:], rhs=sq[:], start=True, stop=True),
                         reads=["sq", "onesb"], writes=[f"ps{pi}"])
                    if ch < 4:
                        p.op("scalar", lambda e: e.activation(out=rn[:], in_=ps[:, :], func=AF.Sqrt, scale=128.0, bias=c.bias_q[:, 0:1]),
                             reads=[f"ps{pi}", "bias_q"], writes=["rn"])
                    else:
                        p.op("scalar", lambda e: e.activation(out=rn[:], in_=ps[:, :], func=AF.Sqrt, scale=1.0, bias=c.bias_k[:, 0:1]),
                             reads=[f"ps{pi}", "bias_k"], writes=["rn"])
                    p.op("vector", lambda e: e.reciprocal(out=rn2[:], in_=rn[:]), reads=["rn"], writes=["rn2"])
                    p.op("gpsimd", lambda e: e.tensor_tensor(out=u[:], in0=sv[:], in1=rn2[:], op=ALU.mult), reads=[sk, "rn2"], writes=[unk])
                    p.dma("sync", o["uT"][ch * 128:(ch + 1) * 128, s * 512:(s + 1) * 512], u[:], reads=[unk])
                    src, srck = u, unk
                else:
                    src, srck = sv, sk
                if ch >= 4:
                    pj = tb
                    sb_, sbk = svb[n % 2], f"svb{n % 2}"
                    p.op("gpsimd", lambda e: e.tensor_copy(out=sb_[:], in_=src[:]), reads=[srck], writes=[sbk])
                    pjb = c.ps[pj][:, :].bitcast(BF16)
                    for ti in range(4):
                        p.op("tensor", lambda e: e.transpose(out=pjb[:, ti * 128:(ti + 1) * 128], in_=sb_[:, ti * 128:(ti + 1) * 128],
                                                             identity=c.identb[:]), reads=[sbk, "identb"], writes=[f"ps{pj}"])
                    t_, tkk = tk[n % 2], f"tk{n % 2}"
                    p.op("scalar", lambda e: e.activation(out=t_[:, :, :], in_=pjb[:, 0:512].rearrange("p (t d) -> p t d", d=128), func=AF.Copy),
                         reads=[f"ps{pj}"], writes=[tkk])
                    dst = o["ktok"] if ch < 8 else o["vtok"]
                    hh = ch % 4
                    p.dma("sync", dst[s * 512:(s + 1) * 512, hh * 128:(hh + 1) * 128].rearrange("(t p) d -> p t d", p=128), t_[:, :, :],
                          reads=[tkk])
                yield
        if not shared:
            p.barrier()


def alloc_gdn_consts(p, c, es):
    nc = p.nc
    def mk(name, fillval_in, pattern, cm, cmp, fill):
        t = es.enter_context(nc.sbuf_tensor(name, [128, 128], F32))
        p.op("gpsimd", lambda e: e.memset(t[:], fillval_in), writes=[name])
        p.op("gpsimd", lambda e: e.affine_select(out=t[:], in_=t[:], pattern=pattern, compare_op=cmp, fill=fill,
                                                  base=0, channel_multiplier=cm), reads=[name], writes=[name])
        if GDN_F32R and name.startswith("tri"):
            t2 = es.enter_context(nc.sbuf_tensor(name + "r", [128, 128], F32))
            p.op("vector", lambda e: e.tensor_copy(out=t2[:].bitcast(mybir.dt.float32r), in_=t[:]), reads=[name], writes=[name])
            return t2
        return t
    c.triU = mk("triU", 1.0, [[1, 128]], -1, ALU.is_ge, 0.0)
    c.triL = mk("triL", 1.0, [[-1, 128]], 1, ALU.is_ge, 0.0)
    c.nm1k = ["nm1f", "nm1b"]
    c.nm2k = ["nm2f", "nm2b"]
    c.nm1 = [mk("nm1f", 0.0, [[-1, 128]], 1, ALU.is_gt, 1.0e4),
             mk("nm1b", 0.0, [[1, 128]], -1, ALU.is_gt, 1.0e4)]
    c.nm2 = [mk("nm2f", 0.0, [[1, 128]], -1, ALU.is_ge, -1.0e4),
             mk("nm2b", 0.0, [[-1, 128]], 1, ALU.is_ge, -1.0e4)]


def phase_b2(p, c, T, uT, ktok, vtok, bdtok, a_log, dt_bias, o_f, o_b):
    nc = p.nc
    NT = T // 128
    F32R = mybir.dt.float32r
    USE_R = GDN_F32R

    def R(ap):
        return ap.bitcast(F32R) if USE_R else ap
    with ExitStack() as es:
        def sb(name, shape, dt=F32):
            return es.enter_context(nc.sbuf_tensor(f"B2_{_uid()}_" + name, shape, dt))
        bd = sb("bd", [128, NT, 16])
        beta = sb("beta", [128, NT, 8])
        nbeta = sb("nbeta", [128, NT, 8])
        g = sb("g", [128, NT, 8])
        G = sb("G", [128, NT, 8])
        tmp = sb("tmp", [128, NT, 8])
        eGr = sb("eGr", [128, NT, 8])
        ge = sb("ge", [128, NT, 8])
        bG = sb("bG", [128, NT, 8])
        dtb = sb("dtb", [128, 8])
        nea = sb("nea", [128, 8])
        for t0 in range(0, NT, 8):
            t1 = min(NT, t0 + 8)
            p.dma("sync", bd[:, t0:t1, :], bdtok[t0 * 128:t1 * 128, :].rearrange("(t p) c -> p t c", p=128), writes=["bd"])
        p.dma("sync", dtb[:], dt_bias.to_broadcast([128, 8]), writes=["dtb"])
        p.dma("sync", nea[:], a_log.to_broadcast([128, 8]), writes=["nea"])
        p.op("scalar", lambda e: e.activation(out=nea[:], in_=nea[:], func=AF.Exp), reads=["nea"], writes=["nea"])
        p.op("vector", lambda e: e.tensor_scalar(out=nea[:], in0=nea[:], scalar1=-1.0, scalar2=None, op0=ALU.mult), reads=["nea"], writes=["nea"])
        p.op("scalar", lambda e: e.activation(out=beta[:, :, :], in_=bd[:, :, 0:8], func=AF.Sigmoid), reads=["bd"], writes=["beta"])
        p.op("vector", lambda e: e.tensor_scalar(out=nbeta[:, :, :], in0=beta[:, :, :], scalar1=-1.0, scalar2=None, op0=ALU.mult),
             reads=["beta"], writes=["nbeta"])
        p.op("vector", lambda e: e.tensor_tensor(out=tmp[:, :, :], in0=bd[:, :, 8:16], in1=dtb[:, :].unsqueeze(1).to_broadcast([128, NT, 8]), op=ALU.add),
             reads=["bd", "dtb"], writes=["tmp"])
        p.op("scalar", lambda e: e.activation(out=tmp[:, :, :], in_=tmp[:, :, :], func=AF.Exp), reads=["tmp"], writes=["tmp"])
        p.op("scalar", lambda e: e.activation(out=tmp[:, :, :], in_=tmp[:, :, :], func=AF.Ln, bias=c.bias_one[:, 0:1], scale=1.0), reads=["tmp", "bias_one"], writes=["tmp"])
        p.op("vector", lambda e: e.tensor_tensor(out=R(g[:, :, :]), in0=tmp[:, :, :], in1=nea[:, :].unsqueeze(1).to_broadcast([128, NT, 8]), op=ALU.mult),
             reads=["tmp", "nea"], writes=["g"])
        for d, tri, tk in ((0, c.triU, "triU"), (1, c.triL, "triL")):
            for t0 in range(0, NT, 64):
                t1 = min(NT, t0 + 64)
                p.op("tensor", lambda e: e.matmul(c.ps[d][:, 0:(t1 - t0) * 4], lhsT=tri[:], rhs=g[:, t0:t1, d * 4:(d + 1) * 4], start=True, stop=True),
                     reads=["g", tk], writes=[f"ps{d}"])
                p.op("vector", lambda e: e.tensor_copy(out=G[:, t0:t1, d * 4:(d + 1) * 4],
                                                       in_=c.ps[d][:, 0:(t1 - t0) * 4].rearrange("p (t c) -> p t c", c=4)),
                     reads=[f"ps{d}"], writes=["G"])
        for t0 in range(0, NT, 64):
            t1 = min(NT, t0 + 64)
            p.op("tensor", lambda e: e.matmul(c.ps[2][:, 0:(t1 - t0) * 8], lhsT=c.onesf[:], rhs=g[:, t0:t1, :], start=True, stop=True),
                 reads=["g", "onesf"], writes=["ps2"])
            p.op("scalar", lambda e: e.activation(out=ge[:, t0:t1, :], in_=c.ps[2][:, 0:(t1 - t0) * 8].rearrange("p (t c) -> p t c", c=8), func=AF.Exp),
                 writes=["ps2", "ge"])
            p.op("vector", lambda e: e.tensor_tensor(out=eGr[:, t0:t1, :], in0=c.ps[2][:, 0:(t1 - t0) * 8].rearrange("p (t c) -> p t c", c=8),
                                                     in1=G[:, t0:t1, :], op=ALU.subtract), reads=["G"], writes=["ps2", "eGr"])
        p.op("scalar", lambda e: e.activation(out=eGr[:, :, :], in_=eGr[:, :, :], func=AF.Exp), reads=["eGr"], writes=["eGr"])
        p.op("scalar", lambda e: e.activation(out=bG[:, :, :], in_=G[:, :, :], func=AF.Exp), reads=["G"], writes=["bG"])
        p.op("vector", lambda e: e.tensor_tensor(out=bG[:, :, :], in0=bG[:, :, :], in1=beta[:, :, :], op=ALU.mult), reads=["bG", "beta"], writes=["bG"])
        p.barrier()

        U = []
        for hd in range(8):
            S_ = sb(f"S_{hd}", [128, 128])
            Sb_ = sb(f"Sb_{hd}", [128, 128], BF16)
            two = []
            for pp in range(2):
                u = {"S": S_, "Sb": Sb_}
                for nm in ["D1", "D2T", "eB", "X0", "X1"]:
                    u[nm] = sb(f"{nm}_{hd}_{pp}", [128, 128])
                for nm in ["XP0", "XP1"]:
                    u[nm] = sb(f"{nm}_{hd}_{pp}", [128, 256])
                for nm in ["aaT", "qdT", "TTb", "TTbg", "nwT", "vnew", "vs"]:
                    u[nm] = sb(f"{nm}_{hd}_{pp}", [128, 128], BF16)
                two.append(u)
            U.append(two)
            p.op("gpsimd", lambda e: e.memset(S_[:], 0.0), writes=[f"S_{hd}"])
            p.op("gpsimd", lambda e: e.memset(Sb_[:], 0.0), writes=[f"Sb_{hd}"])
        inb = []
        for b_ in range(2):
            d_ = {}
            for dr in range(2):
                d_[dr] = dict(kq=sb(f"kq{b_}{dr}", [128, 4, 2, 128]),
                              ktb=sb(f"ktb{b_}{dr}", [128, 512], BF16), vtb=sb(f"vtb{b_}{dr}", [128, 512], BF16),
                              o=sb(f"o{b_}{dr}", [128, 512]))
            inb.append(d_)

        bn = [0]

        def bank():
            i = bn[0] % 8
            bn[0] += 1
            return c.ps[i], f"ps{i}"

        def Q(bk, q):
            return bk[:, q * 128:(q + 1) * 128]

        def ev(eng, out_ap, in_ap, rk, wk, scale=None):
            if eng == "scalar":
                if scale is None:
                    p.op("scalar", lambda e: e.activation(out=out_ap, in_=in_ap, func=AF.Copy), reads=rk, writes=wk)
                else:
                    p.op("scalar", lambda e: e.activation(out=out_ap, in_=in_ap, func=AF.Copy, scale=scale), reads=rk, writes=wk)
            else:
                if scale is None:
                    p.op("vector", lambda e: e.tensor_copy(out=out_ap, in_=in_ap), reads=rk, writes=wk)
                else:
                    p.op("vector", lambda e: e.tensor_scalar(out=out_ap, in0=in_ap, scalar1=scale, scalar2=None, op0=ALU.mult), reads=rk, writes=wk)

        def XT(u, par):
            return u[f"XP{par}"][:, 0:128]

        def PT(u, j):
            return u[f"XP{1 - j % 2}"][:, 128:256]

        for n in range(NT):
            bs = n % 2
            pp = n % 2
            tiles = (n, NT - 1 - n)
            for dr in range(2):
                i = tiles[dr]
                ib = inb[bs][dr]
                for h_ in range(4):
                    p.dma("sync", ib["kq"][:, h_, 0, :], uT[512 + h_ * 128:512 + (h_ + 1) * 128, i * 128:(i + 1) * 128], writes=[f"kq{bs}{dr}"])
                    p.dma("sync", ib["kq"][:, h_, 1, :], uT[h_ * 128:(h_ + 1) * 128, i * 128:(i + 1) * 128], writes=[f"kq{bs}{dr}"])
                p.dma("sync", ib["ktb"][:, :], ktok[i * 128:(i + 1) * 128, :], writes=[f"ktb{bs}{dr}"])
                p.dma("sync", ib["vtb"][:, :], vtok[i * 128:(i + 1) * 128, :], writes=[f"vtb{bs}{dr}"])
            st = {}
            for dr in range(2):
                i = tiles[dr]
                ib = inb[bs][dr]
                tri, trik = (c.triU, "triU") if dr == 0 else (c.triL, "triL")
                bB, bBk = bank()
                bK = [bank(), bank()]
                st[dr] = (bB, bBk, bK)
                for h in range(4):
                    hd = dr * 4 + h
                    p.op("tensor", lambda e: e.matmul(Q(bB, h), lhsT=g[:, i, hd:hd + 1].to_broadcast([128, 128]), rhs=tri[:], start=True, stop=True),
                         reads=["g", trik], writes=[bBk])
                for h in range(4):
                    bk_, bkk = bK[h // 2]
                    p.op("tensor", lambda e: e.matmul(bk_[:, (h % 2) * 256:(h % 2) * 256 + 256], lhsT=ib["kq"][:, h, 0, :],
                                                      rhs=ib["kq"][:, h, :, :].rearrange("p a t -> p (a t)"), start=True, stop=True),
                         reads=[f"kq{bs}{dr}"], writes=[bkk])
            for dr in range(2):
                i = tiles[dr]
                ib = inb[bs][dr]
                bB, bBk, bK = st[dr]
                for h in range(4):
                    hd = dr * 4 + h
                    u = U[hd][pp]
                    Gc = G[:, i, hd:hd + 1]
                    p.op("vector", lambda e: e.scalar_tensor_tensor(out=u["D1"][:], in0=Q(bB, h), scalar=Gc, in1=c.nm1[dr][:], op0=ALU.subtract, op1=ALU.max),
                         reads=["G", c.nm1k[dr]], writes=[bBk, f"D1_{hd}p{pp}"])
                    p.op("scalar", lambda e: e.activation(out=u["D1"][:], in_=u["D1"][:], func=AF.Exp, scale=-1.0), reads=[f"D1_{hd}p{pp}"], writes=[f"D1_{hd}p{pp}"])
                    p.op("vector", lambda e: e.scalar_tensor_tensor(out=u["D2T"][:], in0=Q(bB, h), scalar=Gc, in1=c.nm2[dr][:], op0=ALU.subtract, op1=ALU.min),
                         reads=["G", c.nm2k[dr]], writes=[bBk, f"D2T_{hd}p{pp}"])
                    p.op("scalar", lambda e: e.activation(out=u["D2T"][:], in_=u["D2T"][:], func=AF.Exp), reads=[f"D2T_{hd}p{pp}"], writes=[f"D2T_{hd}p{pp}"])
                for h in range(4):
                    hd = dr * 4 + h
                    u = U[hd][pp]
                    p.op("scalar", lambda e: e.activation(out=u["eB"][:], in_=Q(bB, h), func=AF.Exp), writes=[bBk, f"eB_{hd}p{pp}"])
                for h in range(4):
                    hd = dr * 4 + h
                    u = U[hd][pp]
                    bk_, bkk = bK[h // 2]
                    o0 = (h % 2) * 256
                    p.op("vector", lambda e: e.scalar_tensor_tensor(out=u["X0"][:], in0=bk_[:, o0:o0 + 128], scalar=nbeta[:, i, hd:hd + 1], in1=u["D1"][:],
                                                                    op0=ALU.mult, op1=ALU.mult), reads=["nbeta", f"D1_{hd}p{pp}"], writes=[bkk, f"X0_{hd}p{pp}"])
                    p.op("vector", lambda e: e.tensor_tensor(out=u["aaT"][:], in0=bk_[:, o0 + 128:o0 + 256], in1=u["D2T"][:], op=ALU.mult),
                         reads=[f"D2T_{hd}p{pp}"], writes=[bkk, f"aaT_{hd}p{pp}"])
                    p.op("gpsimd", lambda e: e.tensor_tensor(out=u["qdT"][:], in0=ib["kq"][:, h, 1, :], in1=u["eB"][:], op=ALU.mult),
                         reads=[f"kq{bs}{dr}", f"eB_{hd}p{pp}"], writes=[f"qdT_{hd}p{pp}"])
            for dr in range(2):
                bT, bTk = bank()
                for h in range(4):
                    hd = dr * 4 + h
                    u = U[hd][pp]
                    p.op("tensor", lambda e: e.transpose(out=Q(bT, h), in_=u["X0"][:], identity=c.identf[:]), reads=[f"X0_{hd}p{pp}", "identf"], writes=[bTk])
                for h in range(4):
                    hd = dr * 4 + h
                    u = U[hd][pp]
                    p.op("scalar", lambda e: e.activation(out=XT(u, 0), in_=Q(bT, h), func=AF.Copy), writes=[bTk, f"XT0_{hd}p{pp}"])
                    p.op("gpsimd", lambda e: e.tensor_tensor(out=PT(u, 0), in0=XT(u, 0), in1=c.identf[:], op=ALU.add),
                         reads=[f"XT0_{hd}p{pp}", "identf"], writes=[f"PT0_{hd}p{pp}"])
            for k in range(1, 7):
                a_, b_ = (k - 1) % 2, k % 2
                for dr in range(2):
                    e1, e2 = ("scalar", "vector") if (k + dr) % 3 != 0 else ("scalar", "scalar")
                    b1, b1k = bank()
                    for h in range(4):
                        hd = dr * 4 + h
                        u = U[hd][pp]
                        p.op("tensor", lambda e: e.matmul(Q(b1, h), lhsT=XT(u, a_), rhs=u[f"X{a_}"][:], start=True, stop=True),
                             reads=[f"XT{a_}_{hd}p{pp}", f"X{a_}_{hd}p{pp}"], writes=[b1k])
                    bM = [bank(), bank()]
                    for h in range(4):
                        hd = dr * 4 + h
                        u = U[hd][pp]
                        bm_, bmk = bM[h // 2]
                        o0 = (h % 2) * 256
                        if k == 1:
                            p.op("tensor", lambda e: e.matmul(bm_[:, o0:o0 + 128], lhsT=u[f"X{a_}"][:], rhs=XT(u, a_), start=True, stop=True),
                                 reads=[f"XT{a_}_{hd}p{pp}", f"X{a_}_{hd}p{pp}"], writes=[bmk])
                        else:
                            p.op("tensor", lambda e: e.matmul(bm_[:, o0:o0 + 256], lhsT=u[f"X{a_}"][:], rhs=u[f"XP{a_}"][:, :], start=True, stop=True),
                                 reads=[f"XT{a_}_{hd}p{pp}", f"X{a_}_{hd}p{pp}", f"PT{(k - 2) % 2}_{hd}p{pp}"], writes=[bmk])
                    for h in range(4):
                        hd = dr * 4 + h
                        u = U[hd][pp]
                        ev(e1, u[f"X{b_}"][:], Q(b1, h), [], [b1k, f"X{b_}_{hd}p{pp}"])
                    for h in range(4):
                        hd = dr * 4 + h
                        u = U[hd][pp]
                        bm_, bmk = bM[h // 2]
                        o0 = (h % 2) * 256
                        if k < 6:
                            ev(e2, XT(u, b_), bm_[:, o0:o0 + 128], [], [bmk, f"XT{b_}_{hd}p{pp}"])
                        if k >= 2:
                            p.op("vector", lambda e: e.tensor_tensor(out=PT(u, k - 1), in0=bm_[:, o0 + 128:o0 + 256], in1=PT(u, k - 2), op=ALU.add),
                                 reads=[f"PT{(k - 2) % 2}_{hd}p{pp}"], writes=[bmk, f"PT{(k - 1) % 2}_{hd}p{pp}"])
            for dr in range(2):
                bF, bFk = bank()
                for h in range(4):
                    hd = dr * 4 + h
                    u = U[hd][pp]
                    p.op("tensor", lambda e: e.matmul(Q(bF, h), lhsT=u["X0"][:], rhs=PT(u, 5), start=True, stop=True),
                         reads=[f"X0_{hd}p{pp}", f"PT1_{hd}p{pp}"], writes=[bFk])
                for h in range(4):
                    hd = dr * 4 + h
                    u = U[hd][pp]
                    p.op("vector", lambda e: e.tensor_tensor(out=PT(u, 6), in0=Q(bF, h), in1=PT(u, 5), op=ALU.add),
                         reads=[f"PT1_{hd}p{pp}"], writes=[bFk, f"PT0_{hd}p{pp}"])
            for dr in range(2):
                i = tiles[dr]
                ib = inb[bs][dr]
                for h in range(4):
                    hd = dr * 4 + h
                    u = U[hd][pp]
                    p.op("scalar", lambda e: e.activation(out=u["TTb"][:], in_=PT(u, 6), func=AF.Copy, scale=beta[:, i, hd:hd + 1]),
                         reads=[f"PT0_{hd}p{pp}", "beta"], writes=[f"TTb_{hd}p{pp}"])
                    p.op("vector", lambda e: e.tensor_scalar(out=u["TTbg"][:], in0=PT(u, 6), scalar1=bG[:, i, hd:hd + 1], scalar2=None, op0=ALU.mult),
                         reads=[f"PT0_{hd}p{pp}", "bG"], writes=[f"TTbg_{hd}p{pp}"])
                bw, bwk = bank()
                for h in range(4):
                    hd = dr * 4 + h
                    u = U[hd][pp]
                    p.op("tensor", lambda e: e.matmul(Q(bw, h), lhsT=ib["ktb"][:, h * 128:(h + 1) * 128], rhs=u["TTbg"][:], start=True, stop=True),
                         reads=[f"ktb{bs}{dr}", f"TTbg_{hd}p{pp}"], writes=[bwk])
                for h in range(4):
                    hd = dr * 4 + h
                    u = U[hd][pp]
                    ev("scalar" if dr == 0 else "vector", u["nwT"][:], Q(bw, h), [], [bwk, f"nwT_{hd}p{pp}"], scale=-1.0)
                bv, bvk = bank()
                for h in range(4):
                    hd = dr * 4 + h
                    u = U[hd][pp]
                    p.op("tensor", lambda e: e.matmul(Q(bv, h), lhsT=u["TTb"][:], rhs=ib["vtb"][:, h * 128:(h + 1) * 128], start=True, stop=False),
                         reads=[f"vtb{bs}{dr}", f"TTb_{hd}p{pp}"], writes=[bvk])
                    p.op("tensor", lambda e: e.matmul(Q(bv, h), lhsT=u["nwT"][:], rhs=u["Sb"][:], start=False, stop=True),
                         reads=[f"nwT_{hd}p{pp}", f"Sb_{hd}"], writes=[bvk])
                for h in range(4):
                    hd = dr * 4 + h
                    u = U[hd][pp]
                    p.op("scalar", lambda e: e.activation(out=u["vnew"][:], in_=Q(bv, h), func=AF.Copy), writes=[bvk, f"vnew_{hd}p{pp}"])
                for h in range(4):
                    hd = dr * 4 + h
                    u = U[hd][pp]
                    p.op("vector", lambda e: e.tensor_scalar(out=u["vs"][:], in0=Q(bv, h), scalar1=eGr[:, i, hd:hd + 1], scalar2=None, op0=ALU.mult),
                         reads=["eGr"], writes=[bvk, f"vs_{hd}p{pp}"])
                bo, bok = bank()
                for h in range(4):
                    hd = dr * 4 + h
                    u = U[hd][pp]
                    p.op("tensor", lambda e: e.matmul(Q(bo, h), lhsT=u["qdT"][:], rhs=u["Sb"][:], start=True, stop=False),
                         reads=[f"qdT_{hd}p{pp}", f"Sb_{hd}"], writes=[bok])
                    p.op("tensor", lambda e: e.matmul(Q(bo, h), lhsT=u["aaT"][:], rhs=u["vnew"][:], start=False, stop=True),
                         reads=[f"aaT_{hd}p{pp}", f"vnew_{hd}p{pp}"], writes=[bok])
                p.op("scalar", lambda e: e.activation(out=ib["o"][:, :], in_=bo[:, :], func=AF.Copy), writes=[bok, f"o{bs}{dr}"])
                bS, bSk = bank()
                for h in range(4):
                    hd = dr * 4 + h
                    u = U[hd][pp]
                    p.op("tensor", lambda e: e.matmul(Q(bS, h), lhsT=ib["ktb"][:, h * 128:(h + 1) * 128], rhs=u["vs"][:], start=True, stop=True),
                         reads=[f"ktb{bs}{dr}", f"vs_{hd}p{pp}"], writes=[bSk])
                for h in range(4):
                    hd = dr * 4 + h
                    u = U[hd][pp]
                    p.op("vector", lambda e: e.scalar_tensor_tensor(out=u["S"][:], in0=u["S"][:], scalar=ge[:, i, hd:hd + 1], in1=Q(bS, h),
                                                                    op0=ALU.mult, op1=ALU.add), reads=["ge"], writes=[bSk, f"S_{hd}"])
                    p.op("gpsimd", lambda e: e.tensor_copy(out=u["Sb"][:], in_=u["S"][:]), reads=[f"S_{hd}"], writes=[f"Sb_{hd}"])
                dst = o_f if dr == 0 else o_b
                p.dma("sync", dst[i * 128:(i + 1) * 128, :], ib["o"][:, :], reads=[f"o{bs}{dr}"])
        p.barrier()


def phase_b3(p, c, T, o_f, o_b, ztok, gdn_norm, yaT):
    nc = p.nc
    NT = T // 128
    with ExitStack() as es:
        def sb(name, shape, dt=F32):
            return es.enter_context(nc.sbuf_tensor(f"B3_{_uid()}_" + name, shape, dt))
        gnb = sb("gnb", [128, 128])
        of = [sb(f"of{i}", [128, 512]) for i in range(2)]
        ob = [sb(f"ob{i}", [128, 512]) for i in range(2)]
        zt = [sb(f"zt{i}", [128, 512]) for i in range(2)]
        junk = sb("junk", [128, 128])
        ss = sb("ss", [128, 4])
        rs = sb("rs", [128, 4])
        on = sb("on", [128, 512])
        yb = sb("yb", [128, 512], BF16)
        yT = [sb(f"yT{i}", [128, 4, 128], BF16) for i in range(2)]
        p.dma("sync", gnb[:], gdn_norm.to_broadcast([128, 128]), writes=["gnb"])
        for i in range(NT):
            b = i % 2
            p.dma("sync", of[b][:], o_f[i * 128:(i + 1) * 128, :], writes=[f"of{b}"])
            p.dma("sync", ob[b][:], o_b[i * 128:(i + 1) * 128, :], writes=[f"ob{b}"])
            p.dma("sync", zt[b][:], ztok[i * 128:(i + 1) * 128, :], writes=[f"zt{b}"])
            p.op("gpsimd", lambda e: e.tensor_tensor(out=of[b][:], in0=of[b][:], in1=ob[b][:], op=ALU.add), reads=[f"ob{b}", f"of{b}"], writes=[f"of{b}"])
            for h in range(4):
                p.op("scalar", lambda e: e.activation(out=junk[:], in_=of[b][:, h * 128:(h + 1) * 128], func=AF.Square, accum_out=ss[:, h:h + 1]),
                     reads=[f"of{b}"], writes=["junk", "ss"])
            p.op("vector", lambda e: e.tensor_scalar(out=rs[:], in0=ss[:], scalar1=1.0 / 128, scalar2=RMS_EPS, op0=ALU.mult, op1=ALU.add), reads=["ss"], writes=["rs"])
            p.op("scalar", lambda e: e.activation(out=rs[:], in_=rs[:], func=AF.Sqrt), reads=["rs"], writes=["rs"])
            p.op("vector", lambda e: e.reciprocal(out=rs[:], in_=rs[:]), reads=["rs"], writes=["rs"])
            for h in range(4):
                p.op("vector", lambda e: e.scalar_tensor_tensor(out=on[:, h * 128:(h + 1) * 128], in0=of[b][:, h * 128:(h + 1) * 128], scalar=rs[:, h:h + 1],
                                                                in1=gnb[:], op0=ALU.mult, op1=ALU.mult), reads=[f"of{b}", "rs", "gnb"], writes=["on"])
            p.op("scalar", lambda e: e.activation(out=zt[b][:], in_=zt[b][:], func=AF.Silu), reads=[f"zt{b}"], writes=[f"zt{b}"])
            p.op("gpsimd", lambda e: e.tensor_tensor(out=yb[:], in0=on[:], in1=zt[b][:], op=ALU.mult), reads=["on", f"zt{b}"], writes=["yb"])
            for h in range(4):
                p.op("tensor", lambda e: e.transpose(out=c.psb[:, h * 128:(h + 1) * 128], in_=yb[:, h * 128:(h + 1) * 128], identity=c.identb[:]),
                     reads=["yb", "identb"], writes=["ps7"])
            p.op("vector", lambda e: e.tensor_copy(out=yT[b][:, :, :], in_=c.psb[:, 0:512].rearrange("p (h t) -> p h t", t=128)), writes=["ps7", f"yT{b}"])
            for h_ in range(4):
                p.dma("sync", yaT[h_ * 128:(h_ + 1) * 128, i * 128:(i + 1) * 128], yT[b][:, h_, :], reads=[f"yT{b}"])
        p.barrier()


ATT_DIL = (1, 4, 16)


def alloc_att_consts(p, c, es):
    nc = p.nc
    c.amask = es.enter_context(nc.sbuf_tensor("amask", [128, 256], F32))
    p.op("gpsimd", lambda e: e.memset(c.amask[:], 0.0), writes=["amask"])
    p.op("gpsimd", lambda e: e.affine_select(out=c.amask[:], in_=c.amask[:], pattern=[[1, 256]], compare_op=ALU.is_ge, fill=-30000.0,
                                              base=0, channel_multiplier=-1), reads=["amask"], writes=["amask"])
    p.op("gpsimd", lambda e: e.affine_select(out=c.amask[:], in_=c.amask[:], pattern=[[-1, 256]], compare_op=ALU.is_ge, fill=-30000.0,
                                              base=128, channel_multiplier=1), reads=["amask"], writes=["amask"])
    c.amask0 = es.enter_context(nc.sbuf_tensor("amask0", [128, 128], F32))
    p.op("gpsimd", lambda e: e.memset(c.amask0[:], 0.0), writes=["amask0"])
    p.op("gpsimd", lambda e: e.affine_select(out=c.amask0[:], in_=c.amask0[:], pattern=[[-1, 128]], compare_op=ALU.is_ge, fill=-30000.0,
                                              base=64, channel_multiplier=1), reads=["amask0"], writes=["amask0"])


def phase_c(p, c, T, qkrope, qkrest, vTb, att):
    for _ in phase_c_gen(p, c, T, qkrope, qkrest, vTb, att):
        pass


def phase_c_gen(p, c, T, qkrope, qkrest, vTb, att, es_ext=None, shared=False):
    nc = p.nc
    NT = T // 128
    with ExitStack() as es_own:
        es = es_ext if es_ext is not None else es_own
        def sb(name, shape, dt=F32):
            return es.enter_context(nc.sbuf_tensor(f"C_{_uid()}_" + name, shape, dt))
        qT = [sb(f"qT{i}", [64, T], BF16) for i in range(2)]
        kT = [sb(f"kT{i}", [64, T], BF16) for i in range(2)]
        vT = [sb(f"vT{i}", [64, T], BF16) for i in range(2)]
        vtk = [sb(f"vtk{i}", [128, NT, 64], BF16) for i in range(2)]
        sm = [sb(f"sm{i}", [128, 256]) for i in range(2)]
        P = [sb(f"P{i}", [128, 256], BF16) for i in range(2)]
        PT = [sb(f"PT{i}", [128, 2, 128], BF16) for i in range(2)]
        stt = [sb(f"stt{i}", [128, 4]) for i in range(2)]
        oo = [sb(f"oo{i}", [128, 65]) for i in range(4)]
        blk = [0]
        for h in range(12):
            hb = h % 2
            r = ATT_DIL[h // 4]
            M = T // r
            NB = M // 128
            p.dma("sync", qT[hb][0:16, :], qkrope[h * 16:(h + 1) * 16, :], writes=[f"qT{hb}"])
            p.dma("sync", qT[hb][16:64, :], qkrest[h * 48:(h + 1) * 48, :], writes=[f"qT{hb}"])
            p.dma("sync", kT[hb][0:16, :], qkrope[(12 + h) * 16:(13 + h) * 16, :], writes=[f"kT{hb}"])
            p.dma("sync", kT[hb][16:64, :], qkrest[(12 + h) * 48:(13 + h) * 48, :], writes=[f"kT{hb}"])
            p.dma("sync", vT[hb][:, :], vTb[h * 64:(h + 1) * 64, :], writes=[f"vT{hb}"])
            for g0 in range(0, NT, 16):
                bi = 4 if shared else 6 + (g0 // 16) % 2
                pv = c.ps[bi][:, :].bitcast(BF16)
                for kb in range(g0, min(NT, g0 + 16)):
                    s, mb = divmod(kb, NB)
                    c0 = s + r * mb * 128
                    src = vT[hb][:, c0: c0 + r * 127 + 1: r]
                    p.op("tensor", lambda e: e.transpose(out=pv[:, (kb - g0) * 64:(kb - g0 + 1) * 64], in_=src, identity=c.identb[0:64, 0:64]),
                         reads=[f"vT{hb}", "identb"], writes=[f"ps{bi}"])
                n_ = min(NT, g0 + 16) - g0
                p.op("scalar", lambda e: e.activation(out=vtk[hb][:, g0:g0 + n_, :], in_=pv[:, 0:n_ * 64].rearrange("p (k d) -> p k d", d=64), func=AF.Copy),
                     writes=[f"ps{bi}", f"vtk{hb}"])
            for s in range(r):
                for jb in range(NB + 1):
                    n = blk[0]
                    blk[0] += 1
                    b2 = n % 2
                    m0 = jb * 128 - 64
                    qlo = max(m0, 0)
                    qhi = min(m0 + 128, M)
                    nq = qhi - qlo
                    kblocks = [x for x in (jb - 1, jb) if 0 <= x < NB]
                    nk = 128 * len(kblocks)
                    klo = kblocks[0] * 128
                    if jb == 0:
                        mask = c.amask0[0:nq, 0:nk]
                        mk = "amask0"
                    else:
                        coff = 0 if kblocks[0] == jb - 1 else 128
                        mask = c.amask[0:nq, coff:coff + nk]
                        mk = "amask"
                    qcols = qT[hb][:, s + r * qlo: s + r * (qhi - 1) + 1: r]
                    kcols = kT[hb][:, s + r * klo: s + r * (klo + nk - 1) + 1: r]
                    if shared:
                        bS, bP, bO = c.ps[b2], c.ps[2], c.ps[3]
                        bSk, bPk, bOk = f"ps{b2}", "ps2", "ps3"
                    else:
                        bS, bP, bO = c.ps[b2], c.ps[2 + b2], c.ps[4 + b2]
                        bSk, bPk, bOk = f"ps{b2}", f"ps{2 + b2}", f"ps{4 + b2}"
                    p.op("tensor", lambda e: e.matmul(bS[0:nq, 0:nk], lhsT=qcols, rhs=kcols, start=True, stop=True),
                         reads=[f"qT{hb}", f"kT{hb}"], writes=[bSk])
                    p.op("vector", lambda e: e.scalar_tensor_tensor(out=sm[b2][0:nq, 0:nk], in0=bS[0:nq, 0:nk], scalar=0.125, in1=mask,
                                                                    op0=ALU.mult, op1=ALU.add), reads=[mk], writes=[bSk, f"sm{b2}"])
                    st_ = stt[b2]
                    p.op("vector", lambda e: e.reduce_max(out=st_[0:nq, 1:2], in_=sm[b2][0:nq, 0:nk], axis=AX.X, negate=True), reads=[f"sm{b2}"], writes=[f"stt{b2}b"])
                    p.op("scalar", lambda e: e.activation(out=P[b2][0:nq, 0:nk], in_=sm[b2][0:nq, 0:nk], func=AF.Exp, bias=st_[0:nq, 1:2], scale=1.0,
                                                          accum_out=st_[0:nq, 2:3]), reads=[f"sm{b2}", f"stt{b2}b"], writes=[f"P{b2}", f"stt{b2}c"])
                    pt = bP[:, :].bitcast(BF16)
                    for ci in range(len(kblocks)):
                        p.op("tensor", lambda e: e.transpose(out=pt[:, ci * 128: ci * 128 + nq], in_=P[b2][0:nq, ci * 128:(ci + 1) * 128], identity=c.identb[0:nq, 0:nq]),
                             reads=[f"P{b2}", "identb"], writes=[bPk])
                    for ci in range(len(kblocks)):
                        if ci == 0:
                            p.op("scalar", lambda e: e.activation(out=PT[b2][:, ci, 0:nq], in_=pt[:, ci * 128: ci * 128 + nq], func=AF.Copy), writes=[bPk, f"PT{b2}_{ci}"])
                        else:
                            p.op("vector", lambda e: e.tensor_copy(out=PT[b2][:, ci, 0:nq], in_=pt[:, ci * 128: ci * 128 + nq]), writes=[bPk, f"PT{b2}_{ci}"])
                    for ci, kbk in enumerate(kblocks):
                        p.op("tensor", lambda e: e.matmul(bO[0:nq, 0:64], lhsT=PT[b2][:, ci, 0:nq], rhs=vtk[hb][:, s * NB + kbk, :],
                                                          start=(ci == 0), stop=(ci == len(kblocks) - 1)),
                             reads=[f"PT{b2}_{ci}", f"vtk{hb}"], writes=[bOk])
                    p.op("vector", lambda e: e.reciprocal(out=st_[0:nq, 3:4], in_=st_[0:nq, 2:3]), reads=[f"stt{b2}c"], writes=[f"stt{b2}d"])
                    o4 = oo[n % 4]
                    ok = f"oo{n % 4}"
                    p.op("scalar", lambda e: e.activation(out=o4[0:nq, 64:65], in_=st_[0:nq, 2:3], func=AF.Ln), reads=[f"stt{b2}c"], writes=[ok])
                    p.op("gpsimd", lambda e: e.tensor_tensor(out=o4[0:nq, 64:65], in0=o4[0:nq, 64:65], in1=st_[0:nq, 1:2], op=ALU.subtract),
                         reads=[ok, f"stt{b2}b"], writes=[ok])
                    p.op("vector", lambda e: e.tensor_scalar(out=o4[0:nq, 0:64], in0=bO[0:nq, 0:64], scalar1=st_[0:nq, 3:4], scalar2=None, op0=ALU.mult),
                         reads=[f"stt{b2}d"], writes=[bOk, ok + "o"])
                    t0 = s + r * qlo
                    dst = att[t0: t0 + r * (nq - 1) + 1: r, h, :]
                    p.dma("sync", dst, o4[0:nq, :], reads=[ok, ok + "o"])
                    yield
        if not shared:
            p.barrier()


def phase_d(p, c, T, yaT, att, gatesT, xres, w_a, w_b, w_out, norm_w2, w_router, h2b, aff, x_in=None):
    if x_in is None:
        x_in = xres
    nc = p.nc
    NS = T // 512
    with ExitStack() as es:
        def sb(name, shape, dt=F32):
            return es.enter_context(nc.sbuf_tensor(f"D_{_uid()}_" + name, shape, dt))
        Wa = sb("Wa", [128, 4, 1024], BF16)
        Wb = sb("Wb", [128, 2, 1024], BF16)
        Wo = sb("Wo", [128, 8, 1024], BF16)
        Wr = sb("Wr", [128, 8, 16])
        stg = [sb(f"stg{i}", [128, 1024]) for i in range(2)]
        nwb = sb("nwb", [128, 1024])
        at = [sb(f"at{i}", [128, 12, 65]) for i in range(2)]
        wl = sb("wl", [128, 12])
        mx = sb("mx", [128, 4])
        sm = sb("sm", [128, 4])
        tmpw = sb("tmpw", [128, 12, 64])
        ybt = sb("ybt", [128, 256])
        ybb = sb("ybb", [128, 256], BF16)
        ybT = [sb(f"ybT{i}", [128, 2, 512], BF16) for i in range(2)]
        yaTs = [sb(f"yaTs{i}", [128, 4, 512], BF16) for i in range(2)]
        gts = [sb(f"gts{i}", [128, 16, 512], BF16) for i in range(2)]
        t1 = sb("t1", [128, 512])
        t2 = sb("t2", [128, 512])
        mixT = [sb(f"mixT{i}", [128, 8, 512], BF16) for i in range(2)]
        xt = [sb(f"xt{i}", [128, 1024]) for i in range(2)]
        junk = sb("junk", [128, 1024], BF16)
        h2 = sb("h2", [128, 1024])
        h2bf = [sb(f"h2bf{i}", [128, 1024], BF16) for i in range(2)]
        h2T = sb("h2T", [128, 2, 8, 128], BF16)
        h2lo = sb("h2lo", [128, 1024], BF16)
        Wrb = sb("Wrb", [128, 8, 32], BF16)
        Wrt = sb("Wrt", [128, 8, 16])
        lg48 = sb("lg48", [128, 48])
        st = sb("st", [128, 8])
        lg = sb("lg", [128, 16])
        af = [sb(f"af{i}", [128, 16]) for i in range(2)]

        p.dma("sync", nwb[:], norm_w2.to_broadcast([128, 1024]), writes=["nwb"])
        p.dma("sync", Wr[:, :, :], w_router.rearrange("(k p) e -> p k e", p=128), writes=["Wr"])
        p.op("vector", lambda e: e.tensor_copy(out=Wrb[:, :, 0:16], in_=Wr[:, :, :]), reads=["Wr"], writes=["Wrb"])
        p.op("vector", lambda e: e.tensor_tensor(out=Wrt[:, :, :], in0=Wr[:, :, :], in1=Wrb[:, :, 0:16], op=ALU.subtract), reads=["Wr", "Wrb"], writes=["Wrt"])
        p.op("vector", lambda e: e.tensor_copy(out=Wrb[:, :, 16:32], in_=Wrt[:, :, :]), reads=["Wrt"], writes=["Wrb"])
        si = 0
        for (W, src, nk, key) in ((Wa, w_a, 4, "Wa"), (Wb, w_b, 2, "Wb"), (Wo, w_out, 8, "Wo")):
            for k in range(nk):
                s_, sk = stg[si % 2], f"stg{si % 2}"
                si += 1
                p.dma("sync", s_[:], src[k * 128:(k + 1) * 128, :], writes=[sk])
                p.op("gpsimd", lambda e: e.tensor_copy(out=W[:, k, :], in_=s_[:]), reads=[sk], writes=[key])
        bn = [0]

        def bank():
            i = bn[0] % 6
            bn[0] += 1
            return c.ps[i], f"ps{i}"

        for s in range(NS):
            sl = s % 2
            c0 = s * 512
            for k_ in range(4):
                p.dma("sync", yaTs[sl][:, k_, :], yaT[k_ * 128:(k_ + 1) * 128, c0:c0 + 512], writes=[f"yaTs{sl}"])
            for k_ in range(16):
                p.dma("sync", gts[sl][:, k_, :], gatesT[k_ * 128:(k_ + 1) * 128, c0:c0 + 512], writes=[f"gts{sl}"])
            for ti in range(4):
                r0 = c0 + ti * 128
                a_, ak = at[ti % 2], f"at{ti % 2}"
                p.dma("sync", a_[:, :, :], att[r0:r0 + 128, :, :], writes=[ak])
                L = a_[:, :, 64]
                p.op("vector", lambda e: e.tensor_tensor(out=mx[:], in0=L[:, 0:4], in1=L[:, 4:8], op=ALU.max), reads=[ak], writes=["mx"])
                p.op("vector", lambda e: e.tensor_tensor(out=mx[:], in0=mx[:], in1=L[:, 8:12], op=ALU.max), reads=[ak, "mx"], writes=["mx"])
                p.op("vector", lambda e: e.tensor_tensor(out=wl[:, :].rearrange("p (g h) -> p g h", h=4), in0=L.rearrange("p (g h) -> p g h", h=4),
                                                         in1=mx[:, :].unsqueeze(1).to_broadcast([128, 3, 4]), op=ALU.subtract), reads=[ak, "mx"], writes=["wl"])
                p.op("scalar", lambda e: e.activation(out=wl[:], in_=wl[:], func=AF.Exp), reads=["wl"], writes=["wl"])
                p.op("vector", lambda e: e.tensor_tensor(out=sm[:], in0=wl[:, 0:4], in1=wl[:, 4:8], op=ALU.add), reads=["wl"], writes=["sm"])
                p.op("vector", lambda e: e.tensor_tensor(out=sm[:], in0=sm[:], in1=wl[:, 8:12], op=ALU.add), reads=["wl", "sm"], writes=["sm"])
                p.op("vector", lambda e: e.reciprocal(out=sm[:], in_=sm[:]), reads=["sm"], writes=["sm"])
                p.op("vector", lambda e: e.tensor_tensor(out=wl[:, :].rearrange("p (g h) -> p g h", h=4), in0=wl[:, :].rearrange("p (g h) -> p g h", h=4),
                                                         in1=sm[:, :].unsqueeze(1).to_broadcast([128, 3, 4]), op=ALU.mult), reads=["wl", "sm"], writes=["wl"])
                p.op("vector", lambda e: e.tensor_tensor(out=tmpw[:, :, :], in0=a_[:, :, 0:64], in1=wl[:, :].unsqueeze(2).to_broadcast([128, 12, 64]), op=ALU.mult),
                     reads=[ak, "wl"], writes=["tmpw"])
                tv = tmpw[:, :, :].rearrange("p (g h) d -> p g (h d)", h=4)
                p.op("gpsimd", lambda e: e.tensor_tensor(out=ybt[:], in0=tv[:, 0, :], in1=tv[:, 1, :], op=ALU.add), reads=["tmpw"], writes=["ybt"])
                p.op("gpsimd", lambda e: e.tensor_tensor(out=ybb[:], in0=ybt[:], in1=tv[:, 2, :], op=ALU.add), reads=["tmpw", "ybt"], writes=["ybb"])
                for k in range(2):
                    p.op("tensor", lambda e: e.transpose(out=c.psb[:, k * 128:(k + 1) * 128], in_=ybb[:, k * 128:(k + 1) * 128], identity=c.identb[:]),
                         reads=["ybb", "identb"], writes=["ps7"])
                p.op("scalar", lambda e: e.activation(out=ybT[sl][:, :, ti * 128:(ti + 1) * 128], in_=c.psb[:, 0:256].rearrange("p (k t) -> p k t", t=128), func=AF.Copy),
                     writes=["ps7", f"ybT{sl}"])
            for dm in range(8):
                bA, bAk = bank()
                bB, bBk = bank()
                for k in range(4):
                    p.op("tensor", lambda e: e.matmul(bA[:, :], lhsT=Wa[:, k, dm * 128:(dm + 1) * 128], rhs=yaTs[sl][:, k, :], start=(k == 0), stop=(k == 3)),
                         reads=["Wa", f"yaTs{sl}"], writes=[bAk])
                for k in range(2):
                    p.op("tensor", lambda e: e.matmul(bB[:, :], lhsT=Wb[:, k, dm * 128:(dm + 1) * 128], rhs=ybT[sl][:, k, :], start=(k == 0), stop=(k == 1)),
                         reads=["Wb", f"ybT{sl}"], writes=[bBk])
                p.op("vector", lambda e: e.tensor_tensor(out=t1[:], in0=bA[:, :], in1=gts[sl][:, dm, :], op=ALU.mult), reads=[f"gts{sl}"], writes=[bAk, "t1"])
                p.op("vector", lambda e: e.tensor_tensor(out=t2[:], in0=bB[:, :], in1=gts[sl][:, 8 + dm, :], op=ALU.mult), reads=[f"gts{sl}"], writes=[bBk, "t2"])
                p.op("gpsimd", lambda e: e.tensor_tensor(out=mixT[sl][:, dm, :], in0=t1[:], in1=t2[:], op=ALU.add), reads=["t1", "t2"], writes=[f"mixT{sl}"])
            for ti in range(4):
                r0 = c0 + ti * 128
                x_, xk = xt[ti % 2], f"xt{ti % 2}"
                p.dma("sync", x_[:], x_in[r0:r0 + 128, :], writes=[xk])
                for hf in range(2):
                    bX, bXk = bank()
                    for k in range(8):
                        p.op("tensor", lambda e: e.matmul(bX[:, :], lhsT=mixT[sl][:, k, ti * 128:(ti + 1) * 128], rhs=Wo[:, k, hf * 512:(hf + 1) * 512],
                                                          start=(k == 0), stop=(k == 7)), reads=["Wo", f"mixT{sl}"], writes=[bXk])
                    p.op("vector", lambda e: e.tensor_tensor(out=x_[:, hf * 512:(hf + 1) * 512], in0=bX[:, :], in1=x_[:, hf * 512:(hf + 1) * 512], op=ALU.add),
                         reads=[xk], writes=[bXk, xk])
                p.dma("sync", xres[r0:r0 + 128, :], x_[:], reads=[xk])
                p.op("scalar", lambda e: e.activation(out=junk[:], in_=x_[:], func=AF.Square, accum_out=st[:, 0:1]), reads=[xk], writes=["junk", "st0"])
                p.op("vector", lambda e: e.tensor_scalar(out=st[:, 1:2], in0=st[:, 0:1], scalar1=1.0 / 1024, scalar2=RMS_EPS, op0=ALU.mult, op1=ALU.add),
                     reads=["st0"], writes=["st1"])
                p.op("scalar", lambda e: e.activation(out=st[:, 2:3], in_=st[:, 1:2], func=AF.Sqrt), reads=["st1"], writes=["st2"])
                p.op("vector", lambda e: e.reciprocal(out=st[:, 3:4], in_=st[:, 2:3]), reads=["st2"], writes=["st3"])
                p.op("vector", lambda e: e.scalar_tensor_tensor(out=h2[:], in0=x_[:], scalar=st[:, 3:4], in1=nwb[:], op0=ALU.mult, op1=ALU.mult),
                     reads=[xk, "st3", "nwb"], writes=["h2"])
                hb_, hbk = h2bf[ti % 2], f"h2bf{ti % 2}"
                p.op("gpsimd", lambda e: e.tensor_copy(out=hb_[:], in_=h2[:]), reads=["h2"], writes=[hbk])
                p.dma("sync", h2b[r0:r0 + 128, :], hb_[:], reads=[hbk])
                p.op("gpsimd", lambda e: e.tensor_tensor(out=h2lo[:], in0=h2[:], in1=hb_[:], op=ALU.subtract), reads=["h2", hbk], writes=["h2lo"])
                for part, (src_, srck_) in enumerate(((hb_, hbk), (h2lo, "h2lo"))):
                    bT, bTk = bank()
                    tpv = bT[:, :].bitcast(BF16)
                    for k in range(8):
                        p.op("tensor", lambda e: e.transpose(out=tpv[:, k * 128:(k + 1) * 128], in_=src_[:, k * 128:(k + 1) * 128], identity=c.identb[:]),
                             reads=[srck_, "identb"], writes=[bTk])
                    if part == 0:
                        p.op("scalar", lambda e: e.activation(out=h2T[:, part, :, :], in_=tpv[:, :].rearrange("p (k t) -> p k t", t=128), func=AF.Copy),
                             writes=[bTk, f"h2T{part}"])
                    else:
                        p.op("vector", lambda e: e.tensor_copy(out=h2T[:, part, :, :], in_=tpv[:, :].rearrange("p (k t) -> p k t", t=128)),
                             writes=[bTk, f"h2T{part}"])
                bL, bLk = bank()
                for k in range(8):
                    p.op("tensor", lambda e: e.matmul(bL[:, 0:32], lhsT=h2T[:, 0, k, :], rhs=Wrb[:, k, :], start=(k == 0), stop=(k == 7)),
                         reads=["h2T0", "Wrb"], writes=[bLk])
                for k in range(8):
                    p.op("tensor", lambda e: e.matmul(bL[:, 32:48], lhsT=h2T[:, 1, k, :], rhs=Wrb[:, k, 0:16], start=(k == 0), stop=(k == 7)),
                         reads=["h2T1", "Wrb"], writes=[bLk])
                p.op("vector", lambda e: e.tensor_copy(out=lg48[:], in_=bL[:, 0:48]), writes=[bLk, "lg48"])
                p.op("vector", lambda e: e.tensor_tensor(out=lg[:], in0=lg48[:, 0:16], in1=lg48[:, 16:32], op=ALU.add), reads=["lg48"], writes=["lg"])
                p.op("vector", lambda e: e.tensor_tensor(out=lg[:], in0=lg[:], in1=lg48[:, 32:48], op=ALU.add), reads=["lg", "lg48"], writes=["lg"])
                p.op("vector", lambda e: e.reduce_max(out=st[:, 4:5], in_=lg[:], axis=AX.X), reads=["lg"], writes=["st4"])
                p.op("vector", lambda e: e.tensor_scalar(out=st[:, 5:6], in0=st[:, 4:5], scalar1=-1.0, scalar2=None, op0=ALU.mult), reads=["st4"], writes=["st5"])
                p.op("scalar", lambda e: e.activation(out=lg[:], in_=lg[:], func=AF.Exp, bias=st[:, 5:6], scale=1.0, accum_out=st[:, 6:7]),
                     reads=["lg", "st5"], writes=["lg", "st6"])
                p.op("vector", lambda e: e.reciprocal(out=st[:, 7:8], in_=st[:, 6:7]), reads=["st6"], writes=["st7"])
                a2, a2k = af[ti % 2], f"af{ti % 2}"
                p.op("vector", lambda e: e.tensor_scalar(out=a2[:], in0=lg[:], scalar1=st[:, 7:8], scalar2=None, op0=ALU.mult), reads=["lg", "st7"], writes=[a2k])
                p.dma("sync", aff[r0:r0 + 128, :], a2[:], reads=[a2k])
        p.barrier()


def phase_e(p, c, T, aff, h2b, xres, w_gate, w_up, w_down, n_iter=34):
    nc = p.nc
    NT = T // 128
    C = T // 8
    NG = C // 128
    NE = 16
    with ExitStack() as es0:
        def sb0(name, shape, dt=F32):
            return es0.enter_context(nc.sbuf_tensor(f"E_{_uid()}_" + name, shape, dt))
        idx_all = sb0("idx_all", [128, NE, NG], I32)
        gate_all = sb0("gate_all", [128, NE, NG])
        with ExitStack() as es:
            def sb(name, shape, dt=F32):
                return es.enter_context(nc.sbuf_tensor(f"E1_{_uid()}_" + name, shape, dt))
            A = sb("A", [128, NT, NE])
            cmp = sb("cmp", [128, NT, NE])
            lo = sb("lo", [128, NE])
            hi = sb("hi", [128, NE])
            mid = sb("mid", [128, NE])
            cnt = sb("cnt", [128, NE])
            sel = sb("sel", [128, NE])
            d1 = sb("d1", [128, NE])
            triS = sb("triS", [128, 128])
            onesT = sb("onesT", [128, NT])
            rk = sb("rk", [128, NT, NE])
            nn = sb("nn", [128, NT, NE])
            base = sb("base", [128, NT, NE])
            pos = sb("pos", [128, NT, NE])
            posi = sb("posi", [128, NT, NE], I32)
            ai = sb("ai", [128, NT, NE], I32)
            bi_ = sb("bi", [128, NT, NE], I32)
            af_ = sb("af", [128, NT, NE])
            bf_ = sb("bf", [128, NT, NE])
            h1 = sb("h1", [128, NT, NE], BF16)
            h2 = sb("h2", [128, NT, NE], BF16)
            h3 = sb("h3", [128, NT, NE], BF16)
            r1 = sb("r1", [128, NT, NE])
            r2 = sb("r2", [128, NT, NE])
            iotaA = sb("iotaA", [128, 128])
            iotaB = sb("iotaB", [128, NG])
            tval = sb("tval", [128, NT], BF16)
            pval = sb("pval", [128, 1])
            pvalb = sb("pvalb", [128, NT], BF16)
            Aoh = [sb(f"Aoh{i}", [128, NT, 128], BF16) for i in range(2)]
            Boh = [sb(f"Boh{i}", [128, NT, NG]) for i in range(2)]
            rhs = [sb(f"rhs{i}", [128, NT, 5, NG], BF16) for i in range(2)]
            res = sb("res", [128, 5, NG])

            for t0 in range(0, NT, 8):
                t1 = min(NT, t0 + 8)
                p.dma("sync", A[:, t0:t1, :], aff[t0 * 128:t1 * 128, :].rearrange("(t p) e -> p t e", p=128), writes=["A"])
            p.op("gpsimd", lambda e: e.memset(lo[:], 0.0), writes=["lo"])
            p.op("gpsimd", lambda e: e.memset(hi[:], 1.0001), writes=["hi"])
            p.op("gpsimd", lambda e: e.memset(onesT[:], 1.0), writes=["onesT"])
            p.op("gpsimd", lambda e: e.memset(triS[:], 1.0), writes=["triS"])
            p.op("gpsimd", lambda e: e.affine_select(out=triS[:], in_=triS[:], pattern=[[1, 128]], compare_op=ALU.is_gt, fill=0.0, base=0, channel_multiplier=-1),
                 reads=["triS"], writes=["triS"])
            p.op("gpsimd", lambda e: e.iota(iotaA[:], pattern=[[1, 128]], base=0, channel_multiplier=0, allow_small_or_imprecise_dtypes=True), writes=["iotaA"])
            p.op("gpsimd", lambda e: e.iota(iotaB[:], pattern=[[1, NG]], base=0, channel_multiplier=0, allow_small_or_imprecise_dtypes=True), writes=["iotaB"])
            p.op("gpsimd", lambda e: e.iota(tval[:], pattern=[[1, NT]], base=0, channel_multiplier=0, allow_small_or_imprecise_dtypes=True), writes=["tval"])
            p.op("gpsimd", lambda e: e.iota(pval[:], pattern=[[0, 1]], base=0, channel_multiplier=1, allow_small_or_imprecise_dtypes=True), writes=["pval"])
            p.op("vector", lambda e: e.tensor_copy(out=pvalb[:], in_=pval[:, 0:1].to_broadcast([128, NT])), reads=["pval"], writes=["pvalb"])
            for it in range(n_iter):
                p.op("vector", lambda e: e.tensor_tensor(out=mid[:], in0=lo[:], in1=hi[:], op=ALU.add), reads=["lo", "hi"], writes=["mid"])
                p.op("vector", lambda e: e.tensor_scalar(out=mid[:], in0=mid[:], scalar1=0.5, scalar2=None, op0=ALU.mult), reads=["mid"], writes=["mid"])
                p.op("vector", lambda e: e.tensor_tensor(out=cmp[:, :, :], in0=A[:, :, :], in1=mid[:, :].unsqueeze(1).to_broadcast([128, NT, NE]), op=ALU.is_ge),
                     reads=["A", "mid"], writes=["cmp"])
                p.op("vector", lambda e: e.tensor_reduce(out=cnt[:], in_=cmp[:, :, :].rearrange("p t e -> p e t"), axis=AX.X, op=ALU.add), reads=["cmp"], writes=["cnt"])
                p.op("tensor", lambda e: e.matmul(c.ps[0][:, 0:NE], lhsT=c.onesf[:], rhs=cnt[:], start=True, stop=True), reads=["cnt", "onesf"], writes=["ps0"])
                p.op("vector", lambda e: e.tensor_scalar(out=sel[:], in0=c.ps[0][:, 0:NE], scalar1=float(C) - 0.5, scalar2=None, op0=ALU.is_ge), writes=["ps0", "sel"])
                p.op("vector", lambda e: e.tensor_tensor(out=d1[:], in0=mid[:], in1=lo[:], op=ALU.subtract), reads=["mid", "lo"], writes=["d1"])
                p.op("vector", lambda e: e.tensor_tensor(out=d1[:], in0=d1[:], in1=sel[:], op=ALU.mult), reads=["d1", "sel"], writes=["d1"])
                p.op("vector", lambda e: e.tensor_tensor(out=lo[:], in0=lo[:], in1=d1[:], op=ALU.add), reads=["d1", "lo"], writes=["lo"])
                p.op("vector", lambda e: e.tensor_tensor(out=d1[:], in0=hi[:], in1=mid[:], op=ALU.subtract), reads=["mid", "hi"], writes=["d1"])
                p.op("vector", lambda e: e.tensor_tensor(out=d1[:], in0=d1[:], in1=sel[:], op=ALU.mult), reads=["d1", "sel"], writes=["d1"])
                p.op("vector", lambda e: e.tensor_tensor(out=hi[:], in0=mid[:], in1=d1[:], op=ALU.add), reads=["d1", "mid"], writes=["hi"])
            p.op("vector", lambda e: e.tensor_tensor(out=cmp[:, :, :], in0=A[:, :, :], in1=lo[:, :].unsqueeze(1).to_broadcast([128, NT, NE]), op=ALU.is_ge),
                 reads=["A", "lo"], writes=["cmp"])
            cf = cmp[:, :, :].rearrange("p t e -> p (t e)")
            NC_ = NT * NE
            for c0 in range(0, NC_, 512):
                c1 = min(NC_, c0 + 512)
                p.op("tensor", lambda e: e.matmul(c.ps[1][:, 0:c1 - c0], lhsT=triS[:], rhs=cf[:, c0:c1], start=True, stop=True), reads=["cmp", "triS"], writes=["ps1"])
                p.op("vector", lambda e: e.tensor_copy(out=rk[:, :, :].rearrange("p t e -> p (t e)")[:, c0:c1], in_=c.ps[1][:, 0:c1 - c0]), writes=["ps1", "rk"])
                p.op("tensor", lambda e: e.matmul(c.ps[2][:, 0:c1 - c0], lhsT=c.onesf[:], rhs=cf[:, c0:c1], start=True, stop=True), reads=["cmp", "onesf"], writes=["ps2"])
                p.op("vector", lambda e: e.tensor_copy(out=nn[:, :, :].rearrange("p t e -> p (t e)")[:, c0:c1], in_=c.ps[2][:, 0:c1 - c0]), writes=["ps2", "nn"])
            for ex in range(NE):
                p.op("vector", lambda e: e.tensor_tensor_scan(out=base[:, :, ex], data0=onesT[:, :], data1=nn[:, :, ex], initial=0.0, op0=ALU.mult, op1=ALU.add),
                     reads=["nn", "onesT"], writes=["base"])
            p.op("vector", lambda e: e.tensor_tensor(out=pos[:, :, :], in0=base[:, :, :], in1=nn[:, :, :], op=ALU.subtract), reads=["base", "nn"], writes=["pos"])
            p.op("vector", lambda e: e.tensor_tensor(out=pos[:, :, :], in0=pos[:, :, :], in1=rk[:, :, :], op=ALU.add), reads=["pos", "rk"], writes=["pos"])
            p.op("vector", lambda e: e.tensor_copy(out=posi[:, :, :], in_=pos[:, :, :]), reads=["pos"], writes=["posi"])
            p.op("vector", lambda e: e.tensor_single_scalar(out=ai[:, :, :], in_=posi[:, :, :], scalar=(NG.bit_length() - 1), op=ALU.arith_shift_right), reads=["posi"], writes=["ai"])
            p.op("vector", lambda e: e.tensor_single_scalar(out=bi_[:, :, :], in_=posi[:, :, :], scalar=NG - 1, op=ALU.bitwise_and), reads=["posi"], writes=["bi"])
            p.op("vector", lambda e: e.tensor_copy(out=af_[:, :, :], in_=ai[:, :, :]), reads=["ai"], writes=["af"])
            p.op("vector", lambda e: e.tensor_copy(out=bf_[:, :, :], in_=bi_[:, :, :]), reads=["bi"], writes=["bf"])
            p.op("vector", lambda e: e.scalar_tensor_tensor(out=af_[:, :, :], in0=af_[:, :, :], scalar=1.0, in1=cmp[:, :, :], op0=ALU.add, op1=ALU.mult), reads=["af", "cmp"], writes=["af"])
            p.op("vector", lambda e: e.tensor_scalar(out=af_[:, :, :], in0=af_[:, :, :], scalar1=-1.0, scalar2=None, op0=ALU.add), reads=["af"], writes=["af"])
            p.op("vector", lambda e: e.tensor_copy(out=h1[:, :, :], in_=A[:, :, :]), reads=["A"], writes=["h1"])
            p.op("vector", lambda e: e.tensor_tensor(out=r1[:, :, :], in0=A[:, :, :], in1=h1[:, :, :], op=ALU.subtract), reads=["A", "h1"], writes=["r1"])
            p.op("vector", lambda e: e.tensor_copy(out=h2[:, :, :], in_=r1[:, :, :]), reads=["r1"], writes=["h2"])
            p.op("vector", lambda e: e.tensor_tensor(out=r2[:, :, :], in0=r1[:, :, :], in1=h2[:, :, :], op=ALU.subtract), reads=["r1", "h2"], writes=["r2"])
            p.op("vector", lambda e: e.tensor_copy(out=h3[:, :, :], in_=r2[:, :, :]), reads=["r2"], writes=["h3"])
            for ex in range(NE):
                b = ex % 2
                p.op("vector", lambda e: e.tensor_tensor(out=Aoh[b][:, :, :], in0=iotaA[:, :].unsqueeze(1).to_broadcast([128, NT, 128]),
                                                         in1=af_[:, :, ex:ex + 1].to_broadcast([128, NT, 128]), op=ALU.is_equal),
                     reads=["iotaA", "af"], writes=[f"Aoh{b}"])
                p.op("vector", lambda e: e.tensor_tensor(out=Boh[b][:, :, :], in0=iotaB[:, :].unsqueeze(1).to_broadcast([128, NT, NG]),
                                                         in1=bf_[:, :, ex:ex + 1].to_broadcast([128, NT, NG]), op=ALU.is_equal),
                     reads=["iotaB", "bf"], writes=[f"Boh{b}"])
                vals = [tval[:, :].unsqueeze(2).to_broadcast([128, NT, NG]), pvalb[:, :].unsqueeze(2).to_broadcast([128, NT, NG]),
                        h1[:, :, ex:ex + 1].to_broadcast([128, NT, NG]), h2[:, :, ex:ex + 1].to_broadcast([128, NT, NG]),
                        h3[:, :, ex:ex + 1].to_broadcast([128, NT, NG])]
                vkeys = ["tval", "pvalb", "h1", "h2", "h3"]
                for vi in range(5):
                    p.op("gpsimd", lambda e: e.tensor_tensor(out=rhs[b][:, :, vi, :], in0=Boh[b][:, :, :], in1=vals[vi], op=ALU.mult),
                         reads=[f"Boh{b}", vkeys[vi]], writes=[f"rhs{b}"])
                pb = 3 + b
                for t in range(NT):
                    p.op("tensor", lambda e: e.matmul(c.ps[pb][:, 0:5 * NG], lhsT=Aoh[b][:, t, :], rhs=rhs[b][:, t, :, :].rearrange("p v g -> p (v g)"),
                                                      start=(t == 0), stop=(t == NT - 1)), reads=[f"Aoh{b}", f"rhs{b}"], writes=[f"ps{pb}"])
                p.op("vector", lambda e: e.tensor_copy(out=res[:, :, :], in_=c.ps[pb][:, 0:5 * NG].rearrange("p (v g) -> p v g", g=NG)), writes=[f"ps{pb}", "res"])
                p.op("vector", lambda e: e.scalar_tensor_tensor(out=res[:, 0, :], in0=res[:, 0, :], scalar=128.0, in1=res[:, 1, :], op0=ALU.mult, op1=ALU.add),
                     reads=["res"], writes=["res"])
                p.op("vector", lambda e: e.tensor_copy(out=idx_all[:, ex, :], in_=res[:, 0, :]), reads=["res"], writes=["idx_all"])
                p.op("vector", lambda e: e.tensor_tensor(out=res[:, 2, :], in0=res[:, 2, :], in1=res[:, 3, :], op=ALU.add), reads=["res"], writes=["res"])
                p.op("vector", lambda e: e.tensor_tensor(out=gate_all[:, ex, :], in0=res[:, 2, :], in1=res[:, 4, :], op=ALU.add), reads=["res"], writes=["gate_all"])
            p.barrier()
        with ExitStack() as es:
            def sb(name, shape, dt=F32):
                return es.enter_context(nc.sbuf_tensor(f"E2_{_uid()}_" + name, shape, dt))
            NWB = 5
            wbuf = [sb(f"w{i}", [128, 8, 1024], BF16) for i in range(NWB)]
            stg = [sb(f"stg{i}", [128, 2, 1024]) for i in range(2)]
            xes = [sb(f"xe{i}", [128, NG, 1024], BF16) for i in range(2)]
            xeT = sb("xeT", [128, 8, C], BF16)
            hmT = sb("hmT", [128, 8, C], BF16)
            sg = [sb(f"sg{i}", [128, 512]) for i in range(2)]
            yo = [sb(f"yo{i}", [128, 1024]) for i in range(2)]
            wn = [0]
            sn = [0]
            cn = [0]

            def load_w(src):
                i = wn[0] % NWB
                wn[0] += 1
                for k2 in range(4):
                    s_ = stg[sn[0] % 2]
                    sk = f"stg{sn[0] % 2}"
                    sn[0] += 1
                    p.dma("sync", s_[:, :, :], src[k2 * 256:(k2 + 1) * 256, :].rearrange("(k p) f -> p k f", p=128), writes=[sk])
                    eng = ("gpsimd", "gpsimd", "vector", "gpsimd", "scalar", "gpsimd")[cn[0] % 6]
                    cn[0] += 1
                    if eng == "scalar":
                        p.op("scalar", lambda e: e.activation(out=wbuf[i][:, 2 * k2:2 * k2 + 2, :], in_=s_[:, :, :], func=AF.Copy), reads=[sk], writes=[f"w{i}"])
                    else:
                        p.op(eng, lambda e: e.tensor_copy(out=wbuf[i][:, 2 * k2:2 * k2 + 2, :], in_=s_[:, :, :]), reads=[sk], writes=[f"w{i}"])
                return wbuf[i], f"w{i}"

            bn = [0]

            def bank():
                i = bn[0] % 6
                bn[0] += 1
                return c.ps[i], f"ps{i}"

            CH = min(512, C)
            NCH = C // CH
            yn = [0]
            pending = [load_w(w_gate[0]), load_w(w_up[0]), load_w(w_down[0])]
            for ex in range(NE):
                (Wg, Wgk), (Wu, Wuk), (Wd, Wdk) = pending
                xe = xes[ex % 2]
                xek = f"xe{ex % 2}"
                for gi in range(NG):
                    p.dma("gpsimd", None, None, reads=["idx_all"], writes=[xek],
                          fn=lambda e: e.indirect_dma_start(out=xe[:, gi, :], out_offset=None, in_=h2b[:, :],
                                                            in_offset=bass.IndirectOffsetOnAxis(ap=idx_all[:, ex, gi:gi + 1], axis=0)))
                for gi in range(NG):
                    tbk = 6 + gi % 2
                    tps = c.ps[tbk][:, :].bitcast(BF16)
                    for k in range(8):
                        p.op("tensor", lambda e: e.transpose(out=tps[:, k * 128:(k + 1) * 128], in_=xe[:, gi, k * 128:(k + 1) * 128], identity=c.identb[:]),
                             reads=[xek, "identb"], writes=[f"ps{tbk}"])
                    if gi % 2 == 0:
                        p.op("vector", lambda e: e.tensor_copy(out=xeT[:, :, gi * 128:(gi + 1) * 128], in_=tps[:, :].rearrange("p (k t) -> p k t", t=128)),
                             writes=[f"ps{tbk}", "xeT0"])
                    else:
                        p.op("scalar", lambda e: e.activation(out=xeT[:, :, gi * 128:(gi + 1) * 128], in_=tps[:, :].rearrange("p (k t) -> p k t", t=128), func=AF.Copy),
                             writes=[f"ps{tbk}", "xeT1"])
                for fc in range(8):
                    for ch in range(NCH):
                        bG, bGk = bank()
                        bU, bUk = bank()
                        for k in range(8):
                            p.op("tensor", lambda e: e.matmul(bG[:, 0:CH], lhsT=Wg[:, k, fc * 128:(fc + 1) * 128], rhs=xeT[:, k, ch * CH:(ch + 1) * CH],
                                                              start=(k == 0), stop=(k == 7)), reads=[Wgk, "xeT0", "xeT1"], writes=[bGk])
                        for k in range(8):
                            p.op("tensor", lambda e: e.matmul(bU[:, 0:CH], lhsT=Wu[:, k, fc * 128:(fc + 1) * 128], rhs=xeT[:, k, ch * CH:(ch + 1) * CH],
                                                              start=(k == 0), stop=(k == 7)), reads=[Wuk, "xeT0", "xeT1"], writes=[bUk])
                        s_ = sg[(fc * NCH + ch) % 2]
                        sk = f"sg{(fc * NCH + ch) % 2}"
                        p.op("scalar", lambda e: e.activation(out=s_[:, 0:CH], in_=bG[:, 0:CH], func=AF.Silu), writes=[bGk, sk])
                        p.op("vector", lambda e: e.tensor_tensor(out=hmT[:, fc, ch * CH:(ch + 1) * CH], in0=bU[:, 0:CH], in1=s_[:, 0:CH], op=ALU.mult),
                             reads=[sk], writes=[bUk, "hmT"])
                if ex + 1 < NE:
                    nxt = [load_w(w_gate[ex + 1]), load_w(w_up[ex + 1])]
                for cb in range(NG):
                    y_ = yo[yn[0] % 2]
                    yk = f"yo{yn[0] % 2}"
                    yn[0] += 1
                    for hf in range(2):
                        bY, bYk = bank()
                        for f in range(8):
                            p.op("tensor", lambda e: e.matmul(bY[:, :], lhsT=hmT[:, f, cb * 128:(cb + 1) * 128], rhs=Wd[:, f, hf * 512:(hf + 1) * 512],
                                                              start=(f == 0), stop=(f == 7)), reads=[Wdk, "hmT"], writes=[bYk])
                        if hf == 0:
                            p.op("scalar", lambda e: e.activation(out=y_[:, 0:512], in_=bY[:, :], func=AF.Copy, scale=gate_all[:, ex, cb:cb + 1]),
                                 reads=["gate_all"], writes=[bYk, yk])
                        else:
                            p.op("vector", lambda e: e.tensor_scalar(out=y_[:, 512:1024], in0=bY[:, :], scalar1=gate_all[:, ex, cb:cb + 1], scalar2=None, op0=ALU.mult),
                                 reads=["gate_all"], writes=[bYk, yk])
                    p.dma("gpsimd", None, None, reads=[yk, "idx_all"], writes=["xres_scatter"],
                          fn=lambda e: e.indirect_dma_start(out=xres[:, :], out_offset=bass.IndirectOffsetOnAxis(ap=idx_all[:, ex, cb:cb + 1], axis=0),
                                                            in_=y_[:, :], in_offset=None, compute_op=ALU.add))
                if ex + 1 < NE:
                    nxt.append(load_w(w_down[ex + 1]))
                    pending = nxt
            p.barrier()


def phase_f(p, c, T, xres, norm_w, out):
    nc = p.nc
    NT = T // 128
    with ExitStack() as es:
        def sb(name, shape, dt=F32):
            return es.enter_context(nc.sbuf_tensor(f"F_{_uid()}_" + name, shape, dt))
        nwb = sb("nwb", [128, 1024])
        xt = [sb(f"xt{i}", [128, 1024]) for i in range(3)]
        junk = sb("junk", [128, 1024], BF16)
        st = [sb(f"st{i}", [128, 4]) for i in range(2)]
        p.dma("sync", nwb[:], norm_w.to_broadcast([128, 1024]), writes=["nwb"])
        for i in range(NT):
            x_, xk = xt[i % 3], f"xt{i % 3}"
            s_, sk = st[i % 2], f"st{i % 2}"
            p.dma("sync", x_[:], xres[i * 128:(i + 1) * 128, :], writes=[xk])
            p.op("scalar", lambda e: e.activation(out=junk[:], in_=x_[:], func=AF.Square, accum_out=s_[:, 0:1]), reads=[xk], writes=["junk", sk + "a"])
            p.op("vector", lambda e: e.tensor_scalar(out=s_[:, 1:2], in0=s_[:, 0:1], scalar1=1.0 / 1024, scalar2=RMS_EPS, op0=ALU.mult, op1=ALU.add),
                 reads=[sk + "a"], writes=[sk + "b"])
            p.op("scalar", lambda e: e.activation(out=s_[:, 2:3], in_=s_[:, 1:2], func=AF.Sqrt), reads=[sk + "b"], writes=[sk + "c"])
            p.op("vector", lambda e: e.reciprocal(out=s_[:, 3:4], in_=s_[:, 2:3]), reads=[sk + "c"], writes=[sk + "d"])
            p.op("vector", lambda e: e.scalar_tensor_tensor(out=x_[:], in0=x_[:], scalar=s_[:, 3:4], in1=nwb[:], op0=ALU.mult, op1=ALU.mult),
                 reads=[xk, sk + "d", "nwb"], writes=[xk])
            p.dma("sync", out[i * 128:(i + 1) * 128, :], x_[:], reads=[xk])
        p.barrier()


DEPTH = 2


def rope_tables(T):
    half = 8
    inv = np.power(500000.0, -np.arange(half, dtype=np.float64) * 2.0 / 16)
    ang = np.arange(T, dtype=np.float32).astype(np.float64)[None, :] * inv.astype(np.float32).astype(np.float64)[:, None]
    cos = np.cos(ang)
    sin = np.sin(ang)
    c16 = np.concatenate([cos, cos], 0)
    s16 = np.concatenate([sin, sin], 0)
    return np.tile(c16, (8, 1)).astype(np.float32), np.tile(s16, (8, 1)).astype(np.float32)


def build_program(T, depth=DEPTH, phases=None):
    nc = bass.Bass("TRN2", target_bir_lowering=False)
    def din(name, shape, dt=F32):
        return nc.dram_tensor(name, shape, dt, kind="ExternalInput").ap()
    def dint(name, shape, dt=F32):
        return nc.dram_tensor(name, shape, dt, kind="Internal").ap()
    x = din("x", [T, 1024])
    norm_mix = din("norm_mix", [depth, 1024])
    w_in = din("w_in", [depth, 1024, P_IN])
    conv_w = din("conv_w", [depth, 5, 1536])
    a_log = din("a_log", [depth, 8])
    dt_bias = din("dt_bias", [depth, 8])
    gdn_norm = din("gdn_norm", [depth, 128])
    w_a = din("w_branch_a", [depth, 512, 1024])
    w_b = din("w_branch_b", [depth, 256, 1024])
    w_out = din("w_out", [depth, 1024, 1024])
    norm_ffn = din("norm_ffn", [depth, 1024])
    w_router = din("w_router", [depth, 1024, 16])
    w_eg = din("w_expert_gate", [depth, 16, 1024, 1024])
    w_eu = din("w_expert_up", [depth, 16, 1024, 1024])
    w_ed = din("w_expert_down", [depth, 16, 1024, 1024])
    norm_final = din("norm_final", [1, 1024])
    cos_t = din("cos_t", [128, T])
    sin_t = din("sin_t", [128, T])
    out = nc.dram_tensor("out", [T, 1024], F32, kind="ExternalOutput").ap()

    xres = dint("xres", [T, 1024])
    oA = dict(qkvTa=dint("qkvTa", [1536, T + 4], BF16), ztok=dint("ztok", [T, 512]), bdtok=dint("bdtok", [T, 16]), bdT=dint("bdT", [16, T]),
              qkrope=dint("qkrope", [384, T], BF16), qkrest=dint("qkrest", [1152, T], BF16), vTb=dint("vTb", [768, T], BF16),
              gatesT=dint("gatesT", [2048, T], BF16))
    oB1 = dict(uT=dint("uT", [1024, T]), ktok=dint("ktok", [T, 512], BF16), vtok=dint("vtok", [T, 512], BF16))
    o_f = dint("o_f", [T, 512])
    o_b = dint("o_b", [T, 512])
    yaT = dint("yaT", [512, T], BF16)
    att = dint("att", [T, 12, 65])
    h2b = dint("h2b", [T, 1024], BF16)
    aff = dint("aff", [T, 16])

    with ExitStack() as es:
        p = Prog(nc, es)
        c = alloc_common(p, es)
        alloc_gdn_consts(p, c, es)
        alloc_att_consts(p, c, es)
        zt = es.enter_context(nc.sbuf_tensor("zpad", [128, 2], BF16))
        p.op("gpsimd", lambda e: e.memset(zt[:], 0.0), writes=["zpad"])
        for ch in range(12):
            p.dma("sync", oA["qkvTa"][ch * 128:(ch + 1) * 128, 0:2], zt[:], reads=["zpad"], allow_slow_non_contiguous=True)
            p.dma("sync", oA["qkvTa"][ch * 128:(ch + 1) * 128, T + 2:T + 4], zt[:], reads=["zpad"], allow_slow_non_contiguous=True)
        p.barrier()
        ph = phases or "A,B1,B2,B3,C,D,E,F"
        ph = ph.split(",")
        for l in range(depth):
            xin = x if l == 0 else xres
            if "A" in ph:
                phase_a(p, c, T, xin, w_in[l], norm_mix[l:l + 1, :], cos_t, sin_t, oA)
            if "B1" in ph and "C" in ph and INTERLEAVE_B1_C:
                with ExitStack() as es_bc:
                    g1 = phase_b1_gen(p, c, T, oA["qkvTa"], conv_w[l], oB1, es_ext=es_bc, shared=True)
                    g2 = phase_c_gen(p, c, T, oA["qkrope"], oA["qkrest"], oA["vTb"], att, es_ext=es_bc, shared=True)
                    n1 = (T // 512) * 12
                    n2 = sum(4 * r_ * (T // r_ // 128 + 1) for r_ in ATT_DIL)
                    d1 = d2 = 0
                    a1 = a2 = True
                    while a1 or a2:
                        if a1 and (not a2 or d1 * n2 <= d2 * n1):
                            try:
                                next(g1)
                                d1 += 1
                            except StopIteration:
                                a1 = False
                        else:
                            try:
                                next(g2)
                                d2 += 1
                            except StopIteration:
                                a2 = False
                    p.barrier()
            elif "B1" in ph:
                phase_b1(p, c, T, oA["qkvTa"], conv_w[l], oB1)
            if "B2" in ph:
                phase_b2(p, c, T, oB1["uT"], oB1["ktok"], oB1["vtok"], oA["bdtok"], a_log[l:l + 1, :], dt_bias[l:l + 1, :], o_f, o_b)
            if "B3" in ph:
                phase_b3(p, c, T, o_f, o_b, oA["ztok"], gdn_norm[l:l + 1, :], yaT)
            if "C" in ph and not ("B1" in ph and INTERLEAVE_B1_C):
                phase_c(p, c, T, oA["qkrope"], oA["qkrest"], oA["vTb"], att)
            if "D" in ph:
                phase_d(p, c, T, yaT, att, oA["gatesT"], xres, w_a[l], w_b[l], w_out[l], norm_ffn[l:l + 1, :], w_router[l], h2b, aff, x_in=xin)
            if "E" in ph:
                phase_e(p, c, T, aff, h2b, xres, w_eg[l], w_eu[l], w_ed[l])
        if "F" in ph:
            phase_f(p, c, T, xres, norm_final, out)
        p.finish()
        nc._stats = (p.nops, p.nwaits)
    return nc


def make_in_maps(inputs, T, nb):
    cos_t, sin_t = rope_tables(T)
    f32 = lambda a: np.ascontiguousarray(np.asarray(a), dtype=np.float32)
    shared = {
        "norm_mix": f32(inputs["norm_mix"]), "w_in": f32(inputs["w_in"]), "conv_w": f32(inputs["conv_w"]),
        "a_log": f32(inputs["a_log"]).reshape(-1, 8), "dt_bias": f32(inputs["dt_bias"]).reshape(-1, 8),
        "gdn_norm": f32(inputs["gdn_norm"]), "w_branch_a": f32(inputs["w_branch_a"]), "w_branch_b": f32(inputs["w_branch_b"]),
        "w_out": f32(inputs["w_out"]), "norm_ffn": f32(inputs["norm_ffn"]), "w_router": f32(inputs["w_router"]),
        "w_expert_gate": f32(inputs["w_expert_gate"]), "w_expert_up": f32(inputs["w_expert_up"]),
        "w_expert_down": f32(inputs["w_expert_down"]), "norm_final": f32(inputs["norm_final"]).reshape(1, 1024),
        "cos_t": cos_t, "sin_t": sin_t,
    }
    x = f32(inputs["x"])
    return [dict(shared, x=x[b]) for b in range(nb)]


_NC_CACHE = {}


def kernel(**inputs):
    x = np.asarray(inputs["x"])
    B, T, _ = x.shape
    if T not in _NC_CACHE:
        _NC_CACHE[T] = build_program(T)
    nc = _NC_CACHE[T]
    in_maps = make_in_maps(inputs, T, B)
    res = run_bass_kernel_spmd(nc, in_maps, core_ids=list(range(B)))
    return np.stack([np.asarray(r["out"]) for r in res.results], axis=0).astype(np.float32)
```

```python
import numpy as np
import concourse.bass as bass
import concourse.mybir as mybir
from concourse.bass_utils import run_bass_kernel_spmd
from contextlib import ExitStack

F32 = mybir.dt.float32
BF16 = mybir.dt.bfloat16
I32 = mybir.dt.int32
U32 = mybir.dt.uint32
AF = mybir.ActivationFunctionType
ALU = mybir.AluOpType
AX = mybir.AxisListType

import os as _os0
SAME_ENGINE_SYNC = not _os0.environ.get("NO_SES")
INTERLEAVE_B1_C = bool(_os0.environ.get("ILV"))
GDN_F32R = False

D_MODEL = 1024
P_IN = 6416
OFF_Z = 1536
OFF_BETA = 2048
OFF_ATT = 2064
OFF_V = OFF_ATT + 1536
OFF_GATE = 4368
RMS_EPS = 1e-6


class _Rec:
    def __init__(self):
        self.call = None

    def __getattr__(self, name):
        def f(*a, **k):
            self.call = (name, a, k)
            return self
        return f


def _ap_free(ap):
    try:
        sh = tuple(ap.shape)
        n = 1
        for v in sh[1:]:
            n *= int(v)
        return int(sh[0]), n
    except Exception:
        return 128, 128


_DT_SIZE = {}


def _dtsize(dt):
    if dt not in _DT_SIZE:
        _DT_SIZE[dt] = 2 if dt == BF16 else 4
    return _DT_SIZE[dt]


class Prog:
    def __init__(self, nc, es, defer=True):
        self.nc = nc
        self.es = es
        self.engs = {}
        for name in ["tensor", "vector", "scalar", "gpsimd", "sync"]:
            sem = es.enter_context(nc.semaphore(f"sem_{name}"))
            self.engs[name] = dict(h=getattr(nc, name), sem=sem, cnt=0, waited={}, name=name)
        self.rings = {}
        for q in ["sync", "gpsimd", "scalar"]:
            K = 8
            sems = [es.enter_context(nc.semaphore(f"dq_{q}_{i}")) for i in range(K)]
            self.rings[q] = dict(sems=sems, n=0, K=K, vals=[0] * K)
        self.lastw = {}
        self.readers = {}
        self.nwaits = 0
        self.nops = 0
        self.defer = defer
        self.pending = []

    def _wait(self, E, tok):
        sid, sem, val = tok
        if E["waited"].get(sid, 0) >= val:
            return
        if sid == id(E["sem"]):
            if E["name"] == "tensor" or (not SAME_ENGINE_SYNC and E["name"] != "gpsimd"):
                return
        E["h"].wait_ge(sem, val)
        E["waited"][sid] = val
        self.nwaits += 1

    def _deps(self, E, reads, writes):
        for k in reads:
            t = self.lastw.get(k)
            if t is not None:
                self._wait(E, t)
        for k in writes:
            t = self.lastw.get(k)
            if t is not None:
                self._wait(E, t)
            for t in self.readers.get(k, {}).values():
                self._wait(E, t)

    def _record(self, tok, reads, writes):
        for k in reads:
            d = self.readers.setdefault(k, {})
            d[tok[0]] = tok
        for k in writes:
            self.lastw[k] = tok
            self.readers[k] = {}

    def _emit_op(self, eng, call, reads, writes):
        E = self.engs[eng]
        self._deps(E, reads, writes)
        name, a, k = call
        inst = getattr(E["h"], name)(*a, **k)
        E["cnt"] += 1
        inst.then_inc(E["sem"], 1)
        tok = (id(E["sem"]), E["sem"], E["cnt"])
        self._record(tok, reads, writes)
        self.nops += 1

    def _emit_dma(self, q, call, reads, writes):
        E = self.engs[q]
        R = self.rings[q]
        i = R["n"] % R["K"]
        R["n"] += 1
        sem = R["sems"][i]
        if R["vals"][i] > 0:
            self._wait(E, (id(sem), sem, R["vals"][i]))
        self._deps(E, reads, writes)
        name, a, k = call
        inst = getattr(E["h"], name)(*a, **k)
        R["vals"][i] += 16
        inst.then_inc(sem, 16)
        tok = (id(sem), sem, R["vals"][i])
        self._record(tok, reads, writes)
        self.nops += 1

    @staticmethod
    def _cost(eng, call):
        name, a, k = call
        try:
            if eng == "tensor":
                if name == "transpose":
                    ap = k.get("in_", a[1] if len(a) > 1 else None)
                    p_, f_ = _ap_free(ap)
                    c = 0.03 + max(p_, 64) / 2400.0 * (2 if ap.dtype == F32 else 1)
                    return c, c + 0.15
                rhs = k.get("rhs", a[2] if len(a) > 2 else None)
                lhs = k.get("lhsT", a[1] if len(a) > 1 else None)
                _, n = _ap_free(rhs)
                passes = 4 if rhs.dtype == F32 else 1
                c = 0.02 + (max(n, 64) + 0) * passes / 2400.0 + (0.05 if passes == 4 else 0.0)
                return c, c + 0.15
            out = k.get("out", a[0] if a else None)
            _, f_ = _ap_free(out)
            if eng == "vector":
                c = 0.07 + f_ / 960.0
                if name in ("tensor_tensor", "scalar_tensor_tensor", "tensor_tensor_scan"):
                    c = 0.07 + f_ / 700.0
            elif eng == "scalar":
                c = 0.12 + f_ / 1100.0
            else:
                c = 0.16 + f_ / 900.0
            return c, c
        except Exception:
            return 0.3, 0.3

    def op(self, eng, fn, reads=(), writes=()):
        psr = [k for k in reads if isinstance(k, str) and k.startswith("ps")]
        if psr:
            reads = [k for k in reads if k not in psr]
            writes = list(writes) + psr
        rec = _Rec()
        fn(rec)
        occ, lat = self._cost(eng, rec.call)
        self.pending.append(("op", eng, rec.call, list(reads), list(writes), occ, lat))
        if not self.defer:
            self.flush()

    def dma(self, q, out, in_, reads=(), writes=(), fn=None, **kw):
        if fn is None:
            call = ("dma_start", (), dict(out=out, in_=in_, **kw))
            p_, f_ = _ap_free(out)
            nbytes = p_ * f_ * _dtsize(out.dtype)
        else:
            rec = _Rec()
            fn(rec)
            call = rec.call
            nbytes = 128 * 2048
        lat = 2.0 + nbytes / 120000.0
        if q == "gpsimd":
            lat += 3.0
        occ = 0.06 if q != "gpsimd" else 1.0
        self.pending.append(("dma", q, call, list(reads), list(writes), occ, lat))
        if not self.defer:
            self.flush()

    def flush(self, window=20):
        ops = self.pending
        self.pending = []
        n = len(ops)
        if n == 0:
            return
        if n < 4:
            order = range(n)
        else:
            order = self._schedule(ops, window)
        for j in order:
            kind, eng, call, reads, writes, occ, lat = ops[j]
            if kind == "op":
                self._emit_op(eng, call, reads, writes)
            else:
                self._emit_dma(eng, call, reads, writes)

    @staticmethod
    def _schedule(ops, W):
        n = len(ops)
        engof = [o[1] if o[0] == "op" else "q_" + o[1] for o in ops]
        lastw = {}
        readers = {}
        preds = [None] * n
        succ_eng = [None] * n
        for j, o in enumerate(ops):
            ps = set()
            for k in o[3]:
                t = lastw.get(k)
                if t is not None:
                    ps.add(t)
            for k in o[4]:
                t = lastw.get(k)
                if t is not None:
                    ps.add(t)
                for t in readers.get(k, ()):
                    ps.add(t)
            ps.discard(j)
            preds[j] = tuple(ps)
            for k in o[3]:
                readers.setdefault(k, []).append(j)
            for k in o[4]:
                lastw[k] = j
                readers[k] = []
        qhist = {}
        for j, o in enumerate(ops):
            if o[0] == "dma":
                h = qhist.setdefault(o[1], [])
                if len(h) >= 8:
                    preds[j] = preds[j] + (h[-8],)
                h.append(j)
        succs = [[] for _ in range(n)]
        for j in range(n):
            for pj in preds[j]:
                succs[pj].append(j)
        lists = {}
        for j in range(n):
            lists.setdefault(engof[j], []).append(j)
        head = {e: 0 for e in lists}
        done = [False] * n
        fin = [0.0] * n
        start = [0.0] * n
        tE = {e: 0.0 for e in lists}
        LATX, LATS = 0.35, 0.12
        cache = {}

        def candidate(e):
            lst = lists[e]
            h = head[e]
            L = len(lst)
            while h < L and done[lst[h]]:
                h += 1
            head[e] = h
            best = None
            cnt = 0
            i = h
            te = tE[e]
            while i < L and cnt < W:
                idx = lst[i]
                i += 1
                if done[idx]:
                    continue
                cnt += 1
                r = 0.0
                ok = True
                for pj in preds[idx]:
                    if not done[pj]:
                        ok = False
                        break
                    v = fin[pj] + (LATS if engof[pj] == e else LATX)
                    if v > r:
                        r = v
                if not ok:
                    continue
                s_ = r if r > te else te
                if best is None or s_ < best[0] - 1e-9:
                    best = (s_, idx)
                if r <= te:
                    break
            return best

        for e in lists:
            cache[e] = candidate(e)
        remaining = n
        out = []
        while remaining:
            bs = None
            be = None
            for e, cnd in cache.items():
                if cnd is not None and (bs is None or cnd[0] < bs[0] - 1e-9 or (abs(cnd[0] - bs[0]) <= 1e-9 and cnd[1] < bs[1])):
                    bs = cnd
                    be = e
            s_, idx = bs
            o = ops[idx]
            done[idx] = True
            start[idx] = s_
            fin[idx] = s_ + o[6]
            tE[be] = s_ + o[5]
            out.append(idx)
            remaining -= 1
            dirty = {be}
            for sj in succs[idx]:
                dirty.add(engof[sj])
            for e in dirty:
                cache[e] = candidate(e)
        out.sort(key=lambda j: (start[j], j))
        return out

    def barrier(self):
        self.flush()
        toks = []
        for E in self.engs.values():
            if E["cnt"] > 0:
                toks.append((id(E["sem"]), E["sem"], E["cnt"]))
        for R in self.rings.values():
            for i, sem in enumerate(R["sems"]):
                if R["vals"][i] > 0:
                    toks.append((id(sem), sem, R["vals"][i]))
        for E in self.engs.values():
            for t in toks:
                if t[0] != id(E["sem"]):
                    self._wait(E, t)
        self.lastw = {}
        self.readers = {}

    def finish(self):
        self.flush()
        E = self.engs["sync"]
        for X in self.engs.values():
            if X["cnt"] > 0 and X is not E:
                self._wait(E, (id(X["sem"]), X["sem"], X["cnt"]))
        for q, R in self.rings.items():
            for i, sem in enumerate(R["sems"]):
                if R["vals"][i] > 0:
                    self._wait(E, (id(sem), sem, R["vals"][i]))


class Ctx:
    pass


_UIDC = [0]


def _uid():
    _UIDC[0] += 1
    return _UIDC[0]


def alloc_common(p, es):
    nc = p.nc
    c = Ctx()
    c.ps = [es.enter_context(nc.psum_tensor(f"ps{i}", [128, 512], F32)) for i in range(8)]
    c.psb = c.ps[7][:, :].bitcast(BF16)
    c.identf = es.enter_context(nc.sbuf_tensor("identf", [128, 128], F32))
    c.identb = es.enter_context(nc.sbuf_tensor("identb", [128, 128], BF16))
    c.onesf = es.enter_context(nc.sbuf_tensor("onesf", [128, 128], F32))
    p.op("gpsimd", lambda e: e.memset(c.onesf[:], 1.0), writes=["onesf"])
    p.op("gpsimd", lambda e: e.memset(c.identf[:], 1.0), writes=["identf"])
    p.op("gpsimd", lambda e: e.affine_select(out=c.identf[:], in_=c.identf[:], pattern=[[-1, 128]],
                                              compare_op=ALU.is_equal, fill=0.0, base=0, channel_multiplier=1),
         reads=["identf"], writes=["identf"])
    p.op("vector", lambda e: e.tensor_copy(out=c.identb[:], in_=c.identf[:]), reads=["identf"], writes=["identb"])
    c.bias_q = es.enter_context(nc.sbuf_tensor("bias_q", [128, 1], F32))
    c.bias_k = es.enter_context(nc.sbuf_tensor("bias_k", [128, 1], F32))
    p.op("gpsimd", lambda e: e.memset(c.bias_q[:], 128.0e-6), writes=["bias_q"])
    p.op("gpsimd", lambda e: e.memset(c.bias_k[:], 1.0e-6), writes=["bias_k"])
    c.bias_one = es.enter_context(nc.sbuf_tensor("bias_one", [128, 1], F32))
    p.op("gpsimd", lambda e: e.memset(c.bias_one[:], 1.0), writes=["bias_one"])
    return c


def phase_a(p, c, T, x_src, w_in, norm_w, cos_t, sin_t, o):
    nc = p.nc
    NS = T // 512
    with ExitStack() as es:
        def sb(name, shape, dt):
            return es.enter_context(nc.sbuf_tensor(f"A_{_uid()}_" + name, shape, dt))
        NMAIN = 1536 + 528 + 768 + 2048
        Wm = sb("Wm", [128, 8, NMAIN], BF16)
        M_QKV, M_Z, M_BD, M_V, M_G = 0, 1536, 2048, 2064, 2832
        Wrope = sb("Wrope", [128, 8, 384], BF16)
        Wrest = sb("Wrest", [128, 8, 1152], BF16)
        Wp = sb("Wp", [128, 8, 384], BF16)
        stg = [sb(f"stg{i}", [128, 2048], F32) for i in range(2)]
        nwb = sb("nwb", [128, 1024], F32)
        xt = [sb(f"xt{i}", [128, 1024], F32) for i in range(2)]
        junk = sb("junk", [128, 1024], BF16)
        hb = sb("hb", [128, 1024], BF16)
        st = sb("st", [128, 8], F32)
        hT = [sb(f"hT{i}", [128, 8, 512], BF16) for i in range(2)]
        cs = [sb(f"cs{i}", [128, 2, 512], F32) for i in range(2)]
        of32 = [sb(f"of32_{i}", [128, 512], F32) for i in range(3)]
        ob16 = [sb(f"ob16_{i}", [128, 512], BF16) for i in range(3)]
        t1 = sb("t1", [128, 512], F32)
        t2 = sb("t2", [128, 512], F32)
        obd = sb("obd", [128, 4, 16], F32)

        p.dma("sync", nwb[:], norm_w.to_broadcast([128, 1024]), writes=["nwb"])
        pieces = [(0, 1536), (1536, 2064), (2064, 3600), (3600, 4368), (4368, 6416)]
        si = 0
        for k in range(8):
            for pi, (a, b) in enumerate(pieces):
                s = stg[si % 2]
                sk = f"stg{si % 2}"
                si += 1
                p.dma("sync", s[:, 0:b - a], w_in[k * 128:(k + 1) * 128, a:b], writes=[sk])
                if pi == 0:
                    p.op("gpsimd", lambda e: e.tensor_copy(out=Wm[:, k, M_QKV:M_QKV + 1536], in_=s[:, 0:1536]),
                         reads=[sk], writes=["Wm"])
                elif pi == 1:
                    p.op("gpsimd", lambda e: e.tensor_copy(out=Wm[:, k, M_Z:M_Z + 528], in_=s[:, 0:528]),
                         reads=[sk], writes=["Wm"])
                elif pi == 3:
                    p.op("gpsimd", lambda e: e.tensor_copy(out=Wm[:, k, M_V:M_V + 768], in_=s[:, 0:768]),
                         reads=[sk], writes=["Wm"])
                elif pi == 4:
                    p.op("gpsimd", lambda e: e.tensor_copy(out=Wm[:, k, M_G:M_G + 2048], in_=s[:, 0:2048]),
                         reads=[sk], writes=["Wm"])
                else:
                    sv = s[:, 0:1536].rearrange("p (h d) -> p h d", d=64)
                    p.op("gpsimd", lambda e: e.tensor_copy(
                        out=Wrope[:, k, :].rearrange("p (h d) -> p h d", d=16), in_=sv[:, :, 0:16]),
                        reads=[sk], writes=["Wrope"])
                    p.op("gpsimd", lambda e: e.tensor_copy(
                        out=Wrest[:, k, :].rearrange("p (h d) -> p h d", d=48), in_=sv[:, :, 16:64]),
                        reads=[sk], writes=["Wrest"])
                    wpv = Wp[:, k, :].rearrange("p (h d) -> p h d", d=16)
                    p.op("gpsimd", lambda e: e.tensor_scalar(out=wpv[:, :, 0:8], in0=sv[:, :, 8:16], scalar1=-1.0,
                                                             scalar2=None, op0=ALU.mult),
                         reads=[sk], writes=["Wp"])
                    p.op("gpsimd", lambda e: e.tensor_copy(out=wpv[:, :, 8:16], in_=sv[:, :, 0:8]),
                         reads=[sk], writes=["Wp"])

        rot = {"f": 0, "b": 0, "ps": 0, "ev": 0}

        def prep_front(s):
            slot = s % 2
            for ti in range(4):
                r0 = s * 512 + ti * 128
                xs = xt[ti % 2]
                xk = f"xt{ti % 2}"
                p.dma("sync", xs[:], x_src[r0:r0 + 128, :], writes=[xk])
                p.op("scalar", lambda e: e.activation(out=junk[:], in_=xs[:], func=AF.Square, accum_out=st[:, 0:1]),
                     reads=[xk], writes=["junk", "st0"])
                p.op("vector", lambda e: e.tensor_scalar(out=st[:, 1:2], in0=st[:, 0:1], scalar1=1.0 / 1024, scalar2=RMS_EPS,
                                                         op0=ALU.mult, op1=ALU.add), reads=["st0"], writes=["st1"])
                p.op("scalar", lambda e: e.activation(out=st[:, 2:3], in_=st[:, 1:2], func=AF.Sqrt), reads=["st1"], writes=["st2"])
                p.op("vector", lambda e: e.reciprocal(out=st[:, 3:4], in_=st[:, 2:3]), reads=["st2"], writes=["st3"])
                p.op("vector", lambda e: e.scalar_tensor_tensor(out=hb[:], in0=xs[:], scalar=st[:, 3:4], in1=nwb[:],
                                                                op0=ALU.mult, op1=ALU.mult),
                     reads=[xk, "st3", "nwb"], writes=["hb"])
                for k in range(8):
                    p.op("tensor", lambda e: e.transpose(out=c.psb[:, k * 128:(k + 1) * 128], in_=hb[:, k * 128:(k + 1) * 128],
                                                         identity=c.identb[:]),
                         reads=["hb", "identb"], writes=["psb"])
                p.op("scalar", lambda e: e.activation(out=hT[slot][:, :, ti * 128:(ti + 1) * 128],
                                                      in_=c.psb[:, :].rearrange("p (k t) -> p k t", t=128), func=AF.Copy),
                     reads=["psb"], writes=[f"hT{slot}"])

        def fm_group(s, Wsrc, wkey, col0, nchunks, dst, dst_row0, mode):
            slot = s % 2
            hk = f"hT{slot}"
            for ci in range(nchunks):
                pi = rot["ps"] % 6
                rot["ps"] += 1
                ps = c.ps[pi]
                pk = f"ps{pi}"
                for k in range(8):
                    p.op("tensor", lambda e: e.matmul(ps[:, :], lhsT=Wsrc[:, k, col0 + ci * 128: col0 + (ci + 1) * 128],
                                                      rhs=hT[slot][:, k, :], start=(k == 0), stop=(k == 7)),
                         reads=[wkey, hk], writes=[pk])
                if mode == "f32":
                    oi = rot["f"] % 3
                    rot["f"] += 1
                    ot, ok = of32[oi], f"of32_{oi}"
                else:
                    oi = rot["b"] % 3
                    rot["b"] += 1
                    ot, ok = ob16[oi], f"ob16_{oi}"
                ev = "vector" if rot["ev"] % 2 == 0 else "scalar"
                rot["ev"] += 1
                if mode == "sig":
                    p.op("scalar", lambda e: e.activation(out=ot[:], in_=ps[:, :], func=AF.Sigmoid), reads=[pk], writes=[ok])
                elif ev == "vector":
                    p.op("vector", lambda e: e.tensor_copy(out=ot[:], in_=ps[:, :]), reads=[pk], writes=[ok])
                else:
                    p.op("scalar", lambda e: e.activation(out=ot[:], in_=ps[:, :], func=AF.Copy), reads=[pk], writes=[ok])
                r = dst_row0 + ci * 128
                p.dma("sync", dst(r, s), ot[:], reads=[ok])

        def body(s):
            slot = s % 2
            hk = f"hT{slot}"
            p.dma("sync", cs[slot][:, 0, :], cos_t[:, s * 512:(s + 1) * 512], writes=[f"cs{slot}"])
            p.dma("sync", cs[slot][:, 1, :], sin_t[:, s * 512:(s + 1) * 512], writes=[f"cs{slot}"])
            fm_group(s, Wm, "Wm", M_QKV, 12, lambda r, s_: o["qkvTa"][r:r + 128, 2 + s_ * 512: 2 + (s_ + 1) * 512], 0, "bf16")
            for ti in range(4):
                pi = rot["ps"] % 6
                rot["ps"] += 1
                ps = c.ps[pi]
                pk = f"ps{pi}"
                for k in range(8):
                    p.op("tensor", lambda e: e.matmul(ps[:, :], lhsT=hT[slot][:, k, ti * 128:(ti + 1) * 128],
                                                      rhs=Wm[:, k, M_Z:M_Z + 512], start=(k == 0), stop=(k == 7)),
                         reads=["Wm", hk], writes=[pk])
                oi = rot["f"] % 3
                rot["f"] += 1
                p.op("vector", lambda e: e.tensor_copy(out=of32[oi][:], in_=ps[:, :]), reads=[pk], writes=[f"of32_{oi}"])
                r0 = s * 512 + ti * 128
                p.dma("sync", o["ztok"][r0:r0 + 128, :], of32[oi][:], reads=[f"of32_{oi}"])
            ps = c.ps[6]
            for ti in range(4):
                for k in range(8):
                    p.op("tensor", lambda e: e.matmul(ps[:, ti * 16:(ti + 1) * 16], lhsT=hT[slot][:, k, ti * 128:(ti + 1) * 128],
                                                      rhs=Wm[:, k, M_BD:M_BD + 16], start=(k == 0), stop=(k == 7)),
                         reads=["Wm", hk], writes=["ps6"])
            p.op("vector", lambda e: e.tensor_copy(out=obd[:, :, :], in_=ps[:, 0:64].rearrange("p (t c) -> p t c", c=16)),
                 reads=["ps6"], writes=["obd"])
            p.dma("sync", o["bdtok"][s * 512:(s + 1) * 512, :].rearrange("(t p) c -> p t c", p=128), obd[:, :, :],
                  reads=["obd"])
            pi = rot["ps"] % 6
            rot["ps"] += 1
            for k in range(8):
                p.op("tensor", lambda e: e.matmul(c.ps[pi][0:16, :], lhsT=Wm[:, k, M_BD:M_BD + 16],
                                                  rhs=hT[slot][:, k, :], start=(k == 0), stop=(k == 7)),
                     reads=["Wm", hk], writes=[f"ps{pi}"])
            oi = rot["f"] % 3
            rot["f"] += 1
            p.op("vector", lambda e: e.tensor_copy(out=of32[oi][0:16, :], in_=c.ps[pi][0:16, :]), reads=[f"ps{pi}"], writes=[f"of32_{oi}"])
            p.dma("sync", o["bdT"][:, s * 512:(s + 1) * 512], of32[oi][0:16, :], reads=[f"of32_{oi}"])
            for ci in range(3):
                pa = rot["ps"] % 6
                rot["ps"] += 1
                pb = rot["ps"] % 6
                rot["ps"] += 1
                for k in range(8):
                    p.op("tensor", lambda e: e.matmul(c.ps[pa][:, :], lhsT=Wrope[:, k, ci * 128:(ci + 1) * 128],
                                                      rhs=hT[slot][:, k, :], start=(k == 0), stop=(k == 7)),
                         reads=["Wrope", hk], writes=[f"ps{pa}"])
                for k in range(8):
                    p.op("tensor", lambda e: e.matmul(c.ps[pb][:, :], lhsT=Wp[:, k, ci * 128:(ci + 1) * 128],
                                                      rhs=hT[slot][:, k, :], start=(k == 0), stop=(k == 7)),
                         reads=["Wp", hk], writes=[f"ps{pb}"])
                p.op("vector", lambda e: e.tensor_tensor(out=t1[:], in0=c.ps[pa][:, :], in1=cs[slot][:, 0, :], op=ALU.mult),
                     reads=[f"ps{pa}", f"cs{slot}"], writes=["t1"])
                p.op("vector", lambda e: e.tensor_tensor(out=t2[:], in0=c.ps[pb][:, :], in1=cs[slot][:, 1, :], op=ALU.mult),
                     reads=[f"ps{pb}", f"cs{slot}"], writes=["t2"])
                oi = rot["b"] % 3
                rot["b"] += 1
                p.op("gpsimd", lambda e: e.tensor_tensor(out=ob16[oi][:], in0=t1[:], in1=t2[:], op=ALU.add),
                     reads=["t1", "t2"], writes=[f"ob16_{oi}"])
                p.dma("sync", o["qkrope"][ci * 128:(ci + 1) * 128, s * 512:(s + 1) * 512], ob16[oi][:],
                      reads=[f"ob16_{oi}"])
            fm_group(s, Wrest, "Wrest", 0, 9, lambda r, s_: o["qkrest"][r:r + 128, s_ * 512:(s_ + 1) * 512], 0, "bf16")
            fm_group(s, Wm, "Wm", M_V, 6, lambda r, s_: o["vTb"][r:r + 128, s_ * 512:(s_ + 1) * 512], 0, "bf16")
            fm_group(s, Wm, "Wm", M_G, 16, lambda r, s_: o["gatesT"][r:r + 128, s_ * 512:(s_ + 1) * 512], 0, "sig")

        prep_front(0)
        for s in range(NS):
            body(s)
            if s + 1 < NS:
                prep_front(s + 1)
        p.barrier()


def phase_b1(p, c, T, qkvTa, conv_w, o):
    for _ in phase_b1_gen(p, c, T, qkvTa, conv_w, o):
        pass


def phase_b1_gen(p, c, T, qkvTa, conv_w, o, es_ext=None, shared=False):
    nc = p.nc
    NS = T // 512
    with ExitStack() as es_own:
        es = es_ext if es_ext is not None else es_own
        def sb(name, shape, dt):
            return es.enter_context(nc.sbuf_tensor(f"B1_{_uid()}_" + name, shape, dt))
        cw = sb("cw", [128, 12, 5], F32)
        uin = [sb(f"uin{i}", [128, 516], BF16) for i in range(3)]
        Dg = sb("Dg", [128, 60, 128], BF16)
        su = [sb(f"su{i}", [128, 512], F32) for i in range(2)]
        sqs = [sb(f"sq{i}", [128, 512], BF16) for i in range(3)]
        onesb = sb("onesb", [128, 128], BF16)
        svb = [sb(f"svb{i}", [128, 512], BF16) for i in range(2)]
        rns = [sb(f"rn{i}", [128, 512], F32) for i in range(3)]
        rn2s = [sb(f"rn2_{i}", [128, 512], F32) for i in range(3)]
        un = [sb(f"un{i}", [128, 512], F32) for i in range(2)]
        tk = [sb(f"tk{i}", [128, 4, 128], BF16) for i in range(2)]
        p.op("vector", lambda e: e.tensor_copy(out=onesb[:], in_=c.onesf[:]), reads=["onesf"], writes=["onesb"])
        for j in range(5):
            p.dma("sync", cw[:, :, j:j + 1], conv_w[j:j + 1, :].rearrange("j (k c) -> c k j", c=128), writes=["cw"], allow_slow_non_contiguous=True)
        for ch in range(12):
            for j in range(5):
                p.op("vector", lambda e: e.tensor_scalar(out=Dg[:, ch * 5 + j, :], in0=c.identb[:], scalar1=cw[:, ch, j:j + 1], scalar2=None, op0=ALU.mult),
                     reads=["cw", "identb"], writes=["Dg"])
        n = 0
        for s in range(NS):
            for ch in range(12):
                ui, uk = uin[n % 3], f"uin{n % 3}"
                sv, sk = su[n % 2], f"su{n % 2}"
                u, unk = un[n % 2], f"un{n % 2}"
                if shared:
                    cb, pi, tb = 5, 6, 7
                else:
                    cb = n % 3
                    pi = 3 + n % 3
                    tb = 6 + n % 2
                sq, sqk = sqs[n % 3], f"sq{n % 3}"
                rn, rnk = rns[n % 3], f"rn{n % 3}"
                rn2, rn2k = rn2s[n % 3], f"rn2_{n % 3}"
                n += 1
                p.dma("sync", ui[:], qkvTa[ch * 128:(ch + 1) * 128, s * 512: s * 512 + 516], writes=[uk])
                for j in range(5):
                    p.op("tensor", lambda e: e.matmul(c.ps[cb][:, :], lhsT=Dg[:, ch * 5 + j, :], rhs=ui[:, j:j + 512], start=(j == 0), stop=(j == 4)),
                         reads=[uk, "Dg"], writes=[f"ps{cb}"])
                p.op("scalar", lambda e: e.activation(out=sv[:], in_=c.ps[cb][:, :], func=AF.Silu), reads=[f"ps{cb}"], writes=[sk])
                if ch < 8:
                    p.op("gpsimd", lambda e: e.tensor_tensor(out=sq[:], in0=sv[:], in1=sv[:], op=ALU.mult), reads=[sk], writes=[sqk])
                    ps = c.ps[pi]
                    p.op("tensor", lambda e: e.matmul(ps[:, :], lhsT=onesb[:], rhs=sq[:], start=True, stop=True),
                         reads=[sqk, "onesb"], writes=[f"ps{pi}"])
                    if ch < 4:
                        p.op("scalar", lambda e: e.activation(out=rn[:], in_=ps[:, :], func=AF.Sqrt, scale=128.0, bias=c.bias_q[:, 0:1]),
                             reads=[f"ps{pi}", "bias_q"], writes=[rnk])
                    else:
                        p.op("scalar", lambda e: e.activation(out=rn[:], in_=ps[:, :], func=AF.Sqrt, scale=1.0, bias=c.bias_k[:, 0:1]),
                             reads=[f"ps{pi}", "bias_k"], writes=[rnk])
                    p.op("vector", lambda e: e.reciprocal(out=rn2[:], in_=rn[:]), reads=[rnk], writes=[rn2k])
                    p.op("gpsimd", lambda e: e.tensor_tensor(out=u[:], in0=sv[:], in1=rn2[:], op=ALU.mult), reads=[sk, rn2k], writes=[unk])
                    p.dma("sync", o["uT"][ch * 128:(ch + 1) * 128, s * 512:(s + 1) * 512], u[:], reads=[unk])
                    src, srck = u, unk
                else:
                    src, srck = sv, sk
                if ch >= 4:
                    pj = tb
                    sb_, sbk = svb[n % 2], f"svb{n % 2}"
                    p.op("gpsimd", lambda e: e.tensor_copy(out=sb_[:], in_=src[:]), reads=[srck], writes=[sbk])
                    pjb = c.ps[pj][:, :].bitcast(BF16)
                    for ti in range(4):
                        p.op("tensor", lambda e: e.transpose(out=pjb[:, ti * 128:(ti + 1) * 128], in_=sb_[:, ti * 128:(ti + 1) * 128],
                                                             identity=c.identb[:]), reads=[sbk, "identb"], writes=[f"ps{pj}"])
                    t_, tkk = tk[n % 2], f"tk{n % 2}"
                    p.op("scalar", lambda e: e.activation(out=t_[:, :, :], in_=pjb[:, 0:512].rearrange("p (t d) -> p t d", d=128), func=AF.Copy),
                         reads=[f"ps{pj}"], writes=[tkk])
                    dst = o["ktok"] if ch < 8 else o["vtok"]
                    hh = ch % 4
                    p.dma("sync", dst[s * 512:(s + 1) * 512, hh * 128:(hh + 1) * 128].rearrange("(t p) d -> p t d", p=128), t_[:, :, :],
                          reads=[tkk])
                yield
        if not shared:
            p.barrier()


def alloc_gdn_consts(p, c, es):
    nc = p.nc
    def mk(name, fillval_in, pattern, cm, cmp, fill):
        t = es.enter_context(nc.sbuf_tensor(name, [128, 128], F32))
        p.op("gpsimd", lambda e: e.memset(t[:], fillval_in), writes=[name])
        p.op("gpsimd", lambda e: e.affine_select(out=t[:], in_=t[:], pattern=pattern, compare_op=cmp, fill=fill,
                                                  base=0, channel_multiplier=cm), reads=[name], writes=[name])
        if GDN_F32R and name.startswith("tri"):
            t2 = es.enter_context(nc.sbuf_tensor(name + "r", [128, 128], F32))
            p.op("vector", lambda e: e.tensor_copy(out=t2[:].bitcast(mybir.dt.float32r), in_=t[:]), reads=[name], writes=[name])
            return t2
        return t
    c.triU = mk("triU", 1.0, [[1, 128]], -1, ALU.is_ge, 0.0)
    c.triL = mk("triL", 1.0, [[-1, 128]], 1, ALU.is_ge, 0.0)
    c.nm1k = ["nm1f", "nm1b"]
    c.nm2k = ["nm2f", "nm2b"]
    c.nm1 = [mk("nm1f", 0.0, [[-1, 128]], 1, ALU.is_gt, 1.0e4),
             mk("nm1b", 0.0, [[1, 128]], -1, ALU.is_gt, 1.0e4)]
    c.nm2 = [mk("nm2f", 0.0, [[1, 128]], -1, ALU.is_ge, -1.0e4),
             mk("nm2b", 0.0, [[-1, 128]], 1, ALU.is_ge, -1.0e4)]


def phase_b2(p, c, T, uT, ktok, vtok, bdtok, a_log, dt_bias, o_f, o_b):
    nc = p.nc
    NT = T // 128
    F32R = mybir.dt.float32r
    USE_R = GDN_F32R

    def R(ap):
        return ap.bitcast(F32R) if USE_R else ap
    with ExitStack() as es:
        def sb(name, shape, dt=F32):
            return es.enter_context(nc.sbuf_tensor(f"B2_{_uid()}_" + name, shape, dt))
        bd = sb("bd", [128, NT, 16])
        beta = sb("beta", [128, NT, 8])
        nbeta = sb("nbeta", [128, NT, 8])
        g = sb("g", [128, NT, 8])
        G = sb("G", [128, NT, 8])
        tmp = sb("tmp", [128, NT, 8])
        eGr = sb("eGr", [128, NT, 8])
        ge = sb("ge", [128, NT, 8])
        bG = sb("bG", [128, NT, 8])
        dtb = sb("dtb", [128, 8])
        nea = sb("nea", [128, 8])
        for t0 in range(0, NT, 8):
            t1 = min(NT, t0 + 8)
            p.dma("sync", bd[:, t0:t1, :], bdtok[t0 * 128:t1 * 128, :].rearrange("(t p) c -> p t c", p=128), writes=["bd"])
        p.dma("sync", dtb[:], dt_bias.to_broadcast([128, 8]), writes=["dtb"])
        p.dma("sync", nea[:], a_log.to_broadcast([128, 8]), writes=["nea"])
        p.op("scalar", lambda e: e.activation(out=nea[:], in_=nea[:], func=AF.Exp), reads=["nea"], writes=["nea"])
        p.op("vector", lambda e: e.tensor_scalar(out=nea[:], in0=nea[:], scalar1=-1.0, scalar2=None, op0=ALU.mult), reads=["nea"], writes=["nea"])
        p.op("scalar", lambda e: e.activation(out=beta[:, :, :], in_=bd[:, :, 0:8], func=AF.Sigmoid), reads=["bd"], writes=["beta"])
        p.op("vector", lambda e: e.tensor_scalar(out=nbeta[:, :, :], in0=beta[:, :, :], scalar1=-1.0, scalar2=None, op0=ALU.mult),
             reads=["beta"], writes=["nbeta"])
        p.op("vector", lambda e: e.tensor_tensor(out=tmp[:, :, :], in0=bd[:, :, 8:16], in1=dtb[:, :].unsqueeze(1).to_broadcast([128, NT, 8]), op=ALU.add),
             reads=["bd", "dtb"], writes=["tmp"])
        p.op("scalar", lambda e: e.activation(out=tmp[:, :, :], in_=tmp[:, :, :], func=AF.Exp), reads=["tmp"], writes=["tmp"])
        p.op("scalar", lambda e: e.activation(out=tmp[:, :, :], in_=tmp[:, :, :], func=AF.Ln, bias=c.bias_one[:, 0:1], scale=1.0), reads=["tmp", "bias_one"], writes=["tmp"])
        p.op("vector", lambda e: e.tensor_tensor(out=R(g[:, :, :]), in0=tmp[:, :, :], in1=nea[:, :].unsqueeze(1).to_broadcast([128, NT, 8]), op=ALU.mult),
             reads=["tmp", "nea"], writes=["g"])
        for d, tri, tk in ((0, c.triU, "triU"), (1, c.triL, "triL")):
            for t0 in range(0, NT, 64):
                t1 = min(NT, t0 + 64)
                p.op("tensor", lambda e: e.matmul(c.ps[d][:, 0:(t1 - t0) * 4], lhsT=tri[:], rhs=g[:, t0:t1, d * 4:(d + 1) * 4], start=True, stop=True),
                     reads=["g", tk], writes=[f"ps{d}"])
                p.op("vector", lambda e: e.tensor_copy(out=G[:, t0:t1, d * 4:(d + 1) * 4],
                                                       in_=c.ps[d][:, 0:(t1 - t0) * 4].rearrange("p (t c) -> p t c", c=4)),
                     reads=[f"ps{d}"], writes=["G"])
        for t0 in range(0, NT, 64):
            t1 = min(NT, t0 + 64)
            p.op("tensor", lambda e: e.matmul(c.ps[2][:, 0:(t1 - t0) * 8], lhsT=c.onesf[:], rhs=g[:, t0:t1, :], start=True, stop=True),
                 reads=["g", "onesf"], writes=["ps2"])
            p.op("scalar", lambda e: e.activation(out=ge[:, t0:t1, :], in_=c.ps[2][:, 0:(t1 - t0) * 8].rearrange("p (t c) -> p t c", c=8), func=AF.Exp),
                 writes=["ps2", "ge"])
            p.op("vector", lambda e: e.tensor_tensor(out=eGr[:, t0:t1, :], in0=c.ps[2][:, 0:(t1 - t0) * 8].rearrange("p (t c) -> p t c", c=8),
                                                     in1=G[:, t0:t1, :], op=ALU.subtract), reads=["G"], writes=["ps2", "eGr"])
        p.op("scalar", lambda e: e.activation(out=eGr[:, :, :], in_=eGr[:, :, :], func=AF.Exp), reads=["eGr"], writes=["eGr"])
        p.op("scalar", lambda e: e.activation(out=bG[:, :, :], in_=G[:, :, :], func=AF.Exp), reads=["G"], writes=["bG"])
        p.op("vector", lambda e: e.tensor_tensor(out=bG[:, :, :], in0=bG[:, :, :], in1=beta[:, :, :], op=ALU.mult), reads=["bG", "beta"], writes=["bG"])
        p.barrier()

        U = []
        for hd in range(8):
            S_ = sb(f"S_{hd}", [128, 128])
            Sb_ = sb(f"Sb_{hd}", [128, 128], BF16)
            two = []
            for pp in range(2):
                u = {"S": S_, "Sb": Sb_}
                for nm in ["D1", "D2T", "eB", "X0", "X1"]:
                    u[nm] = sb(f"{nm}_{hd}_{pp}", [128, 128])
                for nm in ["XP0", "XP1"]:
                    u[nm] = sb(f"{nm}_{hd}_{pp}", [128, 256])
                for nm in ["aaT", "qdT", "TTb", "TTbg", "nwT", "vnew", "vs"]:
                    u[nm] = sb(f"{nm}_{hd}_{pp}", [128, 128], BF16)
                two.append(u)
            U.append(two)
            p.op("gpsimd", lambda e: e.memset(S_[:], 0.0), writes=[f"S_{hd}"])
            p.op("gpsimd", lambda e: e.memset(Sb_[:], 0.0), writes=[f"Sb_{hd}"])
        inb = []
        for b_ in range(2):
            d_ = {}
            for dr in range(2):
                d_[dr] = dict(kq=sb(f"kq{b_}{dr}", [128, 4, 2, 128]),
                              ktb=sb(f"ktb{b_}{dr}", [128, 512], BF16), vtb=sb(f"vtb{b_}{dr}", [128, 512], BF16),
                              o=sb(f"o{b_}{dr}", [128, 512]))
            inb.append(d_)

        bn = [0]

        def bank():
            i = bn[0] % 8
            bn[0] += 1
            return c.ps[i], f"ps{i}"

        def Q(bk, q):
            return bk[:, q * 128:(q + 1) * 128]

        def ev(eng, out_ap, in_ap, rk, wk, scale=None):
            if eng == "scalar":
                if scale is None:
                    p.op("scalar", lambda e: e.activation(out=out_ap, in_=in_ap, func=AF.Copy), reads=rk, writes=wk)
                else:
                    p.op("scalar", lambda e: e.activation(out=out_ap, in_=in_ap, func=AF.Copy, scale=scale), reads=rk, writes=wk)
            else:
                if scale is None:
                    p.op("vector", lambda e: e.tensor_copy(out=out_ap, in_=in_ap), reads=rk, writes=wk)
                else:
                    p.op("vector", lambda e: e.tensor_scalar(out=out_ap, in0=in_ap, scalar1=scale, scalar2=None, op0=ALU.mult), reads=rk, writes=wk)

        def XT(u, par):
            return u[f"XP{par}"][:, 0:128]

        def PT(u, j):
            return u[f"XP{1 - j % 2}"][:, 128:256]

        for n in range(NT):
            bs = n % 2
            pp = n % 2
            tiles = (n, NT - 1 - n)
            for dr in range(2):
                i = tiles[dr]
                ib = inb[bs][dr]
                for h_ in range(4):
                    p.dma("sync", ib["kq"][:, h_, 0, :], uT[512 + h_ * 128:512 + (h_ + 1) * 128, i * 128:(i + 1) * 128], writes=[f"kq{bs}{dr}"])
                    p.dma("sync", ib["kq"][:, h_, 1, :], uT[h_ * 128:(h_ + 1) * 128, i * 128:(i + 1) * 128], writes=[f"kq{bs}{dr}"])
                p.dma("sync", ib["ktb"][:, :], ktok[i * 128:(i + 1) * 128, :], writes=[f"ktb{bs}{dr}"])
                p.dma("sync", ib["vtb"][:, :], vtok[i * 128:(i + 1) * 128, :], writes=[f"vtb{bs}{dr}"])
            st = {}
            for dr in range(2):
                i = tiles[dr]
                ib = inb[bs][dr]
                tri, trik = (c.triU, "triU") if dr == 0 else (c.triL, "triL")
                bB, bBk = bank()
                bK = [bank(), bank()]
                st[dr] = (bB, bBk, bK)
                for h in range(4):
                    hd = dr * 4 + h
                    p.op("tensor", lambda e: e.matmul(Q(bB, h), lhsT=g[:, i, hd:hd + 1].to_broadcast([128, 128]), rhs=tri[:], start=True, stop=True),
                         reads=["g", trik], writes=[bBk])
                for h in range(4):
                    bk_, bkk = bK[h // 2]
                    p.op("tensor", lambda e: e.matmul(bk_[:, (h % 2) * 256:(h % 2) * 256 + 256], lhsT=ib["kq"][:, h, 0, :],
                                                      rhs=ib["kq"][:, h, :, :].rearrange("p a t -> p (a t)"), start=True, stop=True),
                         reads=[f"kq{bs}{dr}"], writes=[bkk])
            for dr in range(2):
                i = tiles[dr]
                ib = inb[bs][dr]
                bB, bBk, bK = st[dr]
                for h in range(4):
                    hd = dr * 4 + h
                    u = U[hd][pp]
                    Gc = G[:, i, hd:hd + 1]
                    p.op("vector", lambda e: e.scalar_tensor_tensor(out=u["D1"][:], in0=Q(bB, h), scalar=Gc, in1=c.nm1[dr][:], op0=ALU.subtract, op1=ALU.max),
                         reads=["G", c.nm1k[dr]], writes=[bBk, f"D1_{hd}p{pp}"])
                    p.op("scalar", lambda e: e.activation(out=u["D1"][:], in_=u["D1"][:], func=AF.Exp, scale=-1.0), reads=[f"D1_{hd}p{pp}"], writes=[f"D1_{hd}p{pp}"])
                    p.op("vector", lambda e: e.scalar_tensor_tensor(out=u["D2T"][:], in0=Q(bB, h), scalar=Gc, in1=c.nm2[dr][:], op0=ALU.subtract, op1=ALU.min),
                         reads=["G", c.nm2k[dr]], writes=[bBk, f"D2T_{hd}p{pp}"])
                    p.op("scalar", lambda e: e.activation(out=u["D2T"][:], in_=u["D2T"][:], func=AF.Exp), reads=[f"D2T_{hd}p{pp}"], writes=[f"D2T_{hd}p{pp}"])
                for h in range(4):
                    hd = dr * 4 + h
                    u = U[hd][pp]
                    p.op("scalar", lambda e: e.activation(out=u["eB"][:], in_=Q(bB, h), func=AF.Exp), writes=[bBk, f"eB_{hd}p{pp}"])
                for h in range(4):
                    hd = dr * 4 + h
                    u = U[hd][pp]
                    bk_, bkk = bK[h // 2]
                    o0 = (h % 2) * 256
                    p.op("vector", lambda e: e.scalar_tensor_tensor(out=u["X0"][:], in0=bk_[:, o0:o0 + 128], scalar=nbeta[:, i, hd:hd + 1], in1=u["D1"][:],
                                                                    op0=ALU.mult, op1=ALU.mult), reads=["nbeta", f"D1_{hd}p{pp}"], writes=[bkk, f"X0_{hd}p{pp}"])
                    p.op("vector", lambda e: e.tensor_tensor(out=u["aaT"][:], in0=bk_[:, o0 + 128:o0 + 256], in1=u["D2T"][:], op=ALU.mult),
                         reads=[f"D2T_{hd}p{pp}"], writes=[bkk, f"aaT_{hd}p{pp}"])
                    p.op("gpsimd", lambda e: e.tensor_tensor(out=u["qdT"][:], in0=ib["kq"][:, h, 1, :], in1=u["eB"][:], op=ALU.mult),
                         reads=[f"kq{bs}{dr}", f"eB_{hd}p{pp}"], writes=[f"qdT_{hd}p{pp}"])
            for dr in range(2):
                bT, bTk = bank()
                for h in range(4):
                    hd = dr * 4 + h
                    u = U[hd][pp]
                    p.op("tensor", lambda e: e.transpose(out=Q(bT, h), in_=u["X0"][:], identity=c.identf[:]), reads=[f"X0_{hd}p{pp}", "identf"], writes=[bTk])
                for h in range(4):
                    hd = dr * 4 + h
                    u = U[hd][pp]
                    p.op("scalar", lambda e: e.activation(out=XT(u, 0), in_=Q(bT, h), func=AF.Copy), writes=[bTk, f"XT0_{hd}p{pp}"])
                    p.op("gpsimd", lambda e: e.tensor_tensor(out=PT(u, 0), in0=XT(u, 0), in1=c.identf[:], op=ALU.add),
                         reads=[f"XT0_{hd}p{pp}", "identf"], writes=[f"PT0_{hd}p{pp}"])
            for k in range(1, 7):
                a_, b_ = (k - 1) % 2, k % 2
                for dr in range(2):
                    e1, e2 = ("scalar", "vector") if (k + dr) % 3 != 0 else ("scalar", "scalar")
                    b1, b1k = bank()
                    for h in range(4):
                        hd = dr * 4 + h
                        u = U[hd][pp]
                        p.op("tensor", lambda e: e.matmul(Q(b1, h), lhsT=XT(u, a_), rhs=u[f"X{a_}"][:], start=True, stop=True),
                             reads=[f"XT{a_}_{hd}p{pp}", f"X{a_}_{hd}p{pp}"], writes=[b1k])
                    bM = [bank(), bank()]
                    for h in range(4):
                        hd = dr * 4 + h
                        u = U[hd][pp]
                        bm_, bmk = bM[h // 2]
                        o0 = (h % 2) * 256
                        if k == 1:
                            p.op("tensor", lambda e: e.matmul(bm_[:, o0:o0 + 128], lhsT=u[f"X{a_}"][:], rhs=XT(u, a_), start=True, stop=True),
                                 reads=[f"XT{a_}_{hd}p{pp}", f"X{a_}_{hd}p{pp}"], writes=[bmk])
                        else:
                            p.op("tensor", lambda e: e.matmul(bm_[:, o0:o0 + 256], lhsT=u[f"X{a_}"][:], rhs=u[f"XP{a_}"][:, :], start=True, stop=True),
                                 reads=[f"XT{a_}_{hd}p{pp}", f"X{a_}_{hd}p{pp}", f"PT{(k - 2) % 2}_{hd}p{pp}"], writes=[bmk])
                    for h in range(4):
                        hd = dr * 4 + h
                        u = U[hd][pp]
                        ev(e1, u[f"X{b_}"][:], Q(b1, h), [], [b1k, f"X{b_}_{hd}p{pp}"])
                    for h in range(4):
                        hd = dr * 4 + h
                        u = U[hd][pp]
                        bm_, bmk = bM[h // 2]
                        o0 = (h % 2) * 256
                        if k < 6:
                            ev(e2, XT(u, b_), bm_[:, o0:o0 + 128], [], [bmk, f"XT{b_}_{hd}p{pp}"])
                        if k >= 2:
                            p.op("vector", lambda e: e.tensor_tensor(out=PT(u, k - 1), in0=bm_[:, o0 + 128:o0 + 256], in1=PT(u, k - 2), op=ALU.add),
                                 reads=[f"PT{(k - 2) % 2}_{hd}p{pp}"], writes=[bmk, f"PT{(k - 1) % 2}_{hd}p{pp}"])
            for dr in range(2):
                bF, bFk = bank()
                for h in range(4):
                    hd = dr * 4 + h
                    u = U[hd][pp]
                    p.op("tensor", lambda e: e.matmul(Q(bF, h), lhsT=u["X0"][:], rhs=PT(u, 5), start=True, stop=True),
                         reads=[f"X0_{hd}p{pp}", f"PT1_{hd}p{pp}"], writes=[bFk])
                for h in range(4):
                    hd = dr * 4 + h
                    u = U[hd][pp]
                    p.op("vector", lambda e: e.tensor_tensor(out=PT(u, 6), in0=Q(bF, h), in1=PT(u, 5), op=ALU.add),
                         reads=[f"PT1_{hd}p{pp}"], writes=[bFk, f"PT0_{hd}p{pp}"])
            for dr in range(2):
                i = tiles[dr]
                ib = inb[bs][dr]
                for h in range(4):
                    hd = dr * 4 + h
                    u = U[hd][pp]
                    p.op("scalar", lambda e: e.activation(out=u["TTb"][:], in_=PT(u, 6), func=AF.Copy, scale=beta[:, i, hd:hd + 1]),
                         reads=[f"PT0_{hd}p{pp}", "beta"], writes=[f"TTb_{hd}p{pp}"])
                    p.op("vector", lambda e: e.tensor_scalar(out=u["TTbg"][:], in0=PT(u, 6), scalar1=bG[:, i, hd:hd + 1], scalar2=None, op0=ALU.mult),
                         reads=[f"PT0_{hd}p{pp}", "bG"], writes=[f"TTbg_{hd}p{pp}"])
                bw, bwk = bank()
                for h in range(4):
                    hd = dr * 4 + h
                    u = U[hd][pp]
                    p.op("tensor", lambda e: e.matmul(Q(bw, h), lhsT=ib["ktb"][:, h * 128:(h + 1) * 128], rhs=u["TTbg"][:], start=True, stop=True),
                         reads=[f"ktb{bs}{dr}", f"TTbg_{hd}p{pp}"], writes=[bwk])
                for h in range(4):
                    hd = dr * 4 + h
                    u = U[hd][pp]
                    ev("scalar" if dr == 0 else "vector", u["nwT"][:], Q(bw, h), [], [bwk, f"nwT_{hd}p{pp}"], scale=-1.0)
                bv, bvk = bank()
                for h in range(4):
                    hd = dr * 4 + h
                    u = U[hd][pp]
                    p.op("tensor", lambda e: e.matmul(Q(bv, h), lhsT=u["TTb"][:], rhs=ib["vtb"][:, h * 128:(h + 1) * 128], start=True, stop=False),
                         reads=[f"vtb{bs}{dr}", f"TTb_{hd}p{pp}"], writes=[bvk])
                    p.op("tensor", lambda e: e.matmul(Q(bv, h), lhsT=u["nwT"][:], rhs=u["Sb"][:], start=False, stop=True),
                         reads=[f"nwT_{hd}p{pp}", f"Sb_{hd}"], writes=[bvk])
                for h in range(4):
                    hd = dr * 4 + h
                    u = U[hd][pp]
                    p.op("scalar", lambda e: e.activation(out=u["vnew"][:], in_=Q(bv, h), func=AF.Copy), writes=[bvk, f"vnew_{hd}p{pp}"])
                for h in range(4):
                    hd = dr * 4 + h
                    u = U[hd][pp]
                    p.op("vector", lambda e: e.tensor_scalar(out=u["vs"][:], in0=Q(bv, h), scalar1=eGr[:, i, hd:hd + 1], scalar2=None, op0=ALU.mult),
                         reads=["eGr"], writes=[bvk, f"vs_{hd}p{pp}"])
                bo, bok = bank()
                for h in range(4):
                    hd = dr * 4 + h
                    u = U[hd][pp]
                    p.op("tensor", lambda e: e.matmul(Q(bo, h), lhsT=u["qdT"][:], rhs=u["Sb"][:], start=True, stop=False),
                         reads=[f"qdT_{hd}p{pp}", f"Sb_{hd}"], writes=[bok])
                    p.op("tensor", lambda e: e.matmul(Q(bo, h), lhsT=u["aaT"][:], rhs=u["vnew"][:], start=False, stop=True),
                         reads=[f"aaT_{hd}p{pp}", f"vnew_{hd}p{pp}"], writes=[bok])
                p.op("scalar", lambda e: e.activation(out=ib["o"][:, :], in_=bo[:, :], func=AF.Copy), writes=[bok, f"o{bs}{dr}"])
                bS, bSk = bank()
                for h in range(4):
                    hd = dr * 4 + h
                    u = U[hd][pp]
                    p.op("tensor", lambda e: e.matmul(Q(bS, h), lhsT=ib["ktb"][:, h * 128:(h + 1) * 128], rhs=u["vs"][:], start=True, stop=True),
                         reads=[f"ktb{bs}{dr}", f"vs_{hd}p{pp}"], writes=[bSk])
                for h in range(4):
                    hd = dr * 4 + h
                    u = U[hd][pp]
                    p.op("vector", lambda e: e.scalar_tensor_tensor(out=u["S"][:], in0=u["S"][:], scalar=ge[:, i, hd:hd + 1], in1=Q(bS, h),
                                                                    op0=ALU.mult, op1=ALU.add), reads=["ge"], writes=[bSk, f"S_{hd}"])
                    p.op("gpsimd", lambda e: e.tensor_copy(out=u["Sb"][:], in_=u["S"][:]), reads=[f"S_{hd}"], writes=[f"Sb_{hd}"])
                dst = o_f if dr == 0 else o_b
                p.dma("sync", dst[i * 128:(i + 1) * 128, :], ib["o"][:, :], reads=[f"o{bs}{dr}"])
        p.barrier()


def phase_b3(p, c, T, o_f, o_b, ztok, gdn_norm, yaT):
    nc = p.nc
    NT = T // 128
    with ExitStack() as es:
        def sb(name, shape, dt=F32):
            return es.enter_context(nc.sbuf_tensor(f"B3_{_uid()}_" + name, shape, dt))
        gnb = sb("gnb", [128, 128])
        of = [sb(f"of{i}", [128, 512]) for i in range(2)]
        ob = [sb(f"ob{i}", [128, 512]) for i in range(2)]
        zt = [sb(f"zt{i}", [128, 512]) for i in range(2)]
        junk_2 = [sb(f"junk{i}", [128, 128]) for i in range(2)]
        ss_2 = [sb(f"ss{i}", [128, 4]) for i in range(2)]
        rs_2 = [sb(f"rs{i}", [128, 4]) for i in range(2)]
        on_2 = [sb(f"on{i}", [128, 512]) for i in range(2)]
        yb_2 = [sb(f"yb{i}", [128, 512], BF16) for i in range(2)]
        yT = [sb(f"yT{i}", [128, 4, 128], BF16) for i in range(2)]
        p.dma("sync", gnb[:], gdn_norm.to_broadcast([128, 128]), writes=["gnb"])
        for i in range(NT):
            b = i % 2
            junk, ss, rs, on, yb = junk_2[b], ss_2[b], rs_2[b], on_2[b], yb_2[b]
            p.dma("sync", of[b][:], o_f[i * 128:(i + 1) * 128, :], writes=[f"of{b}"])
            p.dma("sync", ob[b][:], o_b[i * 128:(i + 1) * 128, :], writes=[f"ob{b}"])
            p.dma("sync", zt[b][:], ztok[i * 128:(i + 1) * 128, :], writes=[f"zt{b}"])
            p.op("gpsimd", lambda e: e.tensor_tensor(out=of[b][:], in0=of[b][:], in1=ob[b][:], op=ALU.add), reads=[f"ob{b}", f"of{b}"], writes=[f"of{b}"])
            for h in range(4):
                p.op("scalar", lambda e: e.activation(out=junk[:], in_=of[b][:, h * 128:(h + 1) * 128], func=AF.Square, accum_out=ss[:, h:h + 1]),
                     reads=[f"of{b}"], writes=[f"junk_{b}", f"ss_{b}"])
            p.op("vector", lambda e: e.tensor_scalar(out=rs[:], in0=ss[:], scalar1=1.0 / 128, scalar2=RMS_EPS, op0=ALU.mult, op1=ALU.add), reads=[f"ss_{b}"], writes=[f"rs_{b}"])
            p.op("scalar", lambda e: e.activation(out=rs[:], in_=rs[:], func=AF.Sqrt), reads=[f"rs_{b}"], writes=[f"rs_{b}"])
            p.op("vector", lambda e: e.reciprocal(out=rs[:], in_=rs[:]), reads=[f"rs_{b}"], writes=[f"rs_{b}"])
            for h in range(4):
                p.op("vector", lambda e: e.scalar_tensor_tensor(out=on[:, h * 128:(h + 1) * 128], in0=of[b][:, h * 128:(h + 1) * 128], scalar=rs[:, h:h + 1],
                                                                in1=gnb[:], op0=ALU.mult, op1=ALU.mult), reads=[f"of{b}", f"rs_{b}", "gnb"], writes=[f"on_{b}"])
            p.op("scalar", lambda e: e.activation(out=zt[b][:], in_=zt[b][:], func=AF.Silu), reads=[f"zt{b}"], writes=[f"zt{b}"])
            p.op("gpsimd", lambda e: e.tensor_tensor(out=yb[:], in0=on[:], in1=zt[b][:], op=ALU.mult), reads=[f"on_{b}", f"zt{b}"], writes=[f"yb_{b}"])
            for h in range(4):
                p.op("tensor", lambda e: e.transpose(out=c.psb[:, h * 128:(h + 1) * 128], in_=yb[:, h * 128:(h + 1) * 128], identity=c.identb[:]),
                     reads=[f"yb_{b}", "identb"], writes=["ps7"])
            p.op("vector", lambda e: e.tensor_copy(out=yT[b][:, :, :], in_=c.psb[:, 0:512].rearrange("p (h t) -> p h t", t=128)), writes=["ps7", f"yT{b}"])
            for h_ in range(4):
                p.dma("sync", yaT[h_ * 128:(h_ + 1) * 128, i * 128:(i + 1) * 128], yT[b][:, h_, :], reads=[f"yT{b}"])
        p.barrier()


ATT_DIL = (1, 4, 16)


def alloc_att_consts(p, c, es):
    nc = p.nc
    c.amask = es.enter_context(nc.sbuf_tensor("amask", [128, 256], F32))
    p.op("gpsimd", lambda e: e.memset(c.amask[:], 0.0), writes=["amask"])
    p.op("gpsimd", lambda e: e.affine_select(out=c.amask[:], in_=c.amask[:], pattern=[[1, 256]], compare_op=ALU.is_ge, fill=-30000.0,
                                              base=0, channel_multiplier=-1), reads=["amask"], writes=["amask"])
    p.op("gpsimd", lambda e: e.affine_select(out=c.amask[:], in_=c.amask[:], pattern=[[-1, 256]], compare_op=ALU.is_ge, fill=-30000.0,
                                              base=128, channel_multiplier=1), reads=["amask"], writes=["amask"])
    c.amask0 = es.enter_context(nc.sbuf_tensor("amask0", [128, 128], F32))
    p.op("gpsimd", lambda e: e.memset(c.amask0[:], 0.0), writes=["amask0"])
    p.op("gpsimd", lambda e: e.affine_select(out=c.amask0[:], in_=c.amask0[:], pattern=[[-1, 128]], compare_op=ALU.is_ge, fill=-30000.0,
                                              base=64, channel_multiplier=1), reads=["amask0"], writes=["amask0"])


def phase_c(p, c, T, qkrope, qkrest, vTb, att):
    for _ in phase_c_gen(p, c, T, qkrope, qkrest, vTb, att):
        pass


def phase_c_gen(p, c, T, qkrope, qkrest, vTb, att, es_ext=None, shared=False):
    nc = p.nc
    NT = T // 128
    with ExitStack() as es_own:
        es = es_ext if es_ext is not None else es_own
        def sb(name, shape, dt=F32):
            return es.enter_context(nc.sbuf_tensor(f"C_{_uid()}_" + name, shape, dt))
        qT = [sb(f"qT{i}", [64, T], BF16) for i in range(2)]
        kT = [sb(f"kT{i}", [64, T], BF16) for i in range(2)]
        vT = [sb(f"vT{i}", [64, T], BF16) for i in range(2)]
        vtk = [sb(f"vtk{i}", [128, NT, 64], BF16) for i in range(2)]
        sm = [sb(f"sm{i}", [128, 256]) for i in range(2)]
        P = [sb(f"P{i}", [128, 256], BF16) for i in range(2)]
        PT = [sb(f"PT{i}", [128, 2, 128], BF16) for i in range(2)]
        stt = [sb(f"stt{i}", [128, 4]) for i in range(2)]
        oo = [sb(f"oo{i}", [128, 65]) for i in range(4)]
        blk = [0]
        for h in range(12):
            hb = h % 2
            r = ATT_DIL[h // 4]
            M = T // r
            NB = M // 128
            p.dma("sync", qT[hb][0:16, :], qkrope[h * 16:(h + 1) * 16, :], writes=[f"qT{hb}"])
            p.dma("sync", qT[hb][16:64, :], qkrest[h * 48:(h + 1) * 48, :], writes=[f"qT{hb}"])
            p.dma("sync", kT[hb][0:16, :], qkrope[(12 + h) * 16:(13 + h) * 16, :], writes=[f"kT{hb}"])
            p.dma("sync", kT[hb][16:64, :], qkrest[(12 + h) * 48:(13 + h) * 48, :], writes=[f"kT{hb}"])
            p.dma("sync", vT[hb][:, :], vTb[h * 64:(h + 1) * 64, :], writes=[f"vT{hb}"])
            for g0 in range(0, NT, 16):
                bi = 4 if shared else 6 + (g0 // 16) % 2
                pv = c.ps[bi][:, :].bitcast(BF16)
                for kb in range(g0, min(NT, g0 + 16)):
                    s, mb = divmod(kb, NB)
                    c0 = s + r * mb * 128
                    src = vT[hb][:, c0: c0 + r * 127 + 1: r]
                    p.op("tensor", lambda e: e.transpose(out=pv[:, (kb - g0) * 64:(kb - g0 + 1) * 64], in_=src, identity=c.identb[0:64, 0:64]),
                         reads=[f"vT{hb}", "identb"], writes=[f"ps{bi}"])
                n_ = min(NT, g0 + 16) - g0
                p.op("scalar", lambda e: e.activation(out=vtk[hb][:, g0:g0 + n_, :], in_=pv[:, 0:n_ * 64].rearrange("p (k d) -> p k d", d=64), func=AF.Copy),
                     writes=[f"ps{bi}", f"vtk{hb}"])
            for s in range(r):
                for jb in range(NB + 1):
                    n = blk[0]
                    blk[0] += 1
                    b2 = n % 2
                    m0 = jb * 128 - 64
                    qlo = max(m0, 0)
                    qhi = min(m0 + 128, M)
                    nq = qhi - qlo
                    kblocks = [x for x in (jb - 1, jb) if 0 <= x < NB]
                    nk = 128 * len(kblocks)
                    klo = kblocks[0] * 128
                    if jb == 0:
                        mask = c.amask0[0:nq, 0:nk]
                        mk = "amask0"
                    else:
                        coff = 0 if kblocks[0] == jb - 1 else 128
                        mask = c.amask[0:nq, coff:coff + nk]
                        mk = "amask"
                    qcols = qT[hb][:, s + r * qlo: s + r * (qhi - 1) + 1: r]
                    kcols = kT[hb][:, s + r * klo: s + r * (klo + nk - 1) + 1: r]
                    if shared:
                        bS, bP, bO = c.ps[b2], c.ps[2], c.ps[3]
                        bSk, bPk, bOk = f"ps{b2}", "ps2", "ps3"
                    else:
                        bS, bP, bO = c.ps[b2], c.ps[2 + b2], c.ps[4 + b2]
                        bSk, bPk, bOk = f"ps{b2}", f"ps{2 + b2}", f"ps{4 + b2}"
                    p.op("tensor", lambda e: e.matmul(bS[0:nq, 0:nk], lhsT=qcols, rhs=kcols, start=True, stop=True),
                         reads=[f"qT{hb}", f"kT{hb}"], writes=[bSk])
                    p.op("vector", lambda e: e.scalar_tensor_tensor(out=sm[b2][0:nq, 0:nk], in0=bS[0:nq, 0:nk], scalar=0.125, in1=mask,
                                                                    op0=ALU.mult, op1=ALU.add), reads=[mk], writes=[bSk, f"sm{b2}"])
                    st_ = stt[b2]
                    p.op("vector", lambda e: e.reduce_max(out=st_[0:nq, 1:2], in_=sm[b2][0:nq, 0:nk], axis=AX.X, negate=True), reads=[f"sm{b2}"], writes=[f"stt{b2}b"])
                    p.op("scalar", lambda e: e.activation(out=P[b2][0:nq, 0:nk], in_=sm[b2][0:nq, 0:nk], func=AF.Exp, bias=st_[0:nq, 1:2], scale=1.0,
                                                          accum_out=st_[0:nq, 2:3]), reads=[f"sm{b2}", f"stt{b2}b"], writes=[f"P{b2}", f"stt{b2}c"])
                    pt = bP[:, :].bitcast(BF16)
                    for ci in range(len(kblocks)):
                        p.op("tensor", lambda e: e.transpose(out=pt[:, ci * 128: ci * 128 + nq], in_=P[b2][0:nq, ci * 128:(ci + 1) * 128], identity=c.identb[0:nq, 0:nq]),
                             reads=[f"P{b2}", "identb"], writes=[bPk])
                    for ci in range(len(kblocks)):
                        if ci == 0:
                            p.op("scalar", lambda e: e.activation(out=PT[b2][:, ci, 0:nq], in_=pt[:, ci * 128: ci * 128 + nq], func=AF.Copy), writes=[bPk, f"PT{b2}_{ci}"])
                        else:
                            p.op("vector", lambda e: e.tensor_copy(out=PT[b2][:, ci, 0:nq], in_=pt[:, ci * 128: ci * 128 + nq]), writes=[bPk, f"PT{b2}_{ci}"])
                    for ci, kbk in enumerate(kblocks):
                        p.op("tensor", lambda e: e.matmul(bO[0:nq, 0:64], lhsT=PT[b2][:, ci, 0:nq], rhs=vtk[hb][:, s * NB + kbk, :],
                                                          start=(ci == 0), stop=(ci == len(kblocks) - 1)),
                             reads=[f"PT{b2}_{ci}", f"vtk{hb}"], writes=[bOk])
                    p.op("vector", lambda e: e.reciprocal(out=st_[0:nq, 3:4], in_=st_[0:nq, 2:3]), reads=[f"stt{b2}c"], writes=[f"stt{b2}d"])
                    o4 = oo[n % 4]
                    ok = f"oo{n % 4}"
                    p.op("scalar", lambda e: e.activation(out=o4[0:nq, 64:65], in_=st_[0:nq, 2:3], func=AF.Ln), reads=[f"stt{b2}c"], writes=[ok])
                    p.op("gpsimd", lambda e: e.tensor_tensor(out=o4[0:nq, 64:65], in0=o4[0:nq, 64:65], in1=st_[0:nq, 1:2], op=ALU.subtract),
                         reads=[ok, f"stt{b2}b"], writes=[ok])
                    p.op("vector", lambda e: e.tensor_scalar(out=o4[0:nq, 0:64], in0=bO[0:nq, 0:64], scalar1=st_[0:nq, 3:4], scalar2=None, op0=ALU.mult),
                         reads=[f"stt{b2}d"], writes=[bOk, ok + "o"])
                    t0 = s + r * qlo
                    dst = att[t0: t0 + r * (nq - 1) + 1: r, h, :]
                    p.dma("sync", dst, o4[0:nq, :], reads=[ok, ok + "o"])
                    yield
        if not shared:
            p.barrier()


def phase_d(p, c, T, yaT, att, gatesT, xres, w_a, w_b, w_out, norm_w2, w_router, h2b, aff, x_in=None):
    if x_in is None:
        x_in = xres
    nc = p.nc
    NS = T // 512
    with ExitStack() as es:
        def sb(name, shape, dt=F32):
            return es.enter_context(nc.sbuf_tensor(f"D_{_uid()}_" + name, shape, dt))
        Wa = sb("Wa", [128, 4, 1024], BF16)
        Wb = sb("Wb", [128, 2, 1024], BF16)
        Wo = sb("Wo", [128, 8, 1024], BF16)
        Wr = sb("Wr", [128, 8, 16])
        stg = [sb(f"stg{i}", [128, 1024]) for i in range(2)]
        nwb = sb("nwb", [128, 1024])
        at = [sb(f"at{i}", [128, 12, 65]) for i in range(2)]
        wl_2 = [sb(f"wl{i}", [128, 12]) for i in range(2)]
        mx_2 = [sb(f"mx{i}", [128, 4]) for i in range(2)]
        sm_2 = [sb(f"sm{i}", [128, 4]) for i in range(2)]
        tmpw_2 = [sb(f"tmpw{i}", [128, 12, 64]) for i in range(2)]
        ybt_2 = [sb(f"ybt{i}", [128, 256]) for i in range(2)]
        ybb_2 = [sb(f"ybb{i}", [128, 256], BF16) for i in range(2)]
        ybT = [sb(f"ybT{i}", [128, 2, 512], BF16) for i in range(2)]
        yaTs = [sb(f"yaTs{i}", [128, 4, 512], BF16) for i in range(2)]
        gts = [sb(f"gts{i}", [128, 16, 512], BF16) for i in range(2)]
        t1_2 = [sb(f"t1{i}", [128, 512]) for i in range(2)]
        t2_2 = [sb(f"t2{i}", [128, 512]) for i in range(2)]
        mixT = [sb(f"mixT{i}", [128, 8, 512], BF16) for i in range(2)]
        xt = [sb(f"xt{i}", [128, 1024]) for i in range(2)]
        junk_2 = [sb(f"junk{i}", [128, 1024], BF16) for i in range(2)]
        h2_2 = [sb(f"h2{i}", [128, 1024]) for i in range(2)]
        h2bf = [sb(f"h2bf{i}", [128, 1024], BF16) for i in range(2)]
        h2T_2 = [sb(f"h2T{i}", [128, 2, 8, 128], BF16) for i in range(2)]
        h2lo_2 = [sb(f"h2lo{i}", [128, 1024], BF16) for i in range(2)]
        Wrb = sb("Wrb", [128, 8, 32], BF16)
        Wrt = sb("Wrt", [128, 8, 16])
        lg48_2 = [sb(f"lg48{i}", [128, 48]) for i in range(2)]
        st_2 = [sb(f"st{i}", [128, 8]) for i in range(2)]
        lg_2 = [sb(f"lg{i}", [128, 16]) for i in range(2)]
        af = [sb(f"af{i}", [128, 16]) for i in range(2)]

        p.dma("sync", nwb[:], norm_w2.to_broadcast([128, 1024]), writes=["nwb"])
        p.dma("sync", Wr[:, :, :], w_router.rearrange("(k p) e -> p k e", p=128), writes=["Wr"])
        p.op("vector", lambda e: e.tensor_copy(out=Wrb[:, :, 0:16], in_=Wr[:, :, :]), reads=["Wr"], writes=["Wrb"])
        p.op("vector", lambda e: e.tensor_tensor(out=Wrt[:, :, :], in0=Wr[:, :, :], in1=Wrb[:, :, 0:16], op=ALU.subtract), reads=["Wr", "Wrb"], writes=["Wrt"])
        p.op("vector", lambda e: e.tensor_copy(out=Wrb[:, :, 16:32], in_=Wrt[:, :, :]), reads=["Wrt"], writes=["Wrb"])
        si = 0
        for (W, src, nk, key) in ((Wa, w_a, 4, "Wa"), (Wb, w_b, 2, "Wb"), (Wo, w_out, 8, "Wo")):
            for k in range(nk):
                s_, sk = stg[si % 2], f"stg{si % 2}"
                si += 1
                p.dma("sync", s_[:], src[k * 128:(k + 1) * 128, :], writes=[sk])
                p.op("gpsimd", lambda e: e.tensor_copy(out=W[:, k, :], in_=s_[:]), reads=[sk], writes=[key])
        bn = [0]

        def bank():
            i = bn[0] % 6
            bn[0] += 1
            return c.ps[i], f"ps{i}"

        for s in range(NS):
            sl = s % 2
            c0 = s * 512
            for k_ in range(4):
                p.dma("sync", yaTs[sl][:, k_, :], yaT[k_ * 128:(k_ + 1) * 128, c0:c0 + 512], writes=[f"yaTs{sl}"])
            for k_ in range(16):
                p.dma("sync", gts[sl][:, k_, :], gatesT[k_ * 128:(k_ + 1) * 128, c0:c0 + 512], writes=[f"gts{sl}"])
            for ti in range(4):
                r0 = c0 + ti * 128
                a_, ak = at[ti % 2], f"at{ti % 2}"
                pq = ti % 2
                wl, mx, sm, tmpw, ybt, ybb = wl_2[pq], mx_2[pq], sm_2[pq], tmpw_2[pq], ybt_2[pq], ybb_2[pq]
                p.dma("sync", a_[:, :, :], att[r0:r0 + 128, :, :], writes=[ak])
                L = a_[:, :, 64]
                p.op("vector", lambda e: e.tensor_tensor(out=mx[:], in0=L[:, 0:4], in1=L[:, 4:8], op=ALU.max), reads=[ak], writes=[f"mx_{pq}"])
                p.op("vector", lambda e: e.tensor_tensor(out=mx[:], in0=mx[:], in1=L[:, 8:12], op=ALU.max), reads=[ak, f"mx_{pq}"], writes=[f"mx_{pq}"])
                p.op("vector", lambda e: e.tensor_tensor(out=wl[:, :].rearrange("p (g h) -> p g h", h=4), in0=L.rearrange("p (g h) -> p g h", h=4),
                                                         in1=mx[:, :].unsqueeze(1).to_broadcast([128, 3, 4]), op=ALU.subtract), reads=[ak, f"mx_{pq}"], writes=[f"wl_{pq}"])
                p.op("scalar", lambda e: e.activation(out=wl[:], in_=wl[:], func=AF.Exp), reads=[f"wl_{pq}"], writes=[f"wl_{pq}"])
                p.op("vector", lambda e: e.tensor_tensor(out=sm[:], in0=wl[:, 0:4], in1=wl[:, 4:8], op=ALU.add), reads=[f"wl_{pq}"], writes=[f"sm_{pq}"])
                p.op("vector", lambda e: e.tensor_tensor(out=sm[:], in0=sm[:], in1=wl[:, 8:12], op=ALU.add), reads=[f"wl_{pq}", f"sm_{pq}"], writes=[f"sm_{pq}"])
                p.op("vector", lambda e: e.reciprocal(out=sm[:], in_=sm[:]), reads=[f"sm_{pq}"], writes=[f"sm_{pq}"])
                p.op("vector", lambda e: e.tensor_tensor(out=wl[:, :].rearrange("p (g h) -> p g h", h=4), in0=wl[:, :].rearrange("p (g h) -> p g h", h=4),
                                                         in1=sm[:, :].unsqueeze(1).to_broadcast([128, 3, 4]), op=ALU.mult), reads=[f"wl_{pq}", f"sm_{pq}"], writes=[f"wl_{pq}"])
                p.op("vector", lambda e: e.tensor_tensor(out=tmpw[:, :, :], in0=a_[:, :, 0:64], in1=wl[:, :].unsqueeze(2).to_broadcast([128, 12, 64]), op=ALU.mult),
                     reads=[ak, f"wl_{pq}"], writes=[f"tmpw_{pq}"])
                tv = tmpw[:, :, :].rearrange("p (g h) d -> p g (h d)", h=4)
                p.op("gpsimd", lambda e: e.tensor_tensor(out=ybt[:], in0=tv[:, 0, :], in1=tv[:, 1, :], op=ALU.add), reads=[f"tmpw_{pq}"], writes=[f"ybt_{pq}"])
                p.op("gpsimd", lambda e: e.tensor_tensor(out=ybb[:], in0=ybt[:], in1=tv[:, 2, :], op=ALU.add), reads=[f"tmpw_{pq}", f"ybt_{pq}"], writes=[f"ybb_{pq}"])
                for k in range(2):
                    p.op("tensor", lambda e: e.transpose(out=c.psb[:, k * 128:(k + 1) * 128], in_=ybb[:, k * 128:(k + 1) * 128], identity=c.identb[:]),
                         reads=[f"ybb_{pq}", "identb"], writes=["ps7"])
                p.op("scalar", lambda e: e.activation(out=ybT[sl][:, :, ti * 128:(ti + 1) * 128], in_=c.psb[:, 0:256].rearrange("p (k t) -> p k t", t=128), func=AF.Copy),
                     writes=["ps7", f"ybT{sl}"])
            for dm in range(8):
                t1, t2 = t1_2[dm % 2], t2_2[dm % 2]
                bA, bAk = bank()
                bB, bBk = bank()
                for k in range(4):
                    p.op("tensor", lambda e: e.matmul(bA[:, :], lhsT=Wa[:, k, dm * 128:(dm + 1) * 128], rhs=yaTs[sl][:, k, :], start=(k == 0), stop=(k == 3)),
                         reads=["Wa", f"yaTs{sl}"], writes=[bAk])
                for k in range(2):
                    p.op("tensor", lambda e: e.matmul(bB[:, :], lhsT=Wb[:, k, dm * 128:(dm + 1) * 128], rhs=ybT[sl][:, k, :], start=(k == 0), stop=(k == 1)),
                         reads=["Wb", f"ybT{sl}"], writes=[bBk])
                p.op("vector", lambda e: e.tensor_tensor(out=t1[:], in0=bA[:, :], in1=gts[sl][:, dm, :], op=ALU.mult), reads=[f"gts{sl}"], writes=[bAk, f"t1_{dm % 2}"])
                p.op("vector", lambda e: e.tensor_tensor(out=t2[:], in0=bB[:, :], in1=gts[sl][:, 8 + dm, :], op=ALU.mult), reads=[f"gts{sl}"], writes=[bBk, f"t2_{dm % 2}"])
                p.op("gpsimd", lambda e: e.tensor_tensor(out=mixT[sl][:, dm, :], in0=t1[:], in1=t2[:], op=ALU.add), reads=[f"t1_{dm % 2}", f"t2_{dm % 2}"], writes=[f"mixT{sl}"])
            for ti in range(4):
                r0 = c0 + ti * 128
                x_, xk = xt[ti % 2], f"xt{ti % 2}"
                pq = ti % 2
                junk, h2, h2T, h2lo, st, lg, lg48 = junk_2[pq], h2_2[pq], h2T_2[pq], h2lo_2[pq], st_2[pq], lg_2[pq], lg48_2[pq]
                p.dma("sync", x_[:], x_in[r0:r0 + 128, :], writes=[xk])
                for hf in range(2):
                    bX, bXk = bank()
                    for k in range(8):
                        p.op("tensor", lambda e: e.matmul(bX[:, :], lhsT=mixT[sl][:, k, ti * 128:(ti + 1) * 128], rhs=Wo[:, k, hf * 512:(hf + 1) * 512],
                                                          start=(k == 0), stop=(k == 7)), reads=["Wo", f"mixT{sl}"], writes=[bXk])
                    p.op("vector", lambda e: e.tensor_tensor(out=x_[:, hf * 512:(hf + 1) * 512], in0=bX[:, :], in1=x_[:, hf * 512:(hf + 1) * 512], op=ALU.add),
                         reads=[xk], writes=[bXk, xk])
                p.dma("sync", xres[r0:r0 + 128, :], x_[:], reads=[xk])
                p.op("scalar", lambda e: e.activation(out=junk[:], in_=x_[:], func=AF.Square, accum_out=st[:, 0:1]), reads=[xk], writes=[f"junk_{pq}", f"st0_{pq}"])
                p.op("vector", lambda e: e.tensor_scalar(out=st[:, 1:2], in0=st[:, 0:1], scalar1=1.0 / 1024, scalar2=RMS_EPS, op0=ALU.mult, op1=ALU.add),
                     reads=[f"st0_{pq}"], writes=[f"st1_{pq}"])
                p.op("scalar", lambda e: e.activation(out=st[:, 2:3], in_=st[:, 1:2], func=AF.Sqrt), reads=[f"st1_{pq}"], writes=[f"st2_{pq}"])
                p.op("vector", lambda e: e.reciprocal(out=st[:, 3:4], in_=st[:, 2:3]), reads=[f"st2_{pq}"], writes=[f"st3_{pq}"])
                p.op("vector", lambda e: e.scalar_tensor_tensor(out=h2[:], in0=x_[:], scalar=st[:, 3:4], in1=nwb[:], op0=ALU.mult, op1=ALU.mult),
                     reads=[xk, f"st3_{pq}", "nwb"], writes=[f"h2_{pq}"])
                hb_, hbk = h2bf[ti % 2], f"h2bf{ti % 2}"
                p.op("gpsimd", lambda e: e.tensor_copy(out=hb_[:], in_=h2[:]), reads=[f"h2_{pq}"], writes=[hbk])
                p.dma("sync", h2b[r0:r0 + 128, :], hb_[:], reads=[hbk])
                p.op("gpsimd", lambda e: e.tensor_tensor(out=h2lo[:], in0=h2[:], in1=hb_[:], op=ALU.subtract), reads=[f"h2_{pq}", hbk], writes=[f"h2lo_{pq}"])
                for part, (src_, srck_) in enumerate(((hb_, hbk), (h2lo, f"h2lo_{pq}"))):
                    bT, bTk = bank()
                    tpv = bT[:, :].bitcast(BF16)
                    for k in range(8):
                        p.op("tensor", lambda e: e.transpose(out=tpv[:, k * 128:(k + 1) * 128], in_=src_[:, k * 128:(k + 1) * 128], identity=c.identb[:]),
                             reads=[srck_, "identb"], writes=[bTk])
                    if part == 0:
                        p.op("scalar", lambda e: e.activation(out=h2T[:, part, :, :], in_=tpv[:, :].rearrange("p (k t) -> p k t", t=128), func=AF.Copy),
                             writes=[bTk, f"h2T{part}_{pq}"])
                    else:
                        p.op("vector", lambda e: e.tensor_copy(out=h2T[:, part, :, :], in_=tpv[:, :].rearrange("p (k t) -> p k t", t=128)),
                             writes=[bTk, f"h2T{part}_{pq}"])
                bL, bLk = bank()
                for k in range(8):
                    p.op("tensor", lambda e: e.matmul(bL[:, 0:32], lhsT=h2T[:, 0, k, :], rhs=Wrb[:, k, :], start=(k == 0), stop=(k == 7)),
                         reads=[f"h2T0_{pq}", "Wrb"], writes=[bLk])
                for k in range(8):
                    p.op("tensor", lambda e: e.matmul(bL[:, 32:48], lhsT=h2T[:, 1, k, :], rhs=Wrb[:, k, 0:16], start=(k == 0), stop=(k == 7)),
                         reads=[f"h2T1_{pq}", "Wrb"], writes=[bLk])
                p.op("vector", lambda e: e.tensor_copy(out=lg48[:], in_=bL[:, 0:48]), writes=[bLk, f"lg48_{pq}"])
                p.op("vector", lambda e: e.tensor_tensor(out=lg[:], in0=lg48[:, 0:16], in1=lg48[:, 16:32], op=ALU.add), reads=[f"lg48_{pq}"], writes=[f"lg_{pq}"])
                p.op("vector", lambda e: e.tensor_tensor(out=lg[:], in0=lg[:], in1=lg48[:, 32:48], op=ALU.add), reads=[f"lg_{pq}", f"lg48_{pq}"], writes=[f"lg_{pq}"])
                p.op("vector", lambda e: e.reduce_max(out=st[:, 4:5], in_=lg[:], axis=AX.X), reads=[f"lg_{pq}"], writes=[f"st4_{pq}"])
                p.op("vector", lambda e: e.tensor_scalar(out=st[:, 5:6], in0=st[:, 4:5], scalar1=-1.0, scalar2=None, op0=ALU.mult), reads=[f"st4_{pq}"], writes=[f"st5_{pq}"])
                p.op("scalar", lambda e: e.activation(out=lg[:], in_=lg[:], func=AF.Exp, bias=st[:, 5:6], scale=1.0, accum_out=st[:, 6:7]),
                     reads=[f"lg_{pq}", f"st5_{pq}"], writes=[f"lg_{pq}", f"st6_{pq}"])
                p.op("vector", lambda e: e.reciprocal(out=st[:, 7:8], in_=st[:, 6:7]), reads=[f"st6_{pq}"], writes=[f"st7_{pq}"])
                a2, a2k = af[ti % 2], f"af{ti % 2}"
                p.op("vector", lambda e: e.tensor_scalar(out=a2[:], in0=lg[:], scalar1=st[:, 7:8], scalar2=None, op0=ALU.mult), reads=[f"lg_{pq}", f"st7_{pq}"], writes=[a2k])
                p.dma("sync", aff[r0:r0 + 128, :], a2[:], reads=[a2k])
        p.barrier()


def phase_e(p, c, T, aff, h2b, xres, w_gate, w_up, w_down, n_iter=34):
    nc = p.nc
    NT = T // 128
    C = T // 8
    NG = C // 128
    NE = 16
    with ExitStack() as es0:
        def sb0(name, shape, dt=F32):
            return es0.enter_context(nc.sbuf_tensor(f"E_{_uid()}_" + name, shape, dt))
        idx_all = sb0("idx_all", [128, NE, NG], I32)
        gate_all = sb0("gate_all", [128, NE, NG])
        with ExitStack() as es:
            def sb(name, shape, dt=F32):
                return es.enter_context(nc.sbuf_tensor(f"E1_{_uid()}_" + name, shape, dt))
            A = sb("A", [128, NT, NE])
            cmp = sb("cmp", [128, NT, NE])
            lo = sb("lo", [128, NE])
            hi = sb("hi", [128, NE])
            mid = sb("mid", [128, NE])
            cnt = sb("cnt", [128, NE])
            sel = sb("sel", [128, NE])
            d1 = sb("d1", [128, NE])
            triS = sb("triS", [128, 128])
            onesT = sb("onesT", [128, NT])
            rk = sb("rk", [128, NT, NE])
            nn = sb("nn", [128, NT, NE])
            base = sb("base", [128, NT, NE])
            pos = sb("pos", [128, NT, NE])
            posi = sb("posi", [128, NT, NE], I32)
            ai = sb("ai", [128, NT, NE], I32)
            bi_ = sb("bi", [128, NT, NE], I32)
            af_ = sb("af", [128, NT, NE])
            bf_ = sb("bf", [128, NT, NE])
            h1 = sb("h1", [128, NT, NE], BF16)
            h2 = sb("h2", [128, NT, NE], BF16)
            h3 = sb("h3", [128, NT, NE], BF16)
            r1 = sb("r1", [128, NT, NE])
            r2 = sb("r2", [128, NT, NE])
            iotaA = sb("iotaA", [128, 128])
            iotaB = sb("iotaB", [128, NG])
            tval = sb("tval", [128, NT], BF16)
            pval = sb("pval", [128, 1])
            pvalb = sb("pvalb", [128, NT], BF16)
            Aoh = [sb(f"Aoh{i}", [128, NT, 128], BF16) for i in range(2)]
            Boh = [sb(f"Boh{i}", [128, NT, NG]) for i in range(2)]
            rhs = [sb(f"rhs{i}", [128, NT, 5, NG], BF16) for i in range(2)]
            res = sb("res", [128, 5, NG])

            for t0 in range(0, NT, 8):
                t1 = min(NT, t0 + 8)
                p.dma("sync", A[:, t0:t1, :], aff[t0 * 128:t1 * 128, :].rearrange("(t p) e -> p t e", p=128), writes=["A"])
            p.op("gpsimd", lambda e: e.memset(lo[:], 0.0), writes=["lo"])
            p.op("gpsimd", lambda e: e.memset(hi[:], 1.0001), writes=["hi"])
            p.op("gpsimd", lambda e: e.memset(onesT[:], 1.0), writes=["onesT"])
            p.op("gpsimd", lambda e: e.memset(triS[:], 1.0), writes=["triS"])
            p.op("gpsimd", lambda e: e.affine_select(out=triS[:], in_=triS[:], pattern=[[1, 128]], compare_op=ALU.is_gt, fill=0.0, base=0, channel_multiplier=-1),
                 reads=["triS"], writes=["triS"])
            p.op("gpsimd", lambda e: e.iota(iotaA[:], pattern=[[1, 128]], base=0, channel_multiplier=0, allow_small_or_imprecise_dtypes=True), writes=["iotaA"])
            p.op("gpsimd", lambda e: e.iota(iotaB[:], pattern=[[1, NG]], base=0, channel_multiplier=0, allow_small_or_imprecise_dtypes=True), writes=["iotaB"])
            p.op("gpsimd", lambda e: e.iota(tval[:], pattern=[[1, NT]], base=0, channel_multiplier=0, allow_small_or_imprecise_dtypes=True), writes=["tval"])
            p.op("gpsimd", lambda e: e.iota(pval[:], pattern=[[0, 1]], base=0, channel_multiplier=1, allow_small_or_imprecise_dtypes=True), writes=["pval"])
            p.op("vector", lambda e: e.tensor_copy(out=pvalb[:], in_=pval[:, 0:1].to_broadcast([128, NT])), reads=["pval"], writes=["pvalb"])
            for it in range(n_iter):
                p.op("vector", lambda e: e.tensor_tensor(out=mid[:], in0=lo[:], in1=hi[:], op=ALU.add), reads=["lo", "hi"], writes=["mid"])
                p.op("vector", lambda e: e.tensor_scalar(out=mid[:], in0=mid[:], scalar1=0.5, scalar2=None, op0=ALU.mult), reads=["mid"], writes=["mid"])
                p.op("vector", lambda e: e.tensor_tensor(out=cmp[:, :, :], in0=A[:, :, :], in1=mid[:, :].unsqueeze(1).to_broadcast([128, NT, NE]), op=ALU.is_ge),
                     reads=["A", "mid"], writes=["cmp"])
                p.op("vector", lambda e: e.tensor_reduce(out=cnt[:], in_=cmp[:, :, :].rearrange("p t e -> p e t"), axis=AX.X, op=ALU.add), reads=["cmp"], writes=["cnt"])
                p.op("tensor", lambda e: e.matmul(c.ps[0][:, 0:NE], lhsT=c.onesf[:], rhs=cnt[:], start=True, stop=True), reads=["cnt", "onesf"], writes=["ps0"])
                p.op("vector", lambda e: e.tensor_scalar(out=sel[:], in0=c.ps[0][:, 0:NE], scalar1=float(C) - 0.5, scalar2=None, op0=ALU.is_ge), writes=["ps0", "sel"])
                p.op("vector", lambda e: e.tensor_tensor(out=d1[:], in0=mid[:], in1=lo[:], op=ALU.subtract), reads=["mid", "lo"], writes=["d1"])
                p.op("vector", lambda e: e.tensor_tensor(out=d1[:], in0=d1[:], in1=sel[:], op=ALU.mult), reads=["d1", "sel"], writes=["d1"])
                p.op("vector", lambda e: e.tensor_tensor(out=lo[:], in0=lo[:], in1=d1[:], op=ALU.add), reads=["d1", "lo"], writes=["lo"])
                p.op("vector", lambda e: e.tensor_tensor(out=d1[:], in0=hi[:], in1=mid[:], op=ALU.subtract), reads=["mid", "hi"], writes=["d1"])
                p.op("vector", lambda e: e.tensor_tensor(out=d1[:], in0=d1[:], in1=sel[:], op=ALU.mult), reads=["d1", "sel"], writes=["d1"])
                p.op("vector", lambda e: e.tensor_tensor(out=hi[:], in0=mid[:], in1=d1[:], op=ALU.add), reads=["d1", "mid"], writes=["hi"])
            p.op("vector", lambda e: e.tensor_tensor(out=cmp[:, :, :], in0=A[:, :, :], in1=lo[:, :].unsqueeze(1).to_broadcast([128, NT, NE]), op=ALU.is_ge),
                 reads=["A", "lo"], writes=["cmp"])
            cf = cmp[:, :, :].rearrange("p t e -> p (t e)")
            NC_ = NT * NE
            for c0 in range(0, NC_, 512):
                c1 = min(NC_, c0 + 512)
                p.op("tensor", lambda e: e.matmul(c.ps[1][:, 0:c1 - c0], lhsT=triS[:], rhs=cf[:, c0:c1], start=True, stop=True), reads=["cmp", "triS"], writes=["ps1"])
                p.op("vector", lambda e: e.tensor_copy(out=rk[:, :, :].rearrange("p t e -> p (t e)")[:, c0:c1], in_=c.ps[1][:, 0:c1 - c0]), writes=["ps1", "rk"])
                p.op("tensor", lambda e: e.matmul(c.ps[2][:, 0:c1 - c0], lhsT=c.onesf[:], rhs=cf[:, c0:c1], start=True, stop=True), reads=["cmp", "onesf"], writes=["ps2"])
                p.op("vector", lambda e: e.tensor_copy(out=nn[:, :, :].rearrange("p t e -> p (t e)")[:, c0:c1], in_=c.ps[2][:, 0:c1 - c0]), writes=["ps2", "nn"])
            for ex in range(NE):
                p.op("vector", lambda e: e.tensor_tensor_scan(out=base[:, :, ex], data0=onesT[:, :], data1=nn[:, :, ex], initial=0.0, op0=ALU.mult, op1=ALU.add),
                     reads=["nn", "onesT"], writes=["base"])
            p.op("vector", lambda e: e.tensor_tensor(out=pos[:, :, :], in0=base[:, :, :], in1=nn[:, :, :], op=ALU.subtract), reads=["base", "nn"], writes=["pos"])
            p.op("vector", lambda e: e.tensor_tensor(out=pos[:, :, :], in0=pos[:, :, :], in1=rk[:, :, :], op=ALU.add), reads=["pos", "rk"], writes=["pos"])
            p.op("vector", lambda e: e.tensor_copy(out=posi[:, :, :], in_=pos[:, :, :]), reads=["pos"], writes=["posi"])
            p.op("vector", lambda e: e.tensor_single_scalar(out=ai[:, :, :], in_=posi[:, :, :], scalar=(NG.bit_length() - 1), op=ALU.arith_shift_right), reads=["posi"], writes=["ai"])
            p.op("vector", lambda e: e.tensor_single_scalar(out=bi_[:, :, :], in_=posi[:, :, :], scalar=NG - 1, op=ALU.bitwise_and), reads=["posi"], writes=["bi"])
            p.op("vector", lambda e: e.tensor_copy(out=af_[:, :, :], in_=ai[:, :, :]), reads=["ai"], writes=["af"])
            p.op("vector", lambda e: e.tensor_copy(out=bf_[:, :, :], in_=bi_[:, :, :]), reads=["bi"], writes=["bf"])
            p.op("vector", lambda e: e.scalar_tensor_tensor(out=af_[:, :, :], in0=af_[:, :, :], scalar=1.0, in1=cmp[:, :, :], op0=ALU.add, op1=ALU.mult), reads=["af", "cmp"], writes=["af"])
            p.op("vector", lambda e: e.tensor_scalar(out=af_[:, :, :], in0=af_[:, :, :], scalar1=-1.0, scalar2=None, op0=ALU.add), reads=["af"], writes=["af"])
            p.op("vector", lambda e: e.tensor_copy(out=h1[:, :, :], in_=A[:, :, :]), reads=["A"], writes=["h1"])
            p.op("vector", lambda e: e.tensor_tensor(out=r1[:, :, :], in0=A[:, :, :], in1=h1[:, :, :], op=ALU.subtract), reads=["A", "h1"], writes=["r1"])
            p.op("vector", lambda e: e.tensor_copy(out=h2[:, :, :], in_=r1[:, :, :]), reads=["r1"], writes=["h2"])
            p.op("vector", lambda e: e.tensor_tensor(out=r2[:, :, :], in0=r1[:, :, :], in1=h2[:, :, :], op=ALU.subtract), reads=["r1", "h2"], writes=["r2"])
            p.op("vector", lambda e: e.tensor_copy(out=h3[:, :, :], in_=r2[:, :, :]), reads=["r2"], writes=["h3"])
            for ex in range(NE):
                b = ex % 2
                p.op("vector", lambda e: e.tensor_tensor(out=Aoh[b][:, :, :], in0=iotaA[:, :].unsqueeze(1).to_broadcast([128, NT, 128]),
                                                         in1=af_[:, :, ex:ex + 1].to_broadcast([128, NT, 128]), op=ALU.is_equal),
                     reads=["iotaA", "af"], writes=[f"Aoh{b}"])
                p.op("vector", lambda e: e.tensor_tensor(out=Boh[b][:, :, :], in0=iotaB[:, :].unsqueeze(1).to_broadcast([128, NT, NG]),
                                                         in1=bf_[:, :, ex:ex + 1].to_broadcast([128, NT, NG]), op=ALU.is_equal),
                     reads=["iotaB", "bf"], writes=[f"Boh{b}"])
                vals = [tval[:, :].unsqueeze(2).to_broadcast([128, NT, NG]), pvalb[:, :].unsqueeze(2).to_broadcast([128, NT, NG]),
                        h1[:, :, ex:ex + 1].to_broadcast([128, NT, NG]), h2[:, :, ex:ex + 1].to_broadcast([128, NT, NG]),
                        h3[:, :, ex:ex + 1].to_broadcast([128, NT, NG])]
                vkeys = ["tval", "pvalb", "h1", "h2", "h3"]
                for vi in range(5):
                    p.op("gpsimd", lambda e: e.tensor_tensor(out=rhs[b][:, :, vi, :], in0=Boh[b][:, :, :], in1=vals[vi], op=ALU.mult),
                         reads=[f"Boh{b}", vkeys[vi]], writes=[f"rhs{b}"])
                pb = 3 + b
                for t in range(NT):
                    p.op("tensor", lambda e: e.matmul(c.ps[pb][:, 0:5 * NG], lhsT=Aoh[b][:, t, :], rhs=rhs[b][:, t, :, :].rearrange("p v g -> p (v g)"),
                                                      start=(t == 0), stop=(t == NT - 1)), reads=[f"Aoh{b}", f"rhs{b}"], writes=[f"ps{pb}"])
                p.op("vector", lambda e: e.tensor_copy(out=res[:, :, :], in_=c.ps[pb][:, 0:5 * NG].rearrange("p (v g) -> p v g", g=NG)), writes=[f"ps{pb}", "res"])
                p.op("vector", lambda e: e.scalar_tensor_tensor(out=res[:, 0, :], in0=res[:, 0, :], scalar=128.0, in1=res[:, 1, :], op0=ALU.mult, op1=ALU.add),
                     reads=["res"], writes=["res"])
                p.op("vector", lambda e: e.tensor_copy(out=idx_all[:, ex, :], in_=res[:, 0, :]), reads=["res"], writes=["idx_all"])
                p.op("vector", lambda e: e.tensor_tensor(out=res[:, 2, :], in0=res[:, 2, :], in1=res[:, 3, :], op=ALU.add), reads=["res"], writes=["res"])
                p.op("vector", lambda e: e.tensor_tensor(out=gate_all[:, ex, :], in0=res[:, 2, :], in1=res[:, 4, :], op=ALU.add), reads=["res"], writes=["gate_all"])
            p.barrier()
        with ExitStack() as es:
            def sb(name, shape, dt=F32):
                return es.enter_context(nc.sbuf_tensor(f"E2_{_uid()}_" + name, shape, dt))
            NWB = 5
            wbuf = [sb(f"w{i}", [128, 8, 1024], BF16) for i in range(NWB)]
            stg = [sb(f"stg{i}", [128, 2, 1024]) for i in range(2)]
            xes = [sb(f"xe{i}", [128, NG, 1024], BF16) for i in range(2)]
            xeT = sb("xeT", [128, 8, C], BF16)
            hmT = sb("hmT", [128, 8, C], BF16)
            sg = [sb(f"sg{i}", [128, 512]) for i in range(2)]
            yo = [sb(f"yo{i}", [128, 1024]) for i in range(2)]
            wn = [0]
            sn = [0]
            cn = [0]

            def load_w(src):
                i = wn[0] % NWB
                wn[0] += 1
                for k2 in range(4):
                    s_ = stg[sn[0] % 2]
                    sk = f"stg{sn[0] % 2}"
                    sn[0] += 1
                    p.dma("sync", s_[:, :, :], src[k2 * 256:(k2 + 1) * 256, :].rearrange("(k p) f -> p k f", p=128), writes=[sk])
                    eng = ("gpsimd", "gpsimd", "vector", "gpsimd", "scalar", "gpsimd")[cn[0] % 6]
                    cn[0] += 1
                    if eng == "scalar":
                        p.op("scalar", lambda e: e.activation(out=wbuf[i][:, 2 * k2:2 * k2 + 2, :], in_=s_[:, :, :], func=AF.Copy), reads=[sk], writes=[f"w{i}"])
                    else:
                        p.op(eng, lambda e: e.tensor_copy(out=wbuf[i][:, 2 * k2:2 * k2 + 2, :], in_=s_[:, :, :]), reads=[sk], writes=[f"w{i}"])
                return wbuf[i], f"w{i}"

            bn = [0]

            def bank():
                i = bn[0] % 6
                bn[0] += 1
                return c.ps[i], f"ps{i}"

            CH = min(512, C)
            NCH = C // CH
            yn = [0]
            pending = [load_w(w_gate[0]), load_w(w_up[0]), load_w(w_down[0])]
            for ex in range(NE):
                (Wg, Wgk), (Wu, Wuk), (Wd, Wdk) = pending
                xe = xes[ex % 2]
                xek = f"xe{ex % 2}"
                for gi in range(NG):
                    p.dma("gpsimd", None, None, reads=["idx_all"], writes=[xek],
                          fn=lambda e: e.indirect_dma_start(out=xe[:, gi, :], out_offset=None, in_=h2b[:, :],
                                                            in_offset=bass.IndirectOffsetOnAxis(ap=idx_all[:, ex, gi:gi + 1], axis=0)))
                for gi in range(NG):
                    tbk = 6 + gi % 2
                    tps = c.ps[tbk][:, :].bitcast(BF16)
                    for k in range(8):
                        p.op("tensor", lambda e: e.transpose(out=tps[:, k * 128:(k + 1) * 128], in_=xe[:, gi, k * 128:(k + 1) * 128], identity=c.identb[:]),
                             reads=[xek, "identb"], writes=[f"ps{tbk}"])
                    if gi % 2 == 0:
                        p.op("vector", lambda e: e.tensor_copy(out=xeT[:, :, gi * 128:(gi + 1) * 128], in_=tps[:, :].rearrange("p (k t) -> p k t", t=128)),
                             writes=[f"ps{tbk}", "xeT0"])
                    else:
                        p.op("scalar", lambda e: e.activation(out=xeT[:, :, gi * 128:(gi + 1) * 128], in_=tps[:, :].rearrange("p (k t) -> p k t", t=128), func=AF.Copy),
                             writes=[f"ps{tbk}", "xeT1"])
                for fc in range(8):
                    for ch in range(NCH):
                        bG, bGk = bank()
                        bU, bUk = bank()
                        for k in range(8):
                            p.op("tensor", lambda e: e.matmul(bG[:, 0:CH], lhsT=Wg[:, k, fc * 128:(fc + 1) * 128], rhs=xeT[:, k, ch * CH:(ch + 1) * CH],
                                                              start=(k == 0), stop=(k == 7)), reads=[Wgk, "xeT0", "xeT1"], writes=[bGk])
                        for k in range(8):
                            p.op("tensor", lambda e: e.matmul(bU[:, 0:CH], lhsT=Wu[:, k, fc * 128:(fc + 1) * 128], rhs=xeT[:, k, ch * CH:(ch + 1) * CH],
                                                              start=(k == 0), stop=(k == 7)), reads=[Wuk, "xeT0", "xeT1"], writes=[bUk])
                        s_ = sg[(fc * NCH + ch) % 2]
                        sk = f"sg{(fc * NCH + ch) % 2}"
                        p.op("scalar", lambda e: e.activation(out=s_[:, 0:CH], in_=bG[:, 0:CH], func=AF.Silu), writes=[bGk, sk])
                        p.op("vector", lambda e: e.tensor_tensor(out=hmT[:, fc, ch * CH:(ch + 1) * CH], in0=bU[:, 0:CH], in1=s_[:, 0:CH], op=ALU.mult),
                             reads=[sk], writes=[bUk, "hmT"])
                if ex + 1 < NE:
                    nxt = [load_w(w_gate[ex + 1]), load_w(w_up[ex + 1])]
                for cb in range(NG):
                    y_ = yo[yn[0] % 2]
                    yk = f"yo{yn[0] % 2}"
                    yn[0] += 1
                    for hf in range(2):
                        bY, bYk = bank()
                        for f in range(8):
                            p.op("tensor", lambda e: e.matmul(bY[:, :], lhsT=hmT[:, f, cb * 128:(cb + 1) * 128], rhs=Wd[:, f, hf * 512:(hf + 1) * 512],
                                                              start=(f == 0), stop=(f == 7)), reads=[Wdk, "hmT"], writes=[bYk])
                        if hf == 0:
                            p.op("scalar", lambda e: e.activation(out=y_[:, 0:512], in_=bY[:, :], func=AF.Copy, scale=gate_all[:, ex, cb:cb + 1]),
                                 reads=["gate_all"], writes=[bYk, yk])
                        else:
                            p.op("vector", lambda e: e.tensor_scalar(out=y_[:, 512:1024], in0=bY[:, :], scalar1=gate_all[:, ex, cb:cb + 1], scalar2=None, op0=ALU.mult),
                                 reads=["gate_all"], writes=[bYk, yk])
                    p.dma("gpsimd", None, None, reads=[yk, "idx_all"], writes=["xres_scatter"],
                          fn=lambda e: e.indirect_dma_start(out=xres[:, :], out_offset=bass.IndirectOffsetOnAxis(ap=idx_all[:, ex, cb:cb + 1], axis=0),
                                                            in_=y_[:, :], in_offset=None, compute_op=ALU.add))
                if ex + 1 < NE:
                    nxt.append(load_w(w_down[ex + 1]))
                    pending = nxt
            p.barrier()


def phase_f(p, c, T, xres, norm_w, out):
    nc = p.nc
    NT = T // 128
    with ExitStack() as es:
        def sb(name, shape, dt=F32):
            return es.enter_context(nc.sbuf_tensor(f"F_{_uid()}_" + name, shape, dt))
        nwb = sb("nwb", [128, 1024])
        xt = [sb(f"xt{i}", [128, 1024]) for i in range(3)]
        junk = sb("junk", [128, 1024], BF16)
        st = [sb(f"st{i}", [128, 4]) for i in range(2)]
        p.dma("sync", nwb[:], norm_w.to_broadcast([128, 1024]), writes=["nwb"])
        for i in range(NT):
            x_, xk = xt[i % 3], f"xt{i % 3}"
            s_, sk = st[i % 2], f"st{i % 2}"
            p.dma("sync", x_[:], xres[i * 128:(i + 1) * 128, :], writes=[xk])
            p.op("scalar", lambda e: e.activation(out=junk[:], in_=x_[:], func=AF.Square, accum_out=s_[:, 0:1]), reads=[xk], writes=["junk", sk + "a"])
            p.op("vector", lambda e: e.tensor_scalar(out=s_[:, 1:2], in0=s_[:, 0:1], scalar1=1.0 / 1024, scalar2=RMS_EPS, op0=ALU.mult, op1=ALU.add),
                 reads=[sk + "a"], writes=[sk + "b"])
            p.op("scalar", lambda e: e.activation(out=s_[:, 2:3], in_=s_[:, 1:2], func=AF.Sqrt), reads=[sk + "b"], writes=[sk + "c"])
            p.op("vector", lambda e: e.reciprocal(out=s_[:, 3:4], in_=s_[:, 2:3]), reads=[sk + "c"], writes=[sk + "d"])
            p.op("vector", lambda e: e.scalar_tensor_tensor(out=x_[:], in0=x_[:], scalar=s_[:, 3:4], in1=nwb[:], op0=ALU.mult, op1=ALU.mult),
                 reads=[xk, sk + "d", "nwb"], writes=[xk])
            p.dma("sync", out[i * 128:(i + 1) * 128, :], x_[:], reads=[xk])
        p.barrier()


DEPTH = 2


def rope_tables(T):
    half = 8
    inv = np.power(500000.0, -np.arange(half, dtype=np.float64) * 2.0 / 16)
    ang = np.arange(T, dtype=np.float32).astype(np.float64)[None, :] * inv.astype(np.float32).astype(np.float64)[:, None]
    cos = np.cos(ang)
    sin = np.sin(ang)
    c16 = np.concatenate([cos, cos], 0)
    s16 = np.concatenate([sin, sin], 0)
    return np.tile(c16, (8, 1)).astype(np.float32), np.tile(s16, (8, 1)).astype(np.float32)


def build_program(T, depth=DEPTH, phases=None):
    nc = bass.Bass("TRN2", target_bir_lowering=False)
    def din(name, shape, dt=F32):
        return nc.dram_tensor(name, shape, dt, kind="ExternalInput").ap()
    def dint(name, shape, dt=F32):
        return nc.dram_tensor(name, shape, dt, kind="Internal").ap()
    x = din("x", [T, 1024])
    norm_mix = din("norm_mix", [depth, 1024])
    w_in = din("w_in", [depth, 1024, P_IN])
    conv_w = din("conv_w", [depth, 5, 1536])
    a_log = din("a_log", [depth, 8])
    dt_bias = din("dt_bias", [depth, 8])
    gdn_norm = din("gdn_norm", [depth, 128])
    w_a = din("w_branch_a", [depth, 512, 1024])
    w_b = din("w_branch_b", [depth, 256, 1024])
    w_out = din("w_out", [depth, 1024, 1024])
    norm_ffn = din("norm_ffn", [depth, 1024])
    w_router = din("w_router", [depth, 1024, 16])
    w_eg = din("w_expert_gate", [depth, 16, 1024, 1024])
    w_eu = din("w_expert_up", [depth, 16, 1024, 1024])
    w_ed = din("w_expert_down", [depth, 16, 1024, 1024])
    norm_final = din("norm_final", [1, 1024])
    cos_t = din("cos_t", [128, T])
    sin_t = din("sin_t", [128, T])
    out = nc.dram_tensor("out", [T, 1024], F32, kind="ExternalOutput").ap()

    xres = dint("xres", [T, 1024])
    oA = dict(qkvTa=dint("qkvTa", [1536, T + 4], BF16), ztok=dint("ztok", [T, 512]), bdtok=dint("bdtok", [T, 16]), bdT=dint("bdT", [16, T]),
              qkrope=dint("qkrope", [384, T], BF16), qkrest=dint("qkrest", [1152, T], BF16), vTb=dint("vTb", [768, T], BF16),
              gatesT=dint("gatesT", [2048, T], BF16))
    oB1 = dict(uT=dint("uT", [1024, T]), ktok=dint("ktok", [T, 512], BF16), vtok=dint("vtok", [T, 512], BF16))
    o_f = dint("o_f", [T, 512])
    o_b = dint("o_b", [T, 512])
    yaT = dint("yaT", [512, T], BF16)
    att = dint("att", [T, 12, 65])
    h2b = dint("h2b", [T, 1024], BF16)
    aff = dint("aff", [T, 16])

    with ExitStack() as es:
        p = Prog(nc, es)
        c = alloc_common(p, es)
        alloc_gdn_consts(p, c, es)
        alloc_att_consts(p, c, es)
        zt = es.enter_context(nc.sbuf_tensor("zpad", [128, 2], BF16))
        p.op("gpsimd", lambda e: e.memset(zt[:], 0.0), writes=["zpad"])
        for ch in range(12):
            p.dma("sync", oA["qkvTa"][ch * 128:(ch + 1) * 128, 0:2], zt[:], reads=["zpad"], allow_slow_non_contiguous=True)
            p.dma("sync", oA["qkvTa"][ch * 128:(ch + 1) * 128, T + 2:T + 4], zt[:], reads=["zpad"], allow_slow_non_contiguous=True)
        p.barrier()
        ph = phases or "A,B1,B2,B3,C,D,E,F"
        ph = ph.split(",")
        for l in range(depth):
            xin = x if l == 0 else xres
            if "A" in ph:
                phase_a(p, c, T, xin, w_in[l], norm_mix[l:l + 1, :], cos_t, sin_t, oA)
            if "B1" in ph and "C" in ph and INTERLEAVE_B1_C:
                with ExitStack() as es_bc:
                    g1 = phase_b1_gen(p, c, T, oA["qkvTa"], conv_w[l], oB1, es_ext=es_bc, shared=True)
                    g2 = phase_c_gen(p, c, T, oA["qkrope"], oA["qkrest"], oA["vTb"], att, es_ext=es_bc, shared=True)
                    n1 = (T // 512) * 12
                    n2 = sum(4 * r_ * (T // r_ // 128 + 1) for r_ in ATT_DIL)
                    d1 = d2 = 0
                    a1 = a2 = True
                    while a1 or a2:
                        if a1 and (not a2 or d1 * n2 <= d2 * n1):
                            try:
                                next(g1)
                                d1 += 1
                            except StopIteration:
                                a1 = False
                        else:
                            try:
                                next(g2)
                                d2 += 1
                            except StopIteration:
                                a2 = False
                    p.barrier()
            elif "B1" in ph:
                phase_b1(p, c, T, oA["qkvTa"], conv_w[l], oB1)
            if "B2" in ph:
                phase_b2(p, c, T, oB1["uT"], oB1["ktok"], oB1["vtok"], oA["bdtok"], a_log[l:l + 1, :], dt_bias[l:l + 1, :], o_f, o_b)
            if "B3" in ph:
                phase_b3(p, c, T, o_f, o_b, oA["ztok"], gdn_norm[l:l + 1, :], yaT)
            if "C" in ph and not ("B1" in ph and INTERLEAVE_B1_C):
                phase_c(p, c, T, oA["qkrope"], oA["qkrest"], oA["vTb"], att)
            if "D" in ph:
                phase_d(p, c, T, yaT, att, oA["gatesT"], xres, w_a[l], w_b[l], w_out[l], norm_ffn[l:l + 1, :], w_router[l], h2b, aff, x_in=xin)
            if "E" in ph:
                phase_e(p, c, T, aff, h2b, xres, w_eg[l], w_eu[l], w_ed[l])
        if "F" in ph:
            phase_f(p, c, T, xres, norm_final, out)
        p.finish()
        nc._stats = (p.nops, p.nwaits)
    return nc


def make_in_maps(inputs, T, nb):
    cos_t, sin_t = rope_tables(T)
    f32 = lambda a: np.ascontiguousarray(np.asarray(a), dtype=np.float32)
    shared = {
        "norm_mix": f32(inputs["norm_mix"]), "w_in": f32(inputs["w_in"]), "conv_w": f32(inputs["conv_w"]),
        "a_log": f32(inputs["a_log"]).reshape(-1, 8), "dt_bias": f32(inputs["dt_bias"]).reshape(-1, 8),
        "gdn_norm": f32(inputs["gdn_norm"]), "w_branch_a": f32(inputs["w_branch_a"]), "w_branch_b": f32(inputs["w_branch_b"]),
        "w_out": f32(inputs["w_out"]), "norm_ffn": f32(inputs["norm_ffn"]), "w_router": f32(inputs["w_router"]),
        "w_expert_gate": f32(inputs["w_expert_gate"]), "w_expert_up": f32(inputs["w_expert_up"]),
        "w_expert_down": f32(inputs["w_expert_down"]), "norm_final": f32(inputs["norm_final"]).reshape(1, 1024),
        "cos_t": cos_t, "sin_t": sin_t,
    }
    x = f32(inputs["x"])
    return [dict(shared, x=x[b]) for b in range(nb)]


_NC_CACHE = {}


def kernel(**inputs):
    x = np.asarray(inputs["x"])
    B, T, _ = x.shape
    if T not in _NC_CACHE:
        _NC_CACHE[T] = build_program(T)
    nc = _NC_CACHE[T]
    in_maps = make_in_maps(inputs, T, B)
    res = run_bass_kernel_spmd(nc, in_maps, core_ids=list(range(B)))
    return np.stack([np.asarray(r["out"]) for r in res.results], axis=0).astype(np.float32)
```

```python
import numpy as np
import concourse.bass as bass
import concourse.mybir as mybir
from concourse.bass_utils import run_bass_kernel_spmd
from contextlib import ExitStack

F32 = mybir.dt.float32
BF16 = mybir.dt.bfloat16
I32 = mybir.dt.int32
U32 = mybir.dt.uint32
AF = mybir.ActivationFunctionType
ALU = mybir.AluOpType
AX = mybir.AxisListType

import os as _os0
SAME_ENGINE_SYNC = not _os0.environ.get("NO_SES")
INTERLEAVE_B1_C = bool(_os0.environ.get("ILV"))
B1_RSQRT_ACT = bool(int(_os0.environ.get("B1_RSQRT_ACT", "1")))
ROUTER_F32 = bool(int(_os0.environ.get("ROUTER_F32", "1")))
GDN_F32R = False

D_MODEL = 1024
P_IN = 6416
OFF_Z = 1536
OFF_BETA = 2048
OFF_ATT = 2064
OFF_V = OFF_ATT + 1536
OFF_GATE = 4368
RMS_EPS = 1e-6


class _Rec:
    def __init__(self):
        self.call = None

    def __getattr__(self, name):
        def f(*a, **k):
            self.call = (name, a, k)
            return self
        return f


def _ap_free(ap):
    try:
        sh = tuple(ap.shape)
        n = 1
        for v in sh[1:]:
            n *= int(v)
        return int(sh[0]), n
    except Exception:
        return 128, 128


_DT_SIZE = {}


def _dtsize(dt):
    if dt not in _DT_SIZE:
        _DT_SIZE[dt] = 2 if dt == BF16 else 4
    return _DT_SIZE[dt]


class Prog:
    def __init__(self, nc, es, defer=True):
        self.nc = nc
        self.es = es
        self.engs = {}
        for name in ["tensor", "vector", "scalar", "gpsimd", "sync"]:
            sem = es.enter_context(nc.semaphore(f"sem_{name}"))
            self.engs[name] = dict(h=getattr(nc, name), sem=sem, cnt=0, waited={}, name=name)
        self.rings = {}
        for q in ["sync", "gpsimd", "scalar"]:
            K = 8
            sems = [es.enter_context(nc.semaphore(f"dq_{q}_{i}")) for i in range(K)]
            self.rings[q] = dict(sems=sems, n=0, K=K, vals=[0] * K)
        self.lastw = {}
        self.readers = {}
        self.nwaits = 0
        self.nops = 0
        self.defer = defer
        self.pending = []

    def _wait(self, E, tok):
        sid, sem, val = tok
        if E["waited"].get(sid, 0) >= val:
            return
        if sid == id(E["sem"]):
            if E["name"] == "tensor" or (not SAME_ENGINE_SYNC and E["name"] != "gpsimd"):
                return
        E["h"].wait_ge(sem, val)
        E["waited"][sid] = val
        self.nwaits += 1

    def _deps(self, E, reads, writes):
        for k in reads:
            t = self.lastw.get(k)
            if t is not None:
                self._wait(E, t)
        for k in writes:
            t = self.lastw.get(k)
            if t is not None:
                self._wait(E, t)
            for t in self.readers.get(k, {}).values():
                self._wait(E, t)

    def _record(self, tok, reads, writes):
        for k in reads:
            d = self.readers.setdefault(k, {})
            d[tok[0]] = tok
        for k in writes:
            self.lastw[k] = tok
            self.readers[k] = {}

    def _emit_op(self, eng, call, reads, writes):
        E = self.engs[eng]
        self._deps(E, reads, writes)
        name, a, k = call
        inst = getattr(E["h"], name)(*a, **k)
        E["cnt"] += 1
        inst.then_inc(E["sem"], 1)
        tok = (id(E["sem"]), E["sem"], E["cnt"])
        self._record(tok, reads, writes)
        self.nops += 1

    def _emit_dma(self, q, call, reads, writes):
        E = self.engs[q]
        R = self.rings[q]
        i = R["n"] % R["K"]
        R["n"] += 1
        sem = R["sems"][i]
        if R["vals"][i] > 0:
            self._wait(E, (id(sem), sem, R["vals"][i]))
        self._deps(E, reads, writes)
        name, a, k = call
        inst = getattr(E["h"], name)(*a, **k)
        R["vals"][i] += 16
        inst.then_inc(sem, 16)
        tok = (id(sem), sem, R["vals"][i])
        self._record(tok, reads, writes)
        self.nops += 1

    @staticmethod
    def _cost(eng, call):
        name, a, k = call
        try:
            if eng == "tensor":
                if name == "transpose":
                    ap = k.get("in_", a[1] if len(a) > 1 else None)
                    p_, f_ = _ap_free(ap)
                    c = 0.03 + max(p_, 64) / 2400.0 * (2 if ap.dtype == F32 else 1)
                    return c, c + 0.15
                rhs = k.get("rhs", a[2] if len(a) > 2 else None)
                lhs = k.get("lhsT", a[1] if len(a) > 1 else None)
                _, n = _ap_free(rhs)
                passes = 4 if rhs.dtype == F32 else 1
                c = 0.02 + (max(n, 64) + 0) * passes / 2400.0 + (0.05 if passes == 4 else 0.0)
                return c, c + 0.15
            out = k.get("out", a[0] if a else None)
            _, f_ = _ap_free(out)
            if eng == "vector":
                c = 0.07 + f_ / 960.0
                if name in ("tensor_tensor", "scalar_tensor_tensor", "tensor_tensor_scan"):
                    c = 0.07 + f_ / 700.0
            elif eng == "scalar":
                c = 0.12 + f_ / 1100.0
            else:
                c = 0.16 + f_ / 900.0
            return c, c
        except Exception:
            return 0.3, 0.3

    def op(self, eng, fn, reads=(), writes=()):
        psr = [k for k in reads if isinstance(k, str) and k.startswith("ps")]
        if psr:
            reads = [k for k in reads if k not in psr]
            writes = list(writes) + psr
        rec = _Rec()
        fn(rec)
        occ, lat = self._cost(eng, rec.call)
        self.pending.append(("op", eng, rec.call, list(reads), list(writes), occ, lat))
        if not self.defer:
            self.flush()

    def dma(self, q, out, in_, reads=(), writes=(), fn=None, **kw):
        if fn is None:
            call = ("dma_start", (), dict(out=out, in_=in_, **kw))
            p_, f_ = _ap_free(out)
            nbytes = p_ * f_ * _dtsize(out.dtype)
        else:
            rec = _Rec()
            fn(rec)
            call = rec.call
            nbytes = 128 * 2048
        lat = 2.0 + nbytes / 120000.0
        if q == "gpsimd":
            lat += 3.0
        occ = 0.06 if q != "gpsimd" else 1.0
        self.pending.append(("dma", q, call, list(reads), list(writes), occ, lat))
        if not self.defer:
            self.flush()

    def flush(self, window=20):
        ops = self.pending
        self.pending = []
        n = len(ops)
        if n == 0:
            return
        if n < 4:
            order = range(n)
        else:
            order = self._schedule(ops, window)
        for j in order:
            kind, eng, call, reads, writes, occ, lat = ops[j]
            if kind == "op":
                self._emit_op(eng, call, reads, writes)
            else:
                self._emit_dma(eng, call, reads, writes)

    @staticmethod
    def _schedule(ops, W):
        n = len(ops)
        engof = [o[1] if o[0] == "op" else "q_" + o[1] for o in ops]
        lastw = {}
        readers = {}
        preds = [None] * n
        succ_eng = [None] * n
        for j, o in enumerate(ops):
            ps = set()
            for k in o[3]:
                t = lastw.get(k)
                if t is not None:
                    ps.add(t)
            for k in o[4]:
                t = lastw.get(k)
                if t is not None:
                    ps.add(t)
                for t in readers.get(k, ()):
                    ps.add(t)
            ps.discard(j)
            preds[j] = tuple(ps)
            for k in o[3]:
                readers.setdefault(k, []).append(j)
            for k in o[4]:
                lastw[k] = j
                readers[k] = []
        qhist = {}
        for j, o in enumerate(ops):
            if o[0] == "dma":
                h = qhist.setdefault(o[1], [])
                if len(h) >= 8:
                    preds[j] = preds[j] + (h[-8],)
                h.append(j)
        succs = [[] for _ in range(n)]
        for j in range(n):
            for pj in preds[j]:
                succs[pj].append(j)
        lists = {}
        for j in range(n):
            lists.setdefault(engof[j], []).append(j)
        head = {e: 0 for e in lists}
        done = [False] * n
        fin = [0.0] * n
        start = [0.0] * n
        tE = {e: 0.0 for e in lists}
        LATX, LATS = 0.35, 0.12
        cache = {}

        def candidate(e):
            lst = lists[e]
            h = head[e]
            L = len(lst)
            while h < L and done[lst[h]]:
                h += 1
            head[e] = h
            best = None
            cnt = 0
            i = h
            te = tE[e]
            while i < L and cnt < W:
                idx = lst[i]
                i += 1
                if done[idx]:
                    continue
                cnt += 1
                r = 0.0
                ok = True
                for pj in preds[idx]:
                    if not done[pj]:
                        ok = False
                        break
                    v = fin[pj] + (LATS if engof[pj] == e else LATX)
                    if v > r:
                        r = v
                if not ok:
                    continue
                s_ = r if r > te else te
                if best is None or s_ < best[0] - 1e-9:
                    best = (s_, idx)
                if r <= te:
                    break
            return best

        for e in lists:
            cache[e] = candidate(e)
        remaining = n
        out = []
        while remaining:
            bs = None
            be = None
            for e, cnd in cache.items():
                if cnd is not None and (bs is None or cnd[0] < bs[0] - 1e-9 or (abs(cnd[0] - bs[0]) <= 1e-9 and cnd[1] < bs[1])):
                    bs = cnd
                    be = e
            s_, idx = bs
            o = ops[idx]
            done[idx] = True
            start[idx] = s_
            fin[idx] = s_ + o[6]
            tE[be] = s_ + o[5]
            out.append(idx)
            remaining -= 1
            dirty = {be}
            for sj in succs[idx]:
                dirty.add(engof[sj])
            for e in dirty:
                cache[e] = candidate(e)
        out.sort(key=lambda j: (start[j], j))
        return out

    def barrier(self):
        self.flush()
        toks = []
        for E in self.engs.values():
            if E["cnt"] > 0:
                toks.append((id(E["sem"]), E["sem"], E["cnt"]))
        for R in self.rings.values():
            for i, sem in enumerate(R["sems"]):
                if R["vals"][i] > 0:
                    toks.append((id(sem), sem, R["vals"][i]))
        for E in self.engs.values():
            for t in toks:
                if t[0] != id(E["sem"]):
                    self._wait(E, t)
        self.lastw = {}
        self.readers = {}

    def finish(self):
        self.flush()
        E = self.engs["sync"]
        for X in self.engs.values():
            if X["cnt"] > 0 and X is not E:
                self._wait(E, (id(X["sem"]), X["sem"], X["cnt"]))
        for q, R in self.rings.items():
            for i, sem in enumerate(R["sems"]):
                if R["vals"][i] > 0:
                    self._wait(E, (id(sem), sem, R["vals"][i]))


class Ctx:
    pass


_UIDC = [0]


def _uid():
    _UIDC[0] += 1
    return _UIDC[0]


def alloc_common(p, es):
    nc = p.nc
    c = Ctx()
    c.ps = [es.enter_context(nc.psum_tensor(f"ps{i}", [128, 512], F32)) for i in range(8)]
    c.psb = c.ps[7][:, :].bitcast(BF16)
    c.identf = es.enter_context(nc.sbuf_tensor("identf", [128, 128], F32))
    c.identb = es.enter_context(nc.sbuf_tensor("identb", [128, 128], BF16))
    c.onesf = es.enter_context(nc.sbuf_tensor("onesf", [128, 128], F32))
    p.op("gpsimd", lambda e: e.memset(c.onesf[:], 1.0), writes=["onesf"])
    p.op("gpsimd", lambda e: e.memset(c.identf[:], 1.0), writes=["identf"])
    p.op("gpsimd", lambda e: e.affine_select(out=c.identf[:], in_=c.identf[:], pattern=[[-1, 128]],
                                              compare_op=ALU.is_equal, fill=0.0, base=0, channel_multiplier=1),
         reads=["identf"], writes=["identf"])
    p.op("vector", lambda e: e.tensor_copy(out=c.identb[:], in_=c.identf[:]), reads=["identf"], writes=["identb"])
    c.bias_q = es.enter_context(nc.sbuf_tensor("bias_q", [128, 1], F32))
    c.bias_k = es.enter_context(nc.sbuf_tensor("bias_k", [128, 1], F32))
    p.op("gpsimd", lambda e: e.memset(c.bias_q[:], 128.0e-6), writes=["bias_q"])
    p.op("gpsimd", lambda e: e.memset(c.bias_k[:], 1.0e-6), writes=["bias_k"])
    c.bias_one = es.enter_context(nc.sbuf_tensor("bias_one", [128, 1], F32))
    p.op("gpsimd", lambda e: e.memset(c.bias_one[:], 1.0), writes=["bias_one"])
    return c


def phase_a(p, c, T, x_src, w_in, norm_w, cos_t, sin_t, o):
    nc = p.nc
    NS = T // 512
    with ExitStack() as es:
        def sb(name, shape, dt):
            return es.enter_context(nc.sbuf_tensor(f"A_{_uid()}_" + name, shape, dt))
        NMAIN = 1536 + 528 + 768 + 2048
        Wm = sb("Wm", [128, 8, NMAIN], BF16)
        M_QKV, M_Z, M_BD, M_V, M_G = 0, 1536, 2048, 2064, 2832
        Wrope = sb("Wrope", [128, 8, 384], BF16)
        Wrest = sb("Wrest", [128, 8, 1152], BF16)
        Wp = sb("Wp", [128, 8, 384], BF16)
        stg = [sb(f"stg{i}", [128, 2048], F32) for i in range(2)]
        nwb = sb("nwb", [128, 1024], F32)
        xt = [sb(f"xt{i}", [128, 1024], F32) for i in range(2)]
        junk = sb("junk", [128, 1024], BF16)
        hb = sb("hb", [128, 1024], BF16)
        st = sb("st", [128, 8], F32)
        hT = [sb(f"hT{i}", [128, 8, 512], BF16) for i in range(2)]
        cs = [sb(f"cs{i}", [128, 2, 512], F32) for i in range(2)]
        of32 = [sb(f"of32_{i}", [128, 512], F32) for i in range(3)]
        ob16 = [sb(f"ob16_{i}", [128, 512], BF16) for i in range(3)]
        t1 = sb("t1", [128, 512], F32)
        t2 = sb("t2", [128, 512], F32)
        obd = sb("obd", [128, 4, 16], F32)

        p.dma("sync", nwb[:], norm_w.to_broadcast([128, 1024]), writes=["nwb"])
        pieces = [(0, 1536), (1536, 2064), (2064, 3600), (3600, 4368), (4368, 6416)]
        si = 0
        for k in range(8):
            for pi, (a, b) in enumerate(pieces):
                s = stg[si % 2]
                sk = f"stg{si % 2}"
                si += 1
                p.dma("sync", s[:, 0:b - a], w_in[k * 128:(k + 1) * 128, a:b], writes=[sk])
                if pi == 0:
                    p.op("gpsimd", lambda e: e.tensor_copy(out=Wm[:, k, M_QKV:M_QKV + 1536], in_=s[:, 0:1536]),
                         reads=[sk], writes=["Wm"])
                elif pi == 1:
                    p.op("gpsimd", lambda e: e.tensor_copy(out=Wm[:, k, M_Z:M_Z + 528], in_=s[:, 0:528]),
                         reads=[sk], writes=["Wm"])
                elif pi == 3:
                    p.op("gpsimd", lambda e: e.tensor_copy(out=Wm[:, k, M_V:M_V + 768], in_=s[:, 0:768]),
                         reads=[sk], writes=["Wm"])
                elif pi == 4:
                    p.op("gpsimd", lambda e: e.tensor_copy(out=Wm[:, k, M_G:M_G + 2048], in_=s[:, 0:2048]),
                         reads=[sk], writes=["Wm"])
                else:
                    sv = s[:, 0:1536].rearrange("p (h d) -> p h d", d=64)
                    p.op("gpsimd", lambda e: e.tensor_copy(
                        out=Wrope[:, k, :].rearrange("p (h d) -> p h d", d=16), in_=sv[:, :, 0:16]),
                        reads=[sk], writes=["Wrope"])
                    p.op("gpsimd", lambda e: e.tensor_copy(
                        out=Wrest[:, k, :].rearrange("p (h d) -> p h d", d=48), in_=sv[:, :, 16:64]),
                        reads=[sk], writes=["Wrest"])
                    wpv = Wp[:, k, :].rearrange("p (h d) -> p h d", d=16)
                    p.op("gpsimd", lambda e: e.tensor_scalar(out=wpv[:, :, 0:8], in0=sv[:, :, 8:16], scalar1=-1.0,
                                                             scalar2=None, op0=ALU.mult),
                         reads=[sk], writes=["Wp"])
                    p.op("gpsimd", lambda e: e.tensor_copy(out=wpv[:, :, 8:16], in_=sv[:, :, 0:8]),
                         reads=[sk], writes=["Wp"])

        rot = {"f": 0, "b": 0, "ps": 0, "ev": 0}

        def prep_front(s):
            slot = s % 2
            for ti in range(4):
                r0 = s * 512 + ti * 128
                xs = xt[ti % 2]
                xk = f"xt{ti % 2}"
                p.dma("sync", xs[:], x_src[r0:r0 + 128, :], writes=[xk])
                p.op("scalar", lambda e: e.activation(out=junk[:], in_=xs[:], func=AF.Square, accum_out=st[:, 0:1]),
                     reads=[xk], writes=["junk", "st0"])
                p.op("vector", lambda e: e.tensor_scalar(out=st[:, 1:2], in0=st[:, 0:1], scalar1=1.0 / 1024, scalar2=RMS_EPS,
                                                         op0=ALU.mult, op1=ALU.add), reads=["st0"], writes=["st1"])
                p.op("scalar", lambda e: e.activation(out=st[:, 2:3], in_=st[:, 1:2], func=AF.Sqrt), reads=["st1"], writes=["st2"])
                p.op("vector", lambda e: e.reciprocal(out=st[:, 3:4], in_=st[:, 2:3]), reads=["st2"], writes=["st3"])
                p.op("vector", lambda e: e.scalar_tensor_tensor(out=hb[:], in0=xs[:], scalar=st[:, 3:4], in1=nwb[:],
                                                                op0=ALU.mult, op1=ALU.mult),
                     reads=[xk, "st3", "nwb"], writes=["hb"])
                for k in range(8):
                    p.op("tensor", lambda e: e.transpose(out=c.psb[:, k * 128:(k + 1) * 128], in_=hb[:, k * 128:(k + 1) * 128],
                                                         identity=c.identb[:]),
                         reads=["hb", "identb"], writes=["psb"])
                p.op("scalar", lambda e: e.activation(out=hT[slot][:, :, ti * 128:(ti + 1) * 128],
                                                      in_=c.psb[:, :].rearrange("p (k t) -> p k t", t=128), func=AF.Copy),
                     reads=["psb"], writes=[f"hT{slot}"])

        def fm_group(s, Wsrc, wkey, col0, nchunks, dst, dst_row0, mode):
            slot = s % 2
            hk = f"hT{slot}"
            for ci in range(nchunks):
                pi = rot["ps"] % 6
                rot["ps"] += 1
                ps = c.ps[pi]
                pk = f"ps{pi}"
                for k in range(8):
                    p.op("tensor", lambda e: e.matmul(ps[:, :], lhsT=Wsrc[:, k, col0 + ci * 128: col0 + (ci + 1) * 128],
                                                      rhs=hT[slot][:, k, :], start=(k == 0), stop=(k == 7)),
                         reads=[wkey, hk], writes=[pk])
                if mode == "f32":
                    oi = rot["f"] % 3
                    rot["f"] += 1
                    ot, ok = of32[oi], f"of32_{oi}"
                else:
                    oi = rot["b"] % 3
                    rot["b"] += 1
                    ot, ok = ob16[oi], f"ob16_{oi}"
                ev = "vector" if rot["ev"] % 2 == 0 else "scalar"
                rot["ev"] += 1
                if mode == "sig":
                    p.op("scalar", lambda e: e.activation(out=ot[:], in_=ps[:, :], func=AF.Sigmoid), reads=[pk], writes=[ok])
                elif ev == "vector":
                    p.op("vector", lambda e: e.tensor_copy(out=ot[:], in_=ps[:, :]), reads=[pk], writes=[ok])
                else:
                    p.op("scalar", lambda e: e.activation(out=ot[:], in_=ps[:, :], func=AF.Copy), reads=[pk], writes=[ok])
                r = dst_row0 + ci * 128
                p.dma("sync", dst(r, s), ot[:], reads=[ok])

        def body(s):
            slot = s % 2
            hk = f"hT{slot}"
            p.dma("sync", cs[slot][:, 0, :], cos_t[:, s * 512:(s + 1) * 512], writes=[f"cs{slot}"])
            p.dma("sync", cs[slot][:, 1, :], sin_t[:, s * 512:(s + 1) * 512], writes=[f"cs{slot}"])
            fm_group(s, Wm, "Wm", M_QKV, 12, lambda r, s_: o["qkvTa"][r:r + 128, 2 + s_ * 512: 2 + (s_ + 1) * 512], 0, "bf16")
            for ti in range(4):
                pi = rot["ps"] % 6
                rot["ps"] += 1
                ps = c.ps[pi]
                pk = f"ps{pi}"
                for k in range(8):
                    p.op("tensor", lambda e: e.matmul(ps[:, :], lhsT=hT[slot][:, k, ti * 128:(ti + 1) * 128],
                                                      rhs=Wm[:, k, M_Z:M_Z + 512], start=(k == 0), stop=(k == 7)),
                         reads=["Wm", hk], writes=[pk])
                oi = rot["f"] % 3
                rot["f"] += 1
                p.op("vector", lambda e: e.tensor_copy(out=of32[oi][:], in_=ps[:, :]), reads=[pk], writes=[f"of32_{oi}"])
                r0 = s * 512 + ti * 128
                p.dma("sync", o["ztok"][r0:r0 + 128, :], of32[oi][:], reads=[f"of32_{oi}"])
            ps = c.ps[6]
            for ti in range(4):
                for k in range(8):
                    p.op("tensor", lambda e: e.matmul(ps[:, ti * 16:(ti + 1) * 16], lhsT=hT[slot][:, k, ti * 128:(ti + 1) * 128],
                                                      rhs=Wm[:, k, M_BD:M_BD + 16], start=(k == 0), stop=(k == 7)),
                         reads=["Wm", hk], writes=["ps6"])
            p.op("vector", lambda e: e.tensor_copy(out=obd[:, :, :], in_=ps[:, 0:64].rearrange("p (t c) -> p t c", c=16)),
                 reads=["ps6"], writes=["obd"])
            p.dma("sync", o["bdtok"][s * 512:(s + 1) * 512, :].rearrange("(t p) c -> p t c", p=128), obd[:, :, :],
                  reads=["obd"])
            pi = rot["ps"] % 6
            rot["ps"] += 1
            for k in range(8):
                p.op("tensor", lambda e: e.matmul(c.ps[pi][0:16, :], lhsT=Wm[:, k, M_BD:M_BD + 16],
                                                  rhs=hT[slot][:, k, :], start=(k == 0), stop=(k == 7)),
                     reads=["Wm", hk], writes=[f"ps{pi}"])
            oi = rot["f"] % 3
            rot["f"] += 1
            p.op("vector", lambda e: e.tensor_copy(out=of32[oi][0:16, :], in_=c.ps[pi][0:16, :]), reads=[f"ps{pi}"], writes=[f"of32_{oi}"])
            p.dma("sync", o["bdT"][:, s * 512:(s + 1) * 512], of32[oi][0:16, :], reads=[f"of32_{oi}"])
            for ci in range(3):
                pa = rot["ps"] % 6
                rot["ps"] += 1
                pb = rot["ps"] % 6
                rot["ps"] += 1
                for k in range(8):
                    p.op("tensor", lambda e: e.matmul(c.ps[pa][:, :], lhsT=Wrope[:, k, ci * 128:(ci + 1) * 128],
                                                      rhs=hT[slot][:, k, :], start=(k == 0), stop=(k == 7)),
                         reads=["Wrope", hk], writes=[f"ps{pa}"])
                for k in range(8):
                    p.op("tensor", lambda e: e.matmul(c.ps[pb][:, :], lhsT=Wp[:, k, ci * 128:(ci + 1) * 128],
                                                      rhs=hT[slot][:, k, :], start=(k == 0), stop=(k == 7)),
                         reads=["Wp", hk], writes=[f"ps{pb}"])
                p.op("vector", lambda e: e.tensor_tensor(out=t1[:], in0=c.ps[pa][:, :], in1=cs[slot][:, 0, :], op=ALU.mult),
                     reads=[f"ps{pa}", f"cs{slot}"], writes=["t1"])
                p.op("vector", lambda e: e.tensor_tensor(out=t2[:], in0=c.ps[pb][:, :], in1=cs[slot][:, 1, :], op=ALU.mult),
                     reads=[f"ps{pb}", f"cs{slot}"], writes=["t2"])
                oi = rot["b"] % 3
                rot["b"] += 1
                p.op("gpsimd", lambda e: e.tensor_tensor(out=ob16[oi][:], in0=t1[:], in1=t2[:], op=ALU.add),
                     reads=["t1", "t2"], writes=[f"ob16_{oi}"])
                p.dma("sync", o["qkrope"][ci * 128:(ci + 1) * 128, s * 512:(s + 1) * 512], ob16[oi][:],
                      reads=[f"ob16_{oi}"])
            fm_group(s, Wrest, "Wrest", 0, 9, lambda r, s_: o["qkrest"][r:r + 128, s_ * 512:(s_ + 1) * 512], 0, "bf16")
            fm_group(s, Wm, "Wm", M_V, 6, lambda r, s_: o["vTb"][r:r + 128, s_ * 512:(s_ + 1) * 512], 0, "bf16")
            fm_group(s, Wm, "Wm", M_G, 16, lambda r, s_: o["gatesT"][r:r + 128, s_ * 512:(s_ + 1) * 512], 0, "sig")

        prep_front(0)
        for s in range(NS):
            body(s)
            if s + 1 < NS:
                prep_front(s + 1)
        p.barrier()


def phase_b1(p, c, T, qkvTa, conv_w, o):
    for _ in phase_b1_gen(p, c, T, qkvTa, conv_w, o):
        pass


def phase_b1_gen(p, c, T, qkvTa, conv_w, o, es_ext=None, shared=False):
    nc = p.nc
    NS = T // 512
    with ExitStack() as es_own:
        es = es_ext if es_ext is not None else es_own
        def sb(name, shape, dt):
            return es.enter_context(nc.sbuf_tensor(f"B1_{_uid()}_" + name, shape, dt))
        cw = sb("cw", [128, 12, 5], F32)
        uin = [sb(f"uin{i}", [128, 516], BF16) for i in range(3)]
        Dg = sb("Dg", [128, 60, 128], BF16)
        su = [sb(f"su{i}", [128, 512], F32) for i in range(2)]
        sqs = [sb(f"sq{i}", [128, 512], BF16) for i in range(3)]
        onesb = sb("onesb", [128, 128], BF16)
        svb = [sb(f"svb{i}", [128, 512], BF16) for i in range(2)]
        rns = [sb(f"rn{i}", [128, 512], F32) for i in range(3)]
        rn2s = [sb(f"rn2_{i}", [128, 512], F32) for i in range(3)]
        un = [sb(f"un{i}", [128, 512], F32) for i in range(2)]
        tk = [sb(f"tk{i}", [128, 4, 128], BF16) for i in range(2)]
        p.op("vector", lambda e: e.tensor_copy(out=onesb[:], in_=c.onesf[:]), reads=["onesf"], writes=["onesb"])
        for j in range(5):
            p.dma("sync", cw[:, :, j:j + 1], conv_w[j:j + 1, :].rearrange("j (k c) -> c k j", c=128), writes=["cw"], allow_slow_non_contiguous=True)
        for ch in range(12):
            for j in range(5):
                p.op("vector", lambda e: e.tensor_scalar(out=Dg[:, ch * 5 + j, :], in0=c.identb[:], scalar1=cw[:, ch, j:j + 1], scalar2=None, op0=ALU.mult),
                     reads=["cw", "identb"], writes=["Dg"])
        n = 0
        for s in range(NS):
            for ch in range(12):
                ui, uk = uin[n % 3], f"uin{n % 3}"
                sv, sk = su[n % 2], f"su{n % 2}"
                u, unk = un[n % 2], f"un{n % 2}"
                if shared:
                    cb, pi, tb = 5, 6, 7
                else:
                    cb = n % 3
                    pi = 3 + n % 3
                    tb = 6 + n % 2
                sq, sqk = sqs[n % 3], f"sq{n % 3}"
                rn, rnk = rns[n % 3], f"rn{n % 3}"
                rn2, rn2k = rn2s[n % 3], f"rn2_{n % 3}"
                n += 1
                p.dma("sync", ui[:], qkvTa[ch * 128:(ch + 1) * 128, s * 512: s * 512 + 516], writes=[uk])
                for j in range(5):
                    p.op("tensor", lambda e: e.matmul(c.ps[cb][:, :], lhsT=Dg[:, ch * 5 + j, :], rhs=ui[:, j:j + 512], start=(j == 0), stop=(j == 4)),
                         reads=[uk, "Dg"], writes=[f"ps{cb}"])
                p.op("scalar", lambda e: e.activation(out=sv[:], in_=c.ps[cb][:, :], func=AF.Silu), reads=[f"ps{cb}"], writes=[sk])
                if ch < 8:
                    p.op("gpsimd", lambda e: e.tensor_tensor(out=sq[:], in0=sv[:], in1=sv[:], op=ALU.mult), reads=[sk], writes=[sqk])
                    ps = c.ps[pi]
                    p.op("tensor", lambda e: e.matmul(ps[:, :], lhsT=onesb[:], rhs=sq[:], start=True, stop=True),
                         reads=[sqk, "onesb"], writes=[f"ps{pi}"])
                    if B1_RSQRT_ACT:
                        if ch < 4:
                            p.op("scalar", lambda e: e.activation(out=rn[:], in_=ps[:, :], func=AF.Ln, scale=128.0, bias=c.bias_q[:, 0:1]),
                                 reads=[f"ps{pi}", "bias_q"], writes=[rnk])
                        else:
                            p.op("scalar", lambda e: e.activation(out=rn[:], in_=ps[:, :], func=AF.Ln, scale=1.0, bias=c.bias_k[:, 0:1]),
                                 reads=[f"ps{pi}", "bias_k"], writes=[rnk])
                        p.op("scalar", lambda e: e.activation(out=rn2[:], in_=rn[:], func=AF.Exp, scale=-0.5), reads=[rnk], writes=[rn2k])
                    else:
                        if ch < 4:
                            p.op("scalar", lambda e: e.activation(out=rn[:], in_=ps[:, :], func=AF.Sqrt, scale=128.0, bias=c.bias_q[:, 0:1]),
                                 reads=[f"ps{pi}", "bias_q"], writes=[rnk])
                        else:
                            p.op("scalar", lambda e: e.activation(out=rn[:], in_=ps[:, :], func=AF.Sqrt, scale=1.0, bias=c.bias_k[:, 0:1]),
                                 reads=[f"ps{pi}", "bias_k"], writes=[rnk])
                        p.op("vector", lambda e: e.reciprocal(out=rn2[:], in_=rn[:]), reads=[rnk], writes=[rn2k])
                    p.op("gpsimd", lambda e: e.tensor_tensor(out=u[:], in0=sv[:], in1=rn2[:], op=ALU.mult), reads=[sk, rn2k], writes=[unk])
                    p.dma("sync", o["uT"][ch * 128:(ch + 1) * 128, s * 512:(s + 1) * 512], u[:], reads=[unk])
                    src, srck = u, unk
                else:
                    src, srck = sv, sk
                if ch >= 4:
                    pj = tb
                    sb_, sbk = svb[n % 2], f"svb{n % 2}"
                    p.op("gpsimd", lambda e: e.tensor_copy(out=sb_[:], in_=src[:]), reads=[srck], writes=[sbk])
                    pjb = c.ps[pj][:, :].bitcast(BF16)
                    for ti in range(4):
                        p.op("tensor", lambda e: e.transpose(out=pjb[:, ti * 128:(ti + 1) * 128], in_=sb_[:, ti * 128:(ti + 1) * 128],
                                                             identity=c.identb[:]), reads=[sbk, "identb"], writes=[f"ps{pj}"])
                    t_, tkk = tk[n % 2], f"tk{n % 2}"
                    p.op("scalar", lambda e: e.activation(out=t_[:, :, :], in_=pjb[:, 0:512].rearrange("p (t d) -> p t d", d=128), func=AF.Copy),
                         reads=[f"ps{pj}"], writes=[tkk])
                    dst = o["ktok"] if ch < 8 else o["vtok"]
                    hh = ch % 4
                    p.dma("sync", dst[s * 512:(s + 1) * 512, hh * 128:(hh + 1) * 128].rearrange("(t p) d -> p t d", p=128), t_[:, :, :],
                          reads=[tkk])
                yield
        if not shared:
            p.barrier()


def alloc_gdn_consts(p, c, es):
    nc = p.nc
    def mk(name, fillval_in, pattern, cm, cmp, fill):
        t = es.enter_context(nc.sbuf_tensor(name, [128, 128], F32))
        p.op("gpsimd", lambda e: e.memset(t[:], fillval_in), writes=[name])
        p.op("gpsimd", lambda e: e.affine_select(out=t[:], in_=t[:], pattern=pattern, compare_op=cmp, fill=fill,
                                                  base=0, channel_multiplier=cm), reads=[name], writes=[name])
        if GDN_F32R and name.startswith("tri"):
            t2 = es.enter_context(nc.sbuf_tensor(name + "r", [128, 128], F32))
            p.op("vector", lambda e: e.tensor_copy(out=t2[:].bitcast(mybir.dt.float32r), in_=t[:]), reads=[name], writes=[name])
            return t2
        return t
    c.triU = mk("triU", 1.0, [[1, 128]], -1, ALU.is_ge, 0.0)
    c.triL = mk("triL", 1.0, [[-1, 128]], 1, ALU.is_ge, 0.0)
    c.nm1k = ["nm1f", "nm1b"]
    c.nm2k = ["nm2f", "nm2b"]
    c.nm1 = [mk("nm1f", 0.0, [[-1, 128]], 1, ALU.is_gt, 1.0e4),
             mk("nm1b", 0.0, [[1, 128]], -1, ALU.is_gt, 1.0e4)]
    c.nm2 = [mk("nm2f", 0.0, [[1, 128]], -1, ALU.is_ge, -1.0e4),
             mk("nm2b", 0.0, [[-1, 128]], 1, ALU.is_ge, -1.0e4)]


def phase_b2(p, c, T, uT, ktok, vtok, bdtok, a_log, dt_bias, o_f, o_b):
    nc = p.nc
    NT = T // 128
    F32R = mybir.dt.float32r
    USE_R = GDN_F32R

    def R(ap):
        return ap.bitcast(F32R) if USE_R else ap
    with ExitStack() as es:
        def sb(name, shape, dt=F32):
            return es.enter_context(nc.sbuf_tensor(f"B2_{_uid()}_" + name, shape, dt))
        bd = sb("bd", [128, NT, 16])
        beta = sb("beta", [128, NT, 8])
        nbeta = sb("nbeta", [128, NT, 8])
        g = sb("g", [128, NT, 8])
        G = sb("G", [128, NT, 8])
        tmp = sb("tmp", [128, NT, 8])
        eGr = sb("eGr", [128, NT, 8])
        ge = sb("ge", [128, NT, 8])
        bG = sb("bG", [128, NT, 8])
        dtb = sb("dtb", [128, 8])
        nea = sb("nea", [128, 8])
        for t0 in range(0, NT, 8):
            t1 = min(NT, t0 + 8)
            p.dma("sync", bd[:, t0:t1, :], bdtok[t0 * 128:t1 * 128, :].rearrange("(t p) c -> p t c", p=128), writes=["bd"])
        p.dma("sync", dtb[:], dt_bias.to_broadcast([128, 8]), writes=["dtb"])
        p.dma("sync", nea[:], a_log.to_broadcast([128, 8]), writes=["nea"])
        p.op("scalar", lambda e: e.activation(out=nea[:], in_=nea[:], func=AF.Exp), reads=["nea"], writes=["nea"])
        p.op("vector", lambda e: e.tensor_scalar(out=nea[:], in0=nea[:], scalar1=-1.0, scalar2=None, op0=ALU.mult), reads=["nea"], writes=["nea"])
        p.op("scalar", lambda e: e.activation(out=beta[:, :, :], in_=bd[:, :, 0:8], func=AF.Sigmoid), reads=["bd"], writes=["beta"])
        p.op("vector", lambda e: e.tensor_scalar(out=nbeta[:, :, :], in0=beta[:, :, :], scalar1=-1.0, scalar2=None, op0=ALU.mult),
             reads=["beta"], writes=["nbeta"])
        p.op("vector", lambda e: e.tensor_tensor(out=tmp[:, :, :], in0=bd[:, :, 8:16], in1=dtb[:, :].unsqueeze(1).to_broadcast([128, NT, 8]), op=ALU.add),
             reads=["bd", "dtb"], writes=["tmp"])
        p.op("scalar", lambda e: e.activation(out=tmp[:, :, :], in_=tmp[:, :, :], func=AF.Exp), reads=["tmp"], writes=["tmp"])
        p.op("scalar", lambda e: e.activation(out=tmp[:, :, :], in_=tmp[:, :, :], func=AF.Ln, bias=c.bias_one[:, 0:1], scale=1.0), reads=["tmp", "bias_one"], writes=["tmp"])
        p.op("vector", lambda e: e.tensor_tensor(out=R(g[:, :, :]), in0=tmp[:, :, :], in1=nea[:, :].unsqueeze(1).to_broadcast([128, NT, 8]), op=ALU.mult),
             reads=["tmp", "nea"], writes=["g"])
        for d, tri, tk in ((0, c.triU, "triU"), (1, c.triL, "triL")):
            for t0 in range(0, NT, 64):
                t1 = min(NT, t0 + 64)
                p.op("tensor", lambda e: e.matmul(c.ps[d][:, 0:(t1 - t0) * 4], lhsT=tri[:], rhs=g[:, t0:t1, d * 4:(d + 1) * 4], start=True, stop=True),
                     reads=["g", tk], writes=[f"ps{d}"])
                p.op("vector", lambda e: e.tensor_copy(out=G[:, t0:t1, d * 4:(d + 1) * 4],
                                                       in_=c.ps[d][:, 0:(t1 - t0) * 4].rearrange("p (t c) -> p t c", c=4)),
                     reads=[f"ps{d}"], writes=["G"])
        for t0 in range(0, NT, 64):
            t1 = min(NT, t0 + 64)
            p.op("tensor", lambda e: e.matmul(c.ps[2][:, 0:(t1 - t0) * 8], lhsT=c.onesf[:], rhs=g[:, t0:t1, :], start=True, stop=True),
                 reads=["g", "onesf"], writes=["ps2"])
            p.op("scalar", lambda e: e.activation(out=ge[:, t0:t1, :], in_=c.ps[2][:, 0:(t1 - t0) * 8].rearrange("p (t c) -> p t c", c=8), func=AF.Exp),
                 writes=["ps2", "ge"])
            p.op("vector", lambda e: e.tensor_tensor(out=eGr[:, t0:t1, :], in0=c.ps[2][:, 0:(t1 - t0) * 8].rearrange("p (t c) -> p t c", c=8),
                                                     in1=G[:, t0:t1, :], op=ALU.subtract), reads=["G"], writes=["ps2", "eGr"])
        p.op("scalar", lambda e: e.activation(out=eGr[:, :, :], in_=eGr[:, :, :], func=AF.Exp), reads=["eGr"], writes=["eGr"])
        p.op("scalar", lambda e: e.activation(out=bG[:, :, :], in_=G[:, :, :], func=AF.Exp), reads=["G"], writes=["bG"])
        p.op("vector", lambda e: e.tensor_tensor(out=bG[:, :, :], in0=bG[:, :, :], in1=beta[:, :, :], op=ALU.mult), reads=["bG", "beta"], writes=["bG"])
        p.barrier()

        U = []
        for hd in range(8):
            S_ = sb(f"S_{hd}", [128, 128])
            Sb_ = sb(f"Sb_{hd}", [128, 128], BF16)
            two = []
            for pp in range(2):
                u = {"S": S_, "Sb": Sb_}
                for nm in ["D1", "D2T", "eB", "X0", "X1"]:
                    u[nm] = sb(f"{nm}_{hd}_{pp}", [128, 128])
                for nm in ["XP0", "XP1"]:
                    u[nm] = sb(f"{nm}_{hd}_{pp}", [128, 256])
                for nm in ["aaT", "qdT", "TTb", "TTbg", "nwT", "vnew", "vs"]:
                    u[nm] = sb(f"{nm}_{hd}_{pp}", [128, 128], BF16)
                two.append(u)
            U.append(two)
            p.op("gpsimd", lambda e: e.memset(S_[:], 0.0), writes=[f"S_{hd}"])
            p.op("gpsimd", lambda e: e.memset(Sb_[:], 0.0), writes=[f"Sb_{hd}"])
        inb = []
        for b_ in range(2):
            d_ = {}
            for dr in range(2):
                d_[dr] = dict(kq=sb(f"kq{b_}{dr}", [128, 4, 2, 128]),
                              ktb=sb(f"ktb{b_}{dr}", [128, 512], BF16), vtb=sb(f"vtb{b_}{dr}", [128, 512], BF16),
                              o=sb(f"o{b_}{dr}", [128, 512]))
            inb.append(d_)

        bn = [0]

        def bank():
            i = bn[0] % 8
            bn[0] += 1
            return c.ps[i], f"ps{i}"

        def Q(bk, q):
            return bk[:, q * 128:(q + 1) * 128]

        def ev(eng, out_ap, in_ap, rk, wk, scale=None):
            if eng == "scalar":
                if scale is None:
                    p.op("scalar", lambda e: e.activation(out=out_ap, in_=in_ap, func=AF.Copy), reads=rk, writes=wk)
                else:
                    p.op("scalar", lambda e: e.activation(out=out_ap, in_=in_ap, func=AF.Copy, scale=scale), reads=rk, writes=wk)
            else:
                if scale is None:
                    p.op("vector", lambda e: e.tensor_copy(out=out_ap, in_=in_ap), reads=rk, writes=wk)
                else:
                    p.op("vector", lambda e: e.tensor_scalar(out=out_ap, in0=in_ap, scalar1=scale, scalar2=None, op0=ALU.mult), reads=rk, writes=wk)

        def XT(u, par):
            return u[f"XP{par}"][:, 0:128]

        def PT(u, j):
            return u[f"XP{1 - j % 2}"][:, 128:256]

        for n in range(NT):
            bs = n % 2
            pp = n % 2
            tiles = (n, NT - 1 - n)
            for dr in range(2):
                i = tiles[dr]
                ib = inb[bs][dr]
                for h_ in range(4):
                    p.dma("sync", ib["kq"][:, h_, 0, :], uT[512 + h_ * 128:512 + (h_ + 1) * 128, i * 128:(i + 1) * 128], writes=[f"kq{bs}{dr}"])
                    p.dma("sync", ib["kq"][:, h_, 1, :], uT[h_ * 128:(h_ + 1) * 128, i * 128:(i + 1) * 128], writes=[f"kq{bs}{dr}"])
                p.dma("sync", ib["ktb"][:, :], ktok[i * 128:(i + 1) * 128, :], writes=[f"ktb{bs}{dr}"])
                p.dma("sync", ib["vtb"][:, :], vtok[i * 128:(i + 1) * 128, :], writes=[f"vtb{bs}{dr}"])
            st = {}
            for dr in range(2):
                i = tiles[dr]
                ib = inb[bs][dr]
                tri, trik = (c.triU, "triU") if dr == 0 else (c.triL, "triL")
                bB, bBk = bank()
                bK = [bank(), bank()]
                st[dr] = (bB, bBk, bK)
                for h in range(4):
                    hd = dr * 4 + h
                    p.op("tensor", lambda e: e.matmul(Q(bB, h), lhsT=g[:, i, hd:hd + 1].to_broadcast([128, 128]), rhs=tri[:], start=True, stop=True),
                         reads=["g", trik], writes=[bBk])
                for h in range(4):
                    bk_, bkk = bK[h // 2]
                    p.op("tensor", lambda e: e.matmul(bk_[:, (h % 2) * 256:(h % 2) * 256 + 256], lhsT=ib["kq"][:, h, 0, :],
                                                      rhs=ib["kq"][:, h, :, :].rearrange("p a t -> p (a t)"), start=True, stop=True),
                         reads=[f"kq{bs}{dr}"], writes=[bkk])
            for dr in range(2):
                i = tiles[dr]
                ib = inb[bs][dr]
                bB, bBk, bK = st[dr]
                for h in range(4):
                    hd = dr * 4 + h
                    u = U[hd][pp]
                    Gc = G[:, i, hd:hd + 1]
                    p.op("vector", lambda e: e.scalar_tensor_tensor(out=u["D1"][:], in0=Q(bB, h), scalar=Gc, in1=c.nm1[dr][:], op0=ALU.subtract, op1=ALU.max),
                         reads=["G", c.nm1k[dr]], writes=[bBk, f"D1_{hd}p{pp}"])
                    p.op("scalar", lambda e: e.activation(out=u["D1"][:], in_=u["D1"][:], func=AF.Exp, scale=-1.0), reads=[f"D1_{hd}p{pp}"], writes=[f"D1_{hd}p{pp}"])
                    p.op("vector", lambda e: e.scalar_tensor_tensor(out=u["D2T"][:], in0=Q(bB, h), scalar=Gc, in1=c.nm2[dr][:], op0=ALU.subtract, op1=ALU.min),
                         reads=["G", c.nm2k[dr]], writes=[bBk, f"D2T_{hd}p{pp}"])
                    p.op("scalar", lambda e: e.activation(out=u["D2T"][:], in_=u["D2T"][:], func=AF.Exp), reads=[f"D2T_{hd}p{pp}"], writes=[f"D2T_{hd}p{pp}"])
                for h in range(4):
                    hd = dr * 4 + h
                    u = U[hd][pp]
                    p.op("scalar", lambda e: e.activation(out=u["eB"][:], in_=Q(bB, h), func=AF.Exp), writes=[bBk, f"eB_{hd}p{pp}"])
                for h in range(4):
                    hd = dr * 4 + h
                    u = U[hd][pp]
                    bk_, bkk = bK[h // 2]
                    o0 = (h % 2) * 256
                    p.op("vector", lambda e: e.scalar_tensor_tensor(out=u["X0"][:], in0=bk_[:, o0:o0 + 128], scalar=nbeta[:, i, hd:hd + 1], in1=u["D1"][:],
                                                                    op0=ALU.mult, op1=ALU.mult), reads=["nbeta", f"D1_{hd}p{pp}"], writes=[bkk, f"X0_{hd}p{pp}"])
                    p.op("vector", lambda e: e.tensor_tensor(out=u["aaT"][:], in0=bk_[:, o0 + 128:o0 + 256], in1=u["D2T"][:], op=ALU.mult),
                         reads=[f"D2T_{hd}p{pp}"], writes=[bkk, f"aaT_{hd}p{pp}"])
                    p.op("gpsimd", lambda e: e.tensor_tensor(out=u["qdT"][:], in0=ib["kq"][:, h, 1, :], in1=u["eB"][:], op=ALU.mult),
                         reads=[f"kq{bs}{dr}", f"eB_{hd}p{pp}"], writes=[f"qdT_{hd}p{pp}"])
            for dr in range(2):
                bT, bTk = bank()
                for h in range(4):
                    hd = dr * 4 + h
                    u = U[hd][pp]
                    p.op("tensor", lambda e: e.transpose(out=Q(bT, h), in_=u["X0"][:], identity=c.identf[:]), reads=[f"X0_{hd}p{pp}", "identf"], writes=[bTk])
                for h in range(4):
                    hd = dr * 4 + h
                    u = U[hd][pp]
                    p.op("scalar", lambda e: e.activation(out=XT(u, 0), in_=Q(bT, h), func=AF.Copy), writes=[bTk, f"XT0_{hd}p{pp}"])
                    p.op("gpsimd", lambda e: e.tensor_tensor(out=PT(u, 0), in0=XT(u, 0), in1=c.identf[:], op=ALU.add),
                         reads=[f"XT0_{hd}p{pp}", "identf"], writes=[f"PT0_{hd}p{pp}"])
            for k in range(1, 7):
                a_, b_ = (k - 1) % 2, k % 2
                for dr in range(2):
                    e1, e2 = ("scalar", "vector") if (k + dr) % 3 != 0 else ("scalar", "scalar")
                    b1, b1k = bank()
                    for h in range(4):
                        hd = dr * 4 + h
                        u = U[hd][pp]
                        p.op("tensor", lambda e: e.matmul(Q(b1, h), lhsT=XT(u, a_), rhs=u[f"X{a_}"][:], start=True, stop=True),
                             reads=[f"XT{a_}_{hd}p{pp}", f"X{a_}_{hd}p{pp}"], writes=[b1k])
                    bM = [bank(), bank()]
                    for h in range(4):
                        hd = dr * 4 + h
                        u = U[hd][pp]
                        bm_, bmk = bM[h // 2]
                        o0 = (h % 2) * 256
                        if k == 1:
                            p.op("tensor", lambda e: e.matmul(bm_[:, o0:o0 + 128], lhsT=u[f"X{a_}"][:], rhs=XT(u, a_), start=True, stop=True),
                                 reads=[f"XT{a_}_{hd}p{pp}", f"X{a_}_{hd}p{pp}"], writes=[bmk])
                        else:
                            p.op("tensor", lambda e: e.matmul(bm_[:, o0:o0 + 256], lhsT=u[f"X{a_}"][:], rhs=u[f"XP{a_}"][:, :], start=True, stop=True),
                                 reads=[f"XT{a_}_{hd}p{pp}", f"X{a_}_{hd}p{pp}", f"PT{(k - 2) % 2}_{hd}p{pp}"], writes=[bmk])
                    for h in range(4):
                        hd = dr * 4 + h
                        u = U[hd][pp]
                        ev(e1, u[f"X{b_}"][:], Q(b1, h), [], [b1k, f"X{b_}_{hd}p{pp}"])
                    for h in range(4):
                        hd = dr * 4 + h
                        u = U[hd][pp]
                        bm_, bmk = bM[h // 2]
                        o0 = (h % 2) * 256
                        if k < 6:
                            ev(e2, XT(u, b_), bm_[:, o0:o0 + 128], [], [bmk, f"XT{b_}_{hd}p{pp}"])
                        if k >= 2:
                            p.op("vector", lambda e: e.tensor_tensor(out=PT(u, k - 1), in0=bm_[:, o0 + 128:o0 + 256], in1=PT(u, k - 2), op=ALU.add),
                                 reads=[f"PT{(k - 2) % 2}_{hd}p{pp}"], writes=[bmk, f"PT{(k - 1) % 2}_{hd}p{pp}"])
            for dr in range(2):
                bF, bFk = bank()
                for h in range(4):
                    hd = dr * 4 + h
                    u = U[hd][pp]
                    p.op("tensor", lambda e: e.matmul(Q(bF, h), lhsT=u["X0"][:], rhs=PT(u, 5), start=True, stop=True),
                         reads=[f"X0_{hd}p{pp}", f"PT1_{hd}p{pp}"], writes=[bFk])
                for h in range(4):
                    hd = dr * 4 + h
                    u = U[hd][pp]
                    p.op("vector", lambda e: e.tensor_tensor(out=PT(u, 6), in0=Q(bF, h), in1=PT(u, 5), op=ALU.add),
                         reads=[f"PT1_{hd}p{pp}"], writes=[bFk, f"PT0_{hd}p{pp}"])
            for dr in range(2):
                i = tiles[dr]
                ib = inb[bs][dr]
                for h in range(4):
                    hd = dr * 4 + h
                    u = U[hd][pp]
                    p.op("scalar", lambda e: e.activation(out=u["TTb"][:], in_=PT(u, 6), func=AF.Copy, scale=beta[:, i, hd:hd + 1]),
                         reads=[f"PT0_{hd}p{pp}", "beta"], writes=[f"TTb_{hd}p{pp}"])
                    p.op("vector", lambda e: e.tensor_scalar(out=u["TTbg"][:], in0=PT(u, 6), scalar1=bG[:, i, hd:hd + 1], scalar2=None, op0=ALU.mult),
                         reads=[f"PT0_{hd}p{pp}", "bG"], writes=[f"TTbg_{hd}p{pp}"])
                bw, bwk = bank()
                for h in range(4):
                    hd = dr * 4 + h
                    u = U[hd][pp]
                    p.op("tensor", lambda e: e.matmul(Q(bw, h), lhsT=ib["ktb"][:, h * 128:(h + 1) * 128], rhs=u["TTbg"][:], start=True, stop=True),
                         reads=[f"ktb{bs}{dr}", f"TTbg_{hd}p{pp}"], writes=[bwk])
                for h in range(4):
                    hd = dr * 4 + h
                    u = U[hd][pp]
                    ev("scalar" if dr == 0 else "vector", u["nwT"][:], Q(bw, h), [], [bwk, f"nwT_{hd}p{pp}"], scale=-1.0)
                bv, bvk = bank()
                for h in range(4):
                    hd = dr * 4 + h
                    u = U[hd][pp]
                    p.op("tensor", lambda e: e.matmul(Q(bv, h), lhsT=u["TTb"][:], rhs=ib["vtb"][:, h * 128:(h + 1) * 128], start=True, stop=False),
                         reads=[f"vtb{bs}{dr}", f"TTb_{hd}p{pp}"], writes=[bvk])
                    p.op("tensor", lambda e: e.matmul(Q(bv, h), lhsT=u["nwT"][:], rhs=u["Sb"][:], start=False, stop=True),
                         reads=[f"nwT_{hd}p{pp}", f"Sb_{hd}"], writes=[bvk])
                for h in range(4):
                    hd = dr * 4 + h
                    u = U[hd][pp]
                    p.op("scalar", lambda e: e.activation(out=u["vnew"][:], in_=Q(bv, h), func=AF.Copy), writes=[bvk, f"vnew_{hd}p{pp}"])
                for h in range(4):
                    hd = dr * 4 + h
                    u = U[hd][pp]
                    p.op("vector", lambda e: e.tensor_scalar(out=u["vs"][:], in0=Q(bv, h), scalar1=eGr[:, i, hd:hd + 1], scalar2=None, op0=ALU.mult),
                         reads=["eGr"], writes=[bvk, f"vs_{hd}p{pp}"])
                bo, bok = bank()
                for h in range(4):
                    hd = dr * 4 + h
                    u = U[hd][pp]
                    p.op("tensor", lambda e: e.matmul(Q(bo, h), lhsT=u["qdT"][:], rhs=u["Sb"][:], start=True, stop=False),
                         reads=[f"qdT_{hd}p{pp}", f"Sb_{hd}"], writes=[bok])
                    p.op("tensor", lambda e: e.matmul(Q(bo, h), lhsT=u["aaT"][:], rhs=u["vnew"][:], start=False, stop=True),
                         reads=[f"aaT_{hd}p{pp}", f"vnew_{hd}p{pp}"], writes=[bok])
                p.op("scalar", lambda e: e.activation(out=ib["o"][:, :], in_=bo[:, :], func=AF.Copy), writes=[bok, f"o{bs}{dr}"])
                bS, bSk = bank()
                for h in range(4):
                    hd = dr * 4 + h
                    u = U[hd][pp]
                    p.op("tensor", lambda e: e.matmul(Q(bS, h), lhsT=ib["ktb"][:, h * 128:(h + 1) * 128], rhs=u["vs"][:], start=True, stop=True),
                         reads=[f"ktb{bs}{dr}", f"vs_{hd}p{pp}"], writes=[bSk])
                for h in range(4):
                    hd = dr * 4 + h
                    u = U[hd][pp]
                    p.op("vector", lambda e: e.scalar_tensor_tensor(out=u["S"][:], in0=u["S"][:], scalar=ge[:, i, hd:hd + 1], in1=Q(bS, h),
                                                                    op0=ALU.mult, op1=ALU.add), reads=["ge"], writes=[bSk, f"S_{hd}"])
                    p.op("gpsimd", lambda e: e.tensor_copy(out=u["Sb"][:], in_=u["S"][:]), reads=[f"S_{hd}"], writes=[f"Sb_{hd}"])
                dst = o_f if dr == 0 else o_b
                p.dma("sync", dst[i * 128:(i + 1) * 128, :], ib["o"][:, :], reads=[f"o{bs}{dr}"])
        p.barrier()


def phase_b3(p, c, T, o_f, o_b, ztok, gdn_norm, yaT):
    nc = p.nc
    NT = T // 128
    with ExitStack() as es:
        def sb(name, shape, dt=F32):
            return es.enter_context(nc.sbuf_tensor(f"B3_{_uid()}_" + name, shape, dt))
        gnb = sb("gnb", [128, 128])
        of = [sb(f"of{i}", [128, 512]) for i in range(2)]
        ob = [sb(f"ob{i}", [128, 512]) for i in range(2)]
        zt = [sb(f"zt{i}", [128, 512]) for i in range(2)]
        junk_2 = [sb(f"junk{i}", [128, 128]) for i in range(2)]
        ss_2 = [sb(f"ss{i}", [128, 4]) for i in range(2)]
        rs_2 = [sb(f"rs{i}", [128, 4]) for i in range(2)]
        on_2 = [sb(f"on{i}", [128, 512]) for i in range(2)]
        yb_2 = [sb(f"yb{i}", [128, 512], BF16) for i in range(2)]
        yT = [sb(f"yT{i}", [128, 4, 128], BF16) for i in range(2)]
        p.dma("sync", gnb[:], gdn_norm.to_broadcast([128, 128]), writes=["gnb"])
        for i in range(NT):
            b = i % 2
            junk, ss, rs, on, yb = junk_2[b], ss_2[b], rs_2[b], on_2[b], yb_2[b]
            p.dma("sync", of[b][:], o_f[i * 128:(i + 1) * 128, :], writes=[f"of{b}"])
            p.dma("sync", ob[b][:], o_b[i * 128:(i + 1) * 128, :], writes=[f"ob{b}"])
            p.dma("sync", zt[b][:], ztok[i * 128:(i + 1) * 128, :], writes=[f"zt{b}"])
            p.op("gpsimd", lambda e: e.tensor_tensor(out=of[b][:], in0=of[b][:], in1=ob[b][:], op=ALU.add), reads=[f"ob{b}", f"of{b}"], writes=[f"of{b}"])
            for h in range(4):
                p.op("scalar", lambda e: e.activation(out=junk[:], in_=of[b][:, h * 128:(h + 1) * 128], func=AF.Square, accum_out=ss[:, h:h + 1]),
                     reads=[f"of{b}"], writes=[f"junk_{b}", f"ss_{b}"])
            p.op("vector", lambda e: e.tensor_scalar(out=rs[:], in0=ss[:], scalar1=1.0 / 128, scalar2=RMS_EPS, op0=ALU.mult, op1=ALU.add), reads=[f"ss_{b}"], writes=[f"rs_{b}"])
            p.op("scalar", lambda e: e.activation(out=rs[:], in_=rs[:], func=AF.Sqrt), reads=[f"rs_{b}"], writes=[f"rs_{b}"])
            p.op("vector", lambda e: e.reciprocal(out=rs[:], in_=rs[:]), reads=[f"rs_{b}"], writes=[f"rs_{b}"])
            for h in range(4):
                p.op("vector", lambda e: e.scalar_tensor_tensor(out=on[:, h * 128:(h + 1) * 128], in0=of[b][:, h * 128:(h + 1) * 128], scalar=rs[:, h:h + 1],
                                                                in1=gnb[:], op0=ALU.mult, op1=ALU.mult), reads=[f"of{b}", f"rs_{b}", "gnb"], writes=[f"on_{b}"])
            p.op("scalar", lambda e: e.activation(out=zt[b][:], in_=zt[b][:], func=AF.Silu), reads=[f"zt{b}"], writes=[f"zt{b}"])
            p.op("gpsimd", lambda e: e.tensor_tensor(out=yb[:], in0=on[:], in1=zt[b][:], op=ALU.mult), reads=[f"on_{b}", f"zt{b}"], writes=[f"yb_{b}"])
            for h in range(4):
                p.op("tensor", lambda e: e.transpose(out=c.psb[:, h * 128:(h + 1) * 128], in_=yb[:, h * 128:(h + 1) * 128], identity=c.identb[:]),
                     reads=[f"yb_{b}", "identb"], writes=["ps7"])
            p.op("vector", lambda e: e.tensor_copy(out=yT[b][:, :, :], in_=c.psb[:, 0:512].rearrange("p (h t) -> p h t", t=128)), writes=["ps7", f"yT{b}"])
            for h_ in range(4):
                p.dma("sync", yaT[h_ * 128:(h_ + 1) * 128, i * 128:(i + 1) * 128], yT[b][:, h_, :], reads=[f"yT{b}"])
        p.barrier()


ATT_DIL = (1, 4, 16)


def alloc_att_consts(p, c, es):
    nc = p.nc
    c.amask = es.enter_context(nc.sbuf_tensor("amask", [128, 256], F32))
    p.op("gpsimd", lambda e: e.memset(c.amask[:], 0.0), writes=["amask"])
    p.op("gpsimd", lambda e: e.affine_select(out=c.amask[:], in_=c.amask[:], pattern=[[1, 256]], compare_op=ALU.is_ge, fill=-30000.0,
                                              base=0, channel_multiplier=-1), reads=["amask"], writes=["amask"])
    p.op("gpsimd", lambda e: e.affine_select(out=c.amask[:], in_=c.amask[:], pattern=[[-1, 256]], compare_op=ALU.is_ge, fill=-30000.0,
                                              base=128, channel_multiplier=1), reads=["amask"], writes=["amask"])
    c.amask0 = es.enter_context(nc.sbuf_tensor("amask0", [128, 128], F32))
    p.op("gpsimd", lambda e: e.memset(c.amask0[:], 0.0), writes=["amask0"])
    p.op("gpsimd", lambda e: e.affine_select(out=c.amask0[:], in_=c.amask0[:], pattern=[[-1, 128]], compare_op=ALU.is_ge, fill=-30000.0,
                                              base=64, channel_multiplier=1), reads=["amask0"], writes=["amask0"])


def phase_c(p, c, T, qkrope, qkrest, vTb, att):
    for _ in phase_c_gen(p, c, T, qkrope, qkrest, vTb, att):
        pass


def phase_c_gen(p, c, T, qkrope, qkrest, vTb, att, es_ext=None, shared=False):
    nc = p.nc
    NT = T // 128
    with ExitStack() as es_own:
        es = es_ext if es_ext is not None else es_own
        def sb(name, shape, dt=F32):
            return es.enter_context(nc.sbuf_tensor(f"C_{_uid()}_" + name, shape, dt))
        qT = [sb(f"qT{i}", [64, T], BF16) for i in range(2)]
        kT = [sb(f"kT{i}", [64, T], BF16) for i in range(2)]
        vT = [sb(f"vT{i}", [64, T], BF16) for i in range(2)]
        vtk = [sb(f"vtk{i}", [128, NT, 64], BF16) for i in range(2)]
        sm = [sb(f"sm{i}", [128, 256]) for i in range(2)]
        P = [sb(f"P{i}", [128, 256], BF16) for i in range(2)]
        PT = [sb(f"PT{i}", [128, 2, 128], BF16) for i in range(2)]
        stt = [sb(f"stt{i}", [128, 4]) for i in range(2)]
        oo = [sb(f"oo{i}", [128, 65]) for i in range(4)]
        blk = [0]
        for h in range(12):
            hb = h % 2
            r = ATT_DIL[h // 4]
            M = T // r
            NB = M // 128
            p.dma("sync", qT[hb][0:16, :], qkrope[h * 16:(h + 1) * 16, :], writes=[f"qT{hb}"])
            p.dma("sync", qT[hb][16:64, :], qkrest[h * 48:(h + 1) * 48, :], writes=[f"qT{hb}"])
            p.dma("sync", kT[hb][0:16, :], qkrope[(12 + h) * 16:(13 + h) * 16, :], writes=[f"kT{hb}"])
            p.dma("sync", kT[hb][16:64, :], qkrest[(12 + h) * 48:(13 + h) * 48, :], writes=[f"kT{hb}"])
            p.dma("sync", vT[hb][:, :], vTb[h * 64:(h + 1) * 64, :], writes=[f"vT{hb}"])
            for g0 in range(0, NT, 16):
                bi = 4 if shared else 6 + (g0 // 16) % 2
                pv = c.ps[bi][:, :].bitcast(BF16)
                for kb in range(g0, min(NT, g0 + 16)):
                    s, mb = divmod(kb, NB)
                    c0 = s + r * mb * 128
                    src = vT[hb][:, c0: c0 + r * 127 + 1: r]
                    p.op("tensor", lambda e: e.transpose(out=pv[:, (kb - g0) * 64:(kb - g0 + 1) * 64], in_=src, identity=c.identb[0:64, 0:64]),
                         reads=[f"vT{hb}", "identb"], writes=[f"ps{bi}"])
                n_ = min(NT, g0 + 16) - g0
                p.op("scalar", lambda e: e.activation(out=vtk[hb][:, g0:g0 + n_, :], in_=pv[:, 0:n_ * 64].rearrange("p (k d) -> p k d", d=64), func=AF.Copy),
                     writes=[f"ps{bi}", f"vtk{hb}"])
            for s in range(r):
                for jb in range(NB + 1):
                    n = blk[0]
                    blk[0] += 1
                    b2 = n % 2
                    m0 = jb * 128 - 64
                    qlo = max(m0, 0)
                    qhi = min(m0 + 128, M)
                    nq = qhi - qlo
                    kblocks = [x for x in (jb - 1, jb) if 0 <= x < NB]
                    nk = 128 * len(kblocks)
                    klo = kblocks[0] * 128
                    if jb == 0:
                        mask = c.amask0[0:nq, 0:nk]
                        mk = "amask0"
                    else:
                        coff = 0 if kblocks[0] == jb - 1 else 128
                        mask = c.amask[0:nq, coff:coff + nk]
                        mk = "amask"
                    qcols = qT[hb][:, s + r * qlo: s + r * (qhi - 1) + 1: r]
                    kcols = kT[hb][:, s + r * klo: s + r * (klo + nk - 1) + 1: r]
                    if shared:
                        bS, bP, bO = c.ps[b2], c.ps[2], c.ps[3]
                        bSk, bPk, bOk = f"ps{b2}", "ps2", "ps3"
                    else:
                        bS, bP, bO = c.ps[b2], c.ps[2 + b2], c.ps[4 + b2]
                        bSk, bPk, bOk = f"ps{b2}", f"ps{2 + b2}", f"ps{4 + b2}"
                    p.op("tensor", lambda e: e.matmul(bS[0:nq, 0:nk], lhsT=qcols, rhs=kcols, start=True, stop=True),
                         reads=[f"qT{hb}", f"kT{hb}"], writes=[bSk])
                    p.op("vector", lambda e: e.scalar_tensor_tensor(out=sm[b2][0:nq, 0:nk], in0=bS[0:nq, 0:nk], scalar=0.125, in1=mask,
                                                                    op0=ALU.mult, op1=ALU.add), reads=[mk], writes=[bSk, f"sm{b2}"])
                    st_ = stt[b2]
                    p.op("vector", lambda e: e.reduce_max(out=st_[0:nq, 1:2], in_=sm[b2][0:nq, 0:nk], axis=AX.X, negate=True), reads=[f"sm{b2}"], writes=[f"stt{b2}b"])
                    p.op("scalar", lambda e: e.activation(out=P[b2][0:nq, 0:nk], in_=sm[b2][0:nq, 0:nk], func=AF.Exp, bias=st_[0:nq, 1:2], scale=1.0,
                                                          accum_out=st_[0:nq, 2:3]), reads=[f"sm{b2}", f"stt{b2}b"], writes=[f"P{b2}", f"stt{b2}c"])
                    pt = bP[:, :].bitcast(BF16)
                    for ci in range(len(kblocks)):
                        p.op("tensor", lambda e: e.transpose(out=pt[:, ci * 128: ci * 128 + nq], in_=P[b2][0:nq, ci * 128:(ci + 1) * 128], identity=c.identb[0:nq, 0:nq]),
                             reads=[f"P{b2}", "identb"], writes=[bPk])
                    for ci in range(len(kblocks)):
                        if ci == 0:
                            p.op("scalar", lambda e: e.activation(out=PT[b2][:, ci, 0:nq], in_=pt[:, ci * 128: ci * 128 + nq], func=AF.Copy), writes=[bPk, f"PT{b2}_{ci}"])
                        else:
                            p.op("vector", lambda e: e.tensor_copy(out=PT[b2][:, ci, 0:nq], in_=pt[:, ci * 128: ci * 128 + nq]), writes=[bPk, f"PT{b2}_{ci}"])
                    for ci, kbk in enumerate(kblocks):
                        p.op("tensor", lambda e: e.matmul(bO[0:nq, 0:64], lhsT=PT[b2][:, ci, 0:nq], rhs=vtk[hb][:, s * NB + kbk, :],
                                                          start=(ci == 0), stop=(ci == len(kblocks) - 1)),
                             reads=[f"PT{b2}_{ci}", f"vtk{hb}"], writes=[bOk])
                    p.op("vector", lambda e: e.reciprocal(out=st_[0:nq, 3:4], in_=st_[0:nq, 2:3]), reads=[f"stt{b2}c"], writes=[f"stt{b2}d"])
                    o4 = oo[n % 4]
                    ok = f"oo{n % 4}"
                    p.op("scalar", lambda e: e.activation(out=o4[0:nq, 64:65], in_=st_[0:nq, 2:3], func=AF.Ln), reads=[f"stt{b2}c"], writes=[ok])
                    p.op("gpsimd", lambda e: e.tensor_tensor(out=o4[0:nq, 64:65], in0=o4[0:nq, 64:65], in1=st_[0:nq, 1:2], op=ALU.subtract),
                         reads=[ok, f"stt{b2}b"], writes=[ok])
                    p.op("vector", lambda e: e.tensor_scalar(out=o4[0:nq, 0:64], in0=bO[0:nq, 0:64], scalar1=st_[0:nq, 3:4], scalar2=None, op0=ALU.mult),
                         reads=[f"stt{b2}d"], writes=[bOk, ok + "o"])
                    t0 = s + r * qlo
                    dst = att[t0: t0 + r * (nq - 1) + 1: r, h, :]
                    p.dma("sync", dst, o4[0:nq, :], reads=[ok, ok + "o"])
                    yield
        if not shared:
            p.barrier()


def phase_d(p, c, T, yaT, att, gatesT, xres, w_a, w_b, w_out, norm_w2, w_router, h2b, aff, x_in=None):
    if x_in is None:
        x_in = xres
    nc = p.nc
    NS = T // 512
    with ExitStack() as es:
        def sb(name, shape, dt=F32):
            return es.enter_context(nc.sbuf_tensor(f"D_{_uid()}_" + name, shape, dt))
        Wa = sb("Wa", [128, 4, 1024], BF16)
        Wb = sb("Wb", [128, 2, 1024], BF16)
        Wo = sb("Wo", [128, 8, 1024], BF16)
        Wr = sb("Wr", [128, 8, 16])
        stg = [sb(f"stg{i}", [128, 1024]) for i in range(2)]
        nwb = sb("nwb", [128, 1024])
        at = [sb(f"at{i}", [128, 12, 65]) for i in range(2)]
        wl_2 = [sb(f"wl{i}", [128, 12]) for i in range(2)]
        mx_2 = [sb(f"mx{i}", [128, 4]) for i in range(2)]
        sm_2 = [sb(f"sm{i}", [128, 4]) for i in range(2)]
        tmpw_2 = [sb(f"tmpw{i}", [128, 12, 64]) for i in range(2)]
        ybt_2 = [sb(f"ybt{i}", [128, 256]) for i in range(2)]
        ybb_2 = [sb(f"ybb{i}", [128, 256], BF16) for i in range(2)]
        ybT = [sb(f"ybT{i}", [128, 2, 512], BF16) for i in range(2)]
        yaTs = [sb(f"yaTs{i}", [128, 4, 512], BF16) for i in range(2)]
        gts = [sb(f"gts{i}", [128, 16, 512], BF16) for i in range(2)]
        t1_2 = [sb(f"t1{i}", [128, 512]) for i in range(2)]
        t2_2 = [sb(f"t2{i}", [128, 512]) for i in range(2)]
        mixT = [sb(f"mixT{i}", [128, 8, 512], BF16) for i in range(2)]
        xt = [sb(f"xt{i}", [128, 1024]) for i in range(2)]
        junk_2 = [sb(f"junk{i}", [128, 1024], BF16) for i in range(2)]
        h2_2 = [sb(f"h2{i}", [128, 1024]) for i in range(2)]
        h2bf = [sb(f"h2bf{i}", [128, 1024], BF16) for i in range(2)]
        h2T_2 = [sb(f"h2T{i}", [128, 2, 8, 128], BF16) for i in range(2)]
        h2lo_2 = [sb(f"h2lo{i}", [128, 1024], BF16) for i in range(2)]
        Wrb = sb("Wrb", [128, 8, 32], BF16)
        Wrt = sb("Wrt", [128, 8, 16])
        lg48_2 = [sb(f"lg48{i}", [128, 48]) for i in range(2)]
        h2Tf_2 = [sb(f"h2Tf{i}", [128, 8, 128]) for i in range(2)] if ROUTER_F32 else [None, None]
        st_2 = [sb(f"st{i}", [128, 8]) for i in range(2)]
        lg_2 = [sb(f"lg{i}", [128, 16]) for i in range(2)]
        af = [sb(f"af{i}", [128, 16]) for i in range(2)]

        p.dma("sync", nwb[:], norm_w2.to_broadcast([128, 1024]), writes=["nwb"])
        p.dma("sync", Wr[:, :, :], w_router.rearrange("(k p) e -> p k e", p=128), writes=["Wr"])
        p.op("vector", lambda e: e.tensor_copy(out=Wrb[:, :, 0:16], in_=Wr[:, :, :]), reads=["Wr"], writes=["Wrb"])
        p.op("vector", lambda e: e.tensor_tensor(out=Wrt[:, :, :], in0=Wr[:, :, :], in1=Wrb[:, :, 0:16], op=ALU.subtract), reads=["Wr", "Wrb"], writes=["Wrt"])
        p.op("vector", lambda e: e.tensor_copy(out=Wrb[:, :, 16:32], in_=Wrt[:, :, :]), reads=["Wrt"], writes=["Wrb"])
        si = 0
        for (W, src, nk, key) in ((Wa, w_a, 4, "Wa"), (Wb, w_b, 2, "Wb"), (Wo, w_out, 8, "Wo")):
            for k in range(nk):
                s_, sk = stg[si % 2], f"stg{si % 2}"
                si += 1
                p.dma("sync", s_[:], src[k * 128:(k + 1) * 128, :], writes=[sk])
                p.op("gpsimd", lambda e: e.tensor_copy(out=W[:, k, :], in_=s_[:]), reads=[sk], writes=[key])
        bn = [0]

        def bank():
            i = bn[0] % 6
            bn[0] += 1
            return c.ps[i], f"ps{i}"

        for s in range(NS):
            sl = s % 2
            c0 = s * 512
            for k_ in range(4):
                p.dma("sync", yaTs[sl][:, k_, :], yaT[k_ * 128:(k_ + 1) * 128, c0:c0 + 512], writes=[f"yaTs{sl}"])
            for k_ in range(16):
                p.dma("sync", gts[sl][:, k_, :], gatesT[k_ * 128:(k_ + 1) * 128, c0:c0 + 512], writes=[f"gts{sl}"])
            for ti in range(4):
                r0 = c0 + ti * 128
                a_, ak = at[ti % 2], f"at{ti % 2}"
                pq = ti % 2
                wl, mx, sm, tmpw, ybt, ybb = wl_2[pq], mx_2[pq], sm_2[pq], tmpw_2[pq], ybt_2[pq], ybb_2[pq]
                p.dma("sync", a_[:, :, :], att[r0:r0 + 128, :, :], writes=[ak])
                L = a_[:, :, 64]
                p.op("vector", lambda e: e.tensor_tensor(out=mx[:], in0=L[:, 0:4], in1=L[:, 4:8], op=ALU.max), reads=[ak], writes=[f"mx_{pq}"])
                p.op("vector", lambda e: e.tensor_tensor(out=mx[:], in0=mx[:], in1=L[:, 8:12], op=ALU.max), reads=[ak, f"mx_{pq}"], writes=[f"mx_{pq}"])
                p.op("vector", lambda e: e.tensor_tensor(out=wl[:, :].rearrange("p (g h) -> p g h", h=4), in0=L.rearrange("p (g h) -> p g h", h=4),
                                                         in1=mx[:, :].unsqueeze(1).to_broadcast([128, 3, 4]), op=ALU.subtract), reads=[ak, f"mx_{pq}"], writes=[f"wl_{pq}"])
                p.op("scalar", lambda e: e.activation(out=wl[:], in_=wl[:], func=AF.Exp), reads=[f"wl_{pq}"], writes=[f"wl_{pq}"])
                p.op("vector", lambda e: e.tensor_tensor(out=sm[:], in0=wl[:, 0:4], in1=wl[:, 4:8], op=ALU.add), reads=[f"wl_{pq}"], writes=[f"sm_{pq}"])
                p.op("vector", lambda e: e.tensor_tensor(out=sm[:], in0=sm[:], in1=wl[:, 8:12], op=ALU.add), reads=[f"wl_{pq}", f"sm_{pq}"], writes=[f"sm_{pq}"])
                p.op("vector", lambda e: e.reciprocal(out=sm[:], in_=sm[:]), reads=[f"sm_{pq}"], writes=[f"sm_{pq}"])
                p.op("vector", lambda e: e.tensor_tensor(out=wl[:, :].rearrange("p (g h) -> p g h", h=4), in0=wl[:, :].rearrange("p (g h) -> p g h", h=4),
                                                         in1=sm[:, :].unsqueeze(1).to_broadcast([128, 3, 4]), op=ALU.mult), reads=[f"wl_{pq}", f"sm_{pq}"], writes=[f"wl_{pq}"])
                p.op("vector", lambda e: e.tensor_tensor(out=tmpw[:, :, :], in0=a_[:, :, 0:64], in1=wl[:, :].unsqueeze(2).to_broadcast([128, 12, 64]), op=ALU.mult),
                     reads=[ak, f"wl_{pq}"], writes=[f"tmpw_{pq}"])
                tv = tmpw[:, :, :].rearrange("p (g h) d -> p g (h d)", h=4)
                p.op("gpsimd", lambda e: e.tensor_tensor(out=ybt[:], in0=tv[:, 0, :], in1=tv[:, 1, :], op=ALU.add), reads=[f"tmpw_{pq}"], writes=[f"ybt_{pq}"])
                p.op("gpsimd", lambda e: e.tensor_tensor(out=ybb[:], in0=ybt[:], in1=tv[:, 2, :], op=ALU.add), reads=[f"tmpw_{pq}", f"ybt_{pq}"], writes=[f"ybb_{pq}"])
                for k in range(2):
                    p.op("tensor", lambda e: e.transpose(out=c.psb[:, k * 128:(k + 1) * 128], in_=ybb[:, k * 128:(k + 1) * 128], identity=c.identb[:]),
                         reads=[f"ybb_{pq}", "identb"], writes=["ps7"])
                p.op("scalar", lambda e: e.activation(out=ybT[sl][:, :, ti * 128:(ti + 1) * 128], in_=c.psb[:, 0:256].rearrange("p (k t) -> p k t", t=128), func=AF.Copy),
                     writes=["ps7", f"ybT{sl}"])
            for dm in range(8):
                t1, t2 = t1_2[dm % 2], t2_2[dm % 2]
                bA, bAk = bank()
                bB, bBk = bank()
                for k in range(4):
                    p.op("tensor", lambda e: e.matmul(bA[:, :], lhsT=Wa[:, k, dm * 128:(dm + 1) * 128], rhs=yaTs[sl][:, k, :], start=(k == 0), stop=(k == 3)),
                         reads=["Wa", f"yaTs{sl}"], writes=[bAk])
                for k in range(2):
                    p.op("tensor", lambda e: e.matmul(bB[:, :], lhsT=Wb[:, k, dm * 128:(dm + 1) * 128], rhs=ybT[sl][:, k, :], start=(k == 0), stop=(k == 1)),
                         reads=["Wb", f"ybT{sl}"], writes=[bBk])
                p.op("vector", lambda e: e.tensor_tensor(out=t1[:], in0=bA[:, :], in1=gts[sl][:, dm, :], op=ALU.mult), reads=[f"gts{sl}"], writes=[bAk, f"t1_{dm % 2}"])
                p.op("vector", lambda e: e.tensor_tensor(out=t2[:], in0=bB[:, :], in1=gts[sl][:, 8 + dm, :], op=ALU.mult), reads=[f"gts{sl}"], writes=[bBk, f"t2_{dm % 2}"])
                p.op("gpsimd", lambda e: e.tensor_tensor(out=mixT[sl][:, dm, :], in0=t1[:], in1=t2[:], op=ALU.add), reads=[f"t1_{dm % 2}", f"t2_{dm % 2}"], writes=[f"mixT{sl}"])
            for ti in range(4):
                r0 = c0 + ti * 128
                x_, xk = xt[ti % 2], f"xt{ti % 2}"
                pq = ti % 2
                junk, h2, h2T, h2lo, st, lg, lg48 = junk_2[pq], h2_2[pq], h2T_2[pq], h2lo_2[pq], st_2[pq], lg_2[pq], lg48_2[pq]
                p.dma("sync", x_[:], x_in[r0:r0 + 128, :], writes=[xk])
                for hf in range(2):
                    bX, bXk = bank()
                    for k in range(8):
                        p.op("tensor", lambda e: e.matmul(bX[:, :], lhsT=mixT[sl][:, k, ti * 128:(ti + 1) * 128], rhs=Wo[:, k, hf * 512:(hf + 1) * 512],
                                                          start=(k == 0), stop=(k == 7)), reads=["Wo", f"mixT{sl}"], writes=[bXk])
                    p.op("vector", lambda e: e.tensor_tensor(out=x_[:, hf * 512:(hf + 1) * 512], in0=bX[:, :], in1=x_[:, hf * 512:(hf + 1) * 512], op=ALU.add),
                         reads=[xk], writes=[bXk, xk])
                p.dma("sync", xres[r0:r0 + 128, :], x_[:], reads=[xk])
                p.op("scalar", lambda e: e.activation(out=junk[:], in_=x_[:], func=AF.Square, accum_out=st[:, 0:1]), reads=[xk], writes=[f"junk_{pq}", f"st0_{pq}"])
                p.op("vector", lambda e: e.tensor_scalar(out=st[:, 1:2], in0=st[:, 0:1], scalar1=1.0 / 1024, scalar2=RMS_EPS, op0=ALU.mult, op1=ALU.add),
                     reads=[f"st0_{pq}"], writes=[f"st1_{pq}"])
                p.op("scalar", lambda e: e.activation(out=st[:, 2:3], in_=st[:, 1:2], func=AF.Sqrt), reads=[f"st1_{pq}"], writes=[f"st2_{pq}"])
                p.op("vector", lambda e: e.reciprocal(out=st[:, 3:4], in_=st[:, 2:3]), reads=[f"st2_{pq}"], writes=[f"st3_{pq}"])
                p.op("vector", lambda e: e.scalar_tensor_tensor(out=h2[:], in0=x_[:], scalar=st[:, 3:4], in1=nwb[:], op0=ALU.mult, op1=ALU.mult),
                     reads=[xk, f"st3_{pq}", "nwb"], writes=[f"h2_{pq}"])
                hb_, hbk = h2bf[ti % 2], f"h2bf{ti % 2}"
                p.op("gpsimd", lambda e: e.tensor_copy(out=hb_[:], in_=h2[:]), reads=[f"h2_{pq}"], writes=[hbk])
                p.dma("sync", h2b[r0:r0 + 128, :], hb_[:], reads=[hbk])
                if ROUTER_F32:
                    h2Tf = h2Tf_2[pq]
                    for hf in range(2):
                        bT, bTk = bank()
                        for k in range(4):
                            kk = hf * 4 + k
                            p.op("tensor", lambda e: e.transpose(out=bT[:, k * 128:(k + 1) * 128], in_=h2[:, kk * 128:(kk + 1) * 128], identity=c.identf[:]),
                                 reads=[f"h2_{pq}", "identf"], writes=[bTk])
                        p.op("scalar", lambda e: e.activation(out=h2Tf[:, hf * 4:(hf + 1) * 4, :], in_=bT[:, :].rearrange("p (k t) -> p k t", t=128), func=AF.Copy),
                             writes=[bTk, f"h2Tf{hf}_{pq}"])
                    bL, bLk = bank()
                    for k in range(8):
                        p.op("tensor", lambda e: e.matmul(bL[:, 0:16], lhsT=h2Tf[:, k, :], rhs=Wr[:, k, :], start=(k == 0), stop=(k == 7)),
                             reads=[f"h2Tf0_{pq}", f"h2Tf1_{pq}", "Wr"], writes=[bLk])
                    p.op("vector", lambda e: e.tensor_copy(out=lg[:], in_=bL[:, 0:16]), writes=[bLk, f"lg_{pq}"])
                else:
                    p.op("gpsimd", lambda e: e.tensor_tensor(out=h2lo[:], in0=h2[:], in1=hb_[:], op=ALU.subtract), reads=[f"h2_{pq}", hbk], writes=[f"h2lo_{pq}"])
                    for part, (src_, srck_) in enumerate(((hb_, hbk), (h2lo, f"h2lo_{pq}"))):
                        bT, bTk = bank()
                        tpv = bT[:, :].bitcast(BF16)
                        for k in range(8):
                            p.op("tensor", lambda e: e.transpose(out=tpv[:, k * 128:(k + 1) * 128], in_=src_[:, k * 128:(k + 1) * 128], identity=c.identb[:]),
                                 reads=[srck_, "identb"], writes=[bTk])
                        if part == 0:
                            p.op("scalar", lambda e: e.activation(out=h2T[:, part, :, :], in_=tpv[:, :].rearrange("p (k t) -> p k t", t=128), func=AF.Copy),
                                 writes=[bTk, f"h2T{part}_{pq}"])
                        else:
                            p.op("vector", lambda e: e.tensor_copy(out=h2T[:, part, :, :], in_=tpv[:, :].rearrange("p (k t) -> p k t", t=128)),
                                 writes=[bTk, f"h2T{part}_{pq}"])
                    bL, bLk = bank()
                    for k in range(8):
                        p.op("tensor", lambda e: e.matmul(bL[:, 0:32], lhsT=h2T[:, 0, k, :], rhs=Wrb[:, k, :], start=(k == 0), stop=(k == 7)),
                             reads=[f"h2T0_{pq}", "Wrb"], writes=[bLk])
                    for k in range(8):
                        p.op("tensor", lambda e: e.matmul(bL[:, 32:48], lhsT=h2T[:, 1, k, :], rhs=Wrb[:, k, 0:16], start=(k == 0), stop=(k == 7)),
                             reads=[f"h2T1_{pq}", "Wrb"], writes=[bLk])
                    p.op("vector", lambda e: e.tensor_copy(out=lg48[:], in_=bL[:, 0:48]), writes=[bLk, f"lg48_{pq}"])
                    p.op("vector", lambda e: e.tensor_tensor(out=lg[:], in0=lg48[:, 0:16], in1=lg48[:, 16:32], op=ALU.add), reads=[f"lg48_{pq}"], writes=[f"lg_{pq}"])
                    p.op("vector", lambda e: e.tensor_tensor(out=lg[:], in0=lg[:], in1=lg48[:, 32:48], op=ALU.add), reads=[f"lg_{pq}", f"lg48_{pq}"], writes=[f"lg_{pq}"])
                p.op("vector", lambda e: e.reduce_max(out=st[:, 4:5], in_=lg[:], axis=AX.X), reads=[f"lg_{pq}"], writes=[f"st4_{pq}"])
                p.op("vector", lambda e: e.tensor_scalar(out=st[:, 5:6], in0=st[:, 4:5], scalar1=-1.0, scalar2=None, op0=ALU.mult), reads=[f"st4_{pq}"], writes=[f"st5_{pq}"])
                p.op("scalar", lambda e: e.activation(out=lg[:], in_=lg[:], func=AF.Exp, bias=st[:, 5:6], scale=1.0, accum_out=st[:, 6:7]),
                     reads=[f"lg_{pq}", f"st5_{pq}"], writes=[f"lg_{pq}", f"st6_{pq}"])
                p.op("vector", lambda e: e.reciprocal(out=st[:, 7:8], in_=st[:, 6:7]), reads=[f"st6_{pq}"], writes=[f"st7_{pq}"])
                a2, a2k = af[ti % 2], f"af{ti % 2}"
                p.op("vector", lambda e: e.tensor_scalar(out=a2[:], in0=lg[:], scalar1=st[:, 7:8], scalar2=None, op0=ALU.mult), reads=[f"lg_{pq}", f"st7_{pq}"], writes=[a2k])
                p.dma("sync", aff[r0:r0 + 128, :], a2[:], reads=[a2k])
        p.barrier()


def phase_e(p, c, T, aff, h2b, xres, w_gate, w_up, w_down, n_iter=34):
    nc = p.nc
    NT = T // 128
    C = T // 8
    NG = C // 128
    NE = 16
    with ExitStack() as es0:
        def sb0(name, shape, dt=F32):
            return es0.enter_context(nc.sbuf_tensor(f"E_{_uid()}_" + name, shape, dt))
        idx_all = sb0("idx_all", [128, NE, NG], I32)
        gate_all = sb0("gate_all", [128, NE, NG])
        with ExitStack() as es:
            def sb(name, shape, dt=F32):
                return es.enter_context(nc.sbuf_tensor(f"E1_{_uid()}_" + name, shape, dt))
            A = sb("A", [128, NT, NE])
            cmp = sb("cmp", [128, NT, NE])
            lo = sb("lo", [128, NE])
            hi = sb("hi", [128, NE])
            mid = sb("mid", [128, NE])
            cnt = sb("cnt", [128, NE])
            sel = sb("sel", [128, NE])
            d1 = sb("d1", [128, NE])
            triS = sb("triS", [128, 128])
            onesT = sb("onesT", [128, NT])
            rk = sb("rk", [128, NT, NE])
            nn = sb("nn", [128, NT, NE])
            base = sb("base", [128, NT, NE])
            pos = sb("pos", [128, NT, NE])
            posi = sb("posi", [128, NT, NE], I32)
            ai = sb("ai", [128, NT, NE], I32)
            bi_ = sb("bi", [128, NT, NE], I32)
            af_ = sb("af", [128, NT, NE])
            bf_ = sb("bf", [128, NT, NE])
            h1 = sb("h1", [128, NT, NE], BF16)
            h2 = sb("h2", [128, NT, NE], BF16)
            h3 = sb("h3", [128, NT, NE], BF16)
            r1 = sb("r1", [128, NT, NE])
            r2 = sb("r2", [128, NT, NE])
            iotaA = sb("iotaA", [128, 128])
            iotaB = sb("iotaB", [128, NG])
            tval = sb("tval", [128, NT], BF16)
            pval = sb("pval", [128, 1])
            pvalb = sb("pvalb", [128, NT], BF16)
            Aoh = [sb(f"Aoh{i}", [128, NT, 128], BF16) for i in range(2)]
            Boh = [sb(f"Boh{i}", [128, NT, NG]) for i in range(2)]
            rhs = [sb(f"rhs{i}", [128, NT, 5, NG], BF16) for i in range(2)]
            res = sb("res", [128, 5, NG])

            for t0 in range(0, NT, 8):
                t1 = min(NT, t0 + 8)
                p.dma("sync", A[:, t0:t1, :], aff[t0 * 128:t1 * 128, :].rearrange("(t p) e -> p t e", p=128), writes=["A"])
            p.op("gpsimd", lambda e: e.memset(lo[:], 0.0), writes=["lo"])
            p.op("gpsimd", lambda e: e.memset(hi[:], 1.0001), writes=["hi"])
            p.op("gpsimd", lambda e: e.memset(onesT[:], 1.0), writes=["onesT"])
            p.op("gpsimd", lambda e: e.memset(triS[:], 1.0), writes=["triS"])
            p.op("gpsimd", lambda e: e.affine_select(out=triS[:], in_=triS[:], pattern=[[1, 128]], compare_op=ALU.is_gt, fill=0.0, base=0, channel_multiplier=-1),
                 reads=["triS"], writes=["triS"])
            p.op("gpsimd", lambda e: e.iota(iotaA[:], pattern=[[1, 128]], base=0, channel_multiplier=0, allow_small_or_imprecise_dtypes=True), writes=["iotaA"])
            p.op("gpsimd", lambda e: e.iota(iotaB[:], pattern=[[1, NG]], base=0, channel_multiplier=0, allow_small_or_imprecise_dtypes=True), writes=["iotaB"])
            p.op("gpsimd", lambda e: e.iota(tval[:], pattern=[[1, NT]], base=0, channel_multiplier=0, allow_small_or_imprecise_dtypes=True), writes=["tval"])
            p.op("gpsimd", lambda e: e.iota(pval[:], pattern=[[0, 1]], base=0, channel_multiplier=1, allow_small_or_imprecise_dtypes=True), writes=["pval"])
            p.op("vector", lambda e: e.tensor_copy(out=pvalb[:], in_=pval[:, 0:1].to_broadcast([128, NT])), reads=["pval"], writes=["pvalb"])
            for it in range(n_iter):
                p.op("vector", lambda e: e.tensor_tensor(out=mid[:], in0=lo[:], in1=hi[:], op=ALU.add), reads=["lo", "hi"], writes=["mid"])
                p.op("vector", lambda e: e.tensor_scalar(out=mid[:], in0=mid[:], scalar1=0.5, scalar2=None, op0=ALU.mult), reads=["mid"], writes=["mid"])
                p.op("vector", lambda e: e.tensor_tensor(out=cmp[:, :, :], in0=A[:, :, :], in1=mid[:, :].unsqueeze(1).to_broadcast([128, NT, NE]), op=ALU.is_ge),
                     reads=["A", "mid"], writes=["cmp"])
                p.op("vector", lambda e: e.tensor_reduce(out=cnt[:], in_=cmp[:, :, :].rearrange("p t e -> p e t"), axis=AX.X, op=ALU.add), reads=["cmp"], writes=["cnt"])
                p.op("tensor", lambda e: e.matmul(c.ps[0][:, 0:NE], lhsT=c.onesf[:], rhs=cnt[:], start=True, stop=True), reads=["cnt", "onesf"], writes=["ps0"])
                p.op("vector", lambda e: e.tensor_scalar(out=sel[:], in0=c.ps[0][:, 0:NE], scalar1=float(C) - 0.5, scalar2=None, op0=ALU.is_ge), writes=["ps0", "sel"])
                p.op("vector", lambda e: e.tensor_tensor(out=d1[:], in0=mid[:], in1=lo[:], op=ALU.subtract), reads=["mid", "lo"], writes=["d1"])
                p.op("vector", lambda e: e.tensor_tensor(out=d1[:], in0=d1[:], in1=sel[:], op=ALU.mult), reads=["d1", "sel"], writes=["d1"])
                p.op("vector", lambda e: e.tensor_tensor(out=lo[:], in0=lo[:], in1=d1[:], op=ALU.add), reads=["d1", "lo"], writes=["lo"])
                p.op("vector", lambda e: e.tensor_tensor(out=d1[:], in0=hi[:], in1=mid[:], op=ALU.subtract), reads=["mid", "hi"], writes=["d1"])
                p.op("vector", lambda e: e.tensor_tensor(out=d1[:], in0=d1[:], in1=sel[:], op=ALU.mult), reads=["d1", "sel"], writes=["d1"])
                p.op("vector", lambda e: e.tensor_tensor(out=hi[:], in0=mid[:], in1=d1[:], op=ALU.add), reads=["d1", "mid"], writes=["hi"])
            p.op("vector", lambda e: e.tensor_tensor(out=cmp[:, :, :], in0=A[:, :, :], in1=lo[:, :].unsqueeze(1).to_broadcast([128, NT, NE]), op=ALU.is_ge),
                 reads=["A", "lo"], writes=["cmp"])
            cf = cmp[:, :, :].rearrange("p t e -> p (t e)")
            NC_ = NT * NE
            for c0 in range(0, NC_, 512):
                c1 = min(NC_, c0 + 512)
                p.op("tensor", lambda e: e.matmul(c.ps[1][:, 0:c1 - c0], lhsT=triS[:], rhs=cf[:, c0:c1], start=True, stop=True), reads=["cmp", "triS"], writes=["ps1"])
                p.op("vector", lambda e: e.tensor_copy(out=rk[:, :, :].rearrange("p t e -> p (t e)")[:, c0:c1], in_=c.ps[1][:, 0:c1 - c0]), writes=["ps1", "rk"])
                p.op("tensor", lambda e: e.matmul(c.ps[2][:, 0:c1 - c0], lhsT=c.onesf[:], rhs=cf[:, c0:c1], start=True, stop=True), reads=["cmp", "onesf"], writes=["ps2"])
                p.op("vector", lambda e: e.tensor_copy(out=nn[:, :, :].rearrange("p t e -> p (t e)")[:, c0:c1], in_=c.ps[2][:, 0:c1 - c0]), writes=["ps2", "nn"])
            for ex in range(NE):
                p.op("vector", lambda e: e.tensor_tensor_scan(out=base[:, :, ex], data0=onesT[:, :], data1=nn[:, :, ex], initial=0.0, op0=ALU.mult, op1=ALU.add),
                     reads=["nn", "onesT"], writes=["base"])
            p.op("vector", lambda e: e.tensor_tensor(out=pos[:, :, :], in0=base[:, :, :], in1=nn[:, :, :], op=ALU.subtract), reads=["base", "nn"], writes=["pos"])
            p.op("vector", lambda e: e.tensor_tensor(out=pos[:, :, :], in0=pos[:, :, :], in1=rk[:, :, :], op=ALU.add), reads=["pos", "rk"], writes=["pos"])
            p.op("vector", lambda e: e.tensor_copy(out=posi[:, :, :], in_=pos[:, :, :]), reads=["pos"], writes=["posi"])
            p.op("vector", lambda e: e.tensor_single_scalar(out=ai[:, :, :], in_=posi[:, :, :], scalar=(NG.bit_length() - 1), op=ALU.arith_shift_right), reads=["posi"], writes=["ai"])
            p.op("vector", lambda e: e.tensor_single_scalar(out=bi_[:, :, :], in_=posi[:, :, :], scalar=NG - 1, op=ALU.bitwise_and), reads=["posi"], writes=["bi"])
            p.op("vector", lambda e: e.tensor_copy(out=af_[:, :, :], in_=ai[:, :, :]), reads=["ai"], writes=["af"])
            p.op("vector", lambda e: e.tensor_copy(out=bf_[:, :, :], in_=bi_[:, :, :]), reads=["bi"], writes=["bf"])
            p.op("vector", lambda e: e.scalar_tensor_tensor(out=af_[:, :, :], in0=af_[:, :, :], scalar=1.0, in1=cmp[:, :, :], op0=ALU.add, op1=ALU.mult), reads=["af", "cmp"], writes=["af"])
            p.op("vector", lambda e: e.tensor_scalar(out=af_[:, :, :], in0=af_[:, :, :], scalar1=-1.0, scalar2=None, op0=ALU.add), reads=["af"], writes=["af"])
            p.op("vector", lambda e: e.tensor_copy(out=h1[:, :, :], in_=A[:, :, :]), reads=["A"], writes=["h1"])
            p.op("vector", lambda e: e.tensor_tensor(out=r1[:, :, :], in0=A[:, :, :], in1=h1[:, :, :], op=ALU.subtract), reads=["A", "h1"], writes=["r1"])
            p.op("vector", lambda e: e.tensor_copy(out=h2[:, :, :], in_=r1[:, :, :]), reads=["r1"], writes=["h2"])
            p.op("vector", lambda e: e.tensor_tensor(out=r2[:, :, :], in0=r1[:, :, :], in1=h2[:, :, :], op=ALU.subtract), reads=["r1", "h2"], writes=["r2"])
            p.op("vector", lambda e: e.tensor_copy(out=h3[:, :, :], in_=r2[:, :, :]), reads=["r2"], writes=["h3"])
            for ex in range(NE):
                b = ex % 2
                p.op("vector", lambda e: e.tensor_tensor(out=Aoh[b][:, :, :], in0=iotaA[:, :].unsqueeze(1).to_broadcast([128, NT, 128]),
                                                         in1=af_[:, :, ex:ex + 1].to_broadcast([128, NT, 128]), op=ALU.is_equal),
                     reads=["iotaA", "af"], writes=[f"Aoh{b}"])
                p.op("vector", lambda e: e.tensor_tensor(out=Boh[b][:, :, :], in0=iotaB[:, :].unsqueeze(1).to_broadcast([128, NT, NG]),
                                                         in1=bf_[:, :, ex:ex + 1].to_broadcast([128, NT, NG]), op=ALU.is_equal),
                     reads=["iotaB", "bf"], writes=[f"Boh{b}"])
                vals = [tval[:, :].unsqueeze(2).to_broadcast([128, NT, NG]), pvalb[:, :].unsqueeze(2).to_broadcast([128, NT, NG]),
                        h1[:, :, ex:ex + 1].to_broadcast([128, NT, NG]), h2[:, :, ex:ex + 1].to_broadcast([128, NT, NG]),
                        h3[:, :, ex:ex + 1].to_broadcast([128, NT, NG])]
                vkeys = ["tval", "pvalb", "h1", "h2", "h3"]
                for vi in range(5):
                    p.op("gpsimd", lambda e: e.tensor_tensor(out=rhs[b][:, :, vi, :], in0=Boh[b][:, :, :], in1=vals[vi], op=ALU.mult),
                         reads=[f"Boh{b}", vkeys[vi]], writes=[f"rhs{b}"])
                pb = 3 + b
                for t in range(NT):
                    p.op("tensor", lambda e: e.matmul(c.ps[pb][:, 0:5 * NG], lhsT=Aoh[b][:, t, :], rhs=rhs[b][:, t, :, :].rearrange("p v g -> p (v g)"),
                                                      start=(t == 0), stop=(t == NT - 1)), reads=[f"Aoh{b}", f"rhs{b}"], writes=[f"ps{pb}"])
                p.op("vector", lambda e: e.tensor_copy(out=res[:, :, :], in_=c.ps[pb][:, 0:5 * NG].rearrange("p (v g) -> p v g", g=NG)), writes=[f"ps{pb}", "res"])
                p.op("vector", lambda e: e.scalar_tensor_tensor(out=res[:, 0, :], in0=res[:, 0, :], scalar=128.0, in1=res[:, 1, :], op0=ALU.mult, op1=ALU.add),
                     reads=["res"], writes=["res"])
                p.op("vector", lambda e: e.tensor_copy(out=idx_all[:, ex, :], in_=res[:, 0, :]), reads=["res"], writes=["idx_all"])
                p.op("vector", lambda e: e.tensor_tensor(out=res[:, 2, :], in0=res[:, 2, :], in1=res[:, 3, :], op=ALU.add), reads=["res"], writes=["res"])
                p.op("vector", lambda e: e.tensor_tensor(out=gate_all[:, ex, :], in0=res[:, 2, :], in1=res[:, 4, :], op=ALU.add), reads=["res"], writes=["gate_all"])
            p.barrier()
        with ExitStack() as es:
            def sb(name, shape, dt=F32):
                return es.enter_context(nc.sbuf_tensor(f"E2_{_uid()}_" + name, shape, dt))
            NWB = 5
            wbuf = [sb(f"w{i}", [128, 8, 1024], BF16) for i in range(NWB)]
            stg = [sb(f"stg{i}", [128, 2, 1024]) for i in range(2)]
            xes = [sb(f"xe{i}", [128, NG, 1024], BF16) for i in range(2)]
            xeT = sb("xeT", [128, 8, C], BF16)
            hmT = sb("hmT", [128, 8, C], BF16)
            sg = [sb(f"sg{i}", [128, 512]) for i in range(2)]
            yo = [sb(f"yo{i}", [128, 1024]) for i in range(2)]
            wn = [0]
            sn = [0]
            cn = [0]

            def load_w(src):
                i = wn[0] % NWB
                wn[0] += 1
                for k2 in range(4):
                    s_ = stg[sn[0] % 2]
                    sk = f"stg{sn[0] % 2}"
                    sn[0] += 1
                    p.dma("sync", s_[:, :, :], src[k2 * 256:(k2 + 1) * 256, :].rearrange("(k p) f -> p k f", p=128), writes=[sk])
                    eng = ("gpsimd", "gpsimd", "vector", "gpsimd", "scalar", "gpsimd")[cn[0] % 6]
                    cn[0] += 1
                    if eng == "scalar":
                        p.op("scalar", lambda e: e.activation(out=wbuf[i][:, 2 * k2:2 * k2 + 2, :], in_=s_[:, :, :], func=AF.Copy), reads=[sk], writes=[f"w{i}"])
                    else:
                        p.op(eng, lambda e: e.tensor_copy(out=wbuf[i][:, 2 * k2:2 * k2 + 2, :], in_=s_[:, :, :]), reads=[sk], writes=[f"w{i}"])
                return wbuf[i], f"w{i}"

            bn = [0]

            def bank():
                i = bn[0] % 6
                bn[0] += 1
                return c.ps[i], f"ps{i}"

            CH = min(512, C)
            NCH = C // CH
            yn = [0]
            pending = [load_w(w_gate[0]), load_w(w_up[0]), load_w(w_down[0])]
            for ex in range(NE):
                (Wg, Wgk), (Wu, Wuk), (Wd, Wdk) = pending
                xe = xes[ex % 2]
                xek = f"xe{ex % 2}"
                for gi in range(NG):
                    p.dma("gpsimd", None, None, reads=["idx_all"], writes=[xek],
                          fn=lambda e: e.indirect_dma_start(out=xe[:, gi, :], out_offset=None, in_=h2b[:, :],
                                                            in_offset=bass.IndirectOffsetOnAxis(ap=idx_all[:, ex, gi:gi + 1], axis=0)))
                for gi in range(NG):
                    tbk = 6 + gi % 2
                    tps = c.ps[tbk][:, :].bitcast(BF16)
                    for k in range(8):
                        p.op("tensor", lambda e: e.transpose(out=tps[:, k * 128:(k + 1) * 128], in_=xe[:, gi, k * 128:(k + 1) * 128], identity=c.identb[:]),
                             reads=[xek, "identb"], writes=[f"ps{tbk}"])
                    if gi % 2 == 0:
                        p.op("vector", lambda e: e.tensor_copy(out=xeT[:, :, gi * 128:(gi + 1) * 128], in_=tps[:, :].rearrange("p (k t) -> p k t", t=128)),
                             writes=[f"ps{tbk}", "xeT0"])
                    else:
                        p.op("scalar", lambda e: e.activation(out=xeT[:, :, gi * 128:(gi + 1) * 128], in_=tps[:, :].rearrange("p (k t) -> p k t", t=128), func=AF.Copy),
                             writes=[f"ps{tbk}", "xeT1"])
                for fc in range(8):
                    for ch in range(NCH):
                        bG, bGk = bank()
                        bU, bUk = bank()
                        for k in range(8):
                            p.op("tensor", lambda e: e.matmul(bG[:, 0:CH], lhsT=Wg[:, k, fc * 128:(fc + 1) * 128], rhs=xeT[:, k, ch * CH:(ch + 1) * CH],
                                                              start=(k == 0), stop=(k == 7)), reads=[Wgk, "xeT0", "xeT1"], writes=[bGk])
                        for k in range(8):
                            p.op("tensor", lambda e: e.matmul(bU[:, 0:CH], lhsT=Wu[:, k, fc * 128:(fc + 1) * 128], rhs=xeT[:, k, ch * CH:(ch + 1) * CH],
                                                              start=(k == 0), stop=(k == 7)), reads=[Wuk, "xeT0", "xeT1"], writes=[bUk])
                        s_ = sg[(fc * NCH + ch) % 2]
                        sk = f"sg{(fc * NCH + ch) % 2}"
                        p.op("scalar", lambda e: e.activation(out=s_[:, 0:CH], in_=bG[:, 0:CH], func=AF.Silu), writes=[bGk, sk])
                        p.op("vector", lambda e: e.tensor_tensor(out=hmT[:, fc, ch * CH:(ch + 1) * CH], in0=bU[:, 0:CH], in1=s_[:, 0:CH], op=ALU.mult),
                             reads=[sk], writes=[bUk, "hmT"])
                if ex + 1 < NE:
                    nxt = [load_w(w_gate[ex + 1]), load_w(w_up[ex + 1])]
                for cb in range(NG):
                    y_ = yo[yn[0] % 2]
                    yk = f"yo{yn[0] % 2}"
                    yn[0] += 1
                    for hf in range(2):
                        bY, bYk = bank()
                        for f in range(8):
                            p.op("tensor", lambda e: e.matmul(bY[:, :], lhsT=hmT[:, f, cb * 128:(cb + 1) * 128], rhs=Wd[:, f, hf * 512:(hf + 1) * 512],
                                                              start=(f == 0), stop=(f == 7)), reads=[Wdk, "hmT"], writes=[bYk])
                        if hf == 0:
                            p.op("scalar", lambda e: e.activation(out=y_[:, 0:512], in_=bY[:, :], func=AF.Copy, scale=gate_all[:, ex, cb:cb + 1]),
                                 reads=["gate_all"], writes=[bYk, yk])
                        else:
                            p.op("vector", lambda e: e.tensor_scalar(out=y_[:, 512:1024], in0=bY[:, :], scalar1=gate_all[:, ex, cb:cb + 1], scalar2=None, op0=ALU.mult),
                                 reads=["gate_all"], writes=[bYk, yk])
                    p.dma("gpsimd", None, None, reads=[yk, "idx_all"], writes=["xres_scatter"],
                          fn=lambda e: e.indirect_dma_start(out=xres[:, :], out_offset=bass.IndirectOffsetOnAxis(ap=idx_all[:, ex, cb:cb + 1], axis=0),
                                                            in_=y_[:, :], in_offset=None, compute_op=ALU.add))
                if ex + 1 < NE:
                    nxt.append(load_w(w_down[ex + 1]))
                    pending = nxt
            p.barrier()


def phase_f(p, c, T, xres, norm_w, out):
    nc = p.nc
    NT = T // 128
    with ExitStack() as es:
        def sb(name, shape, dt=F32):
            return es.enter_context(nc.sbuf_tensor(f"F_{_uid()}_" + name, shape, dt))
        nwb = sb("nwb", [128, 1024])
        xt = [sb(f"xt{i}", [128, 1024]) for i in range(3)]
        junk = sb("junk", [128, 1024], BF16)
        st = [sb(f"st{i}", [128, 4]) for i in range(2)]
        p.dma("sync", nwb[:], norm_w.to_broadcast([128, 1024]), writes=["nwb"])
        for i in range(NT):
            x_, xk = xt[i % 3], f"xt{i % 3}"
            s_, sk = st[i % 2], f"st{i % 2}"
            p.dma("sync", x_[:], xres[i * 128:(i + 1) * 128, :], writes=[xk])
            p.op("scalar", lambda e: e.activation(out=junk[:], in_=x_[:], func=AF.Square, accum_out=s_[:, 0:1]), reads=[xk], writes=["junk", sk + "a"])
            p.op("vector", lambda e: e.tensor_scalar(out=s_[:, 1:2], in0=s_[:, 0:1], scalar1=1.0 / 1024, scalar2=RMS_EPS, op0=ALU.mult, op1=ALU.add),
                 reads=[sk + "a"], writes=[sk + "b"])
            p.op("scalar", lambda e: e.activation(out=s_[:, 2:3], in_=s_[:, 1:2], func=AF.Sqrt), reads=[sk + "b"], writes=[sk + "c"])
            p.op("vector", lambda e: e.reciprocal(out=s_[:, 3:4], in_=s_[:, 2:3]), reads=[sk + "c"], writes=[sk + "d"])
            p.op("vector", lambda e: e.scalar_tensor_tensor(out=x_[:], in0=x_[:], scalar=s_[:, 3:4], in1=nwb[:], op0=ALU.mult, op1=ALU.mult),
                 reads=[xk, sk + "d", "nwb"], writes=[xk])
            p.dma("sync", out[i * 128:(i + 1) * 128, :], x_[:], reads=[xk])
        p.barrier()


DEPTH = 2


def rope_tables(T):
    half = 8
    inv = np.power(500000.0, -np.arange(half, dtype=np.float64) * 2.0 / 16)
    ang = np.arange(T, dtype=np.float32).astype(np.float64)[None, :] * inv.astype(np.float32).astype(np.float64)[:, None]
    cos = np.cos(ang)
    sin = np.sin(ang)
    c16 = np.concatenate([cos, cos], 0)
    s16 = np.concatenate([sin, sin], 0)
    return np.tile(c16, (8, 1)).astype(np.float32), np.tile(s16, (8, 1)).astype(np.float32)


def build_program(T, depth=DEPTH, phases=None):
    nc = bass.Bass("TRN2", target_bir_lowering=False)
    def din(name, shape, dt=F32):
        return nc.dram_tensor(name, shape, dt, kind="ExternalInput").ap()
    def dint(name, shape, dt=F32):
        return nc.dram_tensor(name, shape, dt, kind="Internal").ap()
    x = din("x", [T, 1024])
    norm_mix = din("norm_mix", [depth, 1024])
    w_in = din("w_in", [depth, 1024, P_IN])
    conv_w = din("conv_w", [depth, 5, 1536])
    a_log = din("a_log", [depth, 8])
    dt_bias = din("dt_bias", [depth, 8])
    gdn_norm = din("gdn_norm", [depth, 128])
    w_a = din("w_branch_a", [depth, 512, 1024])
    w_b = din("w_branch_b", [depth, 256, 1024])
    w_out = din("w_out", [depth, 1024, 1024])
    norm_ffn = din("norm_ffn", [depth, 1024])
    w_router = din("w_router", [depth, 1024, 16])
    w_eg = din("w_expert_gate", [depth, 16, 1024, 1024])
    w_eu = din("w_expert_up", [depth, 16, 1024, 1024])
    w_ed = din("w_expert_down", [depth, 16, 1024, 1024])
    norm_final = din("norm_final", [1, 1024])
    cos_t = din("cos_t", [128, T])
    sin_t = din("sin_t", [128, T])
    out = nc.dram_tensor("out", [T, 1024], F32, kind="ExternalOutput").ap()

    xres = dint("xres", [T, 1024])
    oA = dict(qkvTa=dint("qkvTa", [1536, T + 4], BF16), ztok=dint("ztok", [T, 512]), bdtok=dint("bdtok", [T, 16]), bdT=dint("bdT", [16, T]),
              qkrope=dint("qkrope", [384, T], BF16), qkrest=dint("qkrest", [1152, T], BF16), vTb=dint("vTb", [768, T], BF16),
              gatesT=dint("gatesT", [2048, T], BF16))
    oB1 = dict(uT=dint("uT", [1024, T]), ktok=dint("ktok", [T, 512], BF16), vtok=dint("vtok", [T, 512], BF16))
    o_f = dint("o_f", [T, 512])
    o_b = dint("o_b", [T, 512])
    yaT = dint("yaT", [512, T], BF16)
    att = dint("att", [T, 12, 65])
    h2b = dint("h2b", [T, 1024], BF16)
    aff = dint("aff", [T, 16])

    with ExitStack() as es:
        p = Prog(nc, es)
        c = alloc_common(p, es)
        alloc_gdn_consts(p, c, es)
        alloc_att_consts(p, c, es)
        zt = es.enter_context(nc.sbuf_tensor("zpad", [128, 2], BF16))
        p.op("gpsimd", lambda e: e.memset(zt[:], 0.0), writes=["zpad"])
        for ch in range(12):
            p.dma("sync", oA["qkvTa"][ch * 128:(ch + 1) * 128, 0:2], zt[:], reads=["zpad"], allow_slow_non_contiguous=True)
            p.dma("sync", oA["qkvTa"][ch * 128:(ch + 1) * 128, T + 2:T + 4], zt[:], reads=["zpad"], allow_slow_non_contiguous=True)
        p.barrier()
        ph = phases or "A,B1,B2,B3,C,D,E,F"
        ph = ph.split(",")
        for l in range(depth):
            xin = x if l == 0 else xres
            if "A" in ph:
                phase_a(p, c, T, xin, w_in[l], norm_mix[l:l + 1, :], cos_t, sin_t, oA)
            if "B1" in ph and "C" in ph and INTERLEAVE_B1_C:
                with ExitStack() as es_bc:
                    g1 = phase_b1_gen(p, c, T, oA["qkvTa"], conv_w[l], oB1, es_ext=es_bc, shared=True)
                    g2 = phase_c_gen(p, c, T, oA["qkrope"], oA["qkrest"], oA["vTb"], att, es_ext=es_bc, shared=True)
                    n1 = (T // 512) * 12
                    n2 = sum(4 * r_ * (T // r_ // 128 + 1) for r_ in ATT_DIL)
                    d1 = d2 = 0
                    a1 = a2 = True
                    while a1 or a2:
                        if a1 and (not a2 or d1 * n2 <= d2 * n1):
                            try:
                                next(g1)
                                d1 += 1
                            except StopIteration:
                                a1 = False
                        else:
                            try:
                                next(g2)
                                d2 += 1
                            except StopIteration:
                                a2 = False
                    p.barrier()
            elif "B1" in ph:
                phase_b1(p, c, T, oA["qkvTa"], conv_w[l], oB1)
            if "B2" in ph:
                phase_b2(p, c, T, oB1["uT"], oB1["ktok"], oB1["vtok"], oA["bdtok"], a_log[l:l + 1, :], dt_bias[l:l + 1, :], o_f, o_b)
            if "B3" in ph:
                phase_b3(p, c, T, o_f, o_b, oA["ztok"], gdn_norm[l:l + 1, :], yaT)
            if "C" in ph and not ("B1" in ph and INTERLEAVE_B1_C):
                phase_c(p, c, T, oA["qkrope"], oA["qkrest"], oA["vTb"], att)
            if "D" in ph:
                phase_d(p, c, T, yaT, att, oA["gatesT"], xres, w_a[l], w_b[l], w_out[l], norm_ffn[l:l + 1, :], w_router[l], h2b, aff, x_in=xin)
            if "E" in ph:
                phase_e(p, c, T, aff, h2b, xres, w_eg[l], w_eu[l], w_ed[l])
        if "F" in ph:
            phase_f(p, c, T, xres, norm_final, out)
        p.finish()
        nc._stats = (p.nops, p.nwaits)
    return nc


def make_in_maps(inputs, T, nb):
    cos_t, sin_t = rope_tables(T)
    f32 = lambda a: np.ascontiguousarray(np.asarray(a), dtype=np.float32)
    shared = {
        "norm_mix": f32(inputs["norm_mix"]), "w_in": f32(inputs["w_in"]), "conv_w": f32(inputs["conv_w"]),
        "a_log": f32(inputs["a_log"]).reshape(-1, 8), "dt_bias": f32(inputs["dt_bias"]).reshape(-1, 8),
        "gdn_norm": f32(inputs["gdn_norm"]), "w_branch_a": f32(inputs["w_branch_a"]), "w_branch_b": f32(inputs["w_branch_b"]),
        "w_out": f32(inputs["w_out"]), "norm_ffn": f32(inputs["norm_ffn"]), "w_router": f32(inputs["w_router"]),
        "w_expert_gate": f32(inputs["w_expert_gate"]), "w_expert_up": f32(inputs["w_expert_up"]),
        "w_expert_down": f32(inputs["w_expert_down"]), "norm_final": f32(inputs["norm_final"]).reshape(1, 1024),
        "cos_t": cos_t, "sin_t": sin_t,
    }
    x = f32(inputs["x"])
    return [dict(shared, x=x[b]) for b in range(nb)]


_NC_CACHE = {}


def kernel(**inputs):
    x = np.asarray(inputs["x"])
    B, T, _ = x.shape
    if T not in _NC_CACHE:
        _NC_CACHE[T] = build_program(T)
    nc = _NC_CACHE[T]
    in_maps = make_in_maps(inputs, T, B)
    res = run_bass_kernel_spmd(nc, in_maps, core_ids=list(range(B)))
    return np.stack([np.asarray(r["out"]) for r in res.results], axis=0).astype(np.float32)
```

```python
import numpy as np
import concourse.bass as bass
import concourse.mybir as mybir
from concourse.bass_utils import run_bass_kernel_spmd
from contextlib import ExitStack

F32 = mybir.dt.float32
BF16 = mybir.dt.bfloat16
I32 = mybir.dt.int32
U32 = mybir.dt.uint32
AF = mybir.ActivationFunctionType
ALU = mybir.AluOpType
AX = mybir.AxisListType

import os as _os0
SAME_ENGINE_SYNC = not _os0.environ.get("NO_SES")
INTERLEAVE_B1_C = bool(_os0.environ.get("ILV"))
B1_RSQRT_ACT = bool(int(_os0.environ.get("B1_RSQRT_ACT", "1")))
ROUTER_F32 = bool(int(_os0.environ.get("ROUTER_F32", "1")))
GDN_F32R = False

D_MODEL = 1024
P_IN = 6416
OFF_Z = 1536
OFF_BETA = 2048
OFF_ATT = 2064
OFF_V = OFF_ATT + 1536
OFF_GATE = 4368
RMS_EPS = 1e-6


class _Rec:
    def __init__(self):
        self.call = None

    def __getattr__(self, name):
        def f(*a, **k):
            self.call = (name, a, k)
            return self
        return f


def _ap_free(ap):
    try:
        sh = tuple(ap.shape)
        n = 1
        for v in sh[1:]:
            n *= int(v)
        return int(sh[0]), n
    except Exception:
        return 128, 128


_DT_SIZE = {}


def _dtsize(dt):
    if dt not in _DT_SIZE:
        _DT_SIZE[dt] = 2 if dt == BF16 else 4
    return _DT_SIZE[dt]


class Prog:
    def __init__(self, nc, es, defer=True):
        self.nc = nc
        self.es = es
        self.engs = {}
        for name in ["tensor", "vector", "scalar", "gpsimd", "sync"]:
            sem = es.enter_context(nc.semaphore(f"sem_{name}"))
            self.engs[name] = dict(h=getattr(nc, name), sem=sem, cnt=0, waited={}, name=name)
        self.rings = {}
        for q in ["sync", "gpsimd", "scalar"]:
            K = 8
            sems = [es.enter_context(nc.semaphore(f"dq_{q}_{i}")) for i in range(K)]
            self.rings[q] = dict(sems=sems, n=0, K=K, vals=[0] * K)
        self.lastw = {}
        self.readers = {}
        self.nwaits = 0
        self.nops = 0
        self.defer = defer
        self.pending = []

    def _wait(self, E, tok):
        sid, sem, val = tok
        if E["waited"].get(sid, 0) >= val:
            return
        if sid == id(E["sem"]):
            if E["name"] == "tensor" or (not SAME_ENGINE_SYNC and E["name"] != "gpsimd"):
                return
        E["h"].wait_ge(sem, val)
        E["waited"][sid] = val
        self.nwaits += 1

    def _deps(self, E, reads, writes):
        for k in reads:
            t = self.lastw.get(k)
            if t is not None:
                self._wait(E, t)
        for k in writes:
            t = self.lastw.get(k)
            if t is not None:
                self._wait(E, t)
            for t in self.readers.get(k, {}).values():
                self._wait(E, t)

    def _record(self, tok, reads, writes):
        for k in reads:
            d = self.readers.setdefault(k, {})
            d[tok[0]] = tok
        for k in writes:
            self.lastw[k] = tok
            self.readers[k] = {}

    def _emit_op(self, eng, call, reads, writes):
        E = self.engs[eng]
        self._deps(E, reads, writes)
        name, a, k = call
        inst = getattr(E["h"], name)(*a, **k)
        E["cnt"] += 1
        inst.then_inc(E["sem"], 1)
        tok = (id(E["sem"]), E["sem"], E["cnt"])
        self._record(tok, reads, writes)
        self.nops += 1

    def _emit_dma(self, q, call, reads, writes):
        E = self.engs[q]
        R = self.rings[q]
        i = R["n"] % R["K"]
        R["n"] += 1
        sem = R["sems"][i]
        if R["vals"][i] > 0:
            self._wait(E, (id(sem), sem, R["vals"][i]))
        self._deps(E, reads, writes)
        name, a, k = call
        inst = getattr(E["h"], name)(*a, **k)
        R["vals"][i] += 16
        inst.then_inc(sem, 16)
        tok = (id(sem), sem, R["vals"][i])
        self._record(tok, reads, writes)
        self.nops += 1

    @staticmethod
    def _cost(eng, call):
        name, a, k = call
        try:
            if eng == "tensor":
                if name == "transpose":
                    ap = k.get("in_", a[1] if len(a) > 1 else None)
                    p_, f_ = _ap_free(ap)
                    c = 0.03 + max(p_, 64) / 2400.0 * (2 if ap.dtype == F32 else 1)
                    return c, c + 0.15
                rhs = k.get("rhs", a[2] if len(a) > 2 else None)
                lhs = k.get("lhsT", a[1] if len(a) > 1 else None)
                _, n = _ap_free(rhs)
                passes = 4 if rhs.dtype == F32 else 1
                c = 0.02 + (max(n, 64) + 0) * passes / 2400.0 + (0.05 if passes == 4 else 0.0)
                return c, c + 0.15
            out = k.get("out", a[0] if a else None)
            _, f_ = _ap_free(out)
            if eng == "vector":
                c = 0.07 + f_ / 960.0
                if name in ("tensor_tensor", "scalar_tensor_tensor", "tensor_tensor_scan"):
                    c = 0.07 + f_ / 700.0
            elif eng == "scalar":
                c = 0.12 + f_ / 1100.0
            else:
                c = 0.16 + f_ / 900.0
            return c, c
        except Exception:
            return 0.3, 0.3

    def op(self, eng, fn, reads=(), writes=()):
        psr = [k for k in reads if isinstance(k, str) and k.startswith("ps")]
        if psr:
            reads = [k for k in reads if k not in psr]
            writes = list(writes) + psr
        rec = _Rec()
        fn(rec)
        occ, lat = self._cost(eng, rec.call)
        self.pending.append(("op", eng, rec.call, list(reads), list(writes), occ, lat))
        if not self.defer:
            self.flush()

    def dma(self, q, out, in_, reads=(), writes=(), fn=None, **kw):
        if fn is None:
            call = ("dma_start", (), dict(out=out, in_=in_, **kw))
            p_, f_ = _ap_free(out)
            nbytes = p_ * f_ * _dtsize(out.dtype)
        else:
            rec = _Rec()
            fn(rec)
            call = rec.call
            nbytes = 128 * 2048
        lat = 2.0 + nbytes / 120000.0
        if q == "gpsimd":
            lat += 3.0
        occ = 0.06 if q != "gpsimd" else 1.0
        self.pending.append(("dma", q, call, list(reads), list(writes), occ, lat))
        if not self.defer:
            self.flush()

    def flush(self, window=20):
        ops = self.pending
        self.pending = []
        n = len(ops)
        if n == 0:
            return
        if n < 4:
            order = range(n)
        else:
            order = self._schedule(ops, window)
        for j in order:
            kind, eng, call, reads, writes, occ, lat = ops[j]
            if kind == "op":
                self._emit_op(eng, call, reads, writes)
            else:
                self._emit_dma(eng, call, reads, writes)

    @staticmethod
    def _schedule(ops, W):
        n = len(ops)
        engof = [o[1] if o[0] == "op" else "q_" + o[1] for o in ops]
        lastw = {}
        readers = {}
        preds = [None] * n
        succ_eng = [None] * n
        for j, o in enumerate(ops):
            ps = set()
            for k in o[3]:
                t = lastw.get(k)
                if t is not None:
                    ps.add(t)
            for k in o[4]:
                t = lastw.get(k)
                if t is not None:
                    ps.add(t)
                for t in readers.get(k, ()):
                    ps.add(t)
            ps.discard(j)
            preds[j] = tuple(ps)
            for k in o[3]:
                readers.setdefault(k, []).append(j)
            for k in o[4]:
                lastw[k] = j
                readers[k] = []
        qhist = {}
        for j, o in enumerate(ops):
            if o[0] == "dma":
                h = qhist.setdefault(o[1], [])
                if len(h) >= 8:
                    preds[j] = preds[j] + (h[-8],)
                h.append(j)
        succs = [[] for _ in range(n)]
        for j in range(n):
            for pj in preds[j]:
                succs[pj].append(j)
        lists = {}
        for j in range(n):
            lists.setdefault(engof[j], []).append(j)
        head = {e: 0 for e in lists}
        done = [False] * n
        fin = [0.0] * n
        start = [0.0] * n
        tE = {e: 0.0 for e in lists}
        LATX, LATS = 0.35, 0.12
        cache = {}

        def candidate(e):
            lst = lists[e]
            h = head[e]
            L = len(lst)
            while h < L and done[lst[h]]:
                h += 1
            head[e] = h
            best = None
            cnt = 0
            i = h
            te = tE[e]
            while i < L and cnt < W:
                idx = lst[i]
                i += 1
                if done[idx]:
                    continue
                cnt += 1
                r = 0.0
                ok = True
                for pj in preds[idx]:
                    if not done[pj]:
                        ok = False
                        break
                    v = fin[pj] + (LATS if engof[pj] == e else LATX)
                    if v > r:
                        r = v
                if not ok:
                    continue
                s_ = r if r > te else te
                if best is None or s_ < best[0] - 1e-9:
                    best = (s_, idx)
                if r <= te:
                    break
            return best

        for e in lists:
            cache[e] = candidate(e)
        remaining = n
        out = []
        while remaining:
            bs = None
            be = None
            for e, cnd in cache.items():
                if cnd is not None and (bs is None or cnd[0] < bs[0] - 1e-9 or (abs(cnd[0] - bs[0]) <= 1e-9 and cnd[1] < bs[1])):
                    bs = cnd
                    be = e
            s_, idx = bs
            o = ops[idx]
            done[idx] = True
            start[idx] = s_
            fin[idx] = s_ + o[6]
            tE[be] = s_ + o[5]
            out.append(idx)
            remaining -= 1
            dirty = {be}
            for sj in succs[idx]:
                dirty.add(engof[sj])
            for e in dirty:
                cache[e] = candidate(e)
        out.sort(key=lambda j: (start[j], j))
        return out

    def barrier(self):
        self.flush()
        toks = []
        for E in self.engs.values():
            if E["cnt"] > 0:
                toks.append((id(E["sem"]), E["sem"], E["cnt"]))
        for R in self.rings.values():
            for i, sem in enumerate(R["sems"]):
                if R["vals"][i] > 0:
                    toks.append((id(sem), sem, R["vals"][i]))
        for E in self.engs.values():
            for t in toks:
                if t[0] != id(E["sem"]):
                    self._wait(E, t)
        self.lastw = {}
        self.readers = {}

    def finish(self):
        self.flush()
        E = self.engs["sync"]
        for X in self.engs.values():
            if X["cnt"] > 0 and X is not E:
                self._wait(E, (id(X["sem"]), X["sem"], X["cnt"]))
        for q, R in self.rings.items():
            for i, sem in enumerate(R["sems"]):
                if R["vals"][i] > 0:
                    self._wait(E, (id(sem), sem, R["vals"][i]))


class Ctx:
    pass


_UIDC = [0]


def _uid():
    _UIDC[0] += 1
    return _UIDC[0]


def alloc_common(p, es):
    nc = p.nc
    c = Ctx()
    c.ps = [es.enter_context(nc.psum_tensor(f"ps{i}", [128, 512], F32)) for i in range(8)]
    c.psb = c.ps[7][:, :].bitcast(BF16)
    c.identf = es.enter_context(nc.sbuf_tensor("identf", [128, 128], F32))
    c.identb = es.enter_context(nc.sbuf_tensor("identb", [128, 128], BF16))
    c.onesf = es.enter_context(nc.sbuf_tensor("onesf", [128, 128], F32))
    p.op("gpsimd", lambda e: e.memset(c.onesf[:], 1.0), writes=["onesf"])
    p.op("gpsimd", lambda e: e.memset(c.identf[:], 1.0), writes=["identf"])
    p.op("gpsimd", lambda e: e.affine_select(out=c.identf[:], in_=c.identf[:], pattern=[[-1, 128]],
                                              compare_op=ALU.is_equal, fill=0.0, base=0, channel_multiplier=1),
         reads=["identf"], writes=["identf"])
    p.op("vector", lambda e: e.tensor_copy(out=c.identb[:], in_=c.identf[:]), reads=["identf"], writes=["identb"])
    c.bias_q = es.enter_context(nc.sbuf_tensor("bias_q", [128, 1], F32))
    c.bias_k = es.enter_context(nc.sbuf_tensor("bias_k", [128, 1], F32))
    p.op("gpsimd", lambda e: e.memset(c.bias_q[:], 128.0e-6), writes=["bias_q"])
    p.op("gpsimd", lambda e: e.memset(c.bias_k[:], 1.0e-6), writes=["bias_k"])
    c.bias_one = es.enter_context(nc.sbuf_tensor("bias_one", [128, 1], F32))
    p.op("gpsimd", lambda e: e.memset(c.bias_one[:], 1.0), writes=["bias_one"])
    return c


def phase_a(p, c, T, x_src, w_in, norm_w, cos_t, sin_t, o):
    nc = p.nc
    NS = T // 512
    with ExitStack() as es:
        def sb(name, shape, dt):
            return es.enter_context(nc.sbuf_tensor(f"A_{_uid()}_" + name, shape, dt))
        NMAIN = 1536 + 528 + 768 + 2048
        Wm = sb("Wm", [128, 8, NMAIN], BF16)
        M_QKV, M_Z, M_BD, M_V, M_G = 0, 1536, 2048, 2064, 2832
        Wrope = sb("Wrope", [128, 8, 384], BF16)
        Wrest = sb("Wrest", [128, 8, 1152], BF16)
        Wp = sb("Wp", [128, 8, 384], BF16)
        stg = [sb(f"stg{i}", [128, 2048], F32) for i in range(2)]
        nwb = sb("nwb", [128, 1024], F32)
        xt = [sb(f"xt{i}", [128, 1024], F32) for i in range(2)]
        junk = sb("junk", [128, 1024], BF16)
        hb = sb("hb", [128, 1024], BF16)
        st = sb("st", [128, 8], F32)
        hT = [sb(f"hT{i}", [128, 8, 512], BF16) for i in range(2)]
        cs = [sb(f"cs{i}", [128, 2, 512], F32) for i in range(2)]
        of32 = [sb(f"of32_{i}", [128, 512], F32) for i in range(3)]
        ob16 = [sb(f"ob16_{i}", [128, 512], BF16) for i in range(3)]
        t1 = sb("t1", [128, 512], F32)
        t2 = sb("t2", [128, 512], F32)
        obd = sb("obd", [128, 4, 16], F32)

        p.dma("sync", nwb[:], norm_w.to_broadcast([128, 1024]), writes=["nwb"])
        pieces = [(0, 1536), (1536, 2064), (2064, 3600), (3600, 4368), (4368, 6416)]
        si = 0
        for k in range(8):
            for pi, (a, b) in enumerate(pieces):
                s = stg[si % 2]
                sk = f"stg{si % 2}"
                si += 1
                p.dma("sync", s[:, 0:b - a], w_in[k * 128:(k + 1) * 128, a:b], writes=[sk])
                if pi == 0:
                    p.op("gpsimd", lambda e: e.tensor_copy(out=Wm[:, k, M_QKV:M_QKV + 1536], in_=s[:, 0:1536]),
                         reads=[sk], writes=["Wm"])
                elif pi == 1:
                    p.op("gpsimd", lambda e: e.tensor_copy(out=Wm[:, k, M_Z:M_Z + 528], in_=s[:, 0:528]),
                         reads=[sk], writes=["Wm"])
                elif pi == 3:
                    p.op("gpsimd", lambda e: e.tensor_copy(out=Wm[:, k, M_V:M_V + 768], in_=s[:, 0:768]),
                         reads=[sk], writes=["Wm"])
                elif pi == 4:
                    p.op("gpsimd", lambda e: e.tensor_copy(out=Wm[:, k, M_G:M_G + 2048], in_=s[:, 0:2048]),
                         reads=[sk], writes=["Wm"])
                else:
                    sv = s[:, 0:1536].rearrange("p (h d) -> p h d", d=64)
                    p.op("gpsimd", lambda e: e.tensor_copy(
                        out=Wrope[:, k, :].rearrange("p (h d) -> p h d", d=16), in_=sv[:, :, 0:16]),
                        reads=[sk], writes=["Wrope"])
                    p.op("gpsimd", lambda e: e.tensor_copy(
                        out=Wrest[:, k, :].rearrange("p (h d) -> p h d", d=48), in_=sv[:, :, 16:64]),
                        reads=[sk], writes=["Wrest"])
                    wpv = Wp[:, k, :].rearrange("p (h d) -> p h d", d=16)
                    p.op("gpsimd", lambda e: e.tensor_scalar(out=wpv[:, :, 0:8], in0=sv[:, :, 8:16], scalar1=-1.0,
                                                             scalar2=None, op0=ALU.mult),
                         reads=[sk], writes=["Wp"])
                    p.op("gpsimd", lambda e: e.tensor_copy(out=wpv[:, :, 8:16], in_=sv[:, :, 0:8]),
                         reads=[sk], writes=["Wp"])

        rot = {"f": 0, "b": 0, "ps": 0, "ev": 0}

        def prep_front(s):
            slot = s % 2
            for ti in range(4):
                r0 = s * 512 + ti * 128
                xs = xt[ti % 2]
                xk = f"xt{ti % 2}"
                p.dma("sync", xs[:], x_src[r0:r0 + 128, :], writes=[xk])
                p.op("scalar", lambda e: e.activation(out=junk[:], in_=xs[:], func=AF.Square, accum_out=st[:, 0:1]),
                     reads=[xk], writes=["junk", "st0"])
                p.op("vector", lambda e: e.tensor_scalar(out=st[:, 1:2], in0=st[:, 0:1], scalar1=1.0 / 1024, scalar2=RMS_EPS,
                                                         op0=ALU.mult, op1=ALU.add), reads=["st0"], writes=["st1"])
                p.op("scalar", lambda e: e.activation(out=st[:, 2:3], in_=st[:, 1:2], func=AF.Sqrt), reads=["st1"], writes=["st2"])
                p.op("vector", lambda e: e.reciprocal(out=st[:, 3:4], in_=st[:, 2:3]), reads=["st2"], writes=["st3"])
                p.op("vector", lambda e: e.scalar_tensor_tensor(out=hb[:], in0=xs[:], scalar=st[:, 3:4], in1=nwb[:],
                                                                op0=ALU.mult, op1=ALU.mult),
                     reads=[xk, "st3", "nwb"], writes=["hb"])
                for k in range(8):
                    p.op("tensor", lambda e: e.transpose(out=c.psb[:, k * 128:(k + 1) * 128], in_=hb[:, k * 128:(k + 1) * 128],
                                                         identity=c.identb[:]),
                         reads=["hb", "identb"], writes=["psb"])
                p.op("scalar", lambda e: e.activation(out=hT[slot][:, :, ti * 128:(ti + 1) * 128],
                                                      in_=c.psb[:, :].rearrange("p (k t) -> p k t", t=128), func=AF.Copy),
                     reads=["psb"], writes=[f"hT{slot}"])

        def fm_group(s, Wsrc, wkey, col0, nchunks, dst, dst_row0, mode):
            slot = s % 2
            hk = f"hT{slot}"
            for ci in range(nchunks):
                pi = rot["ps"] % 6
                rot["ps"] += 1
                ps = c.ps[pi]
                pk = f"ps{pi}"
                for k in range(8):
                    p.op("tensor", lambda e: e.matmul(ps[:, :], lhsT=Wsrc[:, k, col0 + ci * 128: col0 + (ci + 1) * 128],
                                                      rhs=hT[slot][:, k, :], start=(k == 0), stop=(k == 7)),
                         reads=[wkey, hk], writes=[pk])
                if mode == "f32":
                    oi = rot["f"] % 3
                    rot["f"] += 1
                    ot, ok = of32[oi], f"of32_{oi}"
                else:
                    oi = rot["b"] % 3
                    rot["b"] += 1
                    ot, ok = ob16[oi], f"ob16_{oi}"
                ev = "vector" if rot["ev"] % 2 == 0 else "scalar"
                rot["ev"] += 1
                if mode == "sig":
                    p.op("scalar", lambda e: e.activation(out=ot[:], in_=ps[:, :], func=AF.Sigmoid), reads=[pk], writes=[ok])
                elif ev == "vector":
                    p.op("vector", lambda e: e.tensor_copy(out=ot[:], in_=ps[:, :]), reads=[pk], writes=[ok])
                else:
                    p.op("scalar", lambda e: e.activation(out=ot[:], in_=ps[:, :], func=AF.Copy), reads=[pk], writes=[ok])
                r = dst_row0 + ci * 128
                p.dma("sync", dst(r, s), ot[:], reads=[ok])

        def body(s):
            slot = s % 2
            hk = f"hT{slot}"
            p.dma("sync", cs[slot][:, 0, :], cos_t[:, s * 512:(s + 1) * 512], writes=[f"cs{slot}"])
            p.dma("sync", cs[slot][:, 1, :], sin_t[:, s * 512:(s + 1) * 512], writes=[f"cs{slot}"])
            fm_group(s, Wm, "Wm", M_QKV, 12, lambda r, s_: o["qkvTa"][r:r + 128, 2 + s_ * 512: 2 + (s_ + 1) * 512], 0, "bf16")
            for ti in range(4):
                pi = rot["ps"] % 6
                rot["ps"] += 1
                ps = c.ps[pi]
                pk = f"ps{pi}"
                for k in range(8):
                    p.op("tensor", lambda e: e.matmul(ps[:, :], lhsT=hT[slot][:, k, ti * 128:(ti + 1) * 128],
                                                      rhs=Wm[:, k, M_Z:M_Z + 512], start=(k == 0), stop=(k == 7)),
                         reads=["Wm", hk], writes=[pk])
                oi = rot["f"] % 3
                rot["f"] += 1
                p.op("vector", lambda e: e.tensor_copy(out=of32[oi][:], in_=ps[:, :]), reads=[pk], writes=[f"of32_{oi}"])
                r0 = s * 512 + ti * 128
                p.dma("sync", o["ztok"][r0:r0 + 128, :], of32[oi][:], reads=[f"of32_{oi}"])
            ps = c.ps[6]
            for ti in range(4):
                for k in range(8):
                    p.op("tensor", lambda e: e.matmul(ps[:, ti * 16:(ti + 1) * 16], lhsT=hT[slot][:, k, ti * 128:(ti + 1) * 128],
                                                      rhs=Wm[:, k, M_BD:M_BD + 16], start=(k == 0), stop=(k == 7)),
                         reads=["Wm", hk], writes=["ps6"])
            p.op("vector", lambda e: e.tensor_copy(out=obd[:, :, :], in_=ps[:, 0:64].rearrange("p (t c) -> p t c", c=16)),
                 reads=["ps6"], writes=["obd"])
            p.dma("sync", o["bdtok"][s * 512:(s + 1) * 512, :].rearrange("(t p) c -> p t c", p=128), obd[:, :, :],
                  reads=["obd"])
            pi = rot["ps"] % 6
            rot["ps"] += 1
            for k in range(8):
                p.op("tensor", lambda e: e.matmul(c.ps[pi][0:16, :], lhsT=Wm[:, k, M_BD:M_BD + 16],
                                                  rhs=hT[slot][:, k, :], start=(k == 0), stop=(k == 7)),
                     reads=["Wm", hk], writes=[f"ps{pi}"])
            oi = rot["f"] % 3
            rot["f"] += 1
            p.op("vector", lambda e: e.tensor_copy(out=of32[oi][0:16, :], in_=c.ps[pi][0:16, :]), reads=[f"ps{pi}"], writes=[f"of32_{oi}"])
            p.dma("sync", o["bdT"][:, s * 512:(s + 1) * 512], of32[oi][0:16, :], reads=[f"of32_{oi}"])
            for ci in range(3):
                pa = rot["ps"] % 6
                rot["ps"] += 1
                pb = rot["ps"] % 6
                rot["ps"] += 1
                for k in range(8):
                    p.op("tensor", lambda e: e.matmul(c.ps[pa][:, :], lhsT=Wrope[:, k, ci * 128:(ci + 1) * 128],
                                                      rhs=hT[slot][:, k, :], start=(k == 0), stop=(k == 7)),
                         reads=["Wrope", hk], writes=[f"ps{pa}"])
                for k in range(8):
                    p.op("tensor", lambda e: e.matmul(c.ps[pb][:, :], lhsT=Wp[:, k, ci * 128:(ci + 1) * 128],
                                                      rhs=hT[slot][:, k, :], start=(k == 0), stop=(k == 7)),
                         reads=["Wp", hk], writes=[f"ps{pb}"])
                p.op("vector", lambda e: e.tensor_tensor(out=t1[:], in0=c.ps[pa][:, :], in1=cs[slot][:, 0, :], op=ALU.mult),
                     reads=[f"ps{pa}", f"cs{slot}"], writes=["t1"])
                p.op("vector", lambda e: e.tensor_tensor(out=t2[:], in0=c.ps[pb][:, :], in1=cs[slot][:, 1, :], op=ALU.mult),
                     reads=[f"ps{pb}", f"cs{slot}"], writes=["t2"])
                oi = rot["b"] % 3
                rot["b"] += 1
                p.op("gpsimd", lambda e: e.tensor_tensor(out=ob16[oi][:], in0=t1[:], in1=t2[:], op=ALU.add),
                     reads=["t1", "t2"], writes=[f"ob16_{oi}"])
                p.dma("sync", o["qkrope"][ci * 128:(ci + 1) * 128, s * 512:(s + 1) * 512], ob16[oi][:],
                      reads=[f"ob16_{oi}"])
            fm_group(s, Wrest, "Wrest", 0, 9, lambda r, s_: o["qkrest"][r:r + 128, s_ * 512:(s_ + 1) * 512], 0, "bf16")
            fm_group(s, Wm, "Wm", M_V, 6, lambda r, s_: o["vTb"][r:r + 128, s_ * 512:(s_ + 1) * 512], 0, "bf16")
            fm_group(s, Wm, "Wm", M_G, 16, lambda r, s_: o["gatesT"][r:r + 128, s_ * 512:(s_ + 1) * 512], 0, "sig")

        prep_front(0)
        for s in range(NS):
            body(s)
            if s + 1 < NS:
                prep_front(s + 1)
        p.barrier()


def phase_b1(p, c, T, qkvTa, conv_w, o):
    for _ in phase_b1_gen(p, c, T, qkvTa, conv_w, o):
        pass


def phase_b1_gen(p, c, T, qkvTa, conv_w, o, es_ext=None, shared=False):
    nc = p.nc
    NS = T // 512
    with ExitStack() as es_own:
        es = es_ext if es_ext is not None else es_own
        def sb(name, shape, dt):
            return es.enter_context(nc.sbuf_tensor(f"B1_{_uid()}_" + name, shape, dt))
        cw = sb("cw", [128, 12, 5], F32)
        uin = [sb(f"uin{i}", [128, 516], BF16) for i in range(3)]
        Dg = sb("Dg", [128, 60, 128], BF16)
        su = [sb(f"su{i}", [128, 512], F32) for i in range(2)]
        sqs = [sb(f"sq{i}", [128, 512], BF16) for i in range(3)]
        onesb = sb("onesb", [128, 128], BF16)
        svb = [sb(f"svb{i}", [128, 512], BF16) for i in range(2)]
        rns = [sb(f"rn{i}", [128, 512], F32) for i in range(3)]
        rn2s = [sb(f"rn2_{i}", [128, 512], F32) for i in range(3)]
        un = [sb(f"un{i}", [128, 512], F32) for i in range(2)]
        tk = [sb(f"tk{i}", [128, 4, 128], BF16) for i in range(2)]
        p.op("vector", lambda e: e.tensor_copy(out=onesb[:], in_=c.onesf[:]), reads=["onesf"], writes=["onesb"])
        for j in range(5):
            p.dma("sync", cw[:, :, j:j + 1], conv_w[j:j + 1, :].rearrange("j (k c) -> c k j", c=128), writes=["cw"], allow_slow_non_contiguous=True)
        for ch in range(12):
            for j in range(5):
                p.op("vector", lambda e: e.tensor_scalar(out=Dg[:, ch * 5 + j, :], in0=c.identb[:], scalar1=cw[:, ch, j:j + 1], scalar2=None, op0=ALU.mult),
                     reads=["cw", "identb"], writes=["Dg"])
        n = 0
        for s in range(NS):
            for ch in range(12):
                ui, uk = uin[n % 3], f"uin{n % 3}"
                sv, sk = su[n % 2], f"su{n % 2}"
                u, unk = un[n % 2], f"un{n % 2}"
                if shared:
                    cb, pi, tb = 5, 6, 7
                else:
                    cb = n % 3
                    pi = 3 + n % 3
                    tb = 6 + n % 2
                sq, sqk = sqs[n % 3], f"sq{n % 3}"
                rn, rnk = rns[n % 3], f"rn{n % 3}"
                rn2, rn2k = rn2s[n % 3], f"rn2_{n % 3}"
                n += 1
                p.dma("sync", ui[:], qkvTa[ch * 128:(ch + 1) * 128, s * 512: s * 512 + 516], writes=[uk])
                for j in range(5):
                    p.op("tensor", lambda e: e.matmul(c.ps[cb][:, :], lhsT=Dg[:, ch * 5 + j, :], rhs=ui[:, j:j + 512], start=(j == 0), stop=(j == 4)),
                         reads=[uk, "Dg"], writes=[f"ps{cb}"])
                p.op("scalar", lambda e: e.activation(out=sv[:], in_=c.ps[cb][:, :], func=AF.Silu), reads=[f"ps{cb}"], writes=[sk])
                if ch < 8:
                    p.op("gpsimd", lambda e: e.tensor_tensor(out=sq[:], in0=sv[:], in1=sv[:], op=ALU.mult), reads=[sk], writes=[sqk])
                    ps = c.ps[pi]
                    p.op("tensor", lambda e: e.matmul(ps[:, :], lhsT=onesb[:], rhs=sq[:], start=True, stop=True),
                         reads=[sqk, "onesb"], writes=[f"ps{pi}"])
                    if B1_RSQRT_ACT:
                        if ch < 4:
                            p.op("scalar", lambda e: e.activation(out=rn[:], in_=ps[:, :], func=AF.Ln, scale=128.0, bias=c.bias_q[:, 0:1]),
                                 reads=[f"ps{pi}", "bias_q"], writes=[rnk])
                        else:
                            p.op("scalar", lambda e: e.activation(out=rn[:], in_=ps[:, :], func=AF.Ln, scale=1.0, bias=c.bias_k[:, 0:1]),
                                 reads=[f"ps{pi}", "bias_k"], writes=[rnk])
                        p.op("scalar", lambda e: e.activation(out=rn2[:], in_=rn[:], func=AF.Exp, scale=-0.5), reads=[rnk], writes=[rn2k])
                    else:
                        if ch < 4:
                            p.op("scalar", lambda e: e.activation(out=rn[:], in_=ps[:, :], func=AF.Sqrt, scale=128.0, bias=c.bias_q[:, 0:1]),
                                 reads=[f"ps{pi}", "bias_q"], writes=[rnk])
                        else:
                            p.op("scalar", lambda e: e.activation(out=rn[:], in_=ps[:, :], func=AF.Sqrt, scale=1.0, bias=c.bias_k[:, 0:1]),
                                 reads=[f"ps{pi}", "bias_k"], writes=[rnk])
                        p.op("vector", lambda e: e.reciprocal(out=rn2[:], in_=rn[:]), reads=[rnk], writes=[rn2k])
                    p.op("gpsimd", lambda e: e.tensor_tensor(out=u[:], in0=sv[:], in1=rn2[:], op=ALU.mult), reads=[sk, rn2k], writes=[unk])
                    p.dma("sync", o["uT"][ch * 128:(ch + 1) * 128, s * 512:(s + 1) * 512], u[:], reads=[unk])
                    src, srck = u, unk
                else:
                    src, srck = sv, sk
                if ch >= 4:
                    pj = tb
                    sb_, sbk = svb[n % 2], f"svb{n % 2}"
                    p.op("gpsimd", lambda e: e.tensor_copy(out=sb_[:], in_=src[:]), reads=[srck], writes=[sbk])
                    pjb = c.ps[pj][:, :].bitcast(BF16)
                    for ti in range(4):
                        p.op("tensor", lambda e: e.transpose(out=pjb[:, ti * 128:(ti + 1) * 128], in_=sb_[:, ti * 128:(ti + 1) * 128],
                                                             identity=c.identb[:]), reads=[sbk, "identb"], writes=[f"ps{pj}"])
                    t_, tkk = tk[n % 2], f"tk{n % 2}"
                    p.op("scalar", lambda e: e.activation(out=t_[:, :, :], in_=pjb[:, 0:512].rearrange("p (t d) -> p t d", d=128), func=AF.Copy),
                         reads=[f"ps{pj}"], writes=[tkk])
                    dst = o["ktok"] if ch < 8 else o["vtok"]
                    hh = ch % 4
                    p.dma("sync", dst[s * 512:(s + 1) * 512, hh * 128:(hh + 1) * 128].rearrange("(t p) d -> p t d", p=128), t_[:, :, :],
                          reads=[tkk])
                yield
        if not shared:
            p.barrier()


def alloc_gdn_consts(p, c, es):
    nc = p.nc
    def mk(name, fillval_in, pattern, cm, cmp, fill):
        t = es.enter_context(nc.sbuf_tensor(name, [128, 128], F32))
        p.op("gpsimd", lambda e: e.memset(t[:], fillval_in), writes=[name])
        p.op("gpsimd", lambda e: e.affine_select(out=t[:], in_=t[:], pattern=pattern, compare_op=cmp, fill=fill,
                                                  base=0, channel_multiplier=cm), reads=[name], writes=[name])
        if GDN_F32R and name.startswith("tri"):
            t2 = es.enter_context(nc.sbuf_tensor(name + "r", [128, 128], F32))
            p.op("vector", lambda e: e.tensor_copy(out=t2[:].bitcast(mybir.dt.float32r), in_=t[:]), reads=[name], writes=[name])
            return t2
        return t
    c.triU = mk("triU", 1.0, [[1, 128]], -1, ALU.is_ge, 0.0)
    c.triL = mk("triL", 1.0, [[-1, 128]], 1, ALU.is_ge, 0.0)
    c.nm1k = ["nm1f", "nm1b"]
    c.nm2k = ["nm2f", "nm2b"]
    c.nm1 = [mk("nm1f", 0.0, [[-1, 128]], 1, ALU.is_gt, 1.0e4),
             mk("nm1b", 0.0, [[1, 128]], -1, ALU.is_gt, 1.0e4)]
    c.nm2 = [mk("nm2f", 0.0, [[1, 128]], -1, ALU.is_ge, -1.0e4),
             mk("nm2b", 0.0, [[-1, 128]], 1, ALU.is_ge, -1.0e4)]


def phase_b2(p, c, T, uT, ktok, vtok, bdtok, a_log, dt_bias, o_f, o_b):
    nc = p.nc
    NT = T // 128
    F32R = mybir.dt.float32r
    USE_R = GDN_F32R

    def R(ap):
        return ap.bitcast(F32R) if USE_R else ap
    with ExitStack() as es:
        def sb(name, shape, dt=F32):
            return es.enter_context(nc.sbuf_tensor(f"B2_{_uid()}_" + name, shape, dt))
        bd = sb("bd", [128, NT, 16])
        beta = sb("beta", [128, NT, 8])
        nbeta = sb("nbeta", [128, NT, 8])
        g = sb("g", [128, NT, 8])
        G = sb("G", [128, NT, 8])
        tmp = sb("tmp", [128, NT, 8])
        eGr = sb("eGr", [128, NT, 8])
        ge = sb("ge", [128, NT, 8])
        bG = sb("bG", [128, NT, 8])
        dtb = sb("dtb", [128, 8])
        nea = sb("nea", [128, 8])
        for t0 in range(0, NT, 8):
            t1 = min(NT, t0 + 8)
            p.dma("sync", bd[:, t0:t1, :], bdtok[t0 * 128:t1 * 128, :].rearrange("(t p) c -> p t c", p=128), writes=["bd"])
        p.dma("sync", dtb[:], dt_bias.to_broadcast([128, 8]), writes=["dtb"])
        p.dma("sync", nea[:], a_log.to_broadcast([128, 8]), writes=["nea"])
        p.op("scalar", lambda e: e.activation(out=nea[:], in_=nea[:], func=AF.Exp), reads=["nea"], writes=["nea"])
        p.op("vector", lambda e: e.tensor_scalar(out=nea[:], in0=nea[:], scalar1=-1.0, scalar2=None, op0=ALU.mult), reads=["nea"], writes=["nea"])
        p.op("scalar", lambda e: e.activation(out=beta[:, :, :], in_=bd[:, :, 0:8], func=AF.Sigmoid), reads=["bd"], writes=["beta"])
        p.op("vector", lambda e: e.tensor_scalar(out=nbeta[:, :, :], in0=beta[:, :, :], scalar1=-1.0, scalar2=None, op0=ALU.mult),
             reads=["beta"], writes=["nbeta"])
        p.op("vector", lambda e: e.tensor_tensor(out=tmp[:, :, :], in0=bd[:, :, 8:16], in1=dtb[:, :].unsqueeze(1).to_broadcast([128, NT, 8]), op=ALU.add),
             reads=["bd", "dtb"], writes=["tmp"])
        p.op("scalar", lambda e: e.activation(out=tmp[:, :, :], in_=tmp[:, :, :], func=AF.Exp), reads=["tmp"], writes=["tmp"])
        p.op("scalar", lambda e: e.activation(out=tmp[:, :, :], in_=tmp[:, :, :], func=AF.Ln, bias=c.bias_one[:, 0:1], scale=1.0), reads=["tmp", "bias_one"], writes=["tmp"])
        p.op("vector", lambda e: e.tensor_tensor(out=R(g[:, :, :]), in0=tmp[:, :, :], in1=nea[:, :].unsqueeze(1).to_broadcast([128, NT, 8]), op=ALU.mult),
             reads=["tmp", "nea"], writes=["g"])
        for d, tri, tk in ((0, c.triU, "triU"), (1, c.triL, "triL")):
            for t0 in range(0, NT, 64):
                t1 = min(NT, t0 + 64)
                p.op("tensor", lambda e: e.matmul(c.ps[d][:, 0:(t1 - t0) * 4], lhsT=tri[:], rhs=g[:, t0:t1, d * 4:(d + 1) * 4], start=True, stop=True),
                     reads=["g", tk], writes=[f"ps{d}"])
                p.op("vector", lambda e: e.tensor_copy(out=G[:, t0:t1, d * 4:(d + 1) * 4],
                                                       in_=c.ps[d][:, 0:(t1 - t0) * 4].rearrange("p (t c) -> p t c", c=4)),
                     reads=[f"ps{d}"], writes=["G"])
        for t0 in range(0, NT, 64):
            t1 = min(NT, t0 + 64)
            p.op("tensor", lambda e: e.matmul(c.ps[2][:, 0:(t1 - t0) * 8], lhsT=c.onesf[:], rhs=g[:, t0:t1, :], start=True, stop=True),
                 reads=["g", "onesf"], writes=["ps2"])
            p.op("scalar", lambda e: e.activation(out=ge[:, t0:t1, :], in_=c.ps[2][:, 0:(t1 - t0) * 8].rearrange("p (t c) -> p t c", c=8), func=AF.Exp),
                 writes=["ps2", "ge"])
            p.op("vector", lambda e: e.tensor_tensor(out=eGr[:, t0:t1, :], in0=c.ps[2][:, 0:(t1 - t0) * 8].rearrange("p (t c) -> p t c", c=8),
                                                     in1=G[:, t0:t1, :], op=ALU.subtract), reads=["G"], writes=["ps2", "eGr"])
        p.op("scalar", lambda e: e.activation(out=eGr[:, :, :], in_=eGr[:, :, :], func=AF.Exp), reads=["eGr"], writes=["eGr"])
        p.op("scalar", lambda e: e.activation(out=bG[:, :, :], in_=G[:, :, :], func=AF.Exp), reads=["G"], writes=["bG"])
        p.op("vector", lambda e: e.tensor_tensor(out=bG[:, :, :], in0=bG[:, :, :], in1=beta[:, :, :], op=ALU.mult), reads=["bG", "beta"], writes=["bG"])
        p.barrier()

        U = []
        for hd in range(8):
            S_ = sb(f"S_{hd}", [128, 128])
            Sb_ = sb(f"Sb_{hd}", [128, 128], BF16)
            two = []
            for pp in range(2):
                u = {"S": S_, "Sb": Sb_}
                for nm in ["D1", "D2T", "eB", "X0", "X1"]:
                    u[nm] = sb(f"{nm}_{hd}_{pp}", [128, 128])
                for nm in ["XP0", "XP1"]:
                    u[nm] = sb(f"{nm}_{hd}_{pp}", [128, 256])
                for nm in ["aaT", "qdT", "TTb", "TTbg", "nwT", "vnew", "vs"]:
                    u[nm] = sb(f"{nm}_{hd}_{pp}", [128, 128], BF16)
                two.append(u)
            U.append(two)
            p.op("gpsimd", lambda e: e.memset(S_[:], 0.0), writes=[f"S_{hd}"])
            p.op("gpsimd", lambda e: e.memset(Sb_[:], 0.0), writes=[f"Sb_{hd}"])
        inb = []
        for b_ in range(2):
            d_ = {}
            for dr in range(2):
                d_[dr] = dict(kq=sb(f"kq{b_}{dr}", [128, 4, 2, 128]),
                              ktb=sb(f"ktb{b_}{dr}", [128, 512], BF16), vtb=sb(f"vtb{b_}{dr}", [128, 512], BF16),
                              o=sb(f"o{b_}{dr}", [128, 512]))
            inb.append(d_)

        bn = [0]

        def bank():
            i = bn[0] % 8
            bn[0] += 1
            return c.ps[i], f"ps{i}"

        def Q(bk, q):
            return bk[:, q * 128:(q + 1) * 128]

        def ev(eng, out_ap, in_ap, rk, wk, scale=None):
            if eng == "scalar":
                if scale is None:
                    p.op("scalar", lambda e: e.activation(out=out_ap, in_=in_ap, func=AF.Copy), reads=rk, writes=wk)
                else:
                    p.op("scalar", lambda e: e.activation(out=out_ap, in_=in_ap, func=AF.Copy, scale=scale), reads=rk, writes=wk)
            else:
                if scale is None:
                    p.op("vector", lambda e: e.tensor_copy(out=out_ap, in_=in_ap), reads=rk, writes=wk)
                else:
                    p.op("vector", lambda e: e.tensor_scalar(out=out_ap, in0=in_ap, scalar1=scale, scalar2=None, op0=ALU.mult), reads=rk, writes=wk)

        def XT(u, par):
            return u[f"XP{par}"][:, 0:128]

        def PT(u, j):
            return u[f"XP{1 - j % 2}"][:, 128:256]

        for n in range(NT):
            bs = n % 2
            pp = n % 2
            tiles = (n, NT - 1 - n)
            for dr in range(2):
                i = tiles[dr]
                ib = inb[bs][dr]
                for h_ in range(4):
                    p.dma("sync", ib["kq"][:, h_, 0, :], uT[512 + h_ * 128:512 + (h_ + 1) * 128, i * 128:(i + 1) * 128], writes=[f"kq{bs}{dr}"])
                    p.dma("sync", ib["kq"][:, h_, 1, :], uT[h_ * 128:(h_ + 1) * 128, i * 128:(i + 1) * 128], writes=[f"kq{bs}{dr}"])
                p.dma("sync", ib["ktb"][:, :], ktok[i * 128:(i + 1) * 128, :], writes=[f"ktb{bs}{dr}"])
                p.dma("sync", ib["vtb"][:, :], vtok[i * 128:(i + 1) * 128, :], writes=[f"vtb{bs}{dr}"])
            st = {}
            for dr in range(2):
                i = tiles[dr]
                ib = inb[bs][dr]
                tri, trik = (c.triU, "triU") if dr == 0 else (c.triL, "triL")
                bB, bBk = bank()
                bK = [bank(), bank()]
                st[dr] = (bB, bBk, bK)
                for h in range(4):
                    hd = dr * 4 + h
                    p.op("tensor", lambda e: e.matmul(Q(bB, h), lhsT=g[:, i, hd:hd + 1].to_broadcast([128, 128]), rhs=tri[:], start=True, stop=True),
                         reads=["g", trik], writes=[bBk])
                for h in range(4):
                    bk_, bkk = bK[h // 2]
                    p.op("tensor", lambda e: e.matmul(bk_[:, (h % 2) * 256:(h % 2) * 256 + 256], lhsT=ib["kq"][:, h, 0, :],
                                                      rhs=ib["kq"][:, h, :, :].rearrange("p a t -> p (a t)"), start=True, stop=True),
                         reads=[f"kq{bs}{dr}"], writes=[bkk])
            for dr in range(2):
                i = tiles[dr]
                ib = inb[bs][dr]
                bB, bBk, bK = st[dr]
                for h in range(4):
                    hd = dr * 4 + h
                    u = U[hd][pp]
                    Gc = G[:, i, hd:hd + 1]
                    p.op("vector", lambda e: e.scalar_tensor_tensor(out=u["D1"][:], in0=Q(bB, h), scalar=Gc, in1=c.nm1[dr][:], op0=ALU.subtract, op1=ALU.max),
                         reads=["G", c.nm1k[dr]], writes=[bBk, f"D1_{hd}p{pp}"])
                    p.op("scalar", lambda e: e.activation(out=u["D1"][:], in_=u["D1"][:], func=AF.Exp, scale=-1.0), reads=[f"D1_{hd}p{pp}"], writes=[f"D1_{hd}p{pp}"])
                    p.op("vector", lambda e: e.scalar_tensor_tensor(out=u["D2T"][:], in0=Q(bB, h), scalar=Gc, in1=c.nm2[dr][:], op0=ALU.subtract, op1=ALU.min),
                         reads=["G", c.nm2k[dr]], writes=[bBk, f"D2T_{hd}p{pp}"])
                    p.op("scalar", lambda e: e.activation(out=u["D2T"][:], in_=u["D2T"][:], func=AF.Exp), reads=[f"D2T_{hd}p{pp}"], writes=[f"D2T_{hd}p{pp}"])
                for h in range(4):
                    hd = dr * 4 + h
                    u = U[hd][pp]
                    p.op("scalar", lambda e: e.activation(out=u["eB"][:], in_=Q(bB, h), func=AF.Exp), writes=[bBk, f"eB_{hd}p{pp}"])
                for h in range(4):
                    hd = dr * 4 + h
                    u = U[hd][pp]
                    bk_, bkk = bK[h // 2]
                    o0 = (h % 2) * 256
                    p.op("vector", lambda e: e.scalar_tensor_tensor(out=u["X0"][:], in0=bk_[:, o0:o0 + 128], scalar=nbeta[:, i, hd:hd + 1], in1=u["D1"][:],
                                                                    op0=ALU.mult, op1=ALU.mult), reads=["nbeta", f"D1_{hd}p{pp}"], writes=[bkk, f"X0_{hd}p{pp}"])
                    p.op("vector", lambda e: e.tensor_tensor(out=u["aaT"][:], in0=bk_[:, o0 + 128:o0 + 256], in1=u["D2T"][:], op=ALU.mult),
                         reads=[f"D2T_{hd}p{pp}"], writes=[bkk, f"aaT_{hd}p{pp}"])
                    p.op("gpsimd", lambda e: e.tensor_tensor(out=u["qdT"][:], in0=ib["kq"][:, h, 1, :], in1=u["eB"][:], op=ALU.mult),
                         reads=[f"kq{bs}{dr}", f"eB_{hd}p{pp}"], writes=[f"qdT_{hd}p{pp}"])
            for dr in range(2):
                bT, bTk = bank()
                for h in range(4):
                    hd = dr * 4 + h
                    u = U[hd][pp]
                    p.op("tensor", lambda e: e.transpose(out=Q(bT, h), in_=u["X0"][:], identity=c.identf[:]), reads=[f"X0_{hd}p{pp}", "identf"], writes=[bTk])
                for h in range(4):
                    hd = dr * 4 + h
                    u = U[hd][pp]
                    p.op("scalar", lambda e: e.activation(out=XT(u, 0), in_=Q(bT, h), func=AF.Copy), writes=[bTk, f"XT0_{hd}p{pp}"])
                    p.op("gpsimd", lambda e: e.tensor_tensor(out=PT(u, 0), in0=XT(u, 0), in1=c.identf[:], op=ALU.add),
                         reads=[f"XT0_{hd}p{pp}", "identf"], writes=[f"PT0_{hd}p{pp}"])
            for k in range(1, 7):
                a_, b_ = (k - 1) % 2, k % 2
                for dr in range(2):
                    e1, e2 = ("scalar", "vector") if (k + dr) % 3 != 0 else ("scalar", "scalar")
                    b1, b1k = bank()
                    for h in range(4):
                        hd = dr * 4 + h
                        u = U[hd][pp]
                        p.op("tensor", lambda e: e.matmul(Q(b1, h), lhsT=XT(u, a_), rhs=u[f"X{a_}"][:], start=True, stop=True),
                             reads=[f"XT{a_}_{hd}p{pp}", f"X{a_}_{hd}p{pp}"], writes=[b1k])
                    bM = [bank(), bank()]
                    for h in range(4):
                        hd = dr * 4 + h
                        u = U[hd][pp]
                        bm_, bmk = bM[h // 2]
                        o0 = (h % 2) * 256
                        if k == 1:
                            p.op("tensor", lambda e: e.matmul(bm_[:, o0:o0 + 128], lhsT=u[f"X{a_}"][:], rhs=XT(u, a_), start=True, stop=True),
                                 reads=[f"XT{a_}_{hd}p{pp}", f"X{a_}_{hd}p{pp}"], writes=[bmk])
                        else:
                            p.op("tensor", lambda e: e.matmul(bm_[:, o0:o0 + 256], lhsT=u[f"X{a_}"][:], rhs=u[f"XP{a_}"][:, :], start=True, stop=True),
                                 reads=[f"XT{a_}_{hd}p{pp}", f"X{a_}_{hd}p{pp}", f"PT{(k - 2) % 2}_{hd}p{pp}"], writes=[bmk])
                    for h in range(4):
                        hd = dr * 4 + h
                        u = U[hd][pp]
                        ev(e1, u[f"X{b_}"][:], Q(b1, h), [], [b1k, f"X{b_}_{hd}p{pp}"])
                    for h in range(4):
                        hd = dr * 4 + h
                        u = U[hd][pp]
                        bm_, bmk = bM[h // 2]
                        o0 = (h % 2) * 256
                        if k < 6:
                            ev(e2, XT(u, b_), bm_[:, o0:o0 + 128], [], [bmk, f"XT{b_}_{hd}p{pp}"])
                        if k >= 2:
                            p.op("vector", lambda e: e.tensor_tensor(out=PT(u, k - 1), in0=bm_[:, o0 + 128:o0 + 256], in1=PT(u, k - 2), op=ALU.add),
                                 reads=[f"PT{(k - 2) % 2}_{hd}p{pp}"], writes=[bmk, f"PT{(k - 1) % 2}_{hd}p{pp}"])
            for dr in range(2):
                bF, bFk = bank()
                for h in range(4):
                    hd = dr * 4 + h
                    u = U[hd][pp]
                    p.op("tensor", lambda e: e.matmul(Q(bF, h), lhsT=u["X0"][:], rhs=PT(u, 5), start=True, stop=True),
                         reads=[f"X0_{hd}p{pp}", f"PT1_{hd}p{pp}"], writes=[bFk])
                for h in range(4):
                    hd = dr * 4 + h
                    u = U[hd][pp]
                    p.op("vector", lambda e: e.tensor_tensor(out=PT(u, 6), in0=Q(bF, h), in1=PT(u, 5), op=ALU.add),
                         reads=[f"PT1_{hd}p{pp}"], writes=[bFk, f"PT0_{hd}p{pp}"])
            for dr in range(2):
                i = tiles[dr]
                ib = inb[bs][dr]
                for h in range(4):
                    hd = dr * 4 + h
                    u = U[hd][pp]
                    p.op("scalar", lambda e: e.activation(out=u["TTb"][:], in_=PT(u, 6), func=AF.Copy, scale=beta[:, i, hd:hd + 1]),
                         reads=[f"PT0_{hd}p{pp}", "beta"], writes=[f"TTb_{hd}p{pp}"])
                    p.op("vector", lambda e: e.tensor_scalar(out=u["TTbg"][:], in0=PT(u, 6), scalar1=bG[:, i, hd:hd + 1], scalar2=None, op0=ALU.mult),
                         reads=[f"PT0_{hd}p{pp}", "bG"], writes=[f"TTbg_{hd}p{pp}"])
                bw, bwk = bank()
                for h in range(4):
                    hd = dr * 4 + h
                    u = U[hd][pp]
                    p.op("tensor", lambda e: e.matmul(Q(bw, h), lhsT=ib["ktb"][:, h * 128:(h + 1) * 128], rhs=u["TTbg"][:], start=True, stop=True),
                         reads=[f"ktb{bs}{dr}", f"TTbg_{hd}p{pp}"], writes=[bwk])
                for h in range(4):
                    hd = dr * 4 + h
                    u = U[hd][pp]
                    ev("scalar" if dr == 0 else "vector", u["nwT"][:], Q(bw, h), [], [bwk, f"nwT_{hd}p{pp}"], scale=-1.0)
                bv, bvk = bank()
                for h in range(4):
                    hd = dr * 4 + h
                    u = U[hd][pp]
                    p.op("tensor", lambda e: e.matmul(Q(bv, h), lhsT=u["TTb"][:], rhs=ib["vtb"][:, h * 128:(h + 1) * 128], start=True, stop=False),
                         reads=[f"vtb{bs}{dr}", f"TTb_{hd}p{pp}"], writes=[bvk])
                    p.op("tensor", lambda e: e.matmul(Q(bv, h), lhsT=u["nwT"][:], rhs=u["Sb"][:], start=False, stop=True),
                         reads=[f"nwT_{hd}p{pp}", f"Sb_{hd}"], writes=[bvk])
                for h in range(4):
                    hd = dr * 4 + h
                    u = U[hd][pp]
                    p.op("scalar", lambda e: e.activation(out=u["vnew"][:], in_=Q(bv, h), func=AF.Copy), writes=[bvk, f"vnew_{hd}p{pp}"])
                for h in range(4):
                    hd = dr * 4 + h
                    u = U[hd][pp]
                    p.op("vector", lambda e: e.tensor_scalar(out=u["vs"][:], in0=Q(bv, h), scalar1=eGr[:, i, hd:hd + 1], scalar2=None, op0=ALU.mult),
                         reads=["eGr"], writes=[bvk, f"vs_{hd}p{pp}"])
                bo, bok = bank()
                for h in range(4):
                    hd = dr * 4 + h
                    u = U[hd][pp]
                    p.op("tensor", lambda e: e.matmul(Q(bo, h), lhsT=u["qdT"][:], rhs=u["Sb"][:], start=True, stop=False),
                         reads=[f"qdT_{hd}p{pp}", f"Sb_{hd}"], writes=[bok])
                    p.op("tensor", lambda e: e.matmul(Q(bo, h), lhsT=u["aaT"][:], rhs=u["vnew"][:], start=False, stop=True),
                         reads=[f"aaT_{hd}p{pp}", f"vnew_{hd}p{pp}"], writes=[bok])
                p.op("scalar", lambda e: e.activation(out=ib["o"][:, :], in_=bo[:, :], func=AF.Copy), writes=[bok, f"o{bs}{dr}"])
                bS, bSk = bank()
                for h in range(4):
                    hd = dr * 4 + h
                    u = U[hd][pp]
                    p.op("tensor", lambda e: e.matmul(Q(bS, h), lhsT=ib["ktb"][:, h * 128:(h + 1) * 128], rhs=u["vs"][:], start=True, stop=True),
                         reads=[f"ktb{bs}{dr}", f"vs_{hd}p{pp}"], writes=[bSk])
                for h in range(4):
                    hd = dr * 4 + h
                    u = U[hd][pp]
                    p.op("vector", lambda e: e.scalar_tensor_tensor(out=u["S"][:], in0=u["S"][:], scalar=ge[:, i, hd:hd + 1], in1=Q(bS, h),
                                                                    op0=ALU.mult, op1=ALU.add), reads=["ge"], writes=[bSk, f"S_{hd}"])
                    p.op("gpsimd", lambda e: e.tensor_copy(out=u["Sb"][:], in_=u["S"][:]), reads=[f"S_{hd}"], writes=[f"Sb_{hd}"])
                dst = o_f if dr == 0 else o_b
                p.dma("sync", dst[i * 128:(i + 1) * 128, :], ib["o"][:, :], reads=[f"o{bs}{dr}"])
        p.barrier()


def phase_b3(p, c, T, o_f, o_b, ztok, gdn_norm, yaT):
    nc = p.nc
    NT = T // 128
    with ExitStack() as es:
        def sb(name, shape, dt=F32):
            return es.enter_context(nc.sbuf_tensor(f"B3_{_uid()}_" + name, shape, dt))
        gnb = sb("gnb", [128, 128])
        of = [sb(f"of{i}", [128, 512]) for i in range(2)]
        ob = [sb(f"ob{i}", [128, 512]) for i in range(2)]
        zt = [sb(f"zt{i}", [128, 512]) for i in range(2)]
        junk_2 = [sb(f"junk{i}", [128, 128]) for i in range(2)]
        ss_2 = [sb(f"ss{i}", [128, 4]) for i in range(2)]
        rs_2 = [sb(f"rs{i}", [128, 4]) for i in range(2)]
        on_2 = [sb(f"on{i}", [128, 512]) for i in range(2)]
        yb_2 = [sb(f"yb{i}", [128, 512], BF16) for i in range(2)]
        yT = [sb(f"yT{i}", [128, 4, 128], BF16) for i in range(2)]
        p.dma("sync", gnb[:], gdn_norm.to_broadcast([128, 128]), writes=["gnb"])
        for i in range(NT):
            b = i % 2
            junk, ss, rs, on, yb = junk_2[b], ss_2[b], rs_2[b], on_2[b], yb_2[b]
            p.dma("sync", of[b][:], o_f[i * 128:(i + 1) * 128, :], writes=[f"of{b}"])
            p.dma("sync", ob[b][:], o_b[i * 128:(i + 1) * 128, :], writes=[f"ob{b}"])
            p.dma("sync", zt[b][:], ztok[i * 128:(i + 1) * 128, :], writes=[f"zt{b}"])
            p.op("gpsimd", lambda e: e.tensor_tensor(out=of[b][:], in0=of[b][:], in1=ob[b][:], op=ALU.add), reads=[f"ob{b}", f"of{b}"], writes=[f"of{b}"])
            for h in range(4):
                p.op("scalar", lambda e: e.activation(out=junk[:], in_=of[b][:, h * 128:(h + 1) * 128], func=AF.Square, accum_out=ss[:, h:h + 1]),
                     reads=[f"of{b}"], writes=[f"junk_{b}", f"ss_{b}"])
            p.op("vector", lambda e: e.tensor_scalar(out=rs[:], in0=ss[:], scalar1=1.0 / 128, scalar2=RMS_EPS, op0=ALU.mult, op1=ALU.add), reads=[f"ss_{b}"], writes=[f"rs_{b}"])
            p.op("scalar", lambda e: e.activation(out=rs[:], in_=rs[:], func=AF.Sqrt), reads=[f"rs_{b}"], writes=[f"rs_{b}"])
            p.op("vector", lambda e: e.reciprocal(out=rs[:], in_=rs[:]), reads=[f"rs_{b}"], writes=[f"rs_{b}"])
            for h in range(4):
                p.op("vector", lambda e: e.scalar_tensor_tensor(out=on[:, h * 128:(h + 1) * 128], in0=of[b][:, h * 128:(h + 1) * 128], scalar=rs[:, h:h + 1],
                                                                in1=gnb[:], op0=ALU.mult, op1=ALU.mult), reads=[f"of{b}", f"rs_{b}", "gnb"], writes=[f"on_{b}"])
            p.op("scalar", lambda e: e.activation(out=zt[b][:], in_=zt[b][:], func=AF.Silu), reads=[f"zt{b}"], writes=[f"zt{b}"])
            p.op("gpsimd", lambda e: e.tensor_tensor(out=yb[:], in0=on[:], in1=zt[b][:], op=ALU.mult), reads=[f"on_{b}", f"zt{b}"], writes=[f"yb_{b}"])
            for h in range(4):
                p.op("tensor", lambda e: e.transpose(out=c.psb[:, h * 128:(h + 1) * 128], in_=yb[:, h * 128:(h + 1) * 128], identity=c.identb[:]),
                     reads=[f"yb_{b}", "identb"], writes=["ps7"])
            p.op("vector", lambda e: e.tensor_copy(out=yT[b][:, :, :], in_=c.psb[:, 0:512].rearrange("p (h t) -> p h t", t=128)), writes=["ps7", f"yT{b}"])
            for h_ in range(4):
                p.dma("sync", yaT[h_ * 128:(h_ + 1) * 128, i * 128:(i + 1) * 128], yT[b][:, h_, :], reads=[f"yT{b}"])
        p.barrier()


ATT_DIL = (1, 4, 16)


def alloc_att_consts(p, c, es):
    nc = p.nc
    c.amask = es.enter_context(nc.sbuf_tensor("amask", [128, 256], F32))
    p.op("gpsimd", lambda e: e.memset(c.amask[:], 0.0), writes=["amask"])
    p.op("gpsimd", lambda e: e.affine_select(out=c.amask[:], in_=c.amask[:], pattern=[[1, 256]], compare_op=ALU.is_ge, fill=-30000.0,
                                              base=0, channel_multiplier=-1), reads=["amask"], writes=["amask"])
    p.op("gpsimd", lambda e: e.affine_select(out=c.amask[:], in_=c.amask[:], pattern=[[-1, 256]], compare_op=ALU.is_ge, fill=-30000.0,
                                              base=128, channel_multiplier=1), reads=["amask"], writes=["amask"])
    c.amask0 = es.enter_context(nc.sbuf_tensor("amask0", [128, 128], F32))
    p.op("gpsimd", lambda e: e.memset(c.amask0[:], 0.0), writes=["amask0"])
    p.op("gpsimd", lambda e: e.affine_select(out=c.amask0[:], in_=c.amask0[:], pattern=[[-1, 128]], compare_op=ALU.is_ge, fill=-30000.0,
                                              base=64, channel_multiplier=1), reads=["amask0"], writes=["amask0"])


def phase_c(p, c, T, qkrope, qkrest, vTb, att):
    for _ in phase_c_gen(p, c, T, qkrope, qkrest, vTb, att):
        pass


def phase_c_gen(p, c, T, qkrope, qkrest, vTb, att, es_ext=None, shared=False):
    nc = p.nc
    NT = T // 128
    with ExitStack() as es_own:
        es = es_ext if es_ext is not None else es_own
        def sb(name, shape, dt=F32):
            return es.enter_context(nc.sbuf_tensor(f"C_{_uid()}_" + name, shape, dt))
        qT = [sb(f"qT{i}", [64, T], BF16) for i in range(2)]
        kT = [sb(f"kT{i}", [64, T], BF16) for i in range(2)]
        vT = [sb(f"vT{i}", [64, T], BF16) for i in range(2)]
        vtk = [sb(f"vtk{i}", [128, NT, 64], BF16) for i in range(2)]
        sm = [sb(f"sm{i}", [128, 256]) for i in range(2)]
        P = [sb(f"P{i}", [128, 256], BF16) for i in range(2)]
        PT = [sb(f"PT{i}", [128, 2, 128], BF16) for i in range(2)]
        stt = [sb(f"stt{i}", [128, 4]) for i in range(2)]
        oo = [sb(f"oo{i}", [128, 65]) for i in range(4)]
        blk = [0]
        for h in range(12):
            hb = h % 2
            r = ATT_DIL[h // 4]
            M = T // r
            NB = M // 128
            p.dma("sync", qT[hb][0:16, :], qkrope[h * 16:(h + 1) * 16, :], writes=[f"qT{hb}"])
            p.dma("sync", qT[hb][16:64, :], qkrest[h * 48:(h + 1) * 48, :], writes=[f"qT{hb}"])
            p.dma("sync", kT[hb][0:16, :], qkrope[(12 + h) * 16:(13 + h) * 16, :], writes=[f"kT{hb}"])
            p.dma("sync", kT[hb][16:64, :], qkrest[(12 + h) * 48:(13 + h) * 48, :], writes=[f"kT{hb}"])
            p.dma("sync", vT[hb][:, :], vTb[h * 64:(h + 1) * 64, :], writes=[f"vT{hb}"])
            for g0 in range(0, NT, 16):
                bi = 4 if shared else 6 + (g0 // 16) % 2
                pv = c.ps[bi][:, :].bitcast(BF16)
                for kb in range(g0, min(NT, g0 + 16)):
                    s, mb = divmod(kb, NB)
                    c0 = s + r * mb * 128
                    src = vT[hb][:, c0: c0 + r * 127 + 1: r]
                    p.op("tensor", lambda e: e.transpose(out=pv[:, (kb - g0) * 64:(kb - g0 + 1) * 64], in_=src, identity=c.identb[0:64, 0:64]),
                         reads=[f"vT{hb}", "identb"], writes=[f"ps{bi}"])
                n_ = min(NT, g0 + 16) - g0
                p.op("scalar", lambda e: e.activation(out=vtk[hb][:, g0:g0 + n_, :], in_=pv[:, 0:n_ * 64].rearrange("p (k d) -> p k d", d=64), func=AF.Copy),
                     writes=[f"ps{bi}", f"vtk{hb}"])
            for s in range(r):
                for jb in range(NB + 1):
                    n = blk[0]
                    blk[0] += 1
                    b2 = n % 2
                    m0 = jb * 128 - 64
                    qlo = max(m0, 0)
                    qhi = min(m0 + 128, M)
                    nq = qhi - qlo
                    kblocks = [x for x in (jb - 1, jb) if 0 <= x < NB]
                    nk = 128 * len(kblocks)
                    klo = kblocks[0] * 128
                    if jb == 0:
                        mask = c.amask0[0:nq, 0:nk]
                        mk = "amask0"
                    else:
                        coff = 0 if kblocks[0] == jb - 1 else 128
                        mask = c.amask[0:nq, coff:coff + nk]
                        mk = "amask"
                    qcols = qT[hb][:, s + r * qlo: s + r * (qhi - 1) + 1: r]
                    kcols = kT[hb][:, s + r * klo: s + r * (klo + nk - 1) + 1: r]
                    if shared:
                        bS, bP, bO = c.ps[b2], c.ps[2], c.ps[3]
                        bSk, bPk, bOk = f"ps{b2}", "ps2", "ps3"
                    else:
                        bS, bP, bO = c.ps[b2], c.ps[2 + b2], c.ps[4 + b2]
                        bSk, bPk, bOk = f"ps{b2}", f"ps{2 + b2}", f"ps{4 + b2}"
                    p.op("tensor", lambda e: e.matmul(bS[0:nq, 0:nk], lhsT=qcols, rhs=kcols, start=True, stop=True),
                         reads=[f"qT{hb}", f"kT{hb}"], writes=[bSk])
                    p.op("vector", lambda e: e.scalar_tensor_tensor(out=sm[b2][0:nq, 0:nk], in0=bS[0:nq, 0:nk], scalar=0.125, in1=mask,
                                                                    op0=ALU.mult, op1=ALU.add), reads=[mk], writes=[bSk, f"sm{b2}"])
                    st_ = stt[b2]
                    p.op("vector", lambda e: e.reduce_max(out=st_[0:nq, 1:2], in_=sm[b2][0:nq, 0:nk], axis=AX.X, negate=True), reads=[f"sm{b2}"], writes=[f"stt{b2}b"])
                    p.op("scalar", lambda e: e.activation(out=P[b2][0:nq, 0:nk], in_=sm[b2][0:nq, 0:nk], func=AF.Exp, bias=st_[0:nq, 1:2], scale=1.0,
                                                          accum_out=st_[0:nq, 2:3]), reads=[f"sm{b2}", f"stt{b2}b"], writes=[f"P{b2}", f"stt{b2}c"])
                    pt = bP[:, :].bitcast(BF16)
                    for ci in range(len(kblocks)):
                        p.op("tensor", lambda e: e.transpose(out=pt[:, ci * 128: ci * 128 + nq], in_=P[b2][0:nq, ci * 128:(ci + 1) * 128], identity=c.identb[0:nq, 0:nq]),
                             reads=[f"P{b2}", "identb"], writes=[bPk])
                    for ci in range(len(kblocks)):
                        if ci == 0:
                            p.op("scalar", lambda e: e.activation(out=PT[b2][:, ci, 0:nq], in_=pt[:, ci * 128: ci * 128 + nq], func=AF.Copy), writes=[bPk, f"PT{b2}_{ci}"])
                        else:
                            p.op("vector", lambda e: e.tensor_copy(out=PT[b2][:, ci, 0:nq], in_=pt[:, ci * 128: ci * 128 + nq]), writes=[bPk, f"PT{b2}_{ci}"])
                    for ci, kbk in enumerate(kblocks):
                        p.op("tensor", lambda e: e.matmul(bO[0:nq, 0:64], lhsT=PT[b2][:, ci, 0:nq], rhs=vtk[hb][:, s * NB + kbk, :],
                                                          start=(ci == 0), stop=(ci == len(kblocks) - 1)),
                             reads=[f"PT{b2}_{ci}", f"vtk{hb}"], writes=[bOk])
                    p.op("vector", lambda e: e.reciprocal(out=st_[0:nq, 3:4], in_=st_[0:nq, 2:3]), reads=[f"stt{b2}c"], writes=[f"stt{b2}d"])
                    o4 = oo[n % 4]
                    ok = f"oo{n % 4}"
                    p.op("scalar", lambda e: e.activation(out=o4[0:nq, 64:65], in_=st_[0:nq, 2:3], func=AF.Ln), reads=[f"stt{b2}c"], writes=[ok])
                    p.op("gpsimd", lambda e: e.tensor_tensor(out=o4[0:nq, 64:65], in0=o4[0:nq, 64:65], in1=st_[0:nq, 1:2], op=ALU.subtract),
                         reads=[ok, f"stt{b2}b"], writes=[ok])
                    p.op("vector", lambda e: e.tensor_scalar(out=o4[0:nq, 0:64], in0=bO[0:nq, 0:64], scalar1=st_[0:nq, 3:4], scalar2=None, op0=ALU.mult),
                         reads=[f"stt{b2}d"], writes=[bOk, ok + "o"])
                    t0 = s + r * qlo
                    dst = att[t0: t0 + r * (nq - 1) + 1: r, h, :]
                    p.dma("sync", dst, o4[0:nq, :], reads=[ok, ok + "o"])
                    yield
        if not shared:
            p.barrier()


def phase_d(p, c, T, yaT, att, gatesT, xres, w_a, w_b, w_out, norm_w2, w_router, h2b, aff, x_in=None):
    if x_in is None:
        x_in = xres
    nc = p.nc
    NS = T // 512
    with ExitStack() as es:
        def sb(name, shape, dt=F32):
            return es.enter_context(nc.sbuf_tensor(f"D_{_uid()}_" + name, shape, dt))
        Wa = sb("Wa", [128, 4, 1024], BF16)
        Wb = sb("Wb", [128, 2, 1024], BF16)
        Wo = sb("Wo", [128, 8, 1024], BF16)
        Wr = sb("Wr", [128, 8, 16])
        stg = [sb(f"stg{i}", [128, 1024]) for i in range(2)]
        nwb = sb("nwb", [128, 1024])
        at = [sb(f"at{i}", [128, 12, 65]) for i in range(2)]
        wl_2 = [sb(f"wl{i}", [128, 12]) for i in range(2)]
        mx_2 = [sb(f"mx{i}", [128, 4]) for i in range(2)]
        sm_2 = [sb(f"sm{i}", [128, 4]) for i in range(2)]
        tmpw_2 = [sb(f"tmpw{i}", [128, 12, 64]) for i in range(2)]
        ybt_2 = [sb(f"ybt{i}", [128, 256]) for i in range(2)]
        ybb_2 = [sb(f"ybb{i}", [128, 256], BF16) for i in range(2)]
        ybT = [sb(f"ybT{i}", [128, 2, 512], BF16) for i in range(2)]
        yaTs = [sb(f"yaTs{i}", [128, 4, 512], BF16) for i in range(2)]
        gts = [sb(f"gts{i}", [128, 16, 512], BF16) for i in range(2)]
        t1_2 = [sb(f"t1{i}", [128, 512]) for i in range(2)]
        t2_2 = [sb(f"t2{i}", [128, 512]) for i in range(2)]
        mixT = [sb(f"mixT{i}", [128, 8, 512], BF16) for i in range(2)]
        xt = [sb(f"xt{i}", [128, 1024]) for i in range(2)]
        junk_2 = [sb(f"junk{i}", [128, 1024], BF16) for i in range(2)]
        h2_2 = [sb(f"h2{i}", [128, 1024]) for i in range(2)]
        h2bf = [sb(f"h2bf{i}", [128, 1024], BF16) for i in range(2)]
        h2T_2 = [sb(f"h2T{i}", [128, 2, 8, 128], BF16) for i in range(2)]
        h2lo_2 = [sb(f"h2lo{i}", [128, 1024], BF16) for i in range(2)]
        Wrb = sb("Wrb", [128, 8, 32], BF16)
        Wrt = sb("Wrt", [128, 8, 16])
        lg48_2 = [sb(f"lg48{i}", [128, 48]) for i in range(2)]
        h2Tf_2 = [sb(f"h2Tf{i}", [128, 8, 128]) for i in range(2)] if ROUTER_F32 else [None, None]
        st_2 = [sb(f"st{i}", [128, 8]) for i in range(2)]
        lg_2 = [sb(f"lg{i}", [128, 16]) for i in range(2)]
        af = [sb(f"af{i}", [128, 16]) for i in range(2)]

        p.dma("sync", nwb[:], norm_w2.to_broadcast([128, 1024]), writes=["nwb"])
        p.dma("sync", Wr[:, :, :], w_router.rearrange("(k p) e -> p k e", p=128), writes=["Wr"])
        p.op("vector", lambda e: e.tensor_copy(out=Wrb[:, :, 0:16], in_=Wr[:, :, :]), reads=["Wr"], writes=["Wrb"])
        p.op("vector", lambda e: e.tensor_tensor(out=Wrt[:, :, :], in0=Wr[:, :, :], in1=Wrb[:, :, 0:16], op=ALU.subtract), reads=["Wr", "Wrb"], writes=["Wrt"])
        p.op("vector", lambda e: e.tensor_copy(out=Wrb[:, :, 16:32], in_=Wrt[:, :, :]), reads=["Wrt"], writes=["Wrb"])
        si = 0
        for (W, src, nk, key) in ((Wa, w_a, 4, "Wa"), (Wb, w_b, 2, "Wb"), (Wo, w_out, 8, "Wo")):
            for k in range(nk):
                s_, sk = stg[si % 2], f"stg{si % 2}"
                si += 1
                p.dma("sync", s_[:], src[k * 128:(k + 1) * 128, :], writes=[sk])
                p.op("gpsimd", lambda e: e.tensor_copy(out=W[:, k, :], in_=s_[:]), reads=[sk], writes=[key])
        bn = [0]

        def bank():
            i = bn[0] % 6
            bn[0] += 1
            return c.ps[i], f"ps{i}"

        for s in range(NS):
            sl = s % 2
            c0 = s * 512
            for k_ in range(4):
                p.dma("sync", yaTs[sl][:, k_, :], yaT[k_ * 128:(k_ + 1) * 128, c0:c0 + 512], writes=[f"yaTs{sl}"])
            for k_ in range(16):
                p.dma("sync", gts[sl][:, k_, :], gatesT[k_ * 128:(k_ + 1) * 128, c0:c0 + 512], writes=[f"gts{sl}"])
            for ti in range(4):
                r0 = c0 + ti * 128
                a_, ak = at[ti % 2], f"at{ti % 2}"
                pq = ti % 2
                wl, mx, sm, tmpw, ybt, ybb = wl_2[pq], mx_2[pq], sm_2[pq], tmpw_2[pq], ybt_2[pq], ybb_2[pq]
                p.dma("sync", a_[:, :, :], att[r0:r0 + 128, :, :], writes=[ak])
                L = a_[:, :, 64]
                p.op("vector", lambda e: e.tensor_tensor(out=mx[:], in0=L[:, 0:4], in1=L[:, 4:8], op=ALU.max), reads=[ak], writes=[f"mx_{pq}"])
                p.op("vector", lambda e: e.tensor_tensor(out=mx[:], in0=mx[:], in1=L[:, 8:12], op=ALU.max), reads=[ak, f"mx_{pq}"], writes=[f"mx_{pq}"])
                p.op("vector", lambda e: e.tensor_tensor(out=wl[:, :].rearrange("p (g h) -> p g h", h=4), in0=L.rearrange("p (g h) -> p g h", h=4),
                                                         in1=mx[:, :].unsqueeze(1).to_broadcast([128, 3, 4]), op=ALU.subtract), reads=[ak, f"mx_{pq}"], writes=[f"wl_{pq}"])
                p.op("scalar", lambda e: e.activation(out=wl[:], in_=wl[:], func=AF.Exp), reads=[f"wl_{pq}"], writes=[f"wl_{pq}"])
                p.op("vector", lambda e: e.tensor_tensor(out=sm[:], in0=wl[:, 0:4], in1=wl[:, 4:8], op=ALU.add), reads=[f"wl_{pq}"], writes=[f"sm_{pq}"])
                p.op("vector", lambda e: e.tensor_tensor(out=sm[:], in0=sm[:], in1=wl[:, 8:12], op=ALU.add), reads=[f"wl_{pq}", f"sm_{pq}"], writes=[f"sm_{pq}"])
                p.op("vector", lambda e: e.reciprocal(out=sm[:], in_=sm[:]), reads=[f"sm_{pq}"], writes=[f"sm_{pq}"])
                p.op("vector", lambda e: e.tensor_tensor(out=wl[:, :].rearrange("p (g h) -> p g h", h=4), in0=wl[:, :].rearrange("p (g h) -> p g h", h=4),
                                                         in1=sm[:, :].unsqueeze(1).to_broadcast([128, 3, 4]), op=ALU.mult), reads=[f"wl_{pq}", f"sm_{pq}"], writes=[f"wl_{pq}"])
                p.op("vector", lambda e: e.tensor_tensor(out=tmpw[:, :, :], in0=a_[:, :, 0:64], in1=wl[:, :].unsqueeze(2).to_broadcast([128, 12, 64]), op=ALU.mult),
                     reads=[ak, f"wl_{pq}"], writes=[f"tmpw_{pq}"])
                tv = tmpw[:, :, :].rearrange("p (g h) d -> p g (h d)", h=4)
                p.op("gpsimd", lambda e: e.tensor_tensor(out=ybt[:], in0=tv[:, 0, :], in1=tv[:, 1, :], op=ALU.add), reads=[f"tmpw_{pq}"], writes=[f"ybt_{pq}"])
                p.op("gpsimd", lambda e: e.tensor_tensor(out=ybb[:], in0=ybt[:], in1=tv[:, 2, :], op=ALU.add), reads=[f"tmpw_{pq}", f"ybt_{pq}"], writes=[f"ybb_{pq}"])
                for k in range(2):
                    p.op("tensor", lambda e: e.transpose(out=c.psb[:, k * 128:(k + 1) * 128], in_=ybb[:, k * 128:(k + 1) * 128], identity=c.identb[:]),
                         reads=[f"ybb_{pq}", "identb"], writes=["ps7"])
                p.op("scalar", lambda e: e.activation(out=ybT[sl][:, :, ti * 128:(ti + 1) * 128], in_=c.psb[:, 0:256].rearrange("p (k t) -> p k t", t=128), func=AF.Copy),
                     writes=["ps7", f"ybT{sl}"])
            for dm in range(8):
                t1, t2 = t1_2[dm % 2], t2_2[dm % 2]
                bA, bAk = bank()
                bB, bBk = bank()
                for k in range(4):
                    p.op("tensor", lambda e: e.matmul(bA[:, :], lhsT=Wa[:, k, dm * 128:(dm + 1) * 128], rhs=yaTs[sl][:, k, :], start=(k == 0), stop=(k == 3)),
                         reads=["Wa", f"yaTs{sl}"], writes=[bAk])
                for k in range(2):
                    p.op("tensor", lambda e: e.matmul(bB[:, :], lhsT=Wb[:, k, dm * 128:(dm + 1) * 128], rhs=ybT[sl][:, k, :], start=(k == 0), stop=(k == 1)),
                         reads=["Wb", f"ybT{sl}"], writes=[bBk])
                p.op("vector", lambda e: e.tensor_tensor(out=t1[:], in0=bA[:, :], in1=gts[sl][:, dm, :], op=ALU.mult), reads=[f"gts{sl}"], writes=[bAk, f"t1_{dm % 2}"])
                p.op("vector", lambda e: e.tensor_tensor(out=t2[:], in0=bB[:, :], in1=gts[sl][:, 8 + dm, :], op=ALU.mult), reads=[f"gts{sl}"], writes=[bBk, f"t2_{dm % 2}"])
                p.op("gpsimd", lambda e: e.tensor_tensor(out=mixT[sl][:, dm, :], in0=t1[:], in1=t2[:], op=ALU.add), reads=[f"t1_{dm % 2}", f"t2_{dm % 2}"], writes=[f"mixT{sl}"])
            for ti in range(4):
                r0 = c0 + ti * 128
                x_, xk = xt[ti % 2], f"xt{ti % 2}"
                pq = ti % 2
                junk, h2, h2T, h2lo, st, lg, lg48 = junk_2[pq], h2_2[pq], h2T_2[pq], h2lo_2[pq], st_2[pq], lg_2[pq], lg48_2[pq]
                p.dma("sync", x_[:], x_in[r0:r0 + 128, :], writes=[xk])
                for hf in range(2):
                    bX, bXk = bank()
                    for k in range(8):
                        p.op("tensor", lambda e: e.matmul(bX[:, :], lhsT=mixT[sl][:, k, ti * 128:(ti + 1) * 128], rhs=Wo[:, k, hf * 512:(hf + 1) * 512],
                                                          start=(k == 0), stop=(k == 7)), reads=["Wo", f"mixT{sl}"], writes=[bXk])
                    p.op("vector", lambda e: e.tensor_tensor(out=x_[:, hf * 512:(hf + 1) * 512], in0=bX[:, :], in1=x_[:, hf * 512:(hf + 1) * 512], op=ALU.add),
                         reads=[xk], writes=[bXk, xk])
                p.dma("sync", xres[r0:r0 + 128, :], x_[:], reads=[xk])
                p.op("scalar", lambda e: e.activation(out=junk[:], in_=x_[:], func=AF.Square, accum_out=st[:, 0:1]), reads=[xk], writes=[f"junk_{pq}", f"st0_{pq}"])
                p.op("vector", lambda e: e.tensor_scalar(out=st[:, 1:2], in0=st[:, 0:1], scalar1=1.0 / 1024, scalar2=RMS_EPS, op0=ALU.mult, op1=ALU.add),
                     reads=[f"st0_{pq}"], writes=[f"st1_{pq}"])
                p.op("scalar", lambda e: e.activation(out=st[:, 2:3], in_=st[:, 1:2], func=AF.Sqrt), reads=[f"st1_{pq}"], writes=[f"st2_{pq}"])
                p.op("vector", lambda e: e.reciprocal(out=st[:, 3:4], in_=st[:, 2:3]), reads=[f"st2_{pq}"], writes=[f"st3_{pq}"])
                p.op("vector", lambda e: e.scalar_tensor_tensor(out=h2[:], in0=x_[:], scalar=st[:, 3:4], in1=nwb[:], op0=ALU.mult, op1=ALU.mult),
                     reads=[xk, f"st3_{pq}", "nwb"], writes=[f"h2_{pq}"])
                hb_, hbk = h2bf[ti % 2], f"h2bf{ti % 2}"
                p.op("gpsimd", lambda e: e.tensor_copy(out=hb_[:], in_=h2[:]), reads=[f"h2_{pq}"], writes=[hbk])
                p.dma("sync", h2b[r0:r0 + 128, :], hb_[:], reads=[hbk])
                if ROUTER_F32:
                    h2Tf = h2Tf_2[pq]
                    for hf in range(2):
                        bT, bTk = bank()
                        for k in range(4):
                            kk = hf * 4 + k
                            p.op("tensor", lambda e: e.transpose(out=bT[:, k * 128:(k + 1) * 128], in_=h2[:, kk * 128:(kk + 1) * 128], identity=c.identf[:]),
                                 reads=[f"h2_{pq}", "identf"], writes=[bTk])
                        p.op("scalar", lambda e: e.activation(out=h2Tf[:, hf * 4:(hf + 1) * 4, :], in_=bT[:, :].rearrange("p (k t) -> p k t", t=128), func=AF.Copy),
                             writes=[bTk, f"h2Tf{hf}_{pq}"])
                    bL, bLk = bank()
                    for k in range(8):
                        p.op("tensor", lambda e: e.matmul(bL[:, 0:16], lhsT=h2Tf[:, k, :], rhs=Wr[:, k, :], start=(k == 0), stop=(k == 7)),
                             reads=[f"h2Tf0_{pq}", f"h2Tf1_{pq}", "Wr"], writes=[bLk])
                    p.op("vector", lambda e: e.tensor_copy(out=lg[:], in_=bL[:, 0:16]), writes=[bLk, f"lg_{pq}"])
                else:
                    p.op("gpsimd", lambda e: e.tensor_tensor(out=h2lo[:], in0=h2[:], in1=hb_[:], op=ALU.subtract), reads=[f"h2_{pq}", hbk], writes=[f"h2lo_{pq}"])
                    for part, (src_, srck_) in enumerate(((hb_, hbk), (h2lo, f"h2lo_{pq}"))):
                        bT, bTk = bank()
                        tpv = bT[:, :].bitcast(BF16)
                        for k in range(8):
                            p.op("tensor", lambda e: e.transpose(out=tpv[:, k * 128:(k + 1) * 128], in_=src_[:, k * 128:(k + 1) * 128], identity=c.identb[:]),
                                 reads=[srck_, "identb"], writes=[bTk])
                        if part == 0:
                            p.op("scalar", lambda e: e.activation(out=h2T[:, part, :, :], in_=tpv[:, :].rearrange("p (k t) -> p k t", t=128), func=AF.Copy),
                                 writes=[bTk, f"h2T{part}_{pq}"])
                        else:
                            p.op("vector", lambda e: e.tensor_copy(out=h2T[:, part, :, :], in_=tpv[:, :].rearrange("p (k t) -> p k t", t=128)),
                                 writes=[bTk, f"h2T{part}_{pq}"])
                    bL, bLk = bank()
                    for k in range(8):
                        p.op("tensor", lambda e: e.matmul(bL[:, 0:32], lhsT=h2T[:, 0, k, :], rhs=Wrb[:, k, :], start=(k == 0), stop=(k == 7)),
                             reads=[f"h2T0_{pq}", "Wrb"], writes=[bLk])
                    for k in range(8):
                        p.op("tensor", lambda e: e.matmul(bL[:, 32:48], lhsT=h2T[:, 1, k, :], rhs=Wrb[:, k, 0:16], start=(k == 0), stop=(k == 7)),
                             reads=[f"h2T1_{pq}", "Wrb"], writes=[bLk])
                    p.op("vector", lambda e: e.tensor_copy(out=lg48[:], in_=bL[:, 0:48]), writes=[bLk, f"lg48_{pq}"])
                    p.op("vector", lambda e: e.tensor_tensor(out=lg[:], in0=lg48[:, 0:16], in1=lg48[:, 16:32], op=ALU.add), reads=[f"lg48_{pq}"], writes=[f"lg_{pq}"])
                    p.op("vector", lambda e: e.tensor_tensor(out=lg[:], in0=lg[:], in1=lg48[:, 32:48], op=ALU.add), reads=[f"lg_{pq}", f"lg48_{pq}"], writes=[f"lg_{pq}"])
                p.op("vector", lambda e: e.reduce_max(out=st[:, 4:5], in_=lg[:], axis=AX.X), reads=[f"lg_{pq}"], writes=[f"st4_{pq}"])
                p.op("vector", lambda e: e.tensor_scalar(out=st[:, 5:6], in0=st[:, 4:5], scalar1=-1.0, scalar2=None, op0=ALU.mult), reads=[f"st4_{pq}"], writes=[f"st5_{pq}"])
                p.op("scalar", lambda e: e.activation(out=lg[:], in_=lg[:], func=AF.Exp, bias=st[:, 5:6], scale=1.0, accum_out=st[:, 6:7]),
                     reads=[f"lg_{pq}", f"st5_{pq}"], writes=[f"lg_{pq}", f"st6_{pq}"])
                p.op("vector", lambda e: e.reciprocal(out=st[:, 7:8], in_=st[:, 6:7]), reads=[f"st6_{pq}"], writes=[f"st7_{pq}"])
                a2, a2k = af[ti % 2], f"af{ti % 2}"
                p.op("vector", lambda e: e.tensor_scalar(out=a2[:], in0=lg[:], scalar1=st[:, 7:8], scalar2=None, op0=ALU.mult), reads=[f"lg_{pq}", f"st7_{pq}"], writes=[a2k])
                p.dma("sync", aff[r0:r0 + 128, :], a2[:], reads=[a2k])
        p.barrier()


def phase_e(p, c, T, aff, h2b, xres, w_gate, w_up, w_down, n_iter=30):
    nc = p.nc
    NT = T // 128
    C = T // 8
    NG = C // 128
    NE = 16
    with ExitStack() as es0:
        def sb0(name, shape, dt=F32):
            return es0.enter_context(nc.sbuf_tensor(f"E_{_uid()}_" + name, shape, dt))
        idx_all = sb0("idx_all", [128, NE, NG], I32)
        gate_all = sb0("gate_all", [128, NE, NG])
        with ExitStack() as es:
            def sb(name, shape, dt=F32):
                return es.enter_context(nc.sbuf_tensor(f"E1_{_uid()}_" + name, shape, dt))
            A = sb("A", [128, NT, NE])
            cmp = sb("cmp", [128, NT, NE])
            lo = sb("lo", [128, NE])
            hi = sb("hi", [128, NE])
            mid = sb("mid", [128, NE])
            cnt = sb("cnt", [128, NE])
            sel = sb("sel", [128, NE])
            d1 = sb("d1", [128, NE])
            triS = sb("triS", [128, 128])
            onesT = sb("onesT", [128, NT])
            rk = sb("rk", [128, NT, NE])
            nn = sb("nn", [128, NT, NE])
            base = sb("base", [128, NT, NE])
            pos = sb("pos", [128, NT, NE])
            posi = sb("posi", [128, NT, NE], I32)
            ai = sb("ai", [128, NT, NE], I32)
            bi_ = sb("bi", [128, NT, NE], I32)
            af_ = sb("af", [128, NT, NE])
            bf_ = sb("bf", [128, NT, NE])
            h1 = sb("h1", [128, NT, NE], BF16)
            h2 = sb("h2", [128, NT, NE], BF16)
            h3 = sb("h3", [128, NT, NE], BF16)
            r1 = sb("r1", [128, NT, NE])
            r2 = sb("r2", [128, NT, NE])
            iotaA = sb("iotaA", [128, 128])
            iotaB = sb("iotaB", [128, NG])
            tval = sb("tval", [128, NT], BF16)
            pval = sb("pval", [128, 1])
            pvalb = sb("pvalb", [128, NT], BF16)
            Aoh = [sb(f"Aoh{i}", [128, NT, 128], BF16) for i in range(2)]
            Boh = [sb(f"Boh{i}", [128, NT, NG]) for i in range(2)]
            rhs = [sb(f"rhs{i}", [128, NT, 5, NG], BF16) for i in range(2)]
            res = sb("res", [128, 5, NG])

            for t0 in range(0, NT, 8):
                t1 = min(NT, t0 + 8)
                p.dma("sync", A[:, t0:t1, :], aff[t0 * 128:t1 * 128, :].rearrange("(t p) e -> p t e", p=128), writes=["A"])
            p.op("gpsimd", lambda e: e.memset(lo[:], 0.0), writes=["lo"])
            p.op("gpsimd", lambda e: e.memset(hi[:], 1.0001), writes=["hi"])
            p.op("gpsimd", lambda e: e.memset(onesT[:], 1.0), writes=["onesT"])
            p.op("gpsimd", lambda e: e.memset(triS[:], 1.0), writes=["triS"])
            p.op("gpsimd", lambda e: e.affine_select(out=triS[:], in_=triS[:], pattern=[[1, 128]], compare_op=ALU.is_gt, fill=0.0, base=0, channel_multiplier=-1),
                 reads=["triS"], writes=["triS"])
            p.op("gpsimd", lambda e: e.iota(iotaA[:], pattern=[[1, 128]], base=0, channel_multiplier=0, allow_small_or_imprecise_dtypes=True), writes=["iotaA"])
            p.op("gpsimd", lambda e: e.iota(iotaB[:], pattern=[[1, NG]], base=0, channel_multiplier=0, allow_small_or_imprecise_dtypes=True), writes=["iotaB"])
            p.op("gpsimd", lambda e: e.iota(tval[:], pattern=[[1, NT]], base=0, channel_multiplier=0, allow_small_or_imprecise_dtypes=True), writes=["tval"])
            p.op("gpsimd", lambda e: e.iota(pval[:], pattern=[[0, 1]], base=0, channel_multiplier=1, allow_small_or_imprecise_dtypes=True), writes=["pval"])
            p.op("vector", lambda e: e.tensor_copy(out=pvalb[:], in_=pval[:, 0:1].to_broadcast([128, NT])), reads=["pval"], writes=["pvalb"])
            for it in range(n_iter):
                step = 2.0 ** -(it + 1)
                p.op("vector", lambda e: e.tensor_scalar(out=mid[:], in0=lo[:], scalar1=step, scalar2=None, op0=ALU.add), reads=["lo"], writes=["mid"])
                p.op("vector", lambda e: e.tensor_tensor(out=cmp[:, :, :], in0=A[:, :, :], in1=mid[:, :].unsqueeze(1).to_broadcast([128, NT, NE]), op=ALU.is_ge),
                     reads=["A", "mid"], writes=["cmp"])
                p.op("vector", lambda e: e.tensor_reduce(out=cnt[:], in_=cmp[:, :, :].rearrange("p t e -> p e t"), axis=AX.X, op=ALU.add), reads=["cmp"], writes=["cnt"])
                p.op("tensor", lambda e: e.matmul(c.ps[0][:, 0:NE], lhsT=c.onesf[:], rhs=cnt[:], start=True, stop=True), reads=["cnt", "onesf"], writes=["ps0"])
                p.op("vector", lambda e: e.tensor_scalar(out=sel[:], in0=c.ps[0][:, 0:NE], scalar1=float(C) - 0.5, scalar2=step, op0=ALU.is_ge, op1=ALU.mult),
                     writes=["ps0", "sel"])
                p.op("vector", lambda e: e.tensor_tensor(out=lo[:], in0=lo[:], in1=sel[:], op=ALU.add), reads=["sel", "lo"], writes=["lo"])
            p.op("vector", lambda e: e.tensor_tensor(out=cmp[:, :, :], in0=A[:, :, :], in1=lo[:, :].unsqueeze(1).to_broadcast([128, NT, NE]), op=ALU.is_ge),
                 reads=["A", "lo"], writes=["cmp"])
            cf = cmp[:, :, :].rearrange("p t e -> p (t e)")
            NC_ = NT * NE
            for c0 in range(0, NC_, 512):
                c1 = min(NC_, c0 + 512)
                p.op("tensor", lambda e: e.matmul(c.ps[1][:, 0:c1 - c0], lhsT=triS[:], rhs=cf[:, c0:c1], start=True, stop=True), reads=["cmp", "triS"], writes=["ps1"])
                p.op("vector", lambda e: e.tensor_copy(out=rk[:, :, :].rearrange("p t e -> p (t e)")[:, c0:c1], in_=c.ps[1][:, 0:c1 - c0]), writes=["ps1", "rk"])
                p.op("tensor", lambda e: e.matmul(c.ps[2][:, 0:c1 - c0], lhsT=c.onesf[:], rhs=cf[:, c0:c1], start=True, stop=True), reads=["cmp", "onesf"], writes=["ps2"])
                p.op("vector", lambda e: e.tensor_copy(out=nn[:, :, :].rearrange("p t e -> p (t e)")[:, c0:c1], in_=c.ps[2][:, 0:c1 - c0]), writes=["ps2", "nn"])
            for ex in range(NE):
                p.op("vector", lambda e: e.tensor_tensor_scan(out=base[:, :, ex], data0=onesT[:, :], data1=nn[:, :, ex], initial=0.0, op0=ALU.mult, op1=ALU.add),
                     reads=["nn", "onesT"], writes=["base"])
            p.op("vector", lambda e: e.tensor_tensor(out=pos[:, :, :], in0=base[:, :, :], in1=nn[:, :, :], op=ALU.subtract), reads=["base", "nn"], writes=["pos"])
            p.op("vector", lambda e: e.tensor_tensor(out=pos[:, :, :], in0=pos[:, :, :], in1=rk[:, :, :], op=ALU.add), reads=["pos", "rk"], writes=["pos"])
            p.op("vector", lambda e: e.tensor_copy(out=posi[:, :, :], in_=pos[:, :, :]), reads=["pos"], writes=["posi"])
            p.op("vector", lambda e: e.tensor_single_scalar(out=ai[:, :, :], in_=posi[:, :, :], scalar=(NG.bit_length() - 1), op=ALU.arith_shift_right), reads=["posi"], writes=["ai"])
            p.op("vector", lambda e: e.tensor_single_scalar(out=bi_[:, :, :], in_=posi[:, :, :], scalar=NG - 1, op=ALU.bitwise_and), reads=["posi"], writes=["bi"])
            p.op("vector", lambda e: e.tensor_copy(out=af_[:, :, :], in_=ai[:, :, :]), reads=["ai"], writes=["af"])
            p.op("vector", lambda e: e.tensor_copy(out=bf_[:, :, :], in_=bi_[:, :, :]), reads=["bi"], writes=["bf"])
            p.op("vector", lambda e: e.scalar_tensor_tensor(out=af_[:, :, :], in0=af_[:, :, :], scalar=1.0, in1=cmp[:, :, :], op0=ALU.add, op1=ALU.mult), reads=["af", "cmp"], writes=["af"])
            p.op("vector", lambda e: e.tensor_scalar(out=af_[:, :, :], in0=af_[:, :, :], scalar1=-1.0, scalar2=None, op0=ALU.add), reads=["af"], writes=["af"])
            p.op("vector", lambda e: e.tensor_copy(out=h1[:, :, :], in_=A[:, :, :]), reads=["A"], writes=["h1"])
            p.op("vector", lambda e: e.tensor_tensor(out=r1[:, :, :], in0=A[:, :, :], in1=h1[:, :, :], op=ALU.subtract), reads=["A", "h1"], writes=["r1"])
            p.op("vector", lambda e: e.tensor_copy(out=h2[:, :, :], in_=r1[:, :, :]), reads=["r1"], writes=["h2"])
            p.op("vector", lambda e: e.tensor_tensor(out=r2[:, :, :], in0=r1[:, :, :], in1=h2[:, :, :], op=ALU.subtract), reads=["r1", "h2"], writes=["r2"])
            p.op("vector", lambda e: e.tensor_copy(out=h3[:, :, :], in_=r2[:, :, :]), reads=["r2"], writes=["h3"])
            for ex in range(NE):
                b = ex % 2
                p.op("vector", lambda e: e.tensor_tensor(out=Aoh[b][:, :, :], in0=iotaA[:, :].unsqueeze(1).to_broadcast([128, NT, 128]),
                                                         in1=af_[:, :, ex:ex + 1].to_broadcast([128, NT, 128]), op=ALU.is_equal),
                     reads=["iotaA", "af"], writes=[f"Aoh{b}"])
                p.op("vector", lambda e: e.tensor_tensor(out=Boh[b][:, :, :], in0=iotaB[:, :].unsqueeze(1).to_broadcast([128, NT, NG]),
                                                         in1=bf_[:, :, ex:ex + 1].to_broadcast([128, NT, NG]), op=ALU.is_equal),
                     reads=["iotaB", "bf"], writes=[f"Boh{b}"])
                vals = [tval[:, :].unsqueeze(2).to_broadcast([128, NT, NG]), pvalb[:, :].unsqueeze(2).to_broadcast([128, NT, NG]),
                        h1[:, :, ex:ex + 1].to_broadcast([128, NT, NG]), h2[:, :, ex:ex + 1].to_broadcast([128, NT, NG]),
                        h3[:, :, ex:ex + 1].to_broadcast([128, NT, NG])]
                vkeys = ["tval", "pvalb", "h1", "h2", "h3"]
                for vi in range(5):
                    p.op("gpsimd", lambda e: e.tensor_tensor(out=rhs[b][:, :, vi, :], in0=Boh[b][:, :, :], in1=vals[vi], op=ALU.mult),
                         reads=[f"Boh{b}", vkeys[vi]], writes=[f"rhs{b}"])
                pb = 3 + b
                for t in range(NT):
                    p.op("tensor", lambda e: e.matmul(c.ps[pb][:, 0:5 * NG], lhsT=Aoh[b][:, t, :], rhs=rhs[b][:, t, :, :].rearrange("p v g -> p (v g)"),
                                                      start=(t == 0), stop=(t == NT - 1)), reads=[f"Aoh{b}", f"rhs{b}"], writes=[f"ps{pb}"])
                p.op("vector", lambda e: e.tensor_copy(out=res[:, :, :], in_=c.ps[pb][:, 0:5 * NG].rearrange("p (v g) -> p v g", g=NG)), writes=[f"ps{pb}", "res"])
                p.op("vector", lambda e: e.scalar_tensor_tensor(out=res[:, 0, :], in0=res[:, 0, :], scalar=128.0, in1=res[:, 1, :], op0=ALU.mult, op1=ALU.add),
                     reads=["res"], writes=["res"])
                p.op("vector", lambda e: e.tensor_copy(out=idx_all[:, ex, :], in_=res[:, 0, :]), reads=["res"], writes=["idx_all"])
                p.op("vector", lambda e: e.tensor_tensor(out=res[:, 2, :], in0=res[:, 2, :], in1=res[:, 3, :], op=ALU.add), reads=["res"], writes=["res"])
                p.op("vector", lambda e: e.tensor_tensor(out=gate_all[:, ex, :], in0=res[:, 2, :], in1=res[:, 4, :], op=ALU.add), reads=["res"], writes=["gate_all"])
            p.barrier()
        with ExitStack() as es:
            def sb(name, shape, dt=F32):
                return es.enter_context(nc.sbuf_tensor(f"E2_{_uid()}_" + name, shape, dt))
            NWB = 5
            wbuf = [sb(f"w{i}", [128, 8, 1024], BF16) for i in range(NWB)]
            stg = [sb(f"stg{i}", [128, 2, 1024]) for i in range(2)]
            xes = [sb(f"xe{i}", [128, NG, 1024], BF16) for i in range(2)]
            xeT = sb("xeT", [128, 8, C], BF16)
            hmT = sb("hmT", [128, 8, C], BF16)
            sg = [sb(f"sg{i}", [128, 512]) for i in range(2)]
            yo = [sb(f"yo{i}", [128, 1024]) for i in range(2)]
            wn = [0]
            sn = [0]
            cn = [0]

            def load_w(src):
                i = wn[0] % NWB
                wn[0] += 1
                for k2 in range(4):
                    s_ = stg[sn[0] % 2]
                    sk = f"stg{sn[0] % 2}"
                    sn[0] += 1
                    p.dma("sync", s_[:, :, :], src[k2 * 256:(k2 + 1) * 256, :].rearrange("(k p) f -> p k f", p=128), writes=[sk])
                    eng = ("gpsimd", "gpsimd", "vector", "gpsimd", "scalar", "gpsimd")[cn[0] % 6]
                    cn[0] += 1
                    if eng == "scalar":
                        p.op("scalar", lambda e: e.activation(out=wbuf[i][:, 2 * k2:2 * k2 + 2, :], in_=s_[:, :, :], func=AF.Copy), reads=[sk], writes=[f"w{i}"])
                    else:
                        p.op(eng, lambda e: e.tensor_copy(out=wbuf[i][:, 2 * k2:2 * k2 + 2, :], in_=s_[:, :, :]), reads=[sk], writes=[f"w{i}"])
                return wbuf[i], f"w{i}"

            bn = [0]

            def bank():
                i = bn[0] % 6
                bn[0] += 1
                return c.ps[i], f"ps{i}"

            CH = min(512, C)
            NCH = C // CH
            yn = [0]
            pending = [load_w(w_gate[0]), load_w(w_up[0]), load_w(w_down[0])]
            for ex in range(NE):
                (Wg, Wgk), (Wu, Wuk), (Wd, Wdk) = pending
                xe = xes[ex % 2]
                xek = f"xe{ex % 2}"
                for gi in range(NG):
                    p.dma("gpsimd", None, None, reads=["idx_all"], writes=[xek],
                          fn=lambda e: e.indirect_dma_start(out=xe[:, gi, :], out_offset=None, in_=h2b[:, :],
                                                            in_offset=bass.IndirectOffsetOnAxis(ap=idx_all[:, ex, gi:gi + 1], axis=0)))
                for gi in range(NG):
                    tbk = 6 + gi % 2
                    tps = c.ps[tbk][:, :].bitcast(BF16)
                    for k in range(8):
                        p.op("tensor", lambda e: e.transpose(out=tps[:, k * 128:(k + 1) * 128], in_=xe[:, gi, k * 128:(k + 1) * 128], identity=c.identb[:]),
                             reads=[xek, "identb"], writes=[f"ps{tbk}"])
                    if gi % 2 == 0:
                        p.op("vector", lambda e: e.tensor_copy(out=xeT[:, :, gi * 128:(gi + 1) * 128], in_=tps[:, :].rearrange("p (k t) -> p k t", t=128)),
                             writes=[f"ps{tbk}", "xeT0"])
                    else:
                        p.op("scalar", lambda e: e.activation(out=xeT[:, :, gi * 128:(gi + 1) * 128], in_=tps[:, :].rearrange("p (k t) -> p k t", t=128), func=AF.Copy),
                             writes=[f"ps{tbk}", "xeT1"])
                for fc in range(8):
                    for ch in range(NCH):
                        bG, bGk = bank()
                        bU, bUk = bank()
                        for k in range(8):
                            p.op("tensor", lambda e: e.matmul(bG[:, 0:CH], lhsT=Wg[:, k, fc * 128:(fc + 1) * 128], rhs=xeT[:, k, ch * CH:(ch + 1) * CH],
                                                              start=(k == 0), stop=(k == 7)), reads=[Wgk, "xeT0", "xeT1"], writes=[bGk])
                        for k in range(8):
                            p.op("tensor", lambda e: e.matmul(bU[:, 0:CH], lhsT=Wu[:, k, fc * 128:(fc + 1) * 128], rhs=xeT[:, k, ch * CH:(ch + 1) * CH],
                                                              start=(k == 0), stop=(k == 7)), reads=[Wuk, "xeT0", "xeT1"], writes=[bUk])
                        s_ = sg[(fc * NCH + ch) % 2]
                        sk = f"sg{(fc * NCH + ch) % 2}"
                        p.op("scalar", lambda e: e.activation(out=s_[:, 0:CH], in_=bG[:, 0:CH], func=AF.Silu), writes=[bGk, sk])
                        p.op("vector", lambda e: e.tensor_tensor(out=hmT[:, fc, ch * CH:(ch + 1) * CH], in0=bU[:, 0:CH], in1=s_[:, 0:CH], op=ALU.mult),
                             reads=[sk], writes=[bUk, "hmT"])
                if ex + 1 < NE:
                    nxt = [load_w(w_gate[ex + 1]), load_w(w_up[ex + 1])]
                for cb in range(NG):
                    y_ = yo[yn[0] % 2]
                    yk = f"yo{yn[0] % 2}"
                    yn[0] += 1
                    for hf in range(2):
                        bY, bYk = bank()
                        for f in range(8):
                            p.op("tensor", lambda e: e.matmul(bY[:, :], lhsT=hmT[:, f, cb * 128:(cb + 1) * 128], rhs=Wd[:, f, hf * 512:(hf + 1) * 512],
                                                              start=(f == 0), stop=(f == 7)), reads=[Wdk, "hmT"], writes=[bYk])
                        if hf == 0:
                            p.op("scalar", lambda e: e.activation(out=y_[:, 0:512], in_=bY[:, :], func=AF.Copy, scale=gate_all[:, ex, cb:cb + 1]),
                                 reads=["gate_all"], writes=[bYk, yk])
                        else:
                            p.op("vector", lambda e: e.tensor_scalar(out=y_[:, 512:1024], in0=bY[:, :], scalar1=gate_all[:, ex, cb:cb + 1], scalar2=None, op0=ALU.mult),
                                 reads=["gate_all"], writes=[bYk, yk])
                    p.dma("gpsimd", None, None, reads=[yk, "idx_all"], writes=["xres_scatter"],
                          fn=lambda e: e.indirect_dma_start(out=xres[:, :], out_offset=bass.IndirectOffsetOnAxis(ap=idx_all[:, ex, cb:cb + 1], axis=0),
                                                            in_=y_[:, :], in_offset=None, compute_op=ALU.add))
                if ex + 1 < NE:
                    nxt.append(load_w(w_down[ex + 1]))
                    pending = nxt
            p.barrier()


def phase_f(p, c, T, xres, norm_w, out):
    nc = p.nc
    NT = T // 128
    with ExitStack() as es:
        def sb(name, shape, dt=F32):
            return es.enter_context(nc.sbuf_tensor(f"F_{_uid()}_" + name, shape, dt))
        nwb = sb("nwb", [128, 1024])
        xt = [sb(f"xt{i}", [128, 1024]) for i in range(3)]
        junk = sb("junk", [128, 1024], BF16)
        st = [sb(f"st{i}", [128, 4]) for i in range(2)]
        p.dma("sync", nwb[:], norm_w.to_broadcast([128, 1024]), writes=["nwb"])
        for i in range(NT):
            x_, xk = xt[i % 3], f"xt{i % 3}"
            s_, sk = st[i % 2], f"st{i % 2}"
            p.dma("sync", x_[:], xres[i * 128:(i + 1) * 128, :], writes=[xk])
            p.op("scalar", lambda e: e.activation(out=junk[:], in_=x_[:], func=AF.Square, accum_out=s_[:, 0:1]), reads=[xk], writes=["junk", sk + "a"])
            p.op("vector", lambda e: e.tensor_scalar(out=s_[:, 1:2], in0=s_[:, 0:1], scalar1=1.0 / 1024, scalar2=RMS_EPS, op0=ALU.mult, op1=ALU.add),
                 reads=[sk + "a"], writes=[sk + "b"])
            p.op("scalar", lambda e: e.activation(out=s_[:, 2:3], in_=s_[:, 1:2], func=AF.Sqrt), reads=[sk + "b"], writes=[sk + "c"])
            p.op("vector", lambda e: e.reciprocal(out=s_[:, 3:4], in_=s_[:, 2:3]), reads=[sk + "c"], writes=[sk + "d"])
            p.op("vector", lambda e: e.scalar_tensor_tensor(out=x_[:], in0=x_[:], scalar=s_[:, 3:4], in1=nwb[:], op0=ALU.mult, op1=ALU.mult),
                 reads=[xk, sk + "d", "nwb"], writes=[xk])
            p.dma("sync", out[i * 128:(i + 1) * 128, :], x_[:], reads=[xk])
        p.barrier()


DEPTH = 2


def rope_tables(T):
    half = 8
    inv = np.power(500000.0, -np.arange(half, dtype=np.float64) * 2.0 / 16)
    ang = np.arange(T, dtype=np.float32).astype(np.float64)[None, :] * inv.astype(np.float32).astype(np.float64)[:, None]
    cos = np.cos(ang)
    sin = np.sin(ang)
    c16 = np.concatenate([cos, cos], 0)
    s16 = np.concatenate([sin, sin], 0)
    return np.tile(c16, (8, 1)).astype(np.float32), np.tile(s16, (8, 1)).astype(np.float32)


def build_program(T, depth=DEPTH, phases=None):
    nc = bass.Bass("TRN2", target_bir_lowering=False)
    def din(name, shape, dt=F32):
        return nc.dram_tensor(name, shape, dt, kind="ExternalInput").ap()
    def dint(name, shape, dt=F32):
        return nc.dram_tensor(name, shape, dt, kind="Internal").ap()
    x = din("x", [T, 1024])
    norm_mix = din("norm_mix", [depth, 1024])
    w_in = din("w_in", [depth, 1024, P_IN])
    conv_w = din("conv_w", [depth, 5, 1536])
    a_log = din("a_log", [depth, 8])
    dt_bias = din("dt_bias", [depth, 8])
    gdn_norm = din("gdn_norm", [depth, 128])
    w_a = din("w_branch_a", [depth, 512, 1024])
    w_b = din("w_branch_b", [depth, 256, 1024])
    w_out = din("w_out", [depth, 1024, 1024])
    norm_ffn = din("norm_ffn", [depth, 1024])
    w_router = din("w_router", [depth, 1024, 16])
    w_eg = din("w_expert_gate", [depth, 16, 1024, 1024])
    w_eu = din("w_expert_up", [depth, 16, 1024, 1024])
    w_ed = din("w_expert_down", [depth, 16, 1024, 1024])
    norm_final = din("norm_final", [1, 1024])
    cos_t = din("cos_t", [128, T])
    sin_t = din("sin_t", [128, T])
    out = nc.dram_tensor("out", [T, 1024], F32, kind="ExternalOutput").ap()

    xres = dint("xres", [T, 1024])
    oA = dict(qkvTa=dint("qkvTa", [1536, T + 4], BF16), ztok=dint("ztok", [T, 512]), bdtok=dint("bdtok", [T, 16]), bdT=dint("bdT", [16, T]),
              qkrope=dint("qkrope", [384, T], BF16), qkrest=dint("qkrest", [1152, T], BF16), vTb=dint("vTb", [768, T], BF16),
              gatesT=dint("gatesT", [2048, T], BF16))
    oB1 = dict(uT=dint("uT", [1024, T]), ktok=dint("ktok", [T, 512], BF16), vtok=dint("vtok", [T, 512], BF16))
    o_f = dint("o_f", [T, 512])
    o_b = dint("o_b", [T, 512])
    yaT = dint("yaT", [512, T], BF16)
    att = dint("att", [T, 12, 65])
    h2b = dint("h2b", [T, 1024], BF16)
    aff = dint("aff", [T, 16])

    with ExitStack() as es:
        p = Prog(nc, es)
        c = alloc_common(p, es)
        alloc_gdn_consts(p, c, es)
        alloc_att_consts(p, c, es)
        zt = es.enter_context(nc.sbuf_tensor("zpad", [128, 2], BF16))
        p.op("gpsimd", lambda e: e.memset(zt[:], 0.0), writes=["zpad"])
        for ch in range(12):
            p.dma("sync", oA["qkvTa"][ch * 128:(ch + 1) * 128, 0:2], zt[:], reads=["zpad"], allow_slow_non_contiguous=True)
            p.dma("sync", oA["qkvTa"][ch * 128:(ch + 1) * 128, T + 2:T + 4], zt[:], reads=["zpad"], allow_slow_non_contiguous=True)
        p.barrier()
        ph = phases or "A,B1,B2,B3,C,D,E,F"
        ph = ph.split(",")
        for l in range(depth):
            xin = x if l == 0 else xres
            if "A" in ph:
                phase_a(p, c, T, xin, w_in[l], norm_mix[l:l + 1, :], cos_t, sin_t, oA)
            if "B1" in ph and "C" in ph and INTERLEAVE_B1_C:
                with ExitStack() as es_bc:
                    g1 = phase_b1_gen(p, c, T, oA["qkvTa"], conv_w[l], oB1, es_ext=es_bc, shared=True)
                    g2 = phase_c_gen(p, c, T, oA["qkrope"], oA["qkrest"], oA["vTb"], att, es_ext=es_bc, shared=True)
                    n1 = (T // 512) * 12
                    n2 = sum(4 * r_ * (T // r_ // 128 + 1) for r_ in ATT_DIL)
                    d1 = d2 = 0
                    a1 = a2 = True
                    while a1 or a2:
                        if a1 and (not a2 or d1 * n2 <= d2 * n1):
                            try:
                                next(g1)
                                d1 += 1
                            except StopIteration:
                                a1 = False
                        else:
                            try:
                                next(g2)
                                d2 += 1
                            except StopIteration:
                                a2 = False
                    p.barrier()
            elif "B1" in ph:
                phase_b1(p, c, T, oA["qkvTa"], conv_w[l], oB1)
            if "B2" in ph:
                phase_b2(p, c, T, oB1["uT"], oB1["ktok"], oB1["vtok"], oA["bdtok"], a_log[l:l + 1, :], dt_bias[l:l + 1, :], o_f, o_b)
            if "B3" in ph:
                phase_b3(p, c, T, o_f, o_b, oA["ztok"], gdn_norm[l:l + 1, :], yaT)
            if "C" in ph and not ("B1" in ph and INTERLEAVE_B1_C):
                phase_c(p, c, T, oA["qkrope"], oA["qkrest"], oA["vTb"], att)
            if "D" in ph:
                phase_d(p, c, T, yaT, att, oA["gatesT"], xres, w_a[l], w_b[l], w_out[l], norm_ffn[l:l + 1, :], w_router[l], h2b, aff, x_in=xin)
            if "E" in ph:
                phase_e(p, c, T, aff, h2b, xres, w_eg[l], w_eu[l], w_ed[l])
        if "F" in ph:
            phase_f(p, c, T, xres, norm_final, out)
        p.finish()
        nc._stats = (p.nops, p.nwaits)
    return nc


def make_in_maps(inputs, T, nb):
    cos_t, sin_t = rope_tables(T)
    f32 = lambda a: np.ascontiguousarray(np.asarray(a), dtype=np.float32)
    shared = {
        "norm_mix": f32(inputs["norm_mix"]), "w_in": f32(inputs["w_in"]), "conv_w": f32(inputs["conv_w"]),
        "a_log": f32(inputs["a_log"]).reshape(-1, 8), "dt_bias": f32(inputs["dt_bias"]).reshape(-1, 8),
        "gdn_norm": f32(inputs["gdn_norm"]), "w_branch_a": f32(inputs["w_branch_a"]), "w_branch_b": f32(inputs["w_branch_b"]),
        "w_out": f32(inputs["w_out"]), "norm_ffn": f32(inputs["norm_ffn"]), "w_router": f32(inputs["w_router"]),
        "w_expert_gate": f32(inputs["w_expert_gate"]), "w_expert_up": f32(inputs["w_expert_up"]),
        "w_expert_down": f32(inputs["w_expert_down"]), "norm_final": f32(inputs["norm_final"]).reshape(1, 1024),
        "cos_t": cos_t, "sin_t": sin_t,
    }
    x = f32(inputs["x"])
    return [dict(shared, x=x[b]) for b in range(nb)]


_NC_CACHE = {}


def kernel(**inputs):
    x = np.asarray(inputs["x"])
    B, T, _ = x.shape
    if T not in _NC_CACHE:
        _NC_CACHE[T] = build_program(T)
    nc = _NC_CACHE[T]
    in_maps = make_in_maps(inputs, T, B)
    res = run_bass_kernel_spmd(nc, in_maps, core_ids=list(range(B)))
    return np.stack([np.asarray(r["out"]) for r in res.results], axis=0).astype(np.float32)
```
